# Optimizing a Trainium2 kernel written in Bass

```python
import math
import jax, jax.numpy as jnp
from jax import lax
import numpy as np

D_MODEL = 1024
BATCH = 16
SEQ = 2048
DEPTH = 1

N_DIFF_HEADS = 8
DIFF_HEAD_DIM = 64
DIFF_V_DIM = 2 * DIFF_HEAD_DIM
D_ATTN_QK = N_DIFF_HEADS * 2 * DIFF_HEAD_DIM
D_ATTN_V = N_DIFF_HEADS * DIFF_V_DIM
Q_BLOCK = 128
N_BUCKETS = 32
MAX_EXACT = 16
MAX_DISTANCE = 128
POOL_WINDOWS = (2, 4, 8, 16)
N_POOL_GROUPS = len(POOL_WINDOWS)
D_POOL = D_MODEL
POOL_GROUP_DIM = D_POOL // N_POOL_GROUPS
D_IN = 2 * D_ATTN_QK + D_ATTN_V + D_POOL + 2 * D_MODEL
SPLIT_POINTS = (D_ATTN_QK, 2 * D_ATTN_QK, 2 * D_ATTN_QK + D_ATTN_V,
                2 * D_ATTN_QK + D_ATTN_V + D_POOL,
                2 * D_ATTN_QK + D_ATTN_V + D_POOL + D_MODEL)
N_RET_HEADS = 8
N_KEYS = 128
N_EXPERTS = N_KEYS * N_KEYS
D_QUERY = 256
D_SUBKEY = D_QUERY // 2
TOPK = 16
TOKEN_CHUNK = 128

NORM_EPS = 1e-6
SUBLN_EPS = 1e-5
NEG_INF = -1e30

kernel_name = 'hybrid_diffattn_pool_peer_adaln'


def rms_norm(x, w, eps=NORM_EPS):
    xf = x.astype(jnp.float32)
    y = xf * lax.rsqrt(jnp.mean(xf * xf, axis=-1, keepdims=True) + eps)
    return (y * w.astype(jnp.float32)).astype(x.dtype)


def modulate(h, shift, scale):
    return h * (1 + scale[:, None, :]) + shift[:, None, :]


def t5_bucket(dist):
    d = jnp.maximum(dist, 0)
    large = MAX_EXACT + (jnp.log(jnp.maximum(d, 1).astype(jnp.float32) / MAX_EXACT)
                         / math.log(MAX_DISTANCE / MAX_EXACT)
                         * (N_BUCKETS - MAX_EXACT)).astype(jnp.int32)
    large = jnp.minimum(large, N_BUCKETS - 1)
    return jnp.where(d < MAX_EXACT, d, large)


def diff_attention(q, k, v, lam, lam_init, subln_w, rel_bias):
    B, S = q.shape[0], q.shape[1]
    q = q.transpose(0, 2, 3, 1, 4)
    k = k.transpose(0, 2, 3, 1, 4)
    q1, q2 = q[:, :, 0], q[:, :, 1]
    k1, k2 = k[:, :, 0], k[:, :, 1]
    v = v.transpose(0, 2, 1, 3)
    scale = DIFF_HEAD_DIM ** -0.5
    outs = []
    for i in range(S // Q_BLOCK):
        lo, hi = i * Q_BLOCK, (i + 1) * Q_BLOCK
        dist = jnp.arange(lo, hi)[:, None] - jnp.arange(hi)[None, :]
        bias = rel_bias[t5_bucket(dist)].astype(jnp.float32).transpose(2, 0, 1)
        causal = dist >= 0

        def probs(qm, km):
            s = jnp.einsum('bhqd,bhkd->bhqk', qm[:, :, lo:hi].astype(jnp.float32),
                           km[:, :, :hi].astype(jnp.float32)) * scale + bias
            s = jnp.where(causal, s, NEG_INF)
            return jax.nn.softmax(s, axis=-1)

        a = probs(q1, k1) - lam * probs(q2, k2)
        outs.append(jnp.einsum('bhqk,bhkv->bhqv', a.astype(v.dtype), v[:, :, :hi]))
    o = jnp.concatenate(outs, axis=2)
    o = rms_norm(o, subln_w, SUBLN_EPS) * (1.0 - lam_init)
    return o.transpose(0, 2, 1, 3).reshape(B, S, N_DIFF_HEADS * DIFF_V_DIM)


def multiscale_pool(p, pool_w, pool_scale):
    B, S, _ = p.shape
    pf = p.astype(jnp.float32)
    cs = jnp.cumsum(pf, axis=1)
    t = jnp.arange(S)
    groups = []
    for g, w in enumerate(POOL_WINDOWS):
        ch = slice(g * POOL_GROUP_DIM, (g + 1) * POOL_GROUP_DIM)
        upper = cs[:, :, ch]
        lower = jnp.pad(cs[:, :S - w, ch], ((0, 0), (w, 0), (0, 0)))
        count = jnp.minimum(t + 1, w).astype(jnp.float32)[None, :, None]
        groups.append((upper - lower) / count - pf[:, :, ch])
    pooled = jnp.stack(groups, axis=2).astype(p.dtype)
    y = jnp.einsum('bsgc,gce->bsge', pooled, pool_w).reshape(B, S, D_POOL)
    return y * pool_scale


def peer(h, w_query, sub_keys_1, sub_keys_2, expert_down, expert_up):
    B, S, D = h.shape
    xs = h.reshape(-1, TOKEN_CHUNK, D)

    def chunk(xc):
        T = xc.shape[0]
        q = (xc @ w_query).reshape(T, N_RET_HEADS, 2, D_SUBKEY).astype(jnp.float32)
        s1 = jnp.einsum('thd,hnd->thn', q[:, :, 0], sub_keys_1.astype(jnp.float32))
        s2 = jnp.einsum('thd,hnd->thn', q[:, :, 1], sub_keys_2.astype(jnp.float32))
        v1, i1 = lax.top_k(s1, TOPK)
        v2, i2 = lax.top_k(s2, TOPK)
        cand = (v1[..., :, None] + v2[..., None, :]).reshape(T, N_RET_HEADS, TOPK * TOPK)
        cidx = (i1[..., :, None] * N_KEYS + i2[..., None, :]).reshape(T, N_RET_HEADS, TOPK * TOPK)
        sc, pos = lax.top_k(cand, TOPK)
        eidx = jnp.take_along_axis(cidx, pos, axis=-1)
        gate = jax.nn.softmax(sc, axis=-1)
        u = expert_down[eidx]
        act = jax.nn.gelu(jnp.einsum('td,thkd->thk', xc, u).astype(jnp.float32), approximate=False)
        wgt = (gate * act).astype(xc.dtype)
        return jnp.einsum('thk,thkd->td', wgt, expert_up[eidx])

    return lax.map(chunk, xs).reshape(B, S, D)


def setup_inputs(seed: int = 0) -> dict:
    key = jax.random.key(seed)
    ks = jax.random.split(key, 22)
    f32 = jnp.float32
    D, L = D_MODEL, DEPTH

    def nrm(k, shape, s):
        return jax.random.normal(k, shape, f32) * s

    def gain(k, shape):
        return 1.0 + 0.02 * jax.random.normal(k, shape, f32)

    return {
        'x': nrm(ks[0], (BATCH, SEQ, D), 1.0),
        'c': nrm(ks[1], (BATCH, D), 1.0),
        'w_ada': nrm(ks[2], (L, D, 6 * D), 0.5 * D ** -0.5),
        'b_ada': nrm(ks[3], (L, 6 * D), 0.02),
        'norm_mix_w': gain(ks[4], (L, D)),
        'w_in': nrm(ks[5], (L, D, D_IN), D ** -0.5),
        'lambda_q1': nrm(ks[6], (L, DIFF_HEAD_DIM), 0.1),
        'lambda_k1': nrm(ks[7], (L, DIFF_HEAD_DIM), 0.1),
        'lambda_q2': nrm(ks[8], (L, DIFF_HEAD_DIM), 0.1),
        'lambda_k2': nrm(ks[9], (L, DIFF_HEAD_DIM), 0.1),
        'subln_w': gain(ks[10], (L, DIFF_V_DIM)),
        'rel_bias': nrm(ks[11], (N_BUCKETS, N_DIFF_HEADS), 0.5),
        'pool_w': nrm(ks[12], (L, N_POOL_GROUPS, POOL_GROUP_DIM, POOL_GROUP_DIM), POOL_GROUP_DIM ** -0.5),
        'pool_scale': gain(ks[13], (L, D_POOL)),
        'w_out': nrm(ks[14], (L, D, D), D ** -0.5),
        'norm_ffn_w': gain(ks[15], (L, D)),
        'w_query': nrm(ks[16], (L, D, N_RET_HEADS * D_QUERY), D ** -0.5),
        'sub_keys_1': nrm(ks[17], (L, N_RET_HEADS, N_KEYS, D_SUBKEY), D_SUBKEY ** -0.5),
        'sub_keys_2': nrm(ks[18], (L, N_RET_HEADS, N_KEYS, D_SUBKEY), D_SUBKEY ** -0.5),
        'expert_down': nrm(ks[19], (L, N_EXPERTS, D), D ** -0.5),
        'expert_up': nrm(ks[20], (L, N_EXPERTS, D), N_RET_HEADS ** -0.5),
        'norm_final_w': gain(ks[21], (D,)),
    }


def reference(x, c, w_ada, b_ada, norm_mix_w, w_in, lambda_q1, lambda_k1, lambda_q2, lambda_k2,
              subln_w, rel_bias, pool_w, pool_scale, w_out, norm_ffn_w, w_query,
              sub_keys_1, sub_keys_2, expert_down, expert_up, norm_final_w):
    B, S, _ = x.shape
    c_act = jax.nn.silu(c)
    for l in range(DEPTH):
        lam_init = 0.8 - 0.6 * math.exp(-0.3 * l)
        mod = c_act @ w_ada[l] + b_ada[l]
        sh1, sc1, g1, sh2, sc2, g2 = jnp.split(mod, 6, axis=-1)

        h = modulate(rms_norm(x, norm_mix_w[l]), sh1, sc1)
        proj = h @ w_in[l]
        q, k, v, p, g_attn, g_pool = jnp.split(proj, SPLIT_POINTS, axis=-1)
        lam = (jnp.exp(jnp.sum(lambda_q1[l].astype(jnp.float32) * lambda_k1[l].astype(jnp.float32)))
               - jnp.exp(jnp.sum(lambda_q2[l].astype(jnp.float32) * lambda_k2[l].astype(jnp.float32)))
               + lam_init)
        attn = diff_attention(q.reshape(B, S, N_DIFF_HEADS, 2, DIFF_HEAD_DIM),
                              k.reshape(B, S, N_DIFF_HEADS, 2, DIFF_HEAD_DIM),
                              v.reshape(B, S, N_DIFF_HEADS, DIFF_V_DIM),
                              lam, lam_init, subln_w[l], rel_bias)
        pool = multiscale_pool(p, pool_w[l], pool_scale[l])
        merged = jax.nn.sigmoid(g_attn) * attn + jax.nn.sigmoid(g_pool) * pool
        x = x + g1[:, None, :] * (merged @ w_out[l])

        h = modulate(rms_norm(x, norm_ffn_w[l]), sh2, sc2)
        x = x + g2[:, None, :] * peer(h, w_query[l], sub_keys_1[l], sub_keys_2[l],
                                      expert_down[l], expert_up[l])
    return rms_norm(x, norm_final_w)
```

```python
import os
import math
import contextlib
import numpy as np
import concourse.bass as bass
import concourse.mybir as mybir
from concourse.bass_utils import run_bass_kernel_spmd

dt = mybir.dt
AF = mybir.ActivationFunctionType
ALU = mybir.AluOpType
AX = mybir.AxisListType
F32, BF16, U32 = dt.float32, dt.bfloat16, dt.uint32

NCORES = 8
NB = 2
S = 2048
NT = 16
D = 1024
KC = 8
H = 8
DIN = 6144
NEXP = 16384
OFF = 8.0
NEG = -1.0e30
G = 512


class Prog:
    def __init__(self):
        self.ops = []
        self.last_w = {}
        self.readers = {}
        self.capture = None

    def add(self, eng, fn, r=(), w=(), dma=False):
        if self.capture is not None:
            self.capture.append((eng, fn, r, w, dma))
            return None
        i = len(self.ops)
        deps = set()
        rk = _flat(r)
        wk = _flat(w)
        for k in rk:
            lw = self.last_w.get(k)
            if lw is not None:
                deps.add(lw)
        for k in wk:
            lw = self.last_w.get(k)
            if lw is not None:
                deps.add(lw)
            deps.update(self.readers.get(k, ()))
        deps.discard(i)
        for k in rk:
            self.readers.setdefault(k, []).append(i)
        for k in wk:
            self.last_w[k] = i
            self.readers[k] = []
        self.ops.append(dict(eng=eng, fn=fn, deps=deps, dma=dma, sig=None, need=False, pre=None))
        return i

    def emit(self, nc, stack):
        ops = self.ops
        EPOCH = 30000
        NSLOT = {'sp': 24, 'pool': 24, 'act': 8}
        for o in ops:
            for d in o['deps']:
                p = ops[d]
                if p['eng'] == 'pe' and o['eng'] == 'pe' and not p['dma'] and not o['dma']:
                    continue
                p['need'] = True
        cnt = {e: 0 for e in ('pe', 'act', 'dve', 'pool', 'sp')}
        esems = {e: [] for e in cnt}
        dsems = {q: [stack.enter_context(nc.semaphore(f"d_{q}_{i}")) for i in range(n)] for q, n in NSLOT.items()}
        duse = {q: [0] * n for q, n in NSLOT.items()}
        dnext = {q: 0 for q in NSLOT}
        for o in ops:
            e = o['eng']
            if o['dma']:
                s = dnext[e]
                dnext[e] = (s + 1) % NSLOT[e]
                if duse[e][s] > 0:
                    o['pre'] = (dsems[e][s], 16 * duse[e][s])
                duse[e][s] += 1
                o['sig'] = (dsems[e][s], 16 * duse[e][s])
            elif o['need']:
                ep = cnt[e] // EPOCH
                while len(esems[e]) <= ep:
                    esems[e].append(stack.enter_context(nc.semaphore(f"e_{e}_{len(esems[e])}")))
                cnt[e] += 1
                o['sig'] = (esems[e][ep], cnt[e] - ep * EPOCH)
        by_eng = {e: [o for o in ops if o['eng'] == e] for e in cnt}
        final_waits = []
        for q in NSLOT:
            for s in range(NSLOT[q]):
                if duse[q][s] > 0:
                    final_waits.append((dsems[q][s], 16 * duse[q][s]))

        def run(ename, eng):
            waited = {}
            for o in by_eng[ename]:
                needs = {}
                for d in o['deps']:
                    p = ops[d]
                    if p['eng'] == 'pe' and ename == 'pe' and not p['dma'] and not o['dma']:
                        continue
                    sem, val = p['sig']
                    key = id(sem)
                    if needs.get(key, (None, 0))[1] < val:
                        needs[key] = (sem, val)
                if o['pre'] is not None:
                    sem, val = o['pre']
                    key = id(sem)
                    if needs.get(key, (None, 0))[1] < val:
                        needs[key] = (sem, val)
                for key, (sem, val) in needs.items():
                    if waited.get(key, 0) < val:
                        eng.wait_ge(sem, val)
                        waited[key] = val
                inst = o['fn'](eng)
                if o['sig'] is not None:
                    inst.then_inc(o['sig'][0], 16 if o['dma'] else 1)
            if ename == 'sp':
                for sem, val in final_waits:
                    eng.wait_ge(sem, val)

        with nc.Block() as block:
            @block.tensor
            def _(e):
                run('pe', e)

            @block.scalar
            def _(e):
                run('act', e)

            @block.vector
            def _(e):
                run('dve', e)

            @block.gpsimd
            def _(e):
                run('pool', e)

            @block.sync
            def _(e):
                run('sp', e)


def _flat(x):
    out = []
    for k in x:
        if isinstance(k, (list, tuple, set, range)):
            out.extend(_flat(k))
        else:
            out.append(k)
    return out


class Buf:
    def __init__(self, arena, off, nbytes):
        assert off % 4 == 0 and nbytes % 4 == 0
        self.A, self.off, self.nbytes = arena, off, nbytes

    def g(self, lo=0, hi=None):
        hi = self.nbytes if hi is None else hi
        return range((self.off + lo) // G, (self.off + hi + G - 1) // G)

    def f32(self):
        return self.A[:, self.off // 4:(self.off + self.nbytes) // 4]

    def bf(self):
        return self.A[:, self.off // 4:(self.off + self.nbytes) // 4].bitcast(BF16)

    def u32(self):
        return self.A[:, self.off // 4:(self.off + self.nbytes) // 4].bitcast(U32)

    def sub(self, lo, n):
        return Buf(self.A, self.off + lo, n)


class Alloc:
    def __init__(self, arena, base, limit):
        self.A, self.p, self.limit = arena, base, limit

    def get(self, nbytes):
        nb = (nbytes + G - 1) // G * G
        b = Buf(self.A, self.p, (nbytes + 3) // 4 * 4)
        self.p += nb
        assert self.p <= self.limit, (self.p, self.limit)
        return b


def mk(ap, pattern, extra_off=0):
    return bass.AP(tensor=ap.tensor, offset=ap.offset + extra_off, ap=[list(ap.ap[0])] + [list(p) for p in pattern])


def _t5_bucket(d):
    d = np.maximum(d, 0)
    x = np.maximum(d, 1).astype(np.float32) / np.float32(16)
    large = 16 + (np.log(x).astype(np.float32) / np.float32(math.log(128 / 16)) * np.float32(16)).astype(np.int32)
    large = np.minimum(large, 31)
    return np.where(d < 16, d, large)


def _host_consts():
    kk = np.arange(128)[:, None]
    qq = np.arange(256)[None, :]
    dist = qq - kk
    valid = dist >= 0
    bk = _t5_bucket(dist)
    masks = np.zeros((128, 32, 256), np.float32)
    for b in range(32):
        masks[:, b, :] = (valid & (bk == b)).astype(np.float32)
    negmask = np.where(valid, 0.0, NEG).astype(np.float32)
    band = np.zeros((128, 12, 128), np.float32)
    s = np.arange(128)[:, None]
    t = np.arange(128)[None, :]
    for gi, w in enumerate((2, 4, 8, 16)):
        cnt0 = np.minimum(t + 1, w).astype(np.float32)
        band[:, gi * 3 + 0, :] = ((s <= t) & (s > t - w)) / cnt0 - (s == t)
        band[:, gi * 3 + 1, :] = ((s <= t) & (s > t - w)) / np.float32(w) - (s == t)
        band[:, gi * 3 + 2, :] = (s > 128 + t - w) / np.float32(w)
    ident = np.eye(128, dtype=np.float32)
    iota16 = np.broadcast_to(np.arange(16, dtype=np.float32)[None, :], (128, 16)).copy()
    sel0 = np.zeros((64, 128), np.float32)
    sel0[0, :] = 1.0
    sel0[32, :] = 1.0
    return dict(masks=masks, negmask=negmask, band=band, ident=ident, iota16=iota16, sel0=sel0)


def build(stage=99, dbg=False):
    nc = bass.Bass("TRN2", target_bir_lowering=False)
    P = Prog()

    def din(name, shape, dtype=F32):
        return nc.dram_tensor(name, list(shape), dtype, kind="ExternalInput")

    x_d = din("x", [NB, S, D])
    cT_d = din("cT", [128, NB * KC])
    wada_d = din("w_ada", [D, DIN])
    bada_d = din("b_ada_bc", [128, DIN])
    nw_d = din("nw_bc", [128, 3 * D])
    win_d = din("w_in", [D, DIN])
    wout_d = din("w_out", [D, D])
    wqry_d = din("w_query", [D, 2048])
    poolw_d = din("pool_w", [4, 256, 256])
    psc_d = din("pool_scaleT", [128, 8])
    subk_d = din("subkT", [128, 16 * 128])
    edown_d = din("e_down", [NEXP, D])
    eup_d = din("e_up", [NEXP, D])
    lam_d = din("lam_bc", [128, 256])
    subln_d = din("sublnT", [128, 1])
    relb_d = din("relb_bc", [128, 256])
    masks_d = din("masks", [128, 32 * 256])
    negm_d = din("negmask", [128, 256])
    band_d = din("band", [128, 12 * 128])
    ident_d = din("ident", [128, 128])
    iota_d = din("iota16", [128, 16])
    sel0_d = din("sel0", [64, 128])
    out_d = nc.dram_tensor("out", [NB, S, D], F32, kind="ExternalOutput")
    etab_d = nc.dram_tensor("etab", [NEXP, 2 * D], BF16, kind="Internal")
    dbg_d = {}

    def dbg_out(name, shape):
        dbg_d[name] = nc.dram_tensor(name, list(shape), F32, kind="ExternalOutput")
        return dbg_d[name]

    stack = contextlib.ExitStack()
    TOT = 212480
    arena = stack.enter_context(nc.sbuf_tensor("arena", [128, TOT // 4], F32))
    banks = [stack.enter_context(nc.psum_tensor(f"B{i}", [128, 512], F32)) for i in range(8)]
    BK = [f"B{i}" for i in range(8)]

    al = Alloc(arena, 0, TOT)
    ident_f = al.get(512)
    ident_b = al.get(256)
    ones_b = al.get(256)
    ones_f = al.get(512)
    sel0 = al.get(512)
    relb = al.get(1024)
    rb31m = al.get(32)
    biasT = al.get(8 * 1024)
    lamv = al.get(1024)
    small = al.get(512)
    pscT = al.get(32)
    band = al.get(12 * 128 * 2)
    subkT = al.get(16 * 128 * 2)
    iota16 = al.get(64)
    nf_bc = al.get(4096)
    sh1 = al.get(4096)
    a1 = al.get(4096)
    g1 = al.get(4096)
    sh2 = al.get(4096)
    a2 = al.get(4096)
    g2 = al.get(4096)
    hT = al.get(KC * S * 2)
    mT = al.get(KC * S * 2)
    wout = al.get(KC * D * 2)
    work_base = al.p
    SM = small.f32()
    (SM_S1, SM_S2, SM_E1, SM_E2, SM_NLAM, SM_WSUB, SM_SS, SM_RSTD, SM_LN, SM_SS2, SM_RSTD2, SM_SS3, SM_RSTD3,
     SM_LN2, SM_LN3, SM_SUBLN) = range(16)

    def sm(i):
        return SM[:, i:i + 1]

    def smg(i):
        return small.g()

    def dma(q, out, in_, r, w):
        P.add(q, lambda e: e.dma_start(out=out, in_=in_), r=r, w=w, dma=True)

    def mm(out, lhsT, rhs, start, stop, r, w, **kw):
        P.add('pe', lambda e: e.matmul(out, lhsT, rhs, start=start, stop=stop, **kw), r=r, w=w)

    def tr(out, in_, ident, r, w):
        P.add('pe', lambda e: e.transpose(out, in_, ident), r=r, w=w)

    def act(out, in_, func, r, w, bias=None, scale=None, accum_out=None, eng='act'):
        def f(e):
            kw = {}
            if bias is not None:
                kw['bias'] = bias
            if scale is not None:
                kw['scale'] = scale
            if accum_out is not None:
                kw['accum_out'] = accum_out
            return e.activation(out=out, in_=in_, func=func, **kw)
        P.add('act', f, r=r, w=w)

    def tt(out, in0, in1, op, r, w, eng='dve'):
        P.add(eng, lambda e: e.tensor_tensor(out=out, in0=in0, in1=in1, op=op), r=r, w=w)

    def ts(out, in0, s1, s2, op0, op1, r, w, eng='dve'):
        if op1 is None:
            P.add(eng, lambda e: e.tensor_scalar(out=out, in0=in0, scalar1=s1, scalar2=None, op0=op0), r=r, w=w)
        else:
            P.add(eng, lambda e: e.tensor_scalar(out=out, in0=in0, scalar1=s1, scalar2=s2, op0=op0, op1=op1), r=r, w=w)

    def stt(out, in0, scalar, in1, op0, op1, r, w):
        P.add('dve', lambda e: e.scalar_tensor_tensor(out=out, in0=in0, scalar=scalar, in1=in1, op0=op0, op1=op1), r=r, w=w)

    def cp(out, in_, r, w, eng='dve'):
        if eng == 'act':
            P.add('act', lambda e: e.copy(out=out, in_=in_), r=r, w=w)
        else:
            P.add(eng, lambda e: e.tensor_copy(out=out, in_=in_), r=r, w=w)

    def ttr(out, in0, in1, accum_out, r, w):
        P.add('dve', lambda e: e.scalar_tensor_tensor(out=out, in0=in0, scalar=1.0, in1=in1, op0=ALU.mult, op1=ALU.mult,
                                                     accum_out=accum_out), r=r, w=w)

    def tred(out, in_, op, r, w):
        P.add('dve', lambda e: e.tensor_reduce(out=out, in_=in_, axis=AX.X, op=op), r=r, w=w)

    def memset(ap, val, r, w, eng='dve'):
        P.add(eng, lambda e: e.memset(ap, val), r=r, w=w)

    def rstd_chain(ss_i, ln_i, rstd_i, n, eps):
        ts(sm(ln_i), sm(ss_i), 1.0 / n, eps, ALU.mult, ALU.add, r=[small.g()], w=[small.g()])
        act(sm(ln_i), sm(ln_i), AF.Ln, r=[small.g()], w=[small.g()])
        act(sm(rstd_i), sm(ln_i), AF.Exp, r=[small.g()], w=[small.g()], scale=-0.5)

    def dump(name, ap_sb, shape, r):
        if name in dbg_d:
            return
        d = dbg_out(name, shape)
        dma('sp', d.ap(), ap_sb, r=r, w=[name])

    dma('sp', ident_f.f32(), ident_d.ap(), r=[], w=[ident_f.g()])
    dma('sp', sel0.f32()[0:64, :], sel0_d.ap(), r=[], w=[sel0.g()])
    dma('sp', relb.f32(), relb_d.ap(), r=[], w=[relb.g()])
    dma('sp', lamv.f32(), lam_d.ap(), r=[], w=[lamv.g()])
    dma('sp', pscT.f32(), psc_d.ap(), r=[], w=[pscT.g()])
    dma('sp', iota16.f32(), iota_d.ap(), r=[], w=[iota16.g()])
    dma('sp', nf_bc.f32(), nw_d.ap()[:, 2 * D:3 * D], r=[], w=[nf_bc.g()])
    dma('sp', sm(SM_SUBLN), subln_d.ap(), r=[], w=[small.g()])
    dma('pool', band.bf(), band_d.ap(), r=[], w=[band.g()])
    dma('pool', subkT.bf(), subk_d.ap(), r=[], w=[subkT.g()])
    ETAB_KEYS = []
    for ti, tbl in enumerate((edown_d, eup_d)):
        for r0 in range(0, NEXP, 1024):
            key = f"etab{ti}_{r0}"
            ETAB_KEYS.append(key)
            dma('pool', etab_d.ap()[r0:r0 + 1024, ti * D:(ti + 1) * D], tbl.ap()[r0:r0 + 1024, :], r=[], w=[key])
    cp(ident_b.bf(), ident_f.f32(), r=[ident_f.g()], w=[ident_b.g()])
    memset(ones_b.bf(), 1.0, r=[], w=[ones_b.g()])
    memset(ones_f.f32(), 1.0, r=[], w=[ones_f.g()])
    ts(rb31m.f32(), relb.f32()[:, 31 * 8:32 * 8], -OFF, None, ALU.add, None, r=[relb.g()], w=[rb31m.g()])
    ts(sm(SM_WSUB), sm(SM_SUBLN), 0.8, None, ALU.mult, None, r=[small.g()], w=[small.g()])
    wa = Alloc(arena, work_base, TOT)
    junk64 = wa.get(256)
    LV = lamv.f32()
    ttr(junk64.f32(), LV[:, 0:64], LV[:, 64:128], sm(SM_S1), r=[lamv.g()], w=[junk64.g(), small.g()])
    ttr(junk64.f32(), LV[:, 128:192], LV[:, 192:256], sm(SM_S2), r=[lamv.g()], w=[junk64.g(), small.g()])
    act(sm(SM_E1), sm(SM_S1), AF.Exp, r=[small.g()], w=[small.g()])
    act(sm(SM_E2), sm(SM_S2), AF.Exp, r=[small.g()], w=[small.g()])
    tt(sm(SM_NLAM), sm(SM_E2), sm(SM_E1), ALU.subtract, r=[small.g()], w=[small.g()])
    ts(sm(SM_NLAM), sm(SM_NLAM), -0.2, None, ALU.add, None, r=[small.g()], w=[small.g()])
    mk_chunk = wa.get(8 * 256 * 4)
    BT3 = biasT.f32().rearrange("p (h q) -> p h q", h=8)
    for h in range(8):
        dma('sp', BT3[:, h, :], negm_d.ap(), r=[], w=[biasT.g(h * 1024, (h + 1) * 1024)])
    for c in range(4):
        dma('sp', mk_chunk.f32(), masks_d.ap()[:, c * 2048:(c + 1) * 2048], r=[], w=[mk_chunk.g()])
        MC = mk_chunk.f32().rearrange("p (b q) -> p b q", b=8)
        for bb in range(8):
            b = c * 8 + bb
            for h in range(8):
                stt(BT3[:, h, :], MC[:, bb, :], relb.f32()[:, b * 8 + h:b * 8 + h + 1], BT3[:, h, :], ALU.mult, ALU.add,
                    r=[mk_chunk.g(), relb.g(), biasT.g(h * 1024, (h + 1) * 1024)], w=[biasT.g(h * 1024, (h + 1) * 1024)])
    ts(biasT.f32(), biasT.f32(), -OFF, None, ALU.add, None, r=[biasT.g()], w=[biasT.g()])
    if dbg:
        dump("dbg_bias", biasT.f32(), [128, 2048], r=[biasT.g()])
        dump("dbg_small", small.f32()[:, 0:16], [128, 16], r=[small.g()])

    hT3 = hT.bf().rearrange("p (k t) -> p k t", k=KC)
    mT3 = mT.bf().rearrange("p (k t) -> p k t", k=KC)

    def hT_g(c0, c1):
        return [hT.g(k * S * 2 + c0 * 2, k * S * 2 + c1 * 2) for k in range(KC)]

    def mT_g(k, c0, c1):
        return mT.g(k * S * 2 + c0 * 2, k * S * 2 + c1 * 2)

    BTb = banks[7][:].bitcast(BF16)

    for b in range(NB):
        wa = Alloc(arena, work_base, TOT)
        cact = wa.get(64)
        crep = wa.get(KC * 128 * 4)
        wblk = [wa.get(KC * 512 * 4) for _ in range(2)]
        bblk = [wa.get(2048) for _ in range(2)]
        mtmp = wa.get(2048)
        nwt = wa.get(4096)
        cin = wa.get(64)
        dma('sp', cin.f32()[:, 0:KC], cT_d.ap()[:, b * KC:(b + 1) * KC], r=[], w=[cin.g()])
        act(cact.f32()[:, 0:KC], cin.f32()[:, 0:KC], AF.Silu, r=[cin.g()], w=[cact.g()])
        cp(crep.f32().rearrange("p (k m) -> p k m", k=KC), mk(cact.f32(), [[1, KC], [0, 128]]), r=[cact.g()], w=[crep.g()])
        CR = crep.f32().rearrange("p (k m) -> p k m", k=KC)
        wada_v = wada_d.ap().rearrange("(k p) n -> p k n", p=128)
        dsts = [sh1, sh1, a1, a1, g1, g1, sh2, sh2, a2, a2, g2, g2]
        for nb_ in range(12):
            wb = wblk[nb_ % 2]
            bb_ = bblk[nb_ % 2]
            dma('sp', wb.f32().rearrange("p (k n) -> p k n", k=KC), wada_v[:, :, nb_ * 512:(nb_ + 1) * 512], r=[], w=[wb.g()])
            dma('sp', bb_.f32(), bada_d.ap()[:, nb_ * 512:(nb_ + 1) * 512], r=[], w=[bb_.g()])
            WB = wb.f32().rearrange("p (k n) -> p k n", k=KC)
            for k in range(KC):
                mm(banks[6][:], CR[:, k, :], WB[:, k, :], k == 0, k == KC - 1, r=[crep.g(), wb.g()], w=[BK[6]])
            dst = dsts[nb_]
            half = nb_ % 2
            dst_ap = dst.f32()[:, half * 512:(half + 1) * 512]
            dg_ = dst.g(half * 2048, (half + 1) * 2048)
            if nb_ in (2, 3, 8, 9):
                which = 0 if nb_ in (2, 3) else 1
                dma('sp', nwt.f32()[:, 0:512], nw_d.ap()[:, which * D + half * 512: which * D + (half + 1) * 512], r=[], w=[nwt.g()])
                tt(mtmp.f32(), banks[6][:], bb_.f32(), ALU.add, r=[BK[6], bb_.g()], w=[mtmp.g()])
                stt(dst_ap, mtmp.f32(), 1.0, nwt.f32()[:, 0:512], ALU.add, ALU.mult, r=[mtmp.g(), nwt.g()], w=[dg_])
            else:
                tt(dst_ap, banks[6][:], bb_.f32(), ALU.add, r=[BK[6], bb_.g()], w=[dg_])
        if dbg and b == 0:
            dump("dbg_a1", a1.f32(), [128, 1024], r=[a1.g()])
            dump("dbg_g2", g2.f32(), [128, 1024], r=[g2.g()])

        wa = Alloc(arena, work_base, TOT)
        xt = [wa.get(4096) for _ in range(2)]
        tmpf = wa.get(4096)
        junkb = wa.get(2048)
        hbf = wa.get(2048)
        for i in range(NT):
            xb = xt[i % 2]
            dma('sp', xb.f32(), x_d.ap()[b, i * 128:(i + 1) * 128, :], r=[], w=[xb.g()])
            act(junkb.bf(), xb.f32(), AF.Square, r=[xb.g()], w=[junkb.g(), small.g()], accum_out=sm(SM_SS))
            rstd_chain(SM_SS, SM_LN, SM_RSTD, D, 1e-6)
            stt(tmpf.f32(), xb.f32(), sm(SM_RSTD), a1.f32(), ALU.mult, ALU.mult, r=[xb.g(), small.g(), a1.g()], w=[tmpf.g()])
            tt(hbf.bf(), tmpf.f32(), sh1.f32(), ALU.add, r=[tmpf.g(), sh1.g()], w=[hbf.g()])
            for k in range(KC):
                tr(BTb[:, k * 128:(k + 1) * 128], hbf.bf()[:, k * 128:(k + 1) * 128], ident_b.bf(), r=[hbf.g(), ident_b.g()], w=[BK[7]])
            cp(hT3[:, :, i * 128:(i + 1) * 128], BTb.rearrange("p (k t) -> p k t", k=KC), r=[BK[7]], w=hT_g(i * 128, (i + 1) * 128), eng='act')
        if dbg and b == 0:
            wa2 = Alloc(arena, wa.p, TOT)
            dtmp = wa2.get(8192)
            cp(dtmp.f32(), hT3[:, 0, :], r=hT_g(0, S), w=[dtmp.g()])
            dump("dbg_hT0", dtmp.f32(), [128, 2048], r=[dtmp.g()])
        if stage <= 1:
            continue

        win_v = win_d.ap().rearrange("(k p) n -> p k n", p=128)
        wa = Alloc(arena, work_base, TOT)
        wp = wa.get(KC * 256 * 2)
        wgp = wa.get(KC * 256 * 2)
        pw = wa.get(2 * 256 * 2)
        pg = wa.get(NT * 256 * 2)
        pT = [wa.get(512 * 2) for _ in range(2)]
        sg = wa.get(512 * 4)
        WP = wp.bf().rearrange("p (k n) -> p k n", k=KC)
        WGP = wgp.bf().rearrange("p (k n) -> p k n", k=KC)
        PW = pw.bf().rearrange("p (c n) -> p c n", c=2)
        PG = pg.bf().rearrange("p (i n) -> p i n", i=NT)
        BAND = band.bf().rearrange("p (j n) -> p j n", j=12)
        for g in range(4):
            dma('pool', WP, win_v[:, :, 3072 + g * 256:3072 + (g + 1) * 256], r=[], w=[wp.g()])
            dma('pool', WGP, win_v[:, :, 5120 + g * 256:5120 + (g + 1) * 256], r=[], w=[wgp.g()])
            dma('pool', PW, poolw_d.ap()[g].rearrange("(c p) n -> p c n", p=128), r=[], w=[pw.g()])
            for i in range(NT):
                for k in range(KC):
                    mm(banks[7][:, (i % 2) * 256:(i % 2 + 1) * 256], hT3[:, k, i * 128:(i + 1) * 128], WP[:, k, :], k == 0, k == KC - 1,
                       r=[hT_g(i * 128, (i + 1) * 128), wp.g()], w=[BK[7]])
                if i % 2 == 1:
                    cp(pg.bf()[:, (i - 1) * 256:(i + 1) * 256], banks[7][:], r=[BK[7]], w=[pg.g((i - 1) * 512, (i + 1) * 512)], eng='act')
            for c in range(4):
                for cc in range(2):
                    for il in range(4):
                        i = c * 4 + il
                        cur = g * 3 + (0 if i == 0 else 1)
                        mm(banks[cc][:, il * 128:(il + 1) * 128], PG[:, i, cc * 128:(cc + 1) * 128], BAND[:, cur, :], True, i == 0,
                           r=[pg.g(i * 512, (i + 1) * 512), band.g()], w=[BK[cc]])
                        if i > 0:
                            mm(banks[cc][:, il * 128:(il + 1) * 128], PG[:, i - 1, cc * 128:(cc + 1) * 128], BAND[:, g * 3 + 2, :], False, True,
                               r=[pg.g((i - 1) * 512, i * 512), band.g()], w=[BK[cc]])
                    cp(pT[cc].bf(), banks[cc][:], r=[BK[cc]], w=[pT[cc].g()], eng='act')
                for ec in range(2):
                    kch = g * 2 + ec
                    mm(banks[2][:], PW[:, 0, ec * 128:(ec + 1) * 128], pT[0].bf(), True, False, r=[pw.g(), pT[0].g()], w=[BK[2]])
                    mm(banks[2][:], PW[:, 1, ec * 128:(ec + 1) * 128], pT[1].bf(), False, True, r=[pw.g(), pT[1].g()], w=[BK[2]])
                    for k in range(KC):
                        mm(banks[3][:], WGP[:, k, ec * 128:(ec + 1) * 128], hT3[:, k, c * 512:(c + 1) * 512], k == 0, k == KC - 1,
                           r=[wgp.g(), hT_g(c * 512, (c + 1) * 512)], w=[BK[3]])
                    act(sg.f32(), banks[3][:], AF.Sigmoid, r=[BK[3]], w=[sg.g()])
                    stt(mT3[:, kch, c * 512:(c + 1) * 512], banks[2][:], pscT.f32()[:, kch:kch + 1], sg.f32(), ALU.mult, ALU.mult,
                        r=[BK[2], pscT.g(), sg.g()], w=[mT_g(kch, c * 512, (c + 1) * 512)])
        if dbg and b == 0 and stage == 2:
            wa2 = Alloc(arena, wa.p, TOT)
            dtmp = wa2.get(8192)
            for kk_ in (0, 5):
                cp(dtmp.f32(), mT3[:, kk_, :], r=[mT.g()], w=[dtmp.g()])
                dump(f"dbg_mT{kk_}", dtmp.f32(), [128, 2048], r=[dtmp.g()])
        if stage <= 2:
            continue

        wa = Alloc(arena, work_base, TOT)
        wq = wa.get(KC * 128 * 2)
        wk_ = wa.get(KC * 128 * 2)
        wv = wa.get(KC * 128 * 2)
        wg = wa.get(KC * 128 * 2)
        QT = wa.get(S * 2)
        KT = wa.get(S * 2)
        Vb = wa.get(NT * 128 * 2)
        sgT = wa.get(S * 2)
        PT = [[wa.get(512 * 2) for _ in range(2)] for _ in range(2)]
        ntmp = [wa.get(256 * 4) for _ in range(2)]
        O1s = wa.get(2048)
        O2s = wa.get(2048)
        lnz = wa.get(2048)
        rz = wa.get(2048)
        sq = wa.get(2048)
        t1 = wa.get(2048)
        rs = wa.get(2048)
        WQ = wq.bf().rearrange("p (k n) -> p k n", k=KC)
        WK = wk_.bf().rearrange("p (k n) -> p k n", k=KC)
        WV = wv.bf().rearrange("p (k n) -> p k n", k=KC)
        WG = wg.bf().rearrange("p (k n) -> p k n", k=KC)
        V3 = Vb.bf().rearrange("p (i n) -> p i n", i=NT)
        nheads = H if stage > 3 or not dbg else 1
        for h in range(nheads):
            dma('pool', WQ, win_v[:, :, h * 128:(h + 1) * 128], r=[], w=[wq.g()])
            dma('pool', WK, win_v[:, :, 1024 + h * 128:1024 + (h + 1) * 128], r=[], w=[wk_.g()])
            dma('pool', WV, win_v[:, :, 2048 + h * 128:2048 + (h + 1) * 128], r=[], w=[wv.g()])
            dma('pool', WG, win_v[:, :, 4096 + h * 128:4096 + (h + 1) * 128], r=[], w=[wg.g()])
            pj = 0
            for (W_, wb_, dst, mode) in ((WQ, wq, QT, 'q'), (WK, wk_, KT, 'k'), (WG, wg, sgT, 'g')):
                for c in range(4):
                    bk = 6 + (pj % 2)
                    pj += 1
                    for k in range(KC):
                        mm(banks[bk][:], W_[:, k, :], hT3[:, k, c * 512:(c + 1) * 512], k == 0, k == KC - 1,
                           r=[wb_.g(), hT_g(c * 512, (c + 1) * 512)], w=[BK[bk]])
                    o_ap = dst.bf()[:, c * 512:(c + 1) * 512]
                    o_g = dst.g(c * 1024, (c + 1) * 1024)
                    if mode == 'q':
                        P.add('act', (lambda o_ap=o_ap, bk=bk: (lambda e: e.mul(out=o_ap, in_=banks[bk][:], mul=0.125)))(), r=[BK[bk]], w=[o_g])
                    elif mode == 'k':
                        cp(o_ap, banks[bk][:], r=[BK[bk]], w=[o_g], eng='act')
                    else:
                        act(o_ap, banks[bk][:], AF.Sigmoid, r=[BK[bk]], w=[o_g])
            for i in range(NT):
                bk = 6 + ((i // 4) % 2)
                for k in range(KC):
                    mm(banks[bk][:, (i % 4) * 128:(i % 4 + 1) * 128], hT3[:, k, i * 128:(i + 1) * 128], WV[:, k, :], k == 0, k == KC - 1,
                       r=[hT_g(i * 128, (i + 1) * 128), wv.g()], w=[BK[bk]])
                if i % 4 == 3:
                    cp(Vb.bf()[:, (i - 3) * 128:(i + 1) * 128], banks[bk][:], r=[BK[bk]], w=[Vb.g((i - 3) * 256, (i + 1) * 256)], eng='act')

            def qk(c, j):
                col0 = max(0, j - 4 * c) * 128
                par = j % 2
                for m in range(2):
                    bk = m * 2 + par
                    mm(banks[bk][:, col0:512], KT.bf()[m * 64:(m + 1) * 64, j * 128:(j + 1) * 128],
                       QT.bf()[m * 64:(m + 1) * 64, c * 512 + col0:(c + 1) * 512], True, True,
                       r=[KT.g(j * 256, (j + 1) * 256), QT.g(c * 1024 + col0 * 2, (c + 1) * 1024)], w=[BK[bk]])

            for c in range(4):
                jmax = 4 * c + 3
                qk(c, 0)
                for j in range(jmax + 1):
                    if j + 1 <= jmax:
                        qk(c, j + 1)
                    col0 = max(0, j - 4 * c) * 128
                    par = j % 2
                    il_lo = max(0, j - 4 * c)
                    il_hi = min(3, j + 1 - 4 * c)
                    far0 = max(0, j + 2 - 4 * c) * 128
                    for m in range(2):
                        bk = m * 2 + par
                        pt = PT[m][par]
                        if il_hi >= il_lo and il_hi >= 0:
                            n0, n1 = il_lo * 128, (il_hi + 1) * 128
                            b0 = (4 * c + il_lo - j) * 128
                            nn = n1 - n0
                            tt(ntmp[m].f32()[:, 0:nn], banks[bk][:, n0:n1], BT3[:, h, b0:b0 + nn], ALU.add,
                               r=[BK[bk], biasT.g(h * 1024, (h + 1) * 1024)], w=[ntmp[m].g()])
                            act(pt.bf()[:, n0:n1], ntmp[m].f32()[:, 0:nn], AF.Exp, r=[ntmp[m].g()], w=[pt.g(n0 * 2, n1 * 2)])
                        if far0 < 512:
                            act(pt.bf()[:, far0:512], banks[bk][:, far0:512], AF.Exp, r=[BK[bk], rb31m.g()], w=[pt.g(far0 * 2, 1024)],
                                bias=rb31m.f32()[:, h:h + 1])
                        mm(banks[4 + m][:, col0:512], V3[:, j, :], pt.bf()[:, col0:512], j == 0, j == jmax,
                           r=[Vb.g(j * 256, (j + 1) * 256), pt.g(col0 * 2, 1024)], w=[BK[4 + m]], skip_group_check=True)
                        mm(banks[6][m * 32:(m + 1) * 32, col0:512], ones_b.bf()[:, 0:32], pt.bf()[:, col0:512], j == 0, j == jmax,
                           r=[ones_b.g(), pt.g(col0 * 2, 1024)], w=[BK[6]], skip_group_check=True)
                cs = slice(c * 512, (c + 1) * 512)
                cp(O1s.f32(), banks[4][:], r=[BK[4]], w=[O1s.g()], eng='act')
                cp(O2s.f32(), banks[5][:], r=[BK[5]], w=[O2s.g()], eng='act')
                act(lnz.f32()[0:64, :], banks[6][0:64, :], AF.Ln, r=[BK[6]], w=[lnz.g()])
                act(rz.f32()[0:64, :], lnz.f32()[0:64, :], AF.Exp, r=[lnz.g()], w=[rz.g()], scale=-1.0)
                ts(rz.f32()[32:64, :], rz.f32()[32:64, :], SM[32:64, SM_NLAM:SM_NLAM + 1], None, ALU.mult, None, r=[rz.g(), small.g()], w=[rz.g()])
                mm(banks[0][:], sel0.f32()[0:32, :], rz.f32()[0:32, :], True, True, r=[sel0.g(), rz.g()], w=[BK[0]])
                mm(banks[1][:], sel0.f32()[32:64, :], rz.f32()[32:64, :], True, True, r=[sel0.g(), rz.g()], w=[BK[1]])
                tt(t1.f32(), O1s.f32(), banks[0][:], ALU.mult, r=[O1s.g(), BK[0]], w=[t1.g()])
                tt(O2s.f32(), O2s.f32(), banks[1][:], ALU.mult, r=[O2s.g(), BK[1]], w=[O2s.g()])
                tt(t1.f32(), t1.f32(), O2s.f32(), ALU.add, r=[t1.g(), O2s.g()], w=[t1.g()])
                act(sq.f32(), t1.f32(), AF.Square, r=[t1.g()], w=[sq.g()])
                mm(banks[2][:], ones_f.f32(), sq.f32(), True, True, r=[ones_f.g(), sq.g()], w=[BK[2]])
                ts(rs.f32(), banks[2][:], 1.0 / 128, 1e-5, ALU.mult, ALU.add, r=[BK[2]], w=[rs.g()])
                act(rs.f32(), rs.f32(), AF.Ln, r=[rs.g()], w=[rs.g()])
                act(rs.f32(), rs.f32(), AF.Exp, r=[rs.g()], w=[rs.g()], scale=-0.5)
                tt(t1.f32(), t1.f32(), rs.f32(), ALU.mult, r=[t1.g(), rs.g()], w=[t1.g()])
                stt(t1.f32(), t1.f32(), sm(SM_WSUB), sgT.bf()[:, cs], ALU.mult, ALU.mult, r=[t1.g(), small.g(), sgT.g(c * 1024, (c + 1) * 1024)], w=[t1.g()])
                tt(mT3[:, h, cs], t1.f32(), mT3[:, h, cs], ALU.add, r=[t1.g(), mT_g(h, c * 512, (c + 1) * 512)], w=[mT_g(h, c * 512, (c + 1) * 512)])
        if dbg and b == 0 and stage == 3:
            wa2 = Alloc(arena, wa.p, TOT)
            dtmp = wa2.get(8192)
            cp(dtmp.f32(), mT3[:, 0, :], r=[mT.g()], w=[dtmp.g()])
            dump("dbg_mT0", dtmp.f32(), [128, 2048], r=[dtmp.g()])
        if stage <= 3:
            continue

        WOUT3 = wout.bf().rearrange("p (k n) -> p k n", k=KC)
        dma('pool', WOUT3, wout_d.ap().rearrange("(k p) n -> p k n", p=128), r=[], w=[wout.g()])
        for k in range(KC):
            tt(WOUT3[:, k, :], WOUT3[:, k, :], g1.f32(), ALU.mult, r=[wout.g(k * 2048, (k + 1) * 2048), g1.g()], w=[wout.g(k * 2048, (k + 1) * 2048)])
        WQ3 = hT.bf().rearrange("p (k n) -> p k n", k=KC)
        dma('pool', WQ3, wqry_d.ap().rearrange("(k p) n -> p k n", p=128), r=[], w=[hT.g()])
        wa = Alloc(arena, work_base, TOT)
        xt5 = g1
        xm = [wa.get(4096) for _ in range(2)]
        h2 = [wa.get(2048) for _ in range(2)]
        eidu = [wa.get(512) for _ in range(2)]
        gate = [wa.get(512) for _ in range(2)]
        tmpf = a1
        ot = sh1
        h2T = wa.get(2048)
        qTs = wa.get(4096)
        junkb = wa.get(2048)
        ssb = wa.get(8192)
        wkb = ssb
        tmpfX = ssb.sub(0, 4096)
        junkbX = ssb.sub(4096, 2048)
        v16 = wa.get(1024)
        ixu = wa.get(1024)
        ixf = wa.get(1024)
        c16, posu, pau, pbu, paf, pbf, i1s, i2s, eidf, scm, ee, actv, gl, wgt = [wa.get(512) for _ in range(14)]
        zz = wa.get(32)
        rzz = wa.get(32)
        dgb = [wa.get(256) for _ in range(4)]
        NBUF = (TOT - wa.p) // 4096
        assert NBUF >= 5, NBUF
        cbuf = [wa.get(4096) for _ in range(NBUF)]
        H2T3 = h2T.bf().rearrange("p (k t) -> p k t", k=KC)
        QTS3 = qTs.bf().rearrange("p (q t) -> p q t", q=16)
        SUBK3 = subkT.bf().rearrange("p (q n) -> p q n", q=16)
        SS3 = ssb.f32().rearrange("p (q n) -> p q n", q=16)
        WK3 = wkb.f32().rearrange("p (q n) -> p q n", q=16)
        V16 = v16.f32().rearrange("p (q j) -> p q j", q=16)
        IXU = ixu.u32().rearrange("p (q j) -> p q j", q=16)
        ntiles = NT if not dbg else 2

        def gen(name, r, w, eng='dve', **kw):
            P.add(eng, lambda e: getattr(e, name)(**kw), r=r, w=w)

        def stageX(i):
            par = i % 2
            xm_, h2_, eidu_, gate_ = xm[par], h2[par], eidu[par], gate[par]
            t0 = i * 128
            dma('sp', xt5.f32(), x_d.ap()[b, t0:t0 + 128, :], r=[], w=[xt5.g()])
            for half in range(2):
                for k in range(KC):
                    mm(banks[2 + half][:], mT3[:, k, t0:t0 + 128], WOUT3[:, k, half * 512:(half + 1) * 512], k == 0, k == KC - 1,
                       r=[mT_g(k, t0, t0 + 128), wout.g()], w=[BK[2 + half]])
            for half in range(2):
                hs = slice(half * 512, (half + 1) * 512)
                tt(xm_.f32()[:, hs], banks[2 + half][:], xt5.f32()[:, hs], ALU.add, r=[BK[2 + half], xt5.g()], w=[xm_.g(half * 2048, (half + 1) * 2048)])
            act(junkbX.bf(), xm_.f32(), AF.Square, r=[xm_.g()], w=[junkbX.g(), small.g()], accum_out=sm(SM_SS2))
            rstd_chain(SM_SS2, SM_LN2, SM_RSTD2, D, 1e-6)
            stt(tmpfX.f32(), xm_.f32(), sm(SM_RSTD2), a2.f32(), ALU.mult, ALU.mult, r=[xm_.g(), small.g(), a2.g()], w=[tmpfX.g()])
            tt(h2_.bf(), tmpfX.f32(), sh2.f32(), ALU.add, r=[tmpfX.g(), sh2.g()], w=[h2_.g()])
            for k in range(KC):
                tr(BTb[:, k * 128:(k + 1) * 128], h2_.bf()[:, k * 128:(k + 1) * 128], ident_b.bf(), r=[h2_.g(), ident_b.g()], w=[BK[7]])
            cp(h2T.bf(), BTb, r=[BK[7]], w=[h2T.g()], eng='act')
            for qc in range(16):
                bk = 2 + (qc // 4) % 2
                for k in range(KC):
                    mm(banks[bk][:, (qc % 4) * 128:(qc % 4 + 1) * 128], WQ3[:, k, qc * 128:(qc + 1) * 128], H2T3[:, k, :], k == 0, k == KC - 1,
                       r=[hT.g(), h2T.g()], w=[BK[bk]])
                if qc % 4 == 3:
                    cp(qTs.bf()[:, (qc - 3) * 128:(qc + 1) * 128], banks[bk][:], r=[BK[bk]], w=[qTs.g((qc - 3) * 256, (qc + 1) * 256)], eng='act')
            for qc in range(16):
                bk = 4 + qc // 4
                mm(banks[bk][:, (qc % 4) * 128:(qc % 4 + 1) * 128], QTS3[:, qc, :], SUBK3[:, qc, :], True, True,
                   r=[qTs.g(qc * 256, (qc + 1) * 256), subkT.g()], w=[BK[bk]])
            for q4 in range(4):
                cp(ssb.f32()[:, q4 * 512:(q4 + 1) * 512], banks[4 + q4][:], r=[BK[4 + q4]], w=[ssb.g(q4 * 2048, (q4 + 1) * 2048)], eng='act')
            if dbg and i == 0:
                dump("dbg_xm", xm_.f32(), [128, 1024], r=[xm_.g()])
                dump("dbg_s", ssb.f32(), [128, 2048], r=[ssb.g()])
            sg_ = lambda qc: ssb.g(qc * 512, (qc + 1) * 512)
            wg_ = lambda qc: wkb.g(qc * 512, (qc + 1) * 512)
            vk = lambda qc, hf: f"v16:{qc}:{hf}"
            ik = lambda qc, hf: f"ixu:{qc}:{hf}"
            ALLV = [vk(q_, h_) for q_ in range(16) for h_ in range(2)]
            ALLI = [ik(q_, h_) for q_ in range(16) for h_ in range(2)]
            for qc in range(16):
                gen('max', [sg_(qc), v16.g()], [vk(qc, 0)], out=V16[:, qc, 0:8], in_=SS3[:, qc, :])
            for qc in range(16):
                gen('max_index', [sg_(qc), vk(qc, 0), ixu.g()], [ik(qc, 0)], out=IXU[:, qc, 0:8], in_max=V16[:, qc, 0:8], in_values=SS3[:, qc, :])
            for qc in range(16):
                gen('match_replace', [sg_(qc), vk(qc, 0)], [wg_(qc)], out=WK3[:, qc, :], in_to_replace=V16[:, qc, 0:8], in_values=SS3[:, qc, :], imm_value=NEG)
            for qc in range(16):
                gen('max', [wg_(qc), v16.g()], [vk(qc, 1)], out=V16[:, qc, 8:16], in_=WK3[:, qc, :])
            for qc in range(16):
                gen('max_index', [wg_(qc), vk(qc, 1), ixu.g()], [ik(qc, 1)], out=IXU[:, qc, 8:16], in_max=V16[:, qc, 8:16], in_values=WK3[:, qc, :])
            cp(ixf.f32(), ixu.u32(), r=ALLI, w=[ixf.g()])
            cand, wk2 = ssb, wkb
            C4 = cand.f32().rearrange("p (h n) -> p h n", h=8)
            W4 = wk2.f32().rearrange("p (h n) -> p h n", h=8)
            vf = v16.f32()
            tt(mk(cand.f32(), [[256, 8], [16, 16], [1, 16]]), mk(vf, [[32, 8], [1, 16], [0, 16]]), mk(vf, [[32, 8], [0, 16], [1, 16]], 16), ALU.add,
               r=ALLV, w=[cand.g()])
            C16 = c16.f32().rearrange("p (h j) -> p h j", h=8)
            POS = posu.u32().rearrange("p (h j) -> p h j", h=8)
            cg_ = lambda h_: cand.g(h_ * 1024, (h_ + 1) * 1024)
            w2g_ = lambda h_: wk2.g(h_ * 1024, (h_ + 1) * 1024)
            ck = lambda h_, hf: f"c16:{h_}:{hf}"
            pk = lambda h_, hf: f"pos:{h_}:{hf}"
            ALLC = [ck(h_, f_) for h_ in range(8) for f_ in range(2)]
            ALLP = [pk(h_, f_) for h_ in range(8) for f_ in range(2)]
            for h_ in range(8):
                gen('max', [cg_(h_), c16.g()], [ck(h_, 0)], out=C16[:, h_, 0:8], in_=C4[:, h_, :])
            for h_ in range(8):
                gen('max_index', [cg_(h_), ck(h_, 0), posu.g()], [pk(h_, 0)], out=POS[:, h_, 0:8], in_max=C16[:, h_, 0:8], in_values=C4[:, h_, :])
            for h_ in range(8):
                gen('match_replace', [cg_(h_), ck(h_, 0)], [w2g_(h_)], out=W4[:, h_, :], in_to_replace=C16[:, h_, 0:8], in_values=C4[:, h_, :], imm_value=NEG)
            for h_ in range(8):
                gen('max', [w2g_(h_), c16.g()], [ck(h_, 1)], out=C16[:, h_, 8:16], in_=W4[:, h_, :])
            for h_ in range(8):
                gen('max_index', [w2g_(h_), ck(h_, 1), posu.g()], [pk(h_, 1)], out=POS[:, h_, 8:16], in_max=C16[:, h_, 8:16], in_values=W4[:, h_, :])
            gen('tensor_single_scalar', ALLP, [pau.g()], out=pau.u32(), in_=posu.u32(), scalar=4, op=ALU.logical_shift_right)
            gen('tensor_single_scalar', ALLP, [pbu.g()], out=pbu.u32(), in_=posu.u32(), scalar=15, op=ALU.bitwise_and)
            cp(paf.f32(), pau.u32(), r=[pau.g()], w=[paf.g()])
            cp(pbf.f32(), pbu.u32(), r=[pbu.g()], w=[pbf.g()])
            oh, prod = ssb, wkb
            for (pf_, off_, dst_) in ((paf, 0, i1s), (pbf, 16, i2s)):
                tt(mk(oh.f32(), [[16, 128], [1, 16]]), mk(pf_.f32(), [[1, 128], [0, 16]]), mk(iota16.f32(), [[0, 128], [1, 16]]), ALU.is_equal,
                   r=[pf_.g(), iota16.g()], w=[oh.g()])
                tt(mk(prod.f32(), [[256, 8], [16, 16], [1, 16]]), mk(oh.f32(), [[256, 8], [16, 16], [1, 16]]), mk(ixf.f32(), [[32, 8], [0, 16], [1, 16]], off_), ALU.mult,
                   r=[oh.g(), ixf.g()], w=[prod.g()])
                tred(dst_.f32(), mk(prod.f32(), [[16, 128], [1, 16]]), ALU.add, r=[prod.g()], w=[dst_.g()])
            stt(eidf.f32(), i1s.f32(), 128.0, i2s.f32(), ALU.mult, ALU.add, r=[i1s.g(), i2s.g()], w=[eidf.g()])
            cp(eidu_.u32(), eidf.f32(), r=[eidf.g()], w=[eidu_.g()])
            tt(mk(scm.f32(), [[16, 8], [1, 16]]), mk(c16.f32(), [[16, 8], [1, 16]]), mk(c16.f32(), [[16, 8], [0, 16]]), ALU.subtract, r=ALLC, w=[scm.g()])
            act(ee.f32(), scm.f32(), AF.Exp, r=[scm.g()], w=[ee.g()])
            tred(zz.f32(), mk(ee.f32(), [[16, 8], [1, 16]]), ALU.add, r=[ee.g()], w=[zz.g()])
            gen('reciprocal', [zz.g()], [rzz.g()], out=rzz.f32(), in_=zz.f32())
            tt(mk(gate_.f32(), [[16, 8], [1, 16]]), mk(ee.f32(), [[16, 8], [1, 16]]), mk(rzz.f32(), [[1, 8], [0, 16]]), ALU.mult, r=[ee.g(), rzz.g()], w=[gate_.g()])
            if dbg and i == 0:
                dump("dbg_eid", eidf.f32(), [128, 128], r=[eidf.g()])
                dump("dbg_gate", gate_.f32(), [128, 128], r=[gate_.g()])

        def stageY(i, pump):
            par = i % 2
            xm_, h2_, eidu_, gate_ = xm[par], h2[par], eidu[par], gate[par]
            t0 = i * 128
            EID = eidu_.u32()

            def gather(out_ap, slot, r, w):
                P.add('pool', lambda e: e.indirect_dma_start(out=out_ap, out_offset=None, in_=etab_d.ap(),
                                                             in_offset=bass.IndirectOffsetOnAxis(ap=EID[:, slot:slot + 1], axis=0)),
                      r=r, w=w, dma=True)

            def slot_tail(sl):
                cb = cbuf[sl % NBUF]
                dg_ = dgb[sl % 4]
                act(wgt.f32()[:, sl:sl + 1], gl.f32()[:, sl:sl + 1], AF.Copy, r=[f"gl{sl}", gate_.g(), wgt.g()], w=[f"wgt{sl}"],
                    scale=gate_.f32()[:, sl:sl + 1])
                P.add('act', (lambda dg_=dg_, sl=sl: (lambda e: e.activation(out=dg_.bf(), in_=ident_f.f32(), func=AF.Copy, scale=wgt.f32()[:, sl:sl + 1])))(),
                      r=[ident_f.g(), f"wgt{sl}"], w=[dg_.g()])
                for half in range(2):
                    mm(banks[half][:], dg_.bf(), cb.bf()[:, D + half * 512:D + (half + 1) * 512], sl == 0, sl == 127,
                       r=[dg_.g(), cb.g(2048, 4096)], w=[BK[half]])

            for sl in range(128):
                cb = cbuf[sl % NBUF]
                gather(cb.bf(), sl, r=[eidu_.g()] + ETAB_KEYS, w=[cb.g()])
                P.add('dve', (lambda cb=cb, sl=sl: (lambda e: e.scalar_tensor_tensor(out=junkb.bf(), in0=cb.bf()[:, 0:D], scalar=1.0, in1=h2_.bf(), op0=ALU.mult, op1=ALU.mult,
                                                                                 accum_out=actv.f32()[:, sl:sl + 1])))(),
                      r=[cb.g(0, 2048), h2_.g(), actv.g()], w=[junkb.g(), f"actv{sl}"])
                act(gl.f32()[:, sl:sl + 1], actv.f32()[:, sl:sl + 1], AF.Gelu, r=[f"actv{sl}", gl.g()], w=[f"gl{sl}"])
                if sl >= 1:
                    slot_tail(sl - 1)
                pump(3)
            slot_tail(127)
            for half in range(2):
                hs = slice(half * 512, (half + 1) * 512)
                tt(tmpf.f32()[:, hs], banks[half][:], g2.f32()[:, hs], ALU.mult, r=[BK[half], g2.g()], w=[tmpf.g(half * 2048, (half + 1) * 2048)])
                tt(ot.f32()[:, hs], tmpf.f32()[:, hs], xm_.f32()[:, hs], ALU.add, r=[tmpf.g(half * 2048, (half + 1) * 2048), xm_.g()], w=[ot.g(half * 2048, (half + 1) * 2048)])
            if dbg and i == 0:
                dump("dbg_xo", ot.f32(), [128, 1024], r=[ot.g()])
            act(junkb.bf(), ot.f32(), AF.Square, r=[ot.g()], w=[junkb.g(), small.g()], accum_out=sm(SM_SS3))
            rstd_chain(SM_SS3, SM_LN3, SM_RSTD3, D, 1e-6)
            stt(ot.f32(), ot.f32(), sm(SM_RSTD3), nf_bc.f32(), ALU.mult, ALU.mult, r=[ot.g(), small.g(), nf_bc.g()], w=[ot.g()])
            dma('sp', out_d.ap()[b, t0:t0 + 128, :], ot.f32(), r=[ot.g()], w=[f"out{b}_{i}"])

        def drain(q, n=None):
            k = 0
            while q and (n is None or k < n):
                P.add(*q.pop(0))
                k += 1

        P.capture = []
        stageX(0)
        q = P.capture
        P.capture = None
        drain(q)
        for i in range(ntiles):
            if i + 1 < ntiles:
                P.capture = []
                stageX(i + 1)
                q = P.capture
                P.capture = None
            else:
                q = []
            stageY(i, lambda n: drain(q, n))
            drain(q)

    P.emit(nc, stack)
    stack.close()
    return nc, list(dbg_d.keys())


def _prep_inputs(inputs):
    f = np.float32
    x = np.asarray(inputs['x'], f)
    c = np.asarray(inputs['c'], f)
    cst = _host_consts()
    rep = lambda v, n=128: np.ascontiguousarray(np.broadcast_to(np.asarray(v, f).reshape(1, -1), (n, np.asarray(v).size)))
    nw = np.concatenate([np.asarray(inputs['norm_mix_w'], f).reshape(-1), np.asarray(inputs['norm_ffn_w'], f).reshape(-1),
                         np.asarray(inputs['norm_final_w'], f).reshape(-1)])
    lam = np.concatenate([np.asarray(inputs[k], f).reshape(-1) for k in ('lambda_q1', 'lambda_k1', 'lambda_q2', 'lambda_k2')])
    sk1 = np.asarray(inputs['sub_keys_1'], f)[0]
    sk2 = np.asarray(inputs['sub_keys_2'], f)[0]
    subkT = np.zeros((128, 16, 128), f)
    for h in range(8):
        subkT[:, h * 2 + 0, :] = sk1[h].T
        subkT[:, h * 2 + 1, :] = sk2[h].T
    shared = {
        'w_ada': np.ascontiguousarray(np.asarray(inputs['w_ada'], f)[0]),
        'b_ada_bc': rep(inputs['b_ada']),
        'nw_bc': rep(nw),
        'w_in': np.ascontiguousarray(np.asarray(inputs['w_in'], f)[0]),
        'w_out': np.ascontiguousarray(np.asarray(inputs['w_out'], f)[0]),
        'w_query': np.ascontiguousarray(np.asarray(inputs['w_query'], f)[0]),
        'pool_w': np.ascontiguousarray(np.asarray(inputs['pool_w'], f)[0]),
        'pool_scaleT': np.ascontiguousarray(np.asarray(inputs['pool_scale'], f).reshape(8, 128).T),
        'subkT': np.ascontiguousarray(subkT.reshape(128, 2048)),
        'e_down': np.ascontiguousarray(np.asarray(inputs['expert_down'], f)[0]),
        'e_up': np.ascontiguousarray(np.asarray(inputs['expert_up'], f)[0]),
        'lam_bc': rep(lam),
        'sublnT': np.ascontiguousarray(np.asarray(inputs['subln_w'], f).reshape(128, 1)),
        'relb_bc': rep(np.asarray(inputs['rel_bias'], f).reshape(-1)),
        'masks': np.ascontiguousarray(cst['masks'].reshape(128, -1)),
        'negmask': cst['negmask'],
        'band': np.ascontiguousarray(cst['band'].reshape(128, -1)),
        'ident': cst['ident'],
        'iota16': cst['iota16'],
        'sel0': cst['sel0'],
    }
    in_maps = []
    for core in range(NCORES):
        m = dict(shared)
        m['x'] = np.ascontiguousarray(x[core * NB:(core + 1) * NB])
        cc = c[core * NB:(core + 1) * NB]
        m['cT'] = np.ascontiguousarray(cc.reshape(NB, KC, 128).transpose(2, 0, 1).reshape(128, NB * KC))
        in_maps.append(m)
    return in_maps


_CACHE = {}


def kernel(**inputs):
    in_maps = _prep_inputs(inputs)
    if 'nc' not in _CACHE:
        _CACHE['nc'] = build()[0]
    nc = _CACHE['nc']
    res = run_bass_kernel_spmd(nc, in_maps, core_ids=list(range(NCORES)))
    out = np.concatenate([np.asarray(r['out']) for r in res.results], axis=0)
    return out.astype(np.float32)
```

```python
import os
import math
import contextlib
import numpy as np
import concourse.bass as bass
import concourse.mybir as mybir
from concourse.bass_utils import run_bass_kernel_spmd

dt = mybir.dt
AF = mybir.ActivationFunctionType
ALU = mybir.AluOpType
AX = mybir.AxisListType
F32, BF16, U32 = dt.float32, dt.bfloat16, dt.uint32

NCORES = 8
NB = 2
S = 2048
NT = 16
D = 1024
KC = 8
H = 8
DIN = 6144
NEXP = 16384
OFF = 8.0
NEG = -1.0e30
G = 512


class Prog:
    def __init__(self):
        self.ops = []
        self.last_w = {}
        self.readers = {}
        self.capture = None

    def add(self, eng, fn, r=(), w=(), dma=False):
        if self.capture is not None:
            self.capture.append((eng, fn, r, w, dma))
            return None
        i = len(self.ops)
        deps = set()
        rk = _flat(r)
        wk = _flat(w)
        for k in rk:
            lw = self.last_w.get(k)
            if lw is not None:
                deps.add(lw)
        for k in wk:
            lw = self.last_w.get(k)
            if lw is not None:
                deps.add(lw)
            deps.update(self.readers.get(k, ()))
        deps.discard(i)
        for k in rk:
            self.readers.setdefault(k, []).append(i)
        for k in wk:
            self.last_w[k] = i
            self.readers[k] = []
        self.ops.append(dict(eng=eng, fn=fn, deps=deps, dma=dma, sig=None, need=False, pre=None))
        return i

    def emit(self, nc, stack):
        ops = self.ops
        EPOCH = 30000
        NSLOT = {'sp': 24, 'pool': 24, 'act': 8}
        for o in ops:
            for d in o['deps']:
                p = ops[d]
                if p['eng'] == 'pe' and o['eng'] == 'pe' and not p['dma'] and not o['dma']:
                    continue
                p['need'] = True
        cnt = {e: 0 for e in ('pe', 'act', 'dve', 'pool', 'sp')}
        esems = {e: [] for e in cnt}
        dsems = {q: [stack.enter_context(nc.semaphore(f"d_{q}_{i}")) for i in range(n)] for q, n in NSLOT.items()}
        duse = {q: [0] * n for q, n in NSLOT.items()}
        dnext = {q: 0 for q in NSLOT}
        for o in ops:
            e = o['eng']
            if o['dma']:
                s = dnext[e]
                dnext[e] = (s + 1) % NSLOT[e]
                if duse[e][s] > 0:
                    o['pre'] = (dsems[e][s], 16 * duse[e][s])
                duse[e][s] += 1
                o['sig'] = (dsems[e][s], 16 * duse[e][s])
            elif o['need']:
                ep = cnt[e] // EPOCH
                while len(esems[e]) <= ep:
                    esems[e].append(stack.enter_context(nc.semaphore(f"e_{e}_{len(esems[e])}")))
                cnt[e] += 1
                o['sig'] = (esems[e][ep], cnt[e] - ep * EPOCH)
        by_eng = {e: [o for o in ops if o['eng'] == e] for e in cnt}
        final_waits = []
        for q in NSLOT:
            for s in range(NSLOT[q]):
                if duse[q][s] > 0:
                    final_waits.append((dsems[q][s], 16 * duse[q][s]))

        def run(ename, eng):
            waited = {}
            for o in by_eng[ename]:
                needs = {}
                for d in o['deps']:
                    p = ops[d]
                    if p['eng'] == 'pe' and ename == 'pe' and not p['dma'] and not o['dma']:
                        continue
                    sem, val = p['sig']
                    key = id(sem)
                    if needs.get(key, (None, 0))[1] < val:
                        needs[key] = (sem, val)
                if o['pre'] is not None:
                    sem, val = o['pre']
                    key = id(sem)
                    if needs.get(key, (None, 0))[1] < val:
                        needs[key] = (sem, val)
                for key, (sem, val) in needs.items():
                    if waited.get(key, 0) < val:
                        eng.wait_ge(sem, val)
                        waited[key] = val
                inst = o['fn'](eng)
                if o['sig'] is not None:
                    inst.then_inc(o['sig'][0], 16 if o['dma'] else 1)
            if ename == 'sp':
                for sem, val in final_waits:
                    eng.wait_ge(sem, val)

        with nc.Block() as block:
            @block.tensor
            def _(e):
                run('pe', e)

            @block.scalar
            def _(e):
                run('act', e)

            @block.vector
            def _(e):
                run('dve', e)

            @block.gpsimd
            def _(e):
                run('pool', e)

            @block.sync
            def _(e):
                run('sp', e)


def _flat(x):
    out = []
    for k in x:
        if isinstance(k, (list, tuple, set, range)):
            out.extend(_flat(k))
        else:
            out.append(k)
    return out


class Buf:
    def __init__(self, arena, off, nbytes):
        assert off % 4 == 0 and nbytes % 4 == 0
        self.A, self.off, self.nbytes = arena, off, nbytes

    def g(self, lo=0, hi=None):
        hi = self.nbytes if hi is None else hi
        return range((self.off + lo) // G, (self.off + hi + G - 1) // G)

    def f32(self):
        return self.A[:, self.off // 4:(self.off + self.nbytes) // 4]

    def bf(self):
        return self.A[:, self.off // 4:(self.off + self.nbytes) // 4].bitcast(BF16)

    def u32(self):
        return self.A[:, self.off // 4:(self.off + self.nbytes) // 4].bitcast(U32)

    def sub(self, lo, n):
        return Buf(self.A, self.off + lo, n)


class Alloc:
    def __init__(self, arena, base, limit):
        self.A, self.p, self.limit = arena, base, limit

    def get(self, nbytes):
        nb = (nbytes + G - 1) // G * G
        b = Buf(self.A, self.p, (nbytes + 3) // 4 * 4)
        self.p += nb
        assert self.p <= self.limit, (self.p, self.limit)
        return b


def mk(ap, pattern, extra_off=0):
    return bass.AP(tensor=ap.tensor, offset=ap.offset + extra_off, ap=[list(ap.ap[0])] + [list(p) for p in pattern])


def _t5_bucket(d):
    d = np.maximum(d, 0)
    x = np.maximum(d, 1).astype(np.float32) / np.float32(16)
    large = 16 + (np.log(x).astype(np.float32) / np.float32(math.log(128 / 16)) * np.float32(16)).astype(np.int32)
    large = np.minimum(large, 31)
    return np.where(d < 16, d, large)


def _host_consts():
    kk = np.arange(128)[:, None]
    qq = np.arange(256)[None, :]
    dist = qq - kk
    valid = dist >= 0
    bk = _t5_bucket(dist)
    masks = np.zeros((128, 32, 256), np.float32)
    for b in range(32):
        masks[:, b, :] = (valid & (bk == b)).astype(np.float32)
    negmask = np.where(valid, 0.0, NEG).astype(np.float32)
    band = np.zeros((128, 12, 128), np.float32)
    s = np.arange(128)[:, None]
    t = np.arange(128)[None, :]
    for gi, w in enumerate((2, 4, 8, 16)):
        cnt0 = np.minimum(t + 1, w).astype(np.float32)
        band[:, gi * 3 + 0, :] = ((s <= t) & (s > t - w)) / cnt0 - (s == t)
        band[:, gi * 3 + 1, :] = ((s <= t) & (s > t - w)) / np.float32(w) - (s == t)
        band[:, gi * 3 + 2, :] = (s > 128 + t - w) / np.float32(w)
    ident = np.eye(128, dtype=np.float32)
    iota16 = np.broadcast_to(np.arange(16, dtype=np.float32)[None, :], (128, 16)).copy()
    sel0 = np.zeros((64, 128), np.float32)
    sel0[0, :] = 1.0
    sel0[32, :] = 1.0
    return dict(masks=masks, negmask=negmask, band=band, ident=ident, iota16=iota16, sel0=sel0)


def build(stage=99, dbg=False):
    nc = bass.Bass("TRN2", target_bir_lowering=False)
    P = Prog()

    def din(name, shape, dtype=F32):
        return nc.dram_tensor(name, list(shape), dtype, kind="ExternalInput")

    x_d = din("x", [NB, S, D])
    cT_d = din("cT", [128, NB * KC])
    wada_d = din("w_ada", [D, DIN])
    bada_d = din("b_ada_bc", [128, DIN])
    nw_d = din("nw_bc", [128, 3 * D])
    win_d = din("w_in", [D, DIN])
    wout_d = din("w_out", [D, D])
    wqry_d = din("w_query", [D, 2048])
    poolw_d = din("pool_w", [4, 256, 256])
    psc_d = din("pool_scaleT", [128, 8])
    subk_d = din("subkT", [128, 16 * 128])
    edown_d = din("e_down", [NEXP, D])
    eup_d = din("e_up", [NEXP, D])
    lam_d = din("lam_bc", [128, 256])
    subln_d = din("sublnT", [128, 1])
    relb_d = din("relb_bc", [128, 256])
    masks_d = din("masks", [128, 32 * 256])
    negm_d = din("negmask", [128, 256])
    band_d = din("band", [128, 12 * 128])
    ident_d = din("ident", [128, 128])
    iota_d = din("iota16", [128, 16])
    sel0_d = din("sel0", [64, 128])
    out_d = nc.dram_tensor("out", [NB, S, D], F32, kind="ExternalOutput")
    etab_d = nc.dram_tensor("etab", [NEXP, 2 * D], BF16, kind="Internal")
    dbg_d = {}

    def dbg_out(name, shape):
        dbg_d[name] = nc.dram_tensor(name, list(shape), F32, kind="ExternalOutput")
        return dbg_d[name]

    stack = contextlib.ExitStack()
    TOT = 212480
    arena = stack.enter_context(nc.sbuf_tensor("arena", [128, TOT // 4], F32))
    banks = [stack.enter_context(nc.psum_tensor(f"B{i}", [128, 512], F32)) for i in range(8)]
    BK = [f"B{i}" for i in range(8)]

    al = Alloc(arena, 0, TOT)
    ident_f = al.get(512)
    ident_b = al.get(256)
    ones_b = al.get(256)
    ones_f = al.get(512)
    sel0 = al.get(512)
    relb = al.get(1024)
    rb31m = al.get(32)
    biasT = al.get(8 * 1024)
    lamv = al.get(1024)
    small = al.get(512)
    pscT = al.get(32)
    band = al.get(12 * 128 * 2)
    subkT = al.get(16 * 128 * 2)
    iota16 = al.get(64)
    nf_bc = al.get(4096)
    sh1 = al.get(4096)
    a1 = al.get(4096)
    g1 = al.get(4096)
    sh2 = al.get(4096)
    a2 = al.get(4096)
    g2 = al.get(4096)
    hT = al.get(KC * S * 2)
    mT = al.get(KC * S * 2)
    wout = al.get(KC * D * 2)
    work_base = al.p
    SM = small.f32()
    (SM_S1, SM_S2, SM_E1, SM_E2, SM_NLAM, SM_WSUB, SM_SS, SM_RSTD, SM_LN, SM_SS2, SM_RSTD2, SM_SS3, SM_RSTD3,
     SM_LN2, SM_LN3, SM_SUBLN) = range(16)

    def sm(i):
        return SM[:, i:i + 1]

    def smg(i):
        return small.g()

    def dma(q, out, in_, r, w):
        P.add(q, lambda e: e.dma_start(out=out, in_=in_), r=r, w=w, dma=True)

    def mm(out, lhsT, rhs, start, stop, r, w, **kw):
        P.add('pe', lambda e: e.matmul(out, lhsT, rhs, start=start, stop=stop, **kw), r=r, w=w)

    def tr(out, in_, ident, r, w):
        P.add('pe', lambda e: e.transpose(out, in_, ident), r=r, w=w)

    def act(out, in_, func, r, w, bias=None, scale=None, accum_out=None, eng='act'):
        def f(e):
            kw = {}
            if bias is not None:
                kw['bias'] = bias
            if scale is not None:
                kw['scale'] = scale
            if accum_out is not None:
                kw['accum_out'] = accum_out
            return e.activation(out=out, in_=in_, func=func, **kw)
        P.add('act', f, r=r, w=w)

    def tt(out, in0, in1, op, r, w, eng='dve'):
        P.add(eng, lambda e: e.tensor_tensor(out=out, in0=in0, in1=in1, op=op), r=r, w=w)

    def ts(out, in0, s1, s2, op0, op1, r, w, eng='dve'):
        if op1 is None:
            P.add(eng, lambda e: e.tensor_scalar(out=out, in0=in0, scalar1=s1, scalar2=None, op0=op0), r=r, w=w)
        else:
            P.add(eng, lambda e: e.tensor_scalar(out=out, in0=in0, scalar1=s1, scalar2=s2, op0=op0, op1=op1), r=r, w=w)

    def stt(out, in0, scalar, in1, op0, op1, r, w):
        P.add('dve', lambda e: e.scalar_tensor_tensor(out=out, in0=in0, scalar=scalar, in1=in1, op0=op0, op1=op1), r=r, w=w)

    def cp(out, in_, r, w, eng='dve'):
        if eng == 'act':
            P.add('act', lambda e: e.copy(out=out, in_=in_), r=r, w=w)
        else:
            P.add(eng, lambda e: e.tensor_copy(out=out, in_=in_), r=r, w=w)

    def ttr(out, in0, in1, accum_out, r, w):
        P.add('dve', lambda e: e.scalar_tensor_tensor(out=out, in0=in0, scalar=1.0, in1=in1, op0=ALU.mult, op1=ALU.mult,
                                                     accum_out=accum_out), r=r, w=w)

    def tred(out, in_, op, r, w):
        P.add('dve', lambda e: e.tensor_reduce(out=out, in_=in_, axis=AX.X, op=op), r=r, w=w)

    def memset(ap, val, r, w, eng='dve'):
        P.add(eng, lambda e: e.memset(ap, val), r=r, w=w)

    def rstd_chain(ss_i, ln_i, rstd_i, n, eps):
        ts(sm(ln_i), sm(ss_i), 1.0 / n, eps, ALU.mult, ALU.add, r=[small.g()], w=[small.g()])
        act(sm(ln_i), sm(ln_i), AF.Ln, r=[small.g()], w=[small.g()])
        act(sm(rstd_i), sm(ln_i), AF.Exp, r=[small.g()], w=[small.g()], scale=-0.5)

    def dump(name, ap_sb, shape, r):
        if name in dbg_d:
            return
        d = dbg_out(name, shape)
        dma('sp', d.ap(), ap_sb, r=r, w=[name])

    dma('sp', ident_f.f32(), ident_d.ap(), r=[], w=[ident_f.g()])
    dma('sp', sel0.f32()[0:64, :], sel0_d.ap(), r=[], w=[sel0.g()])
    dma('sp', relb.f32(), relb_d.ap(), r=[], w=[relb.g()])
    dma('sp', lamv.f32(), lam_d.ap(), r=[], w=[lamv.g()])
    dma('sp', pscT.f32(), psc_d.ap(), r=[], w=[pscT.g()])
    dma('sp', iota16.f32(), iota_d.ap(), r=[], w=[iota16.g()])
    dma('sp', nf_bc.f32(), nw_d.ap()[:, 2 * D:3 * D], r=[], w=[nf_bc.g()])
    dma('sp', sm(SM_SUBLN), subln_d.ap(), r=[], w=[small.g()])
    dma('pool', band.bf(), band_d.ap(), r=[], w=[band.g()])
    dma('pool', subkT.bf(), subk_d.ap(), r=[], w=[subkT.g()])
    ETAB_JOBS = [(ti, r0) for ti in range(2) for r0 in range(0, NEXP, 1024)]
    ETAB_KEYS = [f"etab{ti}_{r0}" for (ti, r0) in ETAB_JOBS]

    def precast(n):
        for _ in range(n):
            if not ETAB_JOBS:
                return
            ti, r0 = ETAB_JOBS.pop(0)
            tbl = (edown_d, eup_d)[ti]
            dma('pool', etab_d.ap()[r0:r0 + 1024, ti * D:(ti + 1) * D], tbl.ap()[r0:r0 + 1024, :], r=[], w=[f"etab{ti}_{r0}"])

    cp(ident_b.bf(), ident_f.f32(), r=[ident_f.g()], w=[ident_b.g()])
    memset(ones_b.bf(), 1.0, r=[], w=[ones_b.g()])
    memset(ones_f.f32(), 1.0, r=[], w=[ones_f.g()])
    ts(rb31m.f32(), relb.f32()[:, 31 * 8:32 * 8], -OFF, None, ALU.add, None, r=[relb.g()], w=[rb31m.g()])
    ts(sm(SM_WSUB), sm(SM_SUBLN), 0.8, None, ALU.mult, None, r=[small.g()], w=[small.g()])
    wa = Alloc(arena, work_base, TOT)
    junk64 = wa.get(256)
    LV = lamv.f32()
    ttr(junk64.f32(), LV[:, 0:64], LV[:, 64:128], sm(SM_S1), r=[lamv.g()], w=[junk64.g(), small.g()])
    ttr(junk64.f32(), LV[:, 128:192], LV[:, 192:256], sm(SM_S2), r=[lamv.g()], w=[junk64.g(), small.g()])
    act(sm(SM_E1), sm(SM_S1), AF.Exp, r=[small.g()], w=[small.g()])
    act(sm(SM_E2), sm(SM_S2), AF.Exp, r=[small.g()], w=[small.g()])
    tt(sm(SM_NLAM), sm(SM_E2), sm(SM_E1), ALU.subtract, r=[small.g()], w=[small.g()])
    ts(sm(SM_NLAM), sm(SM_NLAM), -0.2, None, ALU.add, None, r=[small.g()], w=[small.g()])
    mk_chunk = wa.get(8 * 256 * 4)
    BT3 = biasT.f32().rearrange("p (h q) -> p h q", h=8)
    for h in range(8):
        dma('sp', BT3[:, h, :], negm_d.ap(), r=[], w=[biasT.g(h * 1024, (h + 1) * 1024)])
    for c in range(4):
        dma('sp', mk_chunk.f32(), masks_d.ap()[:, c * 2048:(c + 1) * 2048], r=[], w=[mk_chunk.g()])
        MC = mk_chunk.f32().rearrange("p (b q) -> p b q", b=8)
        for bb in range(8):
            b = c * 8 + bb
            for h in range(8):
                stt(BT3[:, h, :], MC[:, bb, :], relb.f32()[:, b * 8 + h:b * 8 + h + 1], BT3[:, h, :], ALU.mult, ALU.add,
                    r=[mk_chunk.g(), relb.g(), biasT.g(h * 1024, (h + 1) * 1024)], w=[biasT.g(h * 1024, (h + 1) * 1024)])
    ts(biasT.f32(), biasT.f32(), -OFF, None, ALU.add, None, r=[biasT.g()], w=[biasT.g()])
    if dbg:
        dump("dbg_bias", biasT.f32(), [128, 2048], r=[biasT.g()])
        dump("dbg_small", small.f32()[:, 0:16], [128, 16], r=[small.g()])

    hT3 = hT.bf().rearrange("p (k t) -> p k t", k=KC)
    mT3 = mT.bf().rearrange("p (k t) -> p k t", k=KC)

    def hT_g(c0, c1):
        return [hT.g(k * S * 2 + c0 * 2, k * S * 2 + c1 * 2) for k in range(KC)]

    def mT_g(k, c0, c1):
        return mT.g(k * S * 2 + c0 * 2, k * S * 2 + c1 * 2)

    BTb = banks[7][:].bitcast(BF16)

    for b in range(NB):
        wa = Alloc(arena, work_base, TOT)
        cact = wa.get(64)
        crep = wa.get(KC * 128 * 4)
        wblk = [wa.get(KC * 512 * 4) for _ in range(2)]
        bblk = [wa.get(2048) for _ in range(2)]
        mtmp = wa.get(2048)
        nwt = wa.get(4096)
        cin = wa.get(64)
        dma('sp', cin.f32()[:, 0:KC], cT_d.ap()[:, b * KC:(b + 1) * KC], r=[], w=[cin.g()])
        act(cact.f32()[:, 0:KC], cin.f32()[:, 0:KC], AF.Silu, r=[cin.g()], w=[cact.g()])
        cp(crep.f32().rearrange("p (k m) -> p k m", k=KC), mk(cact.f32(), [[1, KC], [0, 128]]), r=[cact.g()], w=[crep.g()])
        CR = crep.f32().rearrange("p (k m) -> p k m", k=KC)
        wada_v = wada_d.ap().rearrange("(k p) n -> p k n", p=128)
        dsts = [sh1, sh1, a1, a1, g1, g1, sh2, sh2, a2, a2, g2, g2]
        for nb_ in range(12):
            wb = wblk[nb_ % 2]
            bb_ = bblk[nb_ % 2]
            dma('sp', wb.f32().rearrange("p (k n) -> p k n", k=KC), wada_v[:, :, nb_ * 512:(nb_ + 1) * 512], r=[], w=[wb.g()])
            dma('sp', bb_.f32(), bada_d.ap()[:, nb_ * 512:(nb_ + 1) * 512], r=[], w=[bb_.g()])
            WB = wb.f32().rearrange("p (k n) -> p k n", k=KC)
            for k in range(KC):
                mm(banks[6][:], CR[:, k, :], WB[:, k, :], k == 0, k == KC - 1, r=[crep.g(), wb.g()], w=[BK[6]])
            dst = dsts[nb_]
            half = nb_ % 2
            dst_ap = dst.f32()[:, half * 512:(half + 1) * 512]
            dg_ = dst.g(half * 2048, (half + 1) * 2048)
            if nb_ in (2, 3, 8, 9):
                which = 0 if nb_ in (2, 3) else 1
                dma('sp', nwt.f32()[:, 0:512], nw_d.ap()[:, which * D + half * 512: which * D + (half + 1) * 512], r=[], w=[nwt.g()])
                tt(mtmp.f32(), banks[6][:], bb_.f32(), ALU.add, r=[BK[6], bb_.g()], w=[mtmp.g()])
                stt(dst_ap, mtmp.f32(), 1.0, nwt.f32()[:, 0:512], ALU.add, ALU.mult, r=[mtmp.g(), nwt.g()], w=[dg_])
            else:
                tt(dst_ap, banks[6][:], bb_.f32(), ALU.add, r=[BK[6], bb_.g()], w=[dg_])
        if dbg and b == 0:
            dump("dbg_a1", a1.f32(), [128, 1024], r=[a1.g()])
            dump("dbg_g2", g2.f32(), [128, 1024], r=[g2.g()])

        wa = Alloc(arena, work_base, TOT)
        xt = [wa.get(4096) for _ in range(2)]
        tmpf = wa.get(4096)
        junkb = wa.get(2048)
        hbf = wa.get(2048)
        for i in range(NT):
            xb = xt[i % 2]
            dma('sp', xb.f32(), x_d.ap()[b, i * 128:(i + 1) * 128, :], r=[], w=[xb.g()])
            act(junkb.bf(), xb.f32(), AF.Square, r=[xb.g()], w=[junkb.g(), small.g()], accum_out=sm(SM_SS))
            rstd_chain(SM_SS, SM_LN, SM_RSTD, D, 1e-6)
            stt(tmpf.f32(), xb.f32(), sm(SM_RSTD), a1.f32(), ALU.mult, ALU.mult, r=[xb.g(), small.g(), a1.g()], w=[tmpf.g()])
            tt(hbf.bf(), tmpf.f32(), sh1.f32(), ALU.add, r=[tmpf.g(), sh1.g()], w=[hbf.g()])
            for k in range(KC):
                tr(BTb[:, k * 128:(k + 1) * 128], hbf.bf()[:, k * 128:(k + 1) * 128], ident_b.bf(), r=[hbf.g(), ident_b.g()], w=[BK[7]])
            cp(hT3[:, :, i * 128:(i + 1) * 128], BTb.rearrange("p (k t) -> p k t", k=KC), r=[BK[7]], w=hT_g(i * 128, (i + 1) * 128), eng='act')
        if dbg and b == 0:
            wa2 = Alloc(arena, wa.p, TOT)
            dtmp = wa2.get(8192)
            cp(dtmp.f32(), hT3[:, 0, :], r=hT_g(0, S), w=[dtmp.g()])
            dump("dbg_hT0", dtmp.f32(), [128, 2048], r=[dtmp.g()])
        if stage <= 1:
            continue

        win_v = win_d.ap().rearrange("(k p) n -> p k n", p=128)
        wa = Alloc(arena, work_base, TOT)
        wp = wa.get(KC * 256 * 2)
        wgp = wa.get(KC * 256 * 2)
        pw = wa.get(2 * 256 * 2)
        pg = wa.get(NT * 256 * 2)
        pT = [wa.get(512 * 2) for _ in range(2)]
        sg = wa.get(512 * 4)
        WP = wp.bf().rearrange("p (k n) -> p k n", k=KC)
        WGP = wgp.bf().rearrange("p (k n) -> p k n", k=KC)
        PW = pw.bf().rearrange("p (c n) -> p c n", c=2)
        PG = pg.bf().rearrange("p (i n) -> p i n", i=NT)
        BAND = band.bf().rearrange("p (j n) -> p j n", j=12)
        for g in range(4):
            dma('pool', WP, win_v[:, :, 3072 + g * 256:3072 + (g + 1) * 256], r=[], w=[wp.g()])
            dma('pool', WGP, win_v[:, :, 5120 + g * 256:5120 + (g + 1) * 256], r=[], w=[wgp.g()])
            dma('pool', PW, poolw_d.ap()[g].rearrange("(c p) n -> p c n", p=128), r=[], w=[pw.g()])
            for i in range(NT):
                for k in range(KC):
                    mm(banks[7][:, (i % 2) * 256:(i % 2 + 1) * 256], hT3[:, k, i * 128:(i + 1) * 128], WP[:, k, :], k == 0, k == KC - 1,
                       r=[hT_g(i * 128, (i + 1) * 128), wp.g()], w=[BK[7]])
                if i % 2 == 1:
                    cp(pg.bf()[:, (i - 1) * 256:(i + 1) * 256], banks[7][:], r=[BK[7]], w=[pg.g((i - 1) * 512, (i + 1) * 512)], eng='act')
            for c in range(4):
                for cc in range(2):
                    for il in range(4):
                        i = c * 4 + il
                        cur = g * 3 + (0 if i == 0 else 1)
                        mm(banks[cc][:, il * 128:(il + 1) * 128], PG[:, i, cc * 128:(cc + 1) * 128], BAND[:, cur, :], True, i == 0,
                           r=[pg.g(i * 512, (i + 1) * 512), band.g()], w=[BK[cc]])
                        if i > 0:
                            mm(banks[cc][:, il * 128:(il + 1) * 128], PG[:, i - 1, cc * 128:(cc + 1) * 128], BAND[:, g * 3 + 2, :], False, True,
                               r=[pg.g((i - 1) * 512, i * 512), band.g()], w=[BK[cc]])
                    cp(pT[cc].bf(), banks[cc][:], r=[BK[cc]], w=[pT[cc].g()], eng='act')
                for ec in range(2):
                    kch = g * 2 + ec
                    mm(banks[2][:], PW[:, 0, ec * 128:(ec + 1) * 128], pT[0].bf(), True, False, r=[pw.g(), pT[0].g()], w=[BK[2]])
                    mm(banks[2][:], PW[:, 1, ec * 128:(ec + 1) * 128], pT[1].bf(), False, True, r=[pw.g(), pT[1].g()], w=[BK[2]])
                    for k in range(KC):
                        mm(banks[3][:], WGP[:, k, ec * 128:(ec + 1) * 128], hT3[:, k, c * 512:(c + 1) * 512], k == 0, k == KC - 1,
                           r=[wgp.g(), hT_g(c * 512, (c + 1) * 512)], w=[BK[3]])
                    act(sg.f32(), banks[3][:], AF.Sigmoid, r=[BK[3]], w=[sg.g()])
                    stt(mT3[:, kch, c * 512:(c + 1) * 512], banks[2][:], pscT.f32()[:, kch:kch + 1], sg.f32(), ALU.mult, ALU.mult,
                        r=[BK[2], pscT.g(), sg.g()], w=[mT_g(kch, c * 512, (c + 1) * 512)])
        if dbg and b == 0 and stage == 2:
            wa2 = Alloc(arena, wa.p, TOT)
            dtmp = wa2.get(8192)
            for kk_ in (0, 5):
                cp(dtmp.f32(), mT3[:, kk_, :], r=[mT.g()], w=[dtmp.g()])
                dump(f"dbg_mT{kk_}", dtmp.f32(), [128, 2048], r=[dtmp.g()])
        if stage <= 2:
            continue

        wa = Alloc(arena, work_base, TOT)
        wq = wa.get(KC * 128 * 2)
        wk_ = wa.get(KC * 128 * 2)
        wv = wa.get(KC * 128 * 2)
        wg = wa.get(KC * 128 * 2)
        QT = wa.get(S * 2)
        KT = wa.get(S * 2)
        Vb = wa.get(NT * 128 * 2)
        sgT = wa.get(S * 2)
        PT = [[wa.get(512 * 2) for _ in range(2)] for _ in range(2)]
        ntmp = [wa.get(256 * 4) for _ in range(2)]
        O1s = wa.get(2048)
        O2s = wa.get(2048)
        lnz = wa.get(2048)
        rz = wa.get(2048)
        sq = wa.get(2048)
        t1 = wa.get(2048)
        rs = wa.get(2048)
        WQ = wq.bf().rearrange("p (k n) -> p k n", k=KC)
        WK = wk_.bf().rearrange("p (k n) -> p k n", k=KC)
        WV = wv.bf().rearrange("p (k n) -> p k n", k=KC)
        WG = wg.bf().rearrange("p (k n) -> p k n", k=KC)
        V3 = Vb.bf().rearrange("p (i n) -> p i n", i=NT)
        nheads = H if stage > 3 or not dbg else 1
        for h in range(nheads):
            dma('pool', WQ, win_v[:, :, h * 128:(h + 1) * 128], r=[], w=[wq.g()])
            dma('pool', WK, win_v[:, :, 1024 + h * 128:1024 + (h + 1) * 128], r=[], w=[wk_.g()])
            dma('pool', WV, win_v[:, :, 2048 + h * 128:2048 + (h + 1) * 128], r=[], w=[wv.g()])
            dma('pool', WG, win_v[:, :, 4096 + h * 128:4096 + (h + 1) * 128], r=[], w=[wg.g()])
            precast(4)
            pj = 0
            for (W_, wb_, dst, mode) in ((WQ, wq, QT, 'q'), (WK, wk_, KT, 'k'), (WG, wg, sgT, 'g')):
                for c in range(4):
                    bk = 6 + (pj % 2)
                    pj += 1
                    for k in range(KC):
                        mm(banks[bk][:], W_[:, k, :], hT3[:, k, c * 512:(c + 1) * 512], k == 0, k == KC - 1,
                           r=[wb_.g(), hT_g(c * 512, (c + 1) * 512)], w=[BK[bk]])
                    o_ap = dst.bf()[:, c * 512:(c + 1) * 512]
                    o_g = dst.g(c * 1024, (c + 1) * 1024)
                    if mode == 'q':
                        P.add('act', (lambda o_ap=o_ap, bk=bk: (lambda e: e.mul(out=o_ap, in_=banks[bk][:], mul=0.125)))(), r=[BK[bk]], w=[o_g])
                    elif mode == 'k':
                        cp(o_ap, banks[bk][:], r=[BK[bk]], w=[o_g], eng='act')
                    else:
                        act(o_ap, banks[bk][:], AF.Sigmoid, r=[BK[bk]], w=[o_g])
            for i in range(NT):
                bk = 6 + ((i // 4) % 2)
                for k in range(KC):
                    mm(banks[bk][:, (i % 4) * 128:(i % 4 + 1) * 128], hT3[:, k, i * 128:(i + 1) * 128], WV[:, k, :], k == 0, k == KC - 1,
                       r=[hT_g(i * 128, (i + 1) * 128), wv.g()], w=[BK[bk]])
                if i % 4 == 3:
                    cp(Vb.bf()[:, (i - 3) * 128:(i + 1) * 128], banks[bk][:], r=[BK[bk]], w=[Vb.g((i - 3) * 256, (i + 1) * 256)], eng='act')

            def qk(c, j):
                col0 = max(0, j - 4 * c) * 128
                par = j % 2
                for m in range(2):
                    bk = m * 2 + par
                    mm(banks[bk][:, col0:512], KT.bf()[m * 64:(m + 1) * 64, j * 128:(j + 1) * 128],
                       QT.bf()[m * 64:(m + 1) * 64, c * 512 + col0:(c + 1) * 512], True, True,
                       r=[KT.g(j * 256, (j + 1) * 256), QT.g(c * 1024 + col0 * 2, (c + 1) * 1024)], w=[BK[bk]])

            for c in range(4):
                jmax = 4 * c + 3
                qk(c, 0)
                for j in range(jmax + 1):
                    if j + 1 <= jmax:
                        qk(c, j + 1)
                    col0 = max(0, j - 4 * c) * 128
                    par = j % 2
                    il_lo = max(0, j - 4 * c)
                    il_hi = min(3, j + 1 - 4 * c)
                    far0 = max(0, j + 2 - 4 * c) * 128
                    for m in range(2):
                        bk = m * 2 + par
                        pt = PT[m][par]
                        if il_hi >= il_lo and il_hi >= 0:
                            n0, n1 = il_lo * 128, (il_hi + 1) * 128
                            b0 = (4 * c + il_lo - j) * 128
                            nn = n1 - n0
                            tt(ntmp[m].f32()[:, 0:nn], banks[bk][:, n0:n1], BT3[:, h, b0:b0 + nn], ALU.add,
                               r=[BK[bk], biasT.g(h * 1024, (h + 1) * 1024)], w=[ntmp[m].g()])
                            act(pt.bf()[:, n0:n1], ntmp[m].f32()[:, 0:nn], AF.Exp, r=[ntmp[m].g()], w=[pt.g(n0 * 2, n1 * 2)])
                        if far0 < 512:
                            act(pt.bf()[:, far0:512], banks[bk][:, far0:512], AF.Exp, r=[BK[bk], rb31m.g()], w=[pt.g(far0 * 2, 1024)],
                                bias=rb31m.f32()[:, h:h + 1])
                        mm(banks[4 + m][:, col0:512], V3[:, j, :], pt.bf()[:, col0:512], j == 0, j == jmax,
                           r=[Vb.g(j * 256, (j + 1) * 256), pt.g(col0 * 2, 1024)], w=[BK[4 + m]], skip_group_check=True)
                        mm(banks[6][m * 32:(m + 1) * 32, col0:512], ones_b.bf()[:, 0:32], pt.bf()[:, col0:512], j == 0, j == jmax,
                           r=[ones_b.g(), pt.g(col0 * 2, 1024)], w=[BK[6]], skip_group_check=True)
                cs = slice(c * 512, (c + 1) * 512)
                cp(O1s.f32(), banks[4][:], r=[BK[4]], w=[O1s.g()], eng='act')
                cp(O2s.f32(), banks[5][:], r=[BK[5]], w=[O2s.g()], eng='act')
                act(lnz.f32()[0:64, :], banks[6][0:64, :], AF.Ln, r=[BK[6]], w=[lnz.g()])
                act(rz.f32()[0:64, :], lnz.f32()[0:64, :], AF.Exp, r=[lnz.g()], w=[rz.g()], scale=-1.0)
                ts(rz.f32()[32:64, :], rz.f32()[32:64, :], SM[32:64, SM_NLAM:SM_NLAM + 1], None, ALU.mult, None, r=[rz.g(), small.g()], w=[rz.g()])
                mm(banks[0][:], sel0.f32()[0:32, :], rz.f32()[0:32, :], True, True, r=[sel0.g(), rz.g()], w=[BK[0]])
                mm(banks[1][:], sel0.f32()[32:64, :], rz.f32()[32:64, :], True, True, r=[sel0.g(), rz.g()], w=[BK[1]])
                tt(t1.f32(), O1s.f32(), banks[0][:], ALU.mult, r=[O1s.g(), BK[0]], w=[t1.g()])
                tt(O2s.f32(), O2s.f32(), banks[1][:], ALU.mult, r=[O2s.g(), BK[1]], w=[O2s.g()])
                tt(t1.f32(), t1.f32(), O2s.f32(), ALU.add, r=[t1.g(), O2s.g()], w=[t1.g()])
                act(sq.f32(), t1.f32(), AF.Square, r=[t1.g()], w=[sq.g()])
                mm(banks[2][:], ones_f.f32(), sq.f32(), True, True, r=[ones_f.g(), sq.g()], w=[BK[2]])
                ts(rs.f32(), banks[2][:], 1.0 / 128, 1e-5, ALU.mult, ALU.add, r=[BK[2]], w=[rs.g()])
                act(rs.f32(), rs.f32(), AF.Ln, r=[rs.g()], w=[rs.g()])
                act(rs.f32(), rs.f32(), AF.Exp, r=[rs.g()], w=[rs.g()], scale=-0.5)
                tt(t1.f32(), t1.f32(), rs.f32(), ALU.mult, r=[t1.g(), rs.g()], w=[t1.g()])
                stt(t1.f32(), t1.f32(), sm(SM_WSUB), sgT.bf()[:, cs], ALU.mult, ALU.mult, r=[t1.g(), small.g(), sgT.g(c * 1024, (c + 1) * 1024)], w=[t1.g()])
                tt(mT3[:, h, cs], t1.f32(), mT3[:, h, cs], ALU.add, r=[t1.g(), mT_g(h, c * 512, (c + 1) * 512)], w=[mT_g(h, c * 512, (c + 1) * 512)])
        if dbg and b == 0 and stage == 3:
            wa2 = Alloc(arena, wa.p, TOT)
            dtmp = wa2.get(8192)
            cp(dtmp.f32(), mT3[:, 0, :], r=[mT.g()], w=[dtmp.g()])
            dump("dbg_mT0", dtmp.f32(), [128, 2048], r=[dtmp.g()])
        if stage <= 3:
            continue

        precast(64)
        WOUT3 = wout.bf().rearrange("p (k n) -> p k n", k=KC)
        dma('pool', WOUT3, wout_d.ap().rearrange("(k p) n -> p k n", p=128), r=[], w=[wout.g()])
        for k in range(KC):
            tt(WOUT3[:, k, :], WOUT3[:, k, :], g1.f32(), ALU.mult, r=[wout.g(k * 2048, (k + 1) * 2048), g1.g()], w=[wout.g(k * 2048, (k + 1) * 2048)])
        WQ3 = hT.bf().rearrange("p (k n) -> p k n", k=KC)
        dma('pool', WQ3, wqry_d.ap().rearrange("(k p) n -> p k n", p=128), r=[], w=[hT.g()])
        wa = Alloc(arena, work_base, TOT)
        xt5 = g1
        xm = [wa.get(4096) for _ in range(2)]
        h2 = [wa.get(2048) for _ in range(2)]
        eidu = [wa.get(512) for _ in range(2)]
        gate = [wa.get(512) for _ in range(2)]
        tmpf = a1
        ot = sh1
        h2T = wa.get(2048)
        qTs = wa.get(4096)
        junkb = wa.get(2048)
        ssb = wa.get(8192)
        wkb = ssb
        tmpfX = ssb.sub(0, 4096)
        junkbX = ssb.sub(4096, 2048)
        v16 = wa.get(1024)
        ixu = wa.get(1024)
        ixf = wa.get(1024)
        c16, posu, pau, pbu, paf, pbf, i1s, i2s, eidf, scm, ee, actv, gl, wgt = [wa.get(512) for _ in range(14)]
        zz = wa.get(32)
        rzz = wa.get(32)
        dgb = [wa.get(256) for _ in range(4)]
        NBUF = (TOT - wa.p) // 4096
        assert NBUF >= 5, NBUF
        cbuf = [wa.get(4096) for _ in range(NBUF)]
        H2T3 = h2T.bf().rearrange("p (k t) -> p k t", k=KC)
        QTS3 = qTs.bf().rearrange("p (q t) -> p q t", q=16)
        SUBK3 = subkT.bf().rearrange("p (q n) -> p q n", q=16)
        SS3 = ssb.f32().rearrange("p (q n) -> p q n", q=16)
        WK3 = wkb.f32().rearrange("p (q n) -> p q n", q=16)
        V16 = v16.f32().rearrange("p (q j) -> p q j", q=16)
        IXU = ixu.u32().rearrange("p (q j) -> p q j", q=16)
        ntiles = NT if not dbg else 2

        def gen(name, r, w, eng='dve', **kw):
            P.add(eng, lambda e: getattr(e, name)(**kw), r=r, w=w)

        def stageX(i):
            par = i % 2
            xm_, h2_, eidu_, gate_ = xm[par], h2[par], eidu[par], gate[par]
            t0 = i * 128
            dma('sp', xt5.f32(), x_d.ap()[b, t0:t0 + 128, :], r=[], w=[xt5.g()])
            for half in range(2):
                for k in range(KC):
                    mm(banks[2 + half][:], mT3[:, k, t0:t0 + 128], WOUT3[:, k, half * 512:(half + 1) * 512], k == 0, k == KC - 1,
                       r=[mT_g(k, t0, t0 + 128), wout.g()], w=[BK[2 + half]])
            for half in range(2):
                hs = slice(half * 512, (half + 1) * 512)
                tt(xm_.f32()[:, hs], banks[2 + half][:], xt5.f32()[:, hs], ALU.add, r=[BK[2 + half], xt5.g()], w=[xm_.g(half * 2048, (half + 1) * 2048)])
            act(junkbX.bf(), xm_.f32(), AF.Square, r=[xm_.g()], w=[junkbX.g(), small.g()], accum_out=sm(SM_SS2))
            rstd_chain(SM_SS2, SM_LN2, SM_RSTD2, D, 1e-6)
            stt(tmpfX.f32(), xm_.f32(), sm(SM_RSTD2), a2.f32(), ALU.mult, ALU.mult, r=[xm_.g(), small.g(), a2.g()], w=[tmpfX.g()])
            tt(h2_.bf(), tmpfX.f32(), sh2.f32(), ALU.add, r=[tmpfX.g(), sh2.g()], w=[h2_.g()])
            for k in range(KC):
                tr(BTb[:, k * 128:(k + 1) * 128], h2_.bf()[:, k * 128:(k + 1) * 128], ident_b.bf(), r=[h2_.g(), ident_b.g()], w=[BK[7]])
            cp(h2T.bf(), BTb, r=[BK[7]], w=[h2T.g()], eng='act')
            for qc in range(16):
                bk = 2 + (qc // 4) % 2
                for k in range(KC):
                    mm(banks[bk][:, (qc % 4) * 128:(qc % 4 + 1) * 128], WQ3[:, k, qc * 128:(qc + 1) * 128], H2T3[:, k, :], k == 0, k == KC - 1,
                       r=[hT.g(), h2T.g()], w=[BK[bk]])
                if qc % 4 == 3:
                    cp(qTs.bf()[:, (qc - 3) * 128:(qc + 1) * 128], banks[bk][:], r=[BK[bk]], w=[qTs.g((qc - 3) * 256, (qc + 1) * 256)], eng='act')
            for qc in range(16):
                bk = 4 + qc // 4
                mm(banks[bk][:, (qc % 4) * 128:(qc % 4 + 1) * 128], QTS3[:, qc, :], SUBK3[:, qc, :], True, True,
                   r=[qTs.g(qc * 256, (qc + 1) * 256), subkT.g()], w=[BK[bk]])
            for q4 in range(4):
                cp(ssb.f32()[:, q4 * 512:(q4 + 1) * 512], banks[4 + q4][:], r=[BK[4 + q4]], w=[ssb.g(q4 * 2048, (q4 + 1) * 2048)], eng='act')
            if dbg and i == 0:
                dump("dbg_xm", xm_.f32(), [128, 1024], r=[xm_.g()])
                dump("dbg_s", ssb.f32(), [128, 2048], r=[ssb.g()])
            sg_ = lambda qc: ssb.g(qc * 512, (qc + 1) * 512)
            wg_ = lambda qc: wkb.g(qc * 512, (qc + 1) * 512)
            vk = lambda qc, hf: f"v16:{qc}:{hf}"
            ik = lambda qc, hf: f"ixu:{qc}:{hf}"
            ALLV = [vk(q_, h_) for q_ in range(16) for h_ in range(2)]
            ALLI = [ik(q_, h_) for q_ in range(16) for h_ in range(2)]
            for qc in range(16):
                gen('max', [sg_(qc), v16.g()], [vk(qc, 0)], out=V16[:, qc, 0:8], in_=SS3[:, qc, :])
            for qc in range(16):
                gen('max_index', [sg_(qc), vk(qc, 0), ixu.g()], [ik(qc, 0)], out=IXU[:, qc, 0:8], in_max=V16[:, qc, 0:8], in_values=SS3[:, qc, :])
            for qc in range(16):
                gen('match_replace', [sg_(qc), vk(qc, 0)], [wg_(qc)], out=WK3[:, qc, :], in_to_replace=V16[:, qc, 0:8], in_values=SS3[:, qc, :], imm_value=NEG)
            for qc in range(16):
                gen('max', [wg_(qc), v16.g()], [vk(qc, 1)], out=V16[:, qc, 8:16], in_=WK3[:, qc, :])
            for qc in range(16):
                gen('max_index', [wg_(qc), vk(qc, 1), ixu.g()], [ik(qc, 1)], out=IXU[:, qc, 8:16], in_max=V16[:, qc, 8:16], in_values=WK3[:, qc, :])
            cp(ixf.f32(), ixu.u32(), r=ALLI, w=[ixf.g()])
            cand, wk2 = ssb, wkb
            C4 = cand.f32().rearrange("p (h n) -> p h n", h=8)
            W4 = wk2.f32().rearrange("p (h n) -> p h n", h=8)
            vf = v16.f32()
            tt(mk(cand.f32(), [[256, 8], [16, 16], [1, 16]]), mk(vf, [[32, 8], [1, 16], [0, 16]]), mk(vf, [[32, 8], [0, 16], [1, 16]], 16), ALU.add,
               r=ALLV, w=[cand.g()])
            C16 = c16.f32().rearrange("p (h j) -> p h j", h=8)
            POS = posu.u32().rearrange("p (h j) -> p h j", h=8)
            cg_ = lambda h_: cand.g(h_ * 1024, (h_ + 1) * 1024)
            w2g_ = lambda h_: wk2.g(h_ * 1024, (h_ + 1) * 1024)
            ck = lambda h_, hf: f"c16:{h_}:{hf}"
            pk = lambda h_, hf: f"pos:{h_}:{hf}"
            ALLC = [ck(h_, f_) for h_ in range(8) for f_ in range(2)]
            ALLP = [pk(h_, f_) for h_ in range(8) for f_ in range(2)]
            for h_ in range(8):
                gen('max', [cg_(h_), c16.g()], [ck(h_, 0)], out=C16[:, h_, 0:8], in_=C4[:, h_, :])
            for h_ in range(8):
                gen('max_index', [cg_(h_), ck(h_, 0), posu.g()], [pk(h_, 0)], out=POS[:, h_, 0:8], in_max=C16[:, h_, 0:8], in_values=C4[:, h_, :])
            for h_ in range(8):
                gen('match_replace', [cg_(h_), ck(h_, 0)], [w2g_(h_)], out=W4[:, h_, :], in_to_replace=C16[:, h_, 0:8], in_values=C4[:, h_, :], imm_value=NEG)
            for h_ in range(8):
                gen('max', [w2g_(h_), c16.g()], [ck(h_, 1)], out=C16[:, h_, 8:16], in_=W4[:, h_, :])
            for h_ in range(8):
                gen('max_index', [w2g_(h_), ck(h_, 1), posu.g()], [pk(h_, 1)], out=POS[:, h_, 8:16], in_max=C16[:, h_, 8:16], in_values=W4[:, h_, :])
            gen('tensor_single_scalar', ALLP, [pau.g()], out=pau.u32(), in_=posu.u32(), scalar=4, op=ALU.logical_shift_right)
            gen('tensor_single_scalar', ALLP, [pbu.g()], out=pbu.u32(), in_=posu.u32(), scalar=15, op=ALU.bitwise_and)
            cp(paf.f32(), pau.u32(), r=[pau.g()], w=[paf.g()])
            cp(pbf.f32(), pbu.u32(), r=[pbu.g()], w=[pbf.g()])
            oh, prod = ssb, wkb
            for (pf_, off_, dst_) in ((paf, 0, i1s), (pbf, 16, i2s)):
                tt(mk(oh.f32(), [[16, 128], [1, 16]]), mk(pf_.f32(), [[1, 128], [0, 16]]), mk(iota16.f32(), [[0, 128], [1, 16]]), ALU.is_equal,
                   r=[pf_.g(), iota16.g()], w=[oh.g()])
                tt(mk(prod.f32(), [[256, 8], [16, 16], [1, 16]]), mk(oh.f32(), [[256, 8], [16, 16], [1, 16]]), mk(ixf.f32(), [[32, 8], [0, 16], [1, 16]], off_), ALU.mult,
                   r=[oh.g(), ixf.g()], w=[prod.g()])
                tred(dst_.f32(), mk(prod.f32(), [[16, 128], [1, 16]]), ALU.add, r=[prod.g()], w=[dst_.g()])
            stt(eidf.f32(), i1s.f32(), 128.0, i2s.f32(), ALU.mult, ALU.add, r=[i1s.g(), i2s.g()], w=[eidf.g()])
            cp(eidu_.u32(), eidf.f32(), r=[eidf.g()], w=[eidu_.g()])
            tt(mk(scm.f32(), [[16, 8], [1, 16]]), mk(c16.f32(), [[16, 8], [1, 16]]), mk(c16.f32(), [[16, 8], [0, 16]]), ALU.subtract, r=ALLC, w=[scm.g()])
            act(ee.f32(), scm.f32(), AF.Exp, r=[scm.g()], w=[ee.g()])
            tred(zz.f32(), mk(ee.f32(), [[16, 8], [1, 16]]), ALU.add, r=[ee.g()], w=[zz.g()])
            gen('reciprocal', [zz.g()], [rzz.g()], out=rzz.f32(), in_=zz.f32())
            tt(mk(gate_.f32(), [[16, 8], [1, 16]]), mk(ee.f32(), [[16, 8], [1, 16]]), mk(rzz.f32(), [[1, 8], [0, 16]]), ALU.mult, r=[ee.g(), rzz.g()], w=[gate_.g()])
            if dbg and i == 0:
                dump("dbg_eid", eidf.f32(), [128, 128], r=[eidf.g()])
                dump("dbg_gate", gate_.f32(), [128, 128], r=[gate_.g()])

        def stageY(i, pump):
            par = i % 2
            xm_, h2_, eidu_, gate_ = xm[par], h2[par], eidu[par], gate[par]
            t0 = i * 128
            EID = eidu_.u32()

            def gather(out_ap, slot, r, w):
                P.add('pool', lambda e: e.indirect_dma_start(out=out_ap, out_offset=None, in_=etab_d.ap(),
                                                             in_offset=bass.IndirectOffsetOnAxis(ap=EID[:, slot:slot + 1], axis=0)),
                      r=r, w=w, dma=True)

            def slot_tail(sl):
                cb = cbuf[sl % NBUF]
                dg_ = dgb[sl % 4]
                act(wgt.f32()[:, sl:sl + 1], gl.f32()[:, sl:sl + 1], AF.Copy, r=[f"gl{sl}", gate_.g(), wgt.g()], w=[f"wgt{sl}"],
                    scale=gate_.f32()[:, sl:sl + 1])
                P.add('act', (lambda dg_=dg_, sl=sl: (lambda e: e.activation(out=dg_.bf(), in_=ident_f.f32(), func=AF.Copy, scale=wgt.f32()[:, sl:sl + 1])))(),
                      r=[ident_f.g(), f"wgt{sl}"], w=[dg_.g()])
                for half in range(2):
                    mm(banks[half][:], dg_.bf(), cb.bf()[:, D + half * 512:D + (half + 1) * 512], sl == 0, sl == 127,
                       r=[dg_.g(), cb.g(2048, 4096)], w=[BK[half]])

            for sl in range(128):
                cb = cbuf[sl % NBUF]
                gather(cb.bf(), sl, r=[eidu_.g()] + ETAB_KEYS, w=[cb.g()])
                P.add('dve', (lambda cb=cb, sl=sl: (lambda e: e.scalar_tensor_tensor(out=junkb.bf(), in0=cb.bf()[:, 0:D], scalar=1.0, in1=h2_.bf(), op0=ALU.mult, op1=ALU.mult,
                                                                                 accum_out=actv.f32()[:, sl:sl + 1])))(),
                      r=[cb.g(0, 2048), h2_.g(), actv.g()], w=[junkb.g(), f"actv{sl}"])
                act(gl.f32()[:, sl:sl + 1], actv.f32()[:, sl:sl + 1], AF.Gelu, r=[f"actv{sl}", gl.g()], w=[f"gl{sl}"])
                if sl >= 1:
                    slot_tail(sl - 1)
                pump(3)
            slot_tail(127)
            for half in range(2):
                hs = slice(half * 512, (half + 1) * 512)
                tt(tmpf.f32()[:, hs], banks[half][:], g2.f32()[:, hs], ALU.mult, r=[BK[half], g2.g()], w=[tmpf.g(half * 2048, (half + 1) * 2048)])
                tt(ot.f32()[:, hs], tmpf.f32()[:, hs], xm_.f32()[:, hs], ALU.add, r=[tmpf.g(half * 2048, (half + 1) * 2048), xm_.g()], w=[ot.g(half * 2048, (half + 1) * 2048)])
            if dbg and i == 0:
                dump("dbg_xo", ot.f32(), [128, 1024], r=[ot.g()])
            act(junkb.bf(), ot.f32(), AF.Square, r=[ot.g()], w=[junkb.g(), small.g()], accum_out=sm(SM_SS3))
            rstd_chain(SM_SS3, SM_LN3, SM_RSTD3, D, 1e-6)
            stt(ot.f32(), ot.f32(), sm(SM_RSTD3), nf_bc.f32(), ALU.mult, ALU.mult, r=[ot.g(), small.g(), nf_bc.g()], w=[ot.g()])
            dma('sp', out_d.ap()[b, t0:t0 + 128, :], ot.f32(), r=[ot.g()], w=[f"out{b}_{i}"])

        def drain(q, n=None):
            k = 0
            while q and (n is None or k < n):
                P.add(*q.pop(0))
                k += 1

        P.capture = []
        stageX(0)
        q = P.capture
        P.capture = None
        drain(q)
        for i in range(ntiles):
            if i + 1 < ntiles:
                P.capture = []
                stageX(i + 1)
                q = P.capture
                P.capture = None
            else:
                q = []
            stageY(i, lambda n: drain(q, n))
            drain(q)

    P.emit(nc, stack)
    stack.close()
    return nc, list(dbg_d.keys())


def _prep_inputs(inputs):
    f = np.float32
    x = np.asarray(inputs['x'], f)
    c = np.asarray(inputs['c'], f)
    cst = _host_consts()
    rep = lambda v, n=128: np.ascontiguousarray(np.broadcast_to(np.asarray(v, f).reshape(1, -1), (n, np.asarray(v).size)))
    nw = np.concatenate([np.asarray(inputs['norm_mix_w'], f).reshape(-1), np.asarray(inputs['norm_ffn_w'], f).reshape(-1),
                         np.asarray(inputs['norm_final_w'], f).reshape(-1)])
    lam = np.concatenate([np.asarray(inputs[k], f).reshape(-1) for k in ('lambda_q1', 'lambda_k1', 'lambda_q2', 'lambda_k2')])
    sk1 = np.asarray(inputs['sub_keys_1'], f)[0]
    sk2 = np.asarray(inputs['sub_keys_2'], f)[0]
    subkT = np.zeros((128, 16, 128), f)
    for h in range(8):
        subkT[:, h * 2 + 0, :] = sk1[h].T
        subkT[:, h * 2 + 1, :] = sk2[h].T
    shared = {
        'w_ada': np.ascontiguousarray(np.asarray(inputs['w_ada'], f)[0]),
        'b_ada_bc': rep(inputs['b_ada']),
        'nw_bc': rep(nw),
        'w_in': np.ascontiguousarray(np.asarray(inputs['w_in'], f)[0]),
        'w_out': np.ascontiguousarray(np.asarray(inputs['w_out'], f)[0]),
        'w_query': np.ascontiguousarray(np.asarray(inputs['w_query'], f)[0]),
        'pool_w': np.ascontiguousarray(np.asarray(inputs['pool_w'], f)[0]),
        'pool_scaleT': np.ascontiguousarray(np.asarray(inputs['pool_scale'], f).reshape(8, 128).T),
        'subkT': np.ascontiguousarray(subkT.reshape(128, 2048)),
        'e_down': np.ascontiguousarray(np.asarray(inputs['expert_down'], f)[0]),
        'e_up': np.ascontiguousarray(np.asarray(inputs['expert_up'], f)[0]),
        'lam_bc': rep(lam),
        'sublnT': np.ascontiguousarray(np.asarray(inputs['subln_w'], f).reshape(128, 1)),
        'relb_bc': rep(np.asarray(inputs['rel_bias'], f).reshape(-1)),
        'masks': np.ascontiguousarray(cst['masks'].reshape(128, -1)),
        'negmask': cst['negmask'],
        'band': np.ascontiguousarray(cst['band'].reshape(128, -1)),
        'ident': cst['ident'],
        'iota16': cst['iota16'],
        'sel0': cst['sel0'],
    }
    in_maps = []
    for core in range(NCORES):
        m = dict(shared)
        m['x'] = np.ascontiguousarray(x[core * NB:(core + 1) * NB])
        cc = c[core * NB:(core + 1) * NB]
        m['cT'] = np.ascontiguousarray(cc.reshape(NB, KC, 128).transpose(2, 0, 1).reshape(128, NB * KC))
        in_maps.append(m)
    return in_maps


_CACHE = {}


def kernel(**inputs):
    in_maps = _prep_inputs(inputs)
    if 'nc' not in _CACHE:
        _CACHE['nc'] = build()[0]
    nc = _CACHE['nc']
    res = run_bass_kernel_spmd(nc, in_maps, core_ids=list(range(NCORES)))
    out = np.concatenate([np.asarray(r['out']) for r in res.results], axis=0)
    return out.astype(np.float32)
```

```python
import os
import math
import contextlib
import numpy as np
import concourse.bass as bass
import concourse.mybir as mybir
from concourse.bass_utils import run_bass_kernel_spmd

dt = mybir.dt
AF = mybir.ActivationFunctionType
ALU = mybir.AluOpType
AX = mybir.AxisListType
F32, BF16, U32 = dt.float32, dt.bfloat16, dt.uint32

NCORES = 8
NB = 2
S = 2048
NT = 16
D = 1024
KC = 8
H = 8
DIN = 6144
NEXP = 16384
OFF = 8.0
NEG = -1.0e30
G = 512


class Prog:
    def __init__(self):
        self.ops = []
        self.last_w = {}
        self.readers = {}
        self.capture = None

    def add(self, eng, fn, r=(), w=(), dma=False):
        if self.capture is not None:
            self.capture.append((eng, fn, r, w, dma))
            return None
        i = len(self.ops)
        deps = set()
        rk = _flat(r)
        wk = _flat(w)
        for k in rk:
            lw = self.last_w.get(k)
            if lw is not None:
                deps.add(lw)
        for k in wk:
            lw = self.last_w.get(k)
            if lw is not None:
                deps.add(lw)
            deps.update(self.readers.get(k, ()))
        deps.discard(i)
        for k in rk:
            self.readers.setdefault(k, []).append(i)
        for k in wk:
            self.last_w[k] = i
            self.readers[k] = []
        self.ops.append(dict(eng=eng, fn=fn, deps=deps, dma=dma, sig=None, need=False, pre=None))
        return i

    def emit(self, nc, stack):
        ops = self.ops
        EPOCH = 30000
        NSLOT = {'sp': 24, 'pool': 24, 'act': 8}
        for o in ops:
            for d in o['deps']:
                p = ops[d]
                if p['eng'] == 'pe' and o['eng'] == 'pe' and not p['dma'] and not o['dma']:
                    continue
                p['need'] = True
        cnt = {e: 0 for e in ('pe', 'act', 'dve', 'pool', 'sp')}
        esems = {e: [] for e in cnt}
        dsems = {q: [stack.enter_context(nc.semaphore(f"d_{q}_{i}")) for i in range(n)] for q, n in NSLOT.items()}
        duse = {q: [0] * n for q, n in NSLOT.items()}
        dnext = {q: 0 for q in NSLOT}
        for o in ops:
            e = o['eng']
            if o['dma']:
                s = dnext[e]
                dnext[e] = (s + 1) % NSLOT[e]
                if duse[e][s] > 0:
                    o['pre'] = (dsems[e][s], 16 * duse[e][s])
                duse[e][s] += 1
                o['sig'] = (dsems[e][s], 16 * duse[e][s])
            elif o['need']:
                ep = cnt[e] // EPOCH
                while len(esems[e]) <= ep:
                    esems[e].append(stack.enter_context(nc.semaphore(f"e_{e}_{len(esems[e])}")))
                cnt[e] += 1
                o['sig'] = (esems[e][ep], cnt[e] - ep * EPOCH)
        by_eng = {e: [o for o in ops if o['eng'] == e] for e in cnt}
        final_waits = []
        for q in NSLOT:
            for s in range(NSLOT[q]):
                if duse[q][s] > 0:
                    final_waits.append((dsems[q][s], 16 * duse[q][s]))

        def run(ename, eng):
            waited = {}
            for o in by_eng[ename]:
                needs = {}
                for d in o['deps']:
                    p = ops[d]
                    if p['eng'] == 'pe' and ename == 'pe' and not p['dma'] and not o['dma']:
                        continue
                    sem, val = p['sig']
                    key = id(sem)
                    if needs.get(key, (None, 0))[1] < val:
                        needs[key] = (sem, val)
                if o['pre'] is not None:
                    sem, val = o['pre']
                    key = id(sem)
                    if needs.get(key, (None, 0))[1] < val:
                        needs[key] = (sem, val)
                for key, (sem, val) in needs.items():
                    if waited.get(key, 0) < val:
                        eng.wait_ge(sem, val)
                        waited[key] = val
                inst = o['fn'](eng)
                if o['sig'] is not None:
                    inst.then_inc(o['sig'][0], 16 if o['dma'] else 1)
            if ename == 'sp':
                for sem, val in final_waits:
                    eng.wait_ge(sem, val)

        with nc.Block() as block:
            @block.tensor
            def _(e):
                run('pe', e)

            @block.scalar
            def _(e):
                run('act', e)

            @block.vector
            def _(e):
                run('dve', e)

            @block.gpsimd
            def _(e):
                run('pool', e)

            @block.sync
            def _(e):
                run('sp', e)


def _flat(x):
    out = []
    for k in x:
        if isinstance(k, (list, tuple, set, range)):
            out.extend(_flat(k))
        else:
            out.append(k)
    return out


class Buf:
    def __init__(self, arena, off, nbytes):
        assert off % 4 == 0 and nbytes % 4 == 0
        self.A, self.off, self.nbytes = arena, off, nbytes

    def g(self, lo=0, hi=None):
        hi = self.nbytes if hi is None else hi
        return range((self.off + lo) // G, (self.off + hi + G - 1) // G)

    def f32(self):
        return self.A[:, self.off // 4:(self.off + self.nbytes) // 4]

    def bf(self):
        return self.A[:, self.off // 4:(self.off + self.nbytes) // 4].bitcast(BF16)

    def u32(self):
        return self.A[:, self.off // 4:(self.off + self.nbytes) // 4].bitcast(U32)

    def sub(self, lo, n):
        return Buf(self.A, self.off + lo, n)


class Alloc:
    def __init__(self, arena, base, limit):
        self.A, self.p, self.limit = arena, base, limit

    def get(self, nbytes):
        nb = (nbytes + G - 1) // G * G
        b = Buf(self.A, self.p, (nbytes + 3) // 4 * 4)
        self.p += nb
        assert self.p <= self.limit, (self.p, self.limit)
        return b


def mk(ap, pattern, extra_off=0):
    return bass.AP(tensor=ap.tensor, offset=ap.offset + extra_off, ap=[list(ap.ap[0])] + [list(p) for p in pattern])


def _t5_bucket(d):
    d = np.maximum(d, 0)
    x = np.maximum(d, 1).astype(np.float32) / np.float32(16)
    large = 16 + (np.log(x).astype(np.float32) / np.float32(math.log(128 / 16)) * np.float32(16)).astype(np.int32)
    large = np.minimum(large, 31)
    return np.where(d < 16, d, large)


def _host_consts():
    kk = np.arange(128)[:, None]
    qq = np.arange(256)[None, :]
    dist = qq - kk
    valid = dist >= 0
    bk = _t5_bucket(dist)
    masks = np.zeros((128, 32, 256), np.float32)
    for b in range(32):
        masks[:, b, :] = (valid & (bk == b)).astype(np.float32)
    negmask = np.where(valid, 0.0, NEG).astype(np.float32)
    band = np.zeros((128, 12, 128), np.float32)
    s = np.arange(128)[:, None]
    t = np.arange(128)[None, :]
    for gi, w in enumerate((2, 4, 8, 16)):
        cnt0 = np.minimum(t + 1, w).astype(np.float32)
        band[:, gi * 3 + 0, :] = ((s <= t) & (s > t - w)) / cnt0 - (s == t)
        band[:, gi * 3 + 1, :] = ((s <= t) & (s > t - w)) / np.float32(w) - (s == t)
        band[:, gi * 3 + 2, :] = (s > 128 + t - w) / np.float32(w)
    ident = np.eye(128, dtype=np.float32)
    iota16 = np.broadcast_to(np.arange(16, dtype=np.float32)[None, :], (128, 16)).copy()
    sel0 = np.zeros((64, 128), np.float32)
    sel0[0, :] = 1.0
    sel0[32, :] = 1.0
    return dict(masks=masks, negmask=negmask, band=band, ident=ident, iota16=iota16, sel0=sel0)


def build(stage=99, dbg=False):
    nc = bass.Bass("TRN2", target_bir_lowering=False)
    P = Prog()

    def din(name, shape, dtype=F32):
        return nc.dram_tensor(name, list(shape), dtype, kind="ExternalInput")

    x_d = din("x", [NB, S, D])
    cT_d = din("cT", [128, NB * KC])
    wada_d = din("w_ada", [D, DIN])
    bada_d = din("b_ada_bc", [128, DIN])
    nw_d = din("nw_bc", [128, 3 * D])
    win_d = din("w_in", [D, DIN])
    wout_d = din("w_out", [D, D])
    wqry_d = din("w_query", [D, 2048])
    poolw_d = din("pool_w", [4, 256, 256])
    psc_d = din("pool_scaleT", [128, 8])
    subk_d = din("subkT", [128, 16 * 128])
    edown_d = din("e_down", [NEXP, D])
    eup_d = din("e_up", [NEXP, D])
    lam_d = din("lam_bc", [128, 256])
    subln_d = din("sublnT", [128, 1])
    relb_d = din("relb_bc", [128, 256])
    masks_d = din("masks", [128, 32 * 256])
    negm_d = din("negmask", [128, 256])
    band_d = din("band", [128, 12 * 128])
    ident_d = din("ident", [128, 128])
    iota_d = din("iota16", [128, 16])
    sel0_d = din("sel0", [64, 128])
    out_d = nc.dram_tensor("out", [NB, S, D], F32, kind="ExternalOutput")
    etab_d = nc.dram_tensor("etab", [NEXP, 2 * D], BF16, kind="Internal")
    dbg_d = {}

    def dbg_out(name, shape):
        dbg_d[name] = nc.dram_tensor(name, list(shape), F32, kind="ExternalOutput")
        return dbg_d[name]

    stack = contextlib.ExitStack()
    TOT = 212480
    arena = stack.enter_context(nc.sbuf_tensor("arena", [128, TOT // 4], F32))
    banks = [stack.enter_context(nc.psum_tensor(f"B{i}", [128, 512], F32)) for i in range(8)]
    BK = [f"B{i}" for i in range(8)]

    al = Alloc(arena, 0, TOT)
    ident_f = al.get(512)
    ident_b = al.get(256)
    ones_b = al.get(256)
    ones_f = al.get(512)
    sel0 = al.get(512)
    relb = al.get(1024)
    rb31m = al.get(32)
    biasT = al.get(8 * 1024)
    lamv = al.get(1024)
    small = al.get(512)
    pscT = al.get(32)
    band = al.get(12 * 128 * 2)
    subkT = al.get(16 * 128 * 2)
    iota16 = al.get(64)
    nf_bc = al.get(4096)
    sh1 = al.get(4096)
    a1 = al.get(4096)
    g1 = al.get(4096)
    sh2 = al.get(4096)
    a2 = al.get(4096)
    g2 = al.get(4096)
    hT = al.get(KC * S * 2)
    mT = al.get(KC * S * 2)
    wout = al.get(KC * D * 2)
    work_base = al.p
    SM = small.f32()
    (SM_S1, SM_S2, SM_E1, SM_E2, SM_NLAM, SM_WSUB, SM_SS, SM_RSTD, SM_LN, SM_SS2, SM_RSTD2, SM_SS3, SM_RSTD3,
     SM_LN2, SM_LN3, SM_SUBLN) = range(16)

    def sm(i):
        return SM[:, i:i + 1]

    def smg(i):
        return small.g()

    def dma(q, out, in_, r, w):
        P.add(q, lambda e: e.dma_start(out=out, in_=in_), r=r, w=w, dma=True)

    def mm(out, lhsT, rhs, start, stop, r, w, **kw):
        P.add('pe', lambda e: e.matmul(out, lhsT, rhs, start=start, stop=stop, **kw), r=r, w=w)

    def tr(out, in_, ident, r, w):
        P.add('pe', lambda e: e.transpose(out, in_, ident), r=r, w=w)

    def act(out, in_, func, r, w, bias=None, scale=None, accum_out=None, eng='act'):
        def f(e):
            kw = {}
            if bias is not None:
                kw['bias'] = bias
            if scale is not None:
                kw['scale'] = scale
            if accum_out is not None:
                kw['accum_out'] = accum_out
            return e.activation(out=out, in_=in_, func=func, **kw)
        P.add('act', f, r=r, w=w)

    def tt(out, in0, in1, op, r, w, eng='dve'):
        P.add(eng, lambda e: e.tensor_tensor(out=out, in0=in0, in1=in1, op=op), r=r, w=w)

    def ts(out, in0, s1, s2, op0, op1, r, w, eng='dve'):
        if op1 is None:
            P.add(eng, lambda e: e.tensor_scalar(out=out, in0=in0, scalar1=s1, scalar2=None, op0=op0), r=r, w=w)
        else:
            P.add(eng, lambda e: e.tensor_scalar(out=out, in0=in0, scalar1=s1, scalar2=s2, op0=op0, op1=op1), r=r, w=w)

    def stt(out, in0, scalar, in1, op0, op1, r, w):
        P.add('dve', lambda e: e.scalar_tensor_tensor(out=out, in0=in0, scalar=scalar, in1=in1, op0=op0, op1=op1), r=r, w=w)

    def cp(out, in_, r, w, eng='dve'):
        if eng == 'act':
            P.add('act', lambda e: e.copy(out=out, in_=in_), r=r, w=w)
        else:
            P.add(eng, lambda e: e.tensor_copy(out=out, in_=in_), r=r, w=w)

    def ttr(out, in0, in1, accum_out, r, w):
        P.add('dve', lambda e: e.scalar_tensor_tensor(out=out, in0=in0, scalar=1.0, in1=in1, op0=ALU.mult, op1=ALU.mult,
                                                     accum_out=accum_out), r=r, w=w)

    def tred(out, in_, op, r, w):
        P.add('dve', lambda e: e.tensor_reduce(out=out, in_=in_, axis=AX.X, op=op), r=r, w=w)

    def memset(ap, val, r, w, eng='dve'):
        P.add(eng, lambda e: e.memset(ap, val), r=r, w=w)

    def rstd_chain(ss_i, ln_i, rstd_i, n, eps):
        ts(sm(ln_i), sm(ss_i), 1.0 / n, eps, ALU.mult, ALU.add, r=[small.g()], w=[small.g()])
        act(sm(ln_i), sm(ln_i), AF.Ln, r=[small.g()], w=[small.g()])
        act(sm(rstd_i), sm(ln_i), AF.Exp, r=[small.g()], w=[small.g()], scale=-0.5)

    def dump(name, ap_sb, shape, r):
        if name in dbg_d:
            return
        d = dbg_out(name, shape)
        dma('sp', d.ap(), ap_sb, r=r, w=[name])

    dma('sp', ident_f.f32(), ident_d.ap(), r=[], w=[ident_f.g()])
    dma('sp', sel0.f32()[0:64, :], sel0_d.ap(), r=[], w=[sel0.g()])
    dma('sp', relb.f32(), relb_d.ap(), r=[], w=[relb.g()])
    dma('sp', lamv.f32(), lam_d.ap(), r=[], w=[lamv.g()])
    dma('sp', pscT.f32(), psc_d.ap(), r=[], w=[pscT.g()])
    dma('sp', iota16.f32(), iota_d.ap(), r=[], w=[iota16.g()])
    dma('sp', nf_bc.f32(), nw_d.ap()[:, 2 * D:3 * D], r=[], w=[nf_bc.g()])
    dma('sp', sm(SM_SUBLN), subln_d.ap(), r=[], w=[small.g()])
    dma('pool', band.bf(), band_d.ap(), r=[], w=[band.g()])
    dma('pool', subkT.bf(), subk_d.ap(), r=[], w=[subkT.g()])
    ETAB_JOBS = [(ti, r0) for ti in range(2) for r0 in range(0, NEXP, 1024)]
    ETAB_KEYS = [f"etab{ti}_{r0}" for (ti, r0) in ETAB_JOBS]

    def precast(n):
        for _ in range(n):
            if not ETAB_JOBS:
                return
            ti, r0 = ETAB_JOBS.pop(0)
            tbl = (edown_d, eup_d)[ti]
            dma('pool', etab_d.ap()[r0:r0 + 1024, ti * D:(ti + 1) * D], tbl.ap()[r0:r0 + 1024, :], r=[], w=[f"etab{ti}_{r0}"])

    cp(ident_b.bf(), ident_f.f32(), r=[ident_f.g()], w=[ident_b.g()])
    memset(ones_b.bf(), 1.0, r=[], w=[ones_b.g()])
    memset(ones_f.f32(), 1.0, r=[], w=[ones_f.g()])
    ts(rb31m.f32(), relb.f32()[:, 31 * 8:32 * 8], -OFF, None, ALU.add, None, r=[relb.g()], w=[rb31m.g()])
    ts(sm(SM_WSUB), sm(SM_SUBLN), 0.8, None, ALU.mult, None, r=[small.g()], w=[small.g()])
    wa = Alloc(arena, work_base, TOT)
    junk64 = wa.get(256)
    LV = lamv.f32()
    ttr(junk64.f32(), LV[:, 0:64], LV[:, 64:128], sm(SM_S1), r=[lamv.g()], w=[junk64.g(), small.g()])
    ttr(junk64.f32(), LV[:, 128:192], LV[:, 192:256], sm(SM_S2), r=[lamv.g()], w=[junk64.g(), small.g()])
    act(sm(SM_E1), sm(SM_S1), AF.Exp, r=[small.g()], w=[small.g()])
    act(sm(SM_E2), sm(SM_S2), AF.Exp, r=[small.g()], w=[small.g()])
    tt(sm(SM_NLAM), sm(SM_E2), sm(SM_E1), ALU.subtract, r=[small.g()], w=[small.g()])
    ts(sm(SM_NLAM), sm(SM_NLAM), -0.2, None, ALU.add, None, r=[small.g()], w=[small.g()])
    mk_chunk = wa.get(8 * 256 * 4)
    BT3 = biasT.f32().rearrange("p (h q) -> p h q", h=8)
    for h in range(8):
        dma('sp', BT3[:, h, :], negm_d.ap(), r=[], w=[biasT.g(h * 1024, (h + 1) * 1024)])
    for c in range(4):
        dma('sp', mk_chunk.f32(), masks_d.ap()[:, c * 2048:(c + 1) * 2048], r=[], w=[mk_chunk.g()])
        MC = mk_chunk.f32().rearrange("p (b q) -> p b q", b=8)
        for bb in range(8):
            b = c * 8 + bb
            for h in range(8):
                stt(BT3[:, h, :], MC[:, bb, :], relb.f32()[:, b * 8 + h:b * 8 + h + 1], BT3[:, h, :], ALU.mult, ALU.add,
                    r=[mk_chunk.g(), relb.g(), biasT.g(h * 1024, (h + 1) * 1024)], w=[biasT.g(h * 1024, (h + 1) * 1024)])
    ts(biasT.f32(), biasT.f32(), -OFF, None, ALU.add, None, r=[biasT.g()], w=[biasT.g()])
    if dbg:
        dump("dbg_bias", biasT.f32(), [128, 2048], r=[biasT.g()])
        dump("dbg_small", small.f32()[:, 0:16], [128, 16], r=[small.g()])

    hT3 = hT.bf().rearrange("p (k t) -> p k t", k=KC)
    mT3 = mT.bf().rearrange("p (k t) -> p k t", k=KC)

    def hT_g(c0, c1):
        return [hT.g(k * S * 2 + c0 * 2, k * S * 2 + c1 * 2) for k in range(KC)]

    def mT_g(k, c0, c1):
        return mT.g(k * S * 2 + c0 * 2, k * S * 2 + c1 * 2)

    BTb = banks[7][:].bitcast(BF16)

    for b in range(NB):
        wa = Alloc(arena, work_base, TOT)
        cact = wa.get(64)
        crep = wa.get(KC * 128 * 4)
        wblk = [wa.get(KC * 512 * 4) for _ in range(2)]
        bblk = [wa.get(2048) for _ in range(2)]
        mtmp = wa.get(2048)
        nwt = wa.get(4096)
        cin = wa.get(64)
        dma('sp', cin.f32()[:, 0:KC], cT_d.ap()[:, b * KC:(b + 1) * KC], r=[], w=[cin.g()])
        act(cact.f32()[:, 0:KC], cin.f32()[:, 0:KC], AF.Silu, r=[cin.g()], w=[cact.g()])
        cp(crep.f32().rearrange("p (k m) -> p k m", k=KC), mk(cact.f32(), [[1, KC], [0, 128]]), r=[cact.g()], w=[crep.g()])
        CR = crep.f32().rearrange("p (k m) -> p k m", k=KC)
        wada_v = wada_d.ap().rearrange("(k p) n -> p k n", p=128)
        dsts = [sh1, sh1, a1, a1, g1, g1, sh2, sh2, a2, a2, g2, g2]
        for nb_ in range(12):
            wb = wblk[nb_ % 2]
            bb_ = bblk[nb_ % 2]
            dma('sp', wb.f32().rearrange("p (k n) -> p k n", k=KC), wada_v[:, :, nb_ * 512:(nb_ + 1) * 512], r=[], w=[wb.g()])
            dma('sp', bb_.f32(), bada_d.ap()[:, nb_ * 512:(nb_ + 1) * 512], r=[], w=[bb_.g()])
            WB = wb.f32().rearrange("p (k n) -> p k n", k=KC)
            for k in range(KC):
                mm(banks[6][:], CR[:, k, :], WB[:, k, :], k == 0, k == KC - 1, r=[crep.g(), wb.g()], w=[BK[6]])
            dst = dsts[nb_]
            half = nb_ % 2
            dst_ap = dst.f32()[:, half * 512:(half + 1) * 512]
            dg_ = dst.g(half * 2048, (half + 1) * 2048)
            if nb_ in (2, 3, 8, 9):
                which = 0 if nb_ in (2, 3) else 1
                dma('sp', nwt.f32()[:, 0:512], nw_d.ap()[:, which * D + half * 512: which * D + (half + 1) * 512], r=[], w=[nwt.g()])
                tt(mtmp.f32(), banks[6][:], bb_.f32(), ALU.add, r=[BK[6], bb_.g()], w=[mtmp.g()])
                stt(dst_ap, mtmp.f32(), 1.0, nwt.f32()[:, 0:512], ALU.add, ALU.mult, r=[mtmp.g(), nwt.g()], w=[dg_])
            else:
                tt(dst_ap, banks[6][:], bb_.f32(), ALU.add, r=[BK[6], bb_.g()], w=[dg_])
        if dbg and b == 0:
            dump("dbg_a1", a1.f32(), [128, 1024], r=[a1.g()])
            dump("dbg_g2", g2.f32(), [128, 1024], r=[g2.g()])

        wa = Alloc(arena, work_base, TOT)
        xt = [wa.get(4096) for _ in range(2)]
        tmpf = wa.get(4096)
        junkb = wa.get(2048)
        hbf = wa.get(2048)
        for i in range(NT):
            xb = xt[i % 2]
            dma('sp', xb.f32(), x_d.ap()[b, i * 128:(i + 1) * 128, :], r=[], w=[xb.g()])
            act(junkb.bf(), xb.f32(), AF.Square, r=[xb.g()], w=[junkb.g(), small.g()], accum_out=sm(SM_SS))
            rstd_chain(SM_SS, SM_LN, SM_RSTD, D, 1e-6)
            stt(tmpf.f32(), xb.f32(), sm(SM_RSTD), a1.f32(), ALU.mult, ALU.mult, r=[xb.g(), small.g(), a1.g()], w=[tmpf.g()])
            tt(hbf.bf(), tmpf.f32(), sh1.f32(), ALU.add, r=[tmpf.g(), sh1.g()], w=[hbf.g()])
            for k in range(KC):
                tr(BTb[:, k * 128:(k + 1) * 128], hbf.bf()[:, k * 128:(k + 1) * 128], ident_b.bf(), r=[hbf.g(), ident_b.g()], w=[BK[7]])
            cp(hT3[:, :, i * 128:(i + 1) * 128], BTb.rearrange("p (k t) -> p k t", k=KC), r=[BK[7]], w=hT_g(i * 128, (i + 1) * 128), eng='act')
        if dbg and b == 0:
            wa2 = Alloc(arena, wa.p, TOT)
            dtmp = wa2.get(8192)
            cp(dtmp.f32(), hT3[:, 0, :], r=hT_g(0, S), w=[dtmp.g()])
            dump("dbg_hT0", dtmp.f32(), [128, 2048], r=[dtmp.g()])
        if stage <= 1:
            continue

        win_v = win_d.ap().rearrange("(k p) n -> p k n", p=128)
        wa = Alloc(arena, work_base, TOT)
        wp = wa.get(KC * 256 * 2)
        wgp = wa.get(KC * 256 * 2)
        pw = wa.get(2 * 256 * 2)
        pg = wa.get(NT * 256 * 2)
        pT = [wa.get(512 * 2) for _ in range(2)]
        sg = wa.get(512 * 4)
        WP = wp.bf().rearrange("p (k n) -> p k n", k=KC)
        WGP = wgp.bf().rearrange("p (k n) -> p k n", k=KC)
        PW = pw.bf().rearrange("p (c n) -> p c n", c=2)
        PG = pg.bf().rearrange("p (i n) -> p i n", i=NT)
        BAND = band.bf().rearrange("p (j n) -> p j n", j=12)
        for g in range(4):
            dma('pool', WP, win_v[:, :, 3072 + g * 256:3072 + (g + 1) * 256], r=[], w=[wp.g()])
            dma('pool', WGP, win_v[:, :, 5120 + g * 256:5120 + (g + 1) * 256], r=[], w=[wgp.g()])
            dma('pool', PW, poolw_d.ap()[g].rearrange("(c p) n -> p c n", p=128), r=[], w=[pw.g()])
            for i in range(NT):
                for k in range(KC):
                    mm(banks[7][:, (i % 2) * 256:(i % 2 + 1) * 256], hT3[:, k, i * 128:(i + 1) * 128], WP[:, k, :], k == 0, k == KC - 1,
                       r=[hT_g(i * 128, (i + 1) * 128), wp.g()], w=[BK[7]])
                if i % 2 == 1:
                    cp(pg.bf()[:, (i - 1) * 256:(i + 1) * 256], banks[7][:], r=[BK[7]], w=[pg.g((i - 1) * 512, (i + 1) * 512)], eng='act')
            for c in range(4):
                for cc in range(2):
                    for il in range(4):
                        i = c * 4 + il
                        cur = g * 3 + (0 if i == 0 else 1)
                        mm(banks[cc][:, il * 128:(il + 1) * 128], PG[:, i, cc * 128:(cc + 1) * 128], BAND[:, cur, :], True, i == 0,
                           r=[pg.g(i * 512, (i + 1) * 512), band.g()], w=[BK[cc]])
                        if i > 0:
                            mm(banks[cc][:, il * 128:(il + 1) * 128], PG[:, i - 1, cc * 128:(cc + 1) * 128], BAND[:, g * 3 + 2, :], False, True,
                               r=[pg.g((i - 1) * 512, i * 512), band.g()], w=[BK[cc]])
                    cp(pT[cc].bf(), banks[cc][:], r=[BK[cc]], w=[pT[cc].g()], eng='act')
                for ec in range(2):
                    kch = g * 2 + ec
                    mm(banks[2][:], PW[:, 0, ec * 128:(ec + 1) * 128], pT[0].bf(), True, False, r=[pw.g(), pT[0].g()], w=[BK[2]])
                    mm(banks[2][:], PW[:, 1, ec * 128:(ec + 1) * 128], pT[1].bf(), False, True, r=[pw.g(), pT[1].g()], w=[BK[2]])
                    for k in range(KC):
                        mm(banks[3][:], WGP[:, k, ec * 128:(ec + 1) * 128], hT3[:, k, c * 512:(c + 1) * 512], k == 0, k == KC - 1,
                           r=[wgp.g(), hT_g(c * 512, (c + 1) * 512)], w=[BK[3]])
                    act(sg.f32(), banks[3][:], AF.Sigmoid, r=[BK[3]], w=[sg.g()])
                    stt(mT3[:, kch, c * 512:(c + 1) * 512], banks[2][:], pscT.f32()[:, kch:kch + 1], sg.f32(), ALU.mult, ALU.mult,
                        r=[BK[2], pscT.g(), sg.g()], w=[mT_g(kch, c * 512, (c + 1) * 512)])
        if dbg and b == 0 and stage == 2:
            wa2 = Alloc(arena, wa.p, TOT)
            dtmp = wa2.get(8192)
            for kk_ in (0, 5):
                cp(dtmp.f32(), mT3[:, kk_, :], r=[mT.g()], w=[dtmp.g()])
                dump(f"dbg_mT{kk_}", dtmp.f32(), [128, 2048], r=[dtmp.g()])
        if stage <= 2:
            continue

        wa = Alloc(arena, work_base, TOT)
        wq = wa.get(KC * 128 * 2)
        wk_ = wa.get(KC * 128 * 2)
        wv = wa.get(KC * 128 * 2)
        wg = wa.get(KC * 128 * 2)
        QT = wa.get(S * 2)
        KT = wa.get(S * 2)
        Vb = wa.get(NT * 128 * 2)
        sgT = wa.get(S * 2)
        PT = [[wa.get(512 * 2) for _ in range(2)] for _ in range(2)]
        ntmp = [wa.get(256 * 4) for _ in range(2)]
        O1s = wa.get(2048)
        O2s = wa.get(2048)
        lnz = wa.get(2048)
        rz = wa.get(2048)
        sq = wa.get(2048)
        t1 = wa.get(2048)
        rs = wa.get(2048)
        WQ = wq.bf().rearrange("p (k n) -> p k n", k=KC)
        WK = wk_.bf().rearrange("p (k n) -> p k n", k=KC)
        WV = wv.bf().rearrange("p (k n) -> p k n", k=KC)
        WG = wg.bf().rearrange("p (k n) -> p k n", k=KC)
        V3 = Vb.bf().rearrange("p (i n) -> p i n", i=NT)
        nheads = H if stage > 3 or not dbg else 1
        tail_q = []

        def drain_tail(n=None):
            k = 0
            while tail_q and (n is None or k < n):
                P.add(*tail_q.pop(0))
                k += 1
        for h in range(nheads):
            dma('pool', WQ, win_v[:, :, h * 128:(h + 1) * 128], r=[], w=[wq.g()])
            dma('pool', WK, win_v[:, :, 1024 + h * 128:1024 + (h + 1) * 128], r=[], w=[wk_.g()])
            dma('pool', WV, win_v[:, :, 2048 + h * 128:2048 + (h + 1) * 128], r=[], w=[wv.g()])
            dma('pool', WG, win_v[:, :, 4096 + h * 128:4096 + (h + 1) * 128], r=[], w=[wg.g()])
            precast(4)
            pj = 0
            for (W_, wb_, dst, mode) in ((WQ, wq, QT, 'q'), (WK, wk_, KT, 'k'), (WG, wg, sgT, 'g')):
                for c in range(4):
                    bk = 6 + (pj % 2)
                    pj += 1
                    for k in range(KC):
                        mm(banks[bk][:], W_[:, k, :], hT3[:, k, c * 512:(c + 1) * 512], k == 0, k == KC - 1,
                           r=[wb_.g(), hT_g(c * 512, (c + 1) * 512)], w=[BK[bk]])
                    o_ap = dst.bf()[:, c * 512:(c + 1) * 512]
                    o_g = dst.g(c * 1024, (c + 1) * 1024)
                    if mode == 'q':
                        P.add('act', (lambda o_ap=o_ap, bk=bk: (lambda e: e.mul(out=o_ap, in_=banks[bk][:], mul=0.125)))(), r=[BK[bk]], w=[o_g])
                    elif mode == 'k':
                        cp(o_ap, banks[bk][:], r=[BK[bk]], w=[o_g], eng='act')
                    else:
                        act(o_ap, banks[bk][:], AF.Sigmoid, r=[BK[bk]], w=[o_g])
            for i in range(NT):
                bk = 6 + ((i // 4) % 2)
                for k in range(KC):
                    mm(banks[bk][:, (i % 4) * 128:(i % 4 + 1) * 128], hT3[:, k, i * 128:(i + 1) * 128], WV[:, k, :], k == 0, k == KC - 1,
                       r=[hT_g(i * 128, (i + 1) * 128), wv.g()], w=[BK[bk]])
                if i % 4 == 3:
                    cp(Vb.bf()[:, (i - 3) * 128:(i + 1) * 128], banks[bk][:], r=[BK[bk]], w=[Vb.g((i - 3) * 256, (i + 1) * 256)], eng='act')

            def qk(c, j):
                col0 = max(0, j - 4 * c) * 128
                par = j % 2
                for m in range(2):
                    bk = m * 2 + par
                    mm(banks[bk][:, col0:512], KT.bf()[m * 64:(m + 1) * 64, j * 128:(j + 1) * 128],
                       QT.bf()[m * 64:(m + 1) * 64, c * 512 + col0:(c + 1) * 512], True, True,
                       r=[KT.g(j * 256, (j + 1) * 256), QT.g(c * 1024 + col0 * 2, (c + 1) * 1024)], w=[BK[bk]])

            for c in range(4):
                jmax = 4 * c + 3
                qk(c, 0)
                for j in range(jmax + 1):
                    if j + 1 <= jmax:
                        qk(c, j + 1)
                    col0 = max(0, j - 4 * c) * 128
                    par = j % 2
                    il_lo = max(0, j - 4 * c)
                    il_hi = min(3, j + 1 - 4 * c)
                    far0 = max(0, j + 2 - 4 * c) * 128
                    for m in range(2):
                        bk = m * 2 + par
                        pt = PT[m][par]
                        if il_hi >= il_lo and il_hi >= 0:
                            n0, n1 = il_lo * 128, (il_hi + 1) * 128
                            b0 = (4 * c + il_lo - j) * 128
                            nn = n1 - n0
                            tt(ntmp[m].f32()[:, 0:nn], banks[bk][:, n0:n1], BT3[:, h, b0:b0 + nn], ALU.add,
                               r=[BK[bk], biasT.g(h * 1024, (h + 1) * 1024)], w=[ntmp[m].g()])
                            act(pt.bf()[:, n0:n1], ntmp[m].f32()[:, 0:nn], AF.Exp, r=[ntmp[m].g()], w=[pt.g(n0 * 2, n1 * 2)])
                        if far0 < 512:
                            act(pt.bf()[:, far0:512], banks[bk][:, far0:512], AF.Exp, r=[BK[bk], rb31m.g()], w=[pt.g(far0 * 2, 1024)],
                                bias=rb31m.f32()[:, h:h + 1])
                        mm(banks[4 + m][:, col0:512], V3[:, j, :], pt.bf()[:, col0:512], j == 0, j == jmax,
                           r=[Vb.g(j * 256, (j + 1) * 256), pt.g(col0 * 2, 1024)], w=[BK[4 + m]], skip_group_check=True)
                        mm(banks[6][m * 32:(m + 1) * 32, col0:512], ones_b.bf()[:, 0:32], pt.bf()[:, col0:512], j == 0, j == jmax,
                           r=[ones_b.g(), pt.g(col0 * 2, 1024)], w=[BK[6]], skip_group_check=True)
                    drain_tail(5)
                drain_tail()
                cs = slice(c * 512, (c + 1) * 512)
                cp(O1s.f32(), banks[4][:], r=[BK[4]], w=[O1s.g()], eng='act')
                cp(O2s.f32(), banks[5][:], r=[BK[5]], w=[O2s.g()], eng='act')
                act(lnz.f32()[0:64, :], banks[6][0:64, :], AF.Ln, r=[BK[6]], w=[lnz.g()])
                P.capture = []
                act(rz.f32()[0:64, :], lnz.f32()[0:64, :], AF.Exp, r=[lnz.g()], w=[rz.g()], scale=-1.0)
                ts(rz.f32()[32:64, :], rz.f32()[32:64, :], SM[32:64, SM_NLAM:SM_NLAM + 1], None, ALU.mult, None, r=[rz.g(), small.g()], w=[rz.g()])
                mm(banks[7][:], sel0.f32()[0:32, :], rz.f32()[0:32, :], True, True, r=[sel0.g(), rz.g()], w=[BK[7]])
                tt(t1.f32(), O1s.f32(), banks[7][:], ALU.mult, r=[O1s.g(), BK[7]], w=[t1.g()])
                mm(banks[7][:], sel0.f32()[32:64, :], rz.f32()[32:64, :], True, True, r=[sel0.g(), rz.g()], w=[BK[7]])
                tt(O2s.f32(), O2s.f32(), banks[7][:], ALU.mult, r=[O2s.g(), BK[7]], w=[O2s.g()])
                tt(t1.f32(), t1.f32(), O2s.f32(), ALU.add, r=[t1.g(), O2s.g()], w=[t1.g()])
                act(sq.f32(), t1.f32(), AF.Square, r=[t1.g()], w=[sq.g()])
                mm(banks[7][:], ones_f.f32(), sq.f32(), True, True, r=[ones_f.g(), sq.g()], w=[BK[7]])
                ts(rs.f32(), banks[7][:], 1.0 / 128, 1e-5, ALU.mult, ALU.add, r=[BK[7]], w=[rs.g()])
                act(rs.f32(), rs.f32(), AF.Ln, r=[rs.g()], w=[rs.g()])
                act(rs.f32(), rs.f32(), AF.Exp, r=[rs.g()], w=[rs.g()], scale=-0.5)
                tt(t1.f32(), t1.f32(), rs.f32(), ALU.mult, r=[t1.g(), rs.g()], w=[t1.g()])
                stt(t1.f32(), t1.f32(), sm(SM_WSUB), sgT.bf()[:, cs], ALU.mult, ALU.mult, r=[t1.g(), small.g(), sgT.g(c * 1024, (c + 1) * 1024)], w=[t1.g()])
                tt(mT3[:, h, cs], t1.f32(), mT3[:, h, cs], ALU.add, r=[t1.g(), mT_g(h, c * 512, (c + 1) * 512)], w=[mT_g(h, c * 512, (c + 1) * 512)])
                tail_q.extend(P.capture)
                P.capture = None
                if c == 3:
                    drain_tail()
        if dbg and b == 0 and stage == 3:
            wa2 = Alloc(arena, wa.p, TOT)
            dtmp = wa2.get(8192)
            cp(dtmp.f32(), mT3[:, 0, :], r=[mT.g()], w=[dtmp.g()])
            dump("dbg_mT0", dtmp.f32(), [128, 2048], r=[dtmp.g()])
        if stage <= 3:
            continue

        precast(64)
        WOUT3 = wout.bf().rearrange("p (k n) -> p k n", k=KC)
        dma('pool', WOUT3, wout_d.ap().rearrange("(k p) n -> p k n", p=128), r=[], w=[wout.g()])
        for k in range(KC):
            tt(WOUT3[:, k, :], WOUT3[:, k, :], g1.f32(), ALU.mult, r=[wout.g(k * 2048, (k + 1) * 2048), g1.g()], w=[wout.g(k * 2048, (k + 1) * 2048)])
        WQ3 = hT.bf().rearrange("p (k n) -> p k n", k=KC)
        dma('pool', WQ3, wqry_d.ap().rearrange("(k p) n -> p k n", p=128), r=[], w=[hT.g()])
        wa = Alloc(arena, work_base, TOT)
        xt5 = g1
        xm = [wa.get(4096) for _ in range(2)]
        h2 = [wa.get(2048) for _ in range(2)]
        eidu = [wa.get(512) for _ in range(2)]
        gate = [wa.get(512) for _ in range(2)]
        tmpf = a1
        ot = sh1
        h2T = wa.get(2048)
        qTs = wa.get(4096)
        junkb = wa.get(2048)
        ssb = wa.get(8192)
        wkb = ssb
        tmpfX = ssb.sub(0, 4096)
        junkbX = ssb.sub(4096, 2048)
        v16 = wa.get(1024)
        ixu = wa.get(1024)
        ixf = wa.get(1024)
        c16, posu, pau, pbu, paf, pbf, i1s, i2s, eidf, scm, ee, actv, gl, wgt = [wa.get(512) for _ in range(14)]
        zz = wa.get(32)
        rzz = wa.get(32)
        dgb = [wa.get(256) for _ in range(4)]
        NBUF = (TOT - wa.p) // 4096
        assert NBUF >= 5, NBUF
        cbuf = [wa.get(4096) for _ in range(NBUF)]
        H2T3 = h2T.bf().rearrange("p (k t) -> p k t", k=KC)
        QTS3 = qTs.bf().rearrange("p (q t) -> p q t", q=16)
        SUBK3 = subkT.bf().rearrange("p (q n) -> p q n", q=16)
        SS3 = ssb.f32().rearrange("p (q n) -> p q n", q=16)
        WK3 = wkb.f32().rearrange("p (q n) -> p q n", q=16)
        V16 = v16.f32().rearrange("p (q j) -> p q j", q=16)
        IXU = ixu.u32().rearrange("p (q j) -> p q j", q=16)
        ntiles = NT if not dbg else 2

        def gen(name, r, w, eng='dve', **kw):
            P.add(eng, lambda e: getattr(e, name)(**kw), r=r, w=w)

        def stageX(i):
            par = i % 2
            xm_, h2_, eidu_, gate_ = xm[par], h2[par], eidu[par], gate[par]
            t0 = i * 128
            dma('sp', xt5.f32(), x_d.ap()[b, t0:t0 + 128, :], r=[], w=[xt5.g()])
            for half in range(2):
                for k in range(KC):
                    mm(banks[2 + half][:], mT3[:, k, t0:t0 + 128], WOUT3[:, k, half * 512:(half + 1) * 512], k == 0, k == KC - 1,
                       r=[mT_g(k, t0, t0 + 128), wout.g()], w=[BK[2 + half]])
            for half in range(2):
                hs = slice(half * 512, (half + 1) * 512)
                tt(xm_.f32()[:, hs], banks[2 + half][:], xt5.f32()[:, hs], ALU.add, r=[BK[2 + half], xt5.g()], w=[xm_.g(half * 2048, (half + 1) * 2048)])
            act(junkbX.bf(), xm_.f32(), AF.Square, r=[xm_.g()], w=[junkbX.g(), small.g()], accum_out=sm(SM_SS2))
            rstd_chain(SM_SS2, SM_LN2, SM_RSTD2, D, 1e-6)
            stt(tmpfX.f32(), xm_.f32(), sm(SM_RSTD2), a2.f32(), ALU.mult, ALU.mult, r=[xm_.g(), small.g(), a2.g()], w=[tmpfX.g()])
            tt(h2_.bf(), tmpfX.f32(), sh2.f32(), ALU.add, r=[tmpfX.g(), sh2.g()], w=[h2_.g()])
            for k in range(KC):
                tr(BTb[:, k * 128:(k + 1) * 128], h2_.bf()[:, k * 128:(k + 1) * 128], ident_b.bf(), r=[h2_.g(), ident_b.g()], w=[BK[7]])
            cp(h2T.bf(), BTb, r=[BK[7]], w=[h2T.g()], eng='act')
            for qc in range(16):
                bk = 2 + (qc // 4) % 2
                for k in range(KC):
                    mm(banks[bk][:, (qc % 4) * 128:(qc % 4 + 1) * 128], WQ3[:, k, qc * 128:(qc + 1) * 128], H2T3[:, k, :], k == 0, k == KC - 1,
                       r=[hT.g(), h2T.g()], w=[BK[bk]])
                if qc % 4 == 3:
                    cp(qTs.bf()[:, (qc - 3) * 128:(qc + 1) * 128], banks[bk][:], r=[BK[bk]], w=[qTs.g((qc - 3) * 256, (qc + 1) * 256)], eng='act')
            for qc in range(16):
                bk = 4 + qc // 4
                mm(banks[bk][:, (qc % 4) * 128:(qc % 4 + 1) * 128], QTS3[:, qc, :], SUBK3[:, qc, :], True, True,
                   r=[qTs.g(qc * 256, (qc + 1) * 256), subkT.g()], w=[BK[bk]])
            for q4 in range(4):
                cp(ssb.f32()[:, q4 * 512:(q4 + 1) * 512], banks[4 + q4][:], r=[BK[4 + q4]], w=[ssb.g(q4 * 2048, (q4 + 1) * 2048)], eng='act')
            if dbg and i == 0:
                dump("dbg_xm", xm_.f32(), [128, 1024], r=[xm_.g()])
                dump("dbg_s", ssb.f32(), [128, 2048], r=[ssb.g()])
            sg_ = lambda qc: ssb.g(qc * 512, (qc + 1) * 512)
            wg_ = lambda qc: wkb.g(qc * 512, (qc + 1) * 512)
            vk = lambda qc, hf: f"v16:{qc}:{hf}"
            ik = lambda qc, hf: f"ixu:{qc}:{hf}"
            ALLV = [vk(q_, h_) for q_ in range(16) for h_ in range(2)]
            ALLI = [ik(q_, h_) for q_ in range(16) for h_ in range(2)]
            for qc in range(16):
                gen('max', [sg_(qc), v16.g()], [vk(qc, 0)], out=V16[:, qc, 0:8], in_=SS3[:, qc, :])
            for qc in range(16):
                gen('max_index', [sg_(qc), vk(qc, 0), ixu.g()], [ik(qc, 0)], out=IXU[:, qc, 0:8], in_max=V16[:, qc, 0:8], in_values=SS3[:, qc, :])
            for qc in range(16):
                gen('match_replace', [sg_(qc), vk(qc, 0)], [wg_(qc)], out=WK3[:, qc, :], in_to_replace=V16[:, qc, 0:8], in_values=SS3[:, qc, :], imm_value=NEG)
            for qc in range(16):
                gen('max', [wg_(qc), v16.g()], [vk(qc, 1)], out=V16[:, qc, 8:16], in_=WK3[:, qc, :])
            for qc in range(16):
                gen('max_index', [wg_(qc), vk(qc, 1), ixu.g()], [ik(qc, 1)], out=IXU[:, qc, 8:16], in_max=V16[:, qc, 8:16], in_values=WK3[:, qc, :])
            cp(ixf.f32(), ixu.u32(), r=ALLI, w=[ixf.g()])
            cand, wk2 = ssb, wkb
            C4 = cand.f32().rearrange("p (h n) -> p h n", h=8)
            W4 = wk2.f32().rearrange("p (h n) -> p h n", h=8)
            vf = v16.f32()
            tt(mk(cand.f32(), [[256, 8], [16, 16], [1, 16]]), mk(vf, [[32, 8], [1, 16], [0, 16]]), mk(vf, [[32, 8], [0, 16], [1, 16]], 16), ALU.add,
               r=ALLV, w=[cand.g()])
            C16 = c16.f32().rearrange("p (h j) -> p h j", h=8)
            POS = posu.u32().rearrange("p (h j) -> p h j", h=8)
            cg_ = lambda h_: cand.g(h_ * 1024, (h_ + 1) * 1024)
            w2g_ = lambda h_: wk2.g(h_ * 1024, (h_ + 1) * 1024)
            ck = lambda h_, hf: f"c16:{h_}:{hf}"
            pk = lambda h_, hf: f"pos:{h_}:{hf}"
            ALLC = [ck(h_, f_) for h_ in range(8) for f_ in range(2)]
            ALLP = [pk(h_, f_) for h_ in range(8) for f_ in range(2)]
            for h_ in range(8):
                gen('max', [cg_(h_), c16.g()], [ck(h_, 0)], out=C16[:, h_, 0:8], in_=C4[:, h_, :])
            for h_ in range(8):
                gen('max_index', [cg_(h_), ck(h_, 0), posu.g()], [pk(h_, 0)], out=POS[:, h_, 0:8], in_max=C16[:, h_, 0:8], in_values=C4[:, h_, :])
            for h_ in range(8):
                gen('match_replace', [cg_(h_), ck(h_, 0)], [w2g_(h_)], out=W4[:, h_, :], in_to_replace=C16[:, h_, 0:8], in_values=C4[:, h_, :], imm_value=NEG)
            for h_ in range(8):
                gen('max', [w2g_(h_), c16.g()], [ck(h_, 1)], out=C16[:, h_, 8:16], in_=W4[:, h_, :])
            for h_ in range(8):
                gen('max_index', [w2g_(h_), ck(h_, 1), posu.g()], [pk(h_, 1)], out=POS[:, h_, 8:16], in_max=C16[:, h_, 8:16], in_values=W4[:, h_, :])
            gen('tensor_single_scalar', ALLP, [pau.g()], out=pau.u32(), in_=posu.u32(), scalar=4, op=ALU.logical_shift_right)
            gen('tensor_single_scalar', ALLP, [pbu.g()], out=pbu.u32(), in_=posu.u32(), scalar=15, op=ALU.bitwise_and)
            cp(paf.f32(), pau.u32(), r=[pau.g()], w=[paf.g()])
            cp(pbf.f32(), pbu.u32(), r=[pbu.g()], w=[pbf.g()])
            oh, prod = ssb, wkb
            for (pf_, off_, dst_) in ((paf, 0, i1s), (pbf, 16, i2s)):
                tt(mk(oh.f32(), [[16, 128], [1, 16]]), mk(pf_.f32(), [[1, 128], [0, 16]]), mk(iota16.f32(), [[0, 128], [1, 16]]), ALU.is_equal,
                   r=[pf_.g(), iota16.g()], w=[oh.g()])
                tt(mk(prod.f32(), [[256, 8], [16, 16], [1, 16]]), mk(oh.f32(), [[256, 8], [16, 16], [1, 16]]), mk(ixf.f32(), [[32, 8], [0, 16], [1, 16]], off_), ALU.mult,
                   r=[oh.g(), ixf.g()], w=[prod.g()])
                tred(dst_.f32(), mk(prod.f32(), [[16, 128], [1, 16]]), ALU.add, r=[prod.g()], w=[dst_.g()])
            stt(eidf.f32(), i1s.f32(), 128.0, i2s.f32(), ALU.mult, ALU.add, r=[i1s.g(), i2s.g()], w=[eidf.g()])
            cp(eidu_.u32(), eidf.f32(), r=[eidf.g()], w=[eidu_.g()])
            tt(mk(scm.f32(), [[16, 8], [1, 16]]), mk(c16.f32(), [[16, 8], [1, 16]]), mk(c16.f32(), [[16, 8], [0, 16]]), ALU.subtract, r=ALLC, w=[scm.g()])
            act(ee.f32(), scm.f32(), AF.Exp, r=[scm.g()], w=[ee.g()])
            tred(zz.f32(), mk(ee.f32(), [[16, 8], [1, 16]]), ALU.add, r=[ee.g()], w=[zz.g()])
            gen('reciprocal', [zz.g()], [rzz.g()], out=rzz.f32(), in_=zz.f32())
            tt(mk(gate_.f32(), [[16, 8], [1, 16]]), mk(ee.f32(), [[16, 8], [1, 16]]), mk(rzz.f32(), [[1, 8], [0, 16]]), ALU.mult, r=[ee.g(), rzz.g()], w=[gate_.g()])
            if dbg and i == 0:
                dump("dbg_eid", eidf.f32(), [128, 128], r=[eidf.g()])
                dump("dbg_gate", gate_.f32(), [128, 128], r=[gate_.g()])

        def stageY(i, pump):
            par = i % 2
            xm_, h2_, eidu_, gate_ = xm[par], h2[par], eidu[par], gate[par]
            t0 = i * 128
            EID = eidu_.u32()

            def gather(out_ap, slot, r, w):
                P.add('pool', lambda e: e.indirect_dma_start(out=out_ap, out_offset=None, in_=etab_d.ap(),
                                                             in_offset=bass.IndirectOffsetOnAxis(ap=EID[:, slot:slot + 1], axis=0)),
                      r=r, w=w, dma=True)

            def slot_tail(sl):
                cb = cbuf[sl % NBUF]
                dg_ = dgb[sl % 4]
                act(wgt.f32()[:, sl:sl + 1], gl.f32()[:, sl:sl + 1], AF.Copy, r=[f"gl{sl}", gate_.g(), wgt.g()], w=[f"wgt{sl}"],
                    scale=gate_.f32()[:, sl:sl + 1])
                P.add('act', (lambda dg_=dg_, sl=sl: (lambda e: e.activation(out=dg_.bf(), in_=ident_f.f32(), func=AF.Copy, scale=wgt.f32()[:, sl:sl + 1])))(),
                      r=[ident_f.g(), f"wgt{sl}"], w=[dg_.g()])
                for half in range(2):
                    mm(banks[half][:], dg_.bf(), cb.bf()[:, D + half * 512:D + (half + 1) * 512], sl == 0, sl == 127,
                       r=[dg_.g(), cb.g(2048, 4096)], w=[BK[half]])

            for sl in range(128):
                cb = cbuf[sl % NBUF]
                gather(cb.bf(), sl, r=[eidu_.g()] + ETAB_KEYS, w=[cb.g()])
                P.add('dve', (lambda cb=cb, sl=sl: (lambda e: e.scalar_tensor_tensor(out=junkb.bf(), in0=cb.bf()[:, 0:D], scalar=1.0, in1=h2_.bf(), op0=ALU.mult, op1=ALU.mult,
                                                                                 accum_out=actv.f32()[:, sl:sl + 1])))(),
                      r=[cb.g(0, 2048), h2_.g(), actv.g()], w=[junkb.g(), f"actv{sl}"])
                act(gl.f32()[:, sl:sl + 1], actv.f32()[:, sl:sl + 1], AF.Gelu, r=[f"actv{sl}", gl.g()], w=[f"gl{sl}"])
                if sl >= 1:
                    slot_tail(sl - 1)
                pump(3)
            slot_tail(127)
            for half in range(2):
                hs = slice(half * 512, (half + 1) * 512)
                tt(tmpf.f32()[:, hs], banks[half][:], g2.f32()[:, hs], ALU.mult, r=[BK[half], g2.g()], w=[tmpf.g(half * 2048, (half + 1) * 2048)])
                tt(ot.f32()[:, hs], tmpf.f32()[:, hs], xm_.f32()[:, hs], ALU.add, r=[tmpf.g(half * 2048, (half + 1) * 2048), xm_.g()], w=[ot.g(half * 2048, (half + 1) * 2048)])
            if dbg and i == 0:
                dump("dbg_xo", ot.f32(), [128, 1024], r=[ot.g()])
            act(junkb.bf(), ot.f32(), AF.Square, r=[ot.g()], w=[junkb.g(), small.g()], accum_out=sm(SM_SS3))
            rstd_chain(SM_SS3, SM_LN3, SM_RSTD3, D, 1e-6)
            stt(ot.f32(), ot.f32(), sm(SM_RSTD3), nf_bc.f32(), ALU.mult, ALU.mult, r=[ot.g(), small.g(), nf_bc.g()], w=[ot.g()])
            dma('sp', out_d.ap()[b, t0:t0 + 128, :], ot.f32(), r=[ot.g()], w=[f"out{b}_{i}"])

        def drain(q, n=None):
            k = 0
            while q and (n is None or k < n):
                P.add(*q.pop(0))
                k += 1

        P.capture = []
        stageX(0)
        q = P.capture
        P.capture = None
        drain(q)
        for i in range(ntiles):
            if i + 1 < ntiles:
                P.capture = []
                stageX(i + 1)
                q = P.capture
                P.capture = None
            else:
                q = []
            stageY(i, lambda n: drain(q, n))
            drain(q)

    P.emit(nc, stack)
    stack.close()
    return nc, list(dbg_d.keys())


def _prep_inputs(inputs):
    f = np.float32
    x = np.asarray(inputs['x'], f)
    c = np.asarray(inputs['c'], f)
    cst = _host_consts()
    rep = lambda v, n=128: np.ascontiguousarray(np.broadcast_to(np.asarray(v, f).reshape(1, -1), (n, np.asarray(v).size)))
    nw = np.concatenate([np.asarray(inputs['norm_mix_w'], f).reshape(-1), np.asarray(inputs['norm_ffn_w'], f).reshape(-1),
                         np.asarray(inputs['norm_final_w'], f).reshape(-1)])
    lam = np.concatenate([np.asarray(inputs[k], f).reshape(-1) for k in ('lambda_q1', 'lambda_k1', 'lambda_q2', 'lambda_k2')])
    sk1 = np.asarray(inputs['sub_keys_1'], f)[0]
    sk2 = np.asarray(inputs['sub_keys_2'], f)[0]
    subkT = np.zeros((128, 16, 128), f)
    for h in range(8):
        subkT[:, h * 2 + 0, :] = sk1[h].T
        subkT[:, h * 2 + 1, :] = sk2[h].T
    shared = {
        'w_ada': np.ascontiguousarray(np.asarray(inputs['w_ada'], f)[0]),
        'b_ada_bc': rep(inputs['b_ada']),
        'nw_bc': rep(nw),
        'w_in': np.ascontiguousarray(np.asarray(inputs['w_in'], f)[0]),
        'w_out': np.ascontiguousarray(np.asarray(inputs['w_out'], f)[0]),
        'w_query': np.ascontiguousarray(np.asarray(inputs['w_query'], f)[0]),
        'pool_w': np.ascontiguousarray(np.asarray(inputs['pool_w'], f)[0]),
        'pool_scaleT': np.ascontiguousarray(np.asarray(inputs['pool_scale'], f).reshape(8, 128).T),
        'subkT': np.ascontiguousarray(subkT.reshape(128, 2048)),
        'e_down': np.ascontiguousarray(np.asarray(inputs['expert_down'], f)[0]),
        'e_up': np.ascontiguousarray(np.asarray(inputs['expert_up'], f)[0]),
        'lam_bc': rep(lam),
        'sublnT': np.ascontiguousarray(np.asarray(inputs['subln_w'], f).reshape(128, 1)),
        'relb_bc': rep(np.asarray(inputs['rel_bias'], f).reshape(-1)),
        'masks': np.ascontiguousarray(cst['masks'].reshape(128, -1)),
        'negmask': cst['negmask'],
        'band': np.ascontiguousarray(cst['band'].reshape(128, -1)),
        'ident': cst['ident'],
        'iota16': cst['iota16'],
        'sel0': cst['sel0'],
    }
    in_maps = []
    for core in range(NCORES):
        m = dict(shared)
        m['x'] = np.ascontiguousarray(x[core * NB:(core + 1) * NB])
        cc = c[core * NB:(core + 1) * NB]
        m['cT'] = np.ascontiguousarray(cc.reshape(NB, KC, 128).transpose(2, 0, 1).reshape(128, NB * KC))
        in_maps.append(m)
    return in_maps


_CACHE = {}


def kernel(**inputs):
    in_maps = _prep_inputs(inputs)
    if 'nc' not in _CACHE:
        _CACHE['nc'] = build()[0]
    nc = _CACHE['nc']
    res = run_bass_kernel_spmd(nc, in_maps, core_ids=list(range(NCORES)))
    out = np.concatenate([np.asarray(r['out']) for r in res.results], axis=0)
    return out.astype(np.float32)
```

```python
import os
import math
import contextlib
import numpy as np
import concourse.bass as bass
import concourse.mybir as mybir
from concourse.bass_utils import run_bass_kernel_spmd

dt = mybir.dt
AF = mybir.ActivationFunctionType
ALU = mybir.AluOpType
AX = mybir.AxisListType
F32, BF16, U32 = dt.float32, dt.bfloat16, dt.uint32

NCORES = 8
NB = 2
S = 2048
NT = 16
D = 1024
KC = 8
H = 8
DIN = 6144
NEXP = 16384
OFF = 8.0
NEG = -1.0e30
G = 512


class Prog:
    def __init__(self):
        self.ops = []
        self.last_w = {}
        self.readers = {}
        self.capture = None

    def add(self, eng, fn, r=(), w=(), dma=False):
        if self.capture is not None:
            self.capture.append((eng, fn, r, w, dma))
            return None
        i = len(self.ops)
        deps = set()
        rk = _flat(r)
        wk = _flat(w)
        for k in rk:
            lw = self.last_w.get(k)
            if lw is not None:
                deps.add(lw)
        for k in wk:
            lw = self.last_w.get(k)
            if lw is not None:
                deps.add(lw)
            deps.update(self.readers.get(k, ()))
        deps.discard(i)
        for k in rk:
            self.readers.setdefault(k, []).append(i)
        for k in wk:
            self.last_w[k] = i
            self.readers[k] = []
        self.ops.append(dict(eng=eng, fn=fn, deps=deps, dma=dma, sig=None, need=False, pre=None))
        return i

    def emit(self, nc, stack):
        ops = self.ops
        EPOCH = 30000
        NSLOT = {'sp': 24, 'pool': 24, 'act': 8}
        for o in ops:
            for d in o['deps']:
                p = ops[d]
                if p['eng'] == 'pe' and o['eng'] == 'pe' and not p['dma'] and not o['dma']:
                    continue
                p['need'] = True
        cnt = {e: 0 for e in ('pe', 'act', 'dve', 'pool', 'sp')}
        esems = {e: [] for e in cnt}
        dsems = {q: [stack.enter_context(nc.semaphore(f"d_{q}_{i}")) for i in range(n)] for q, n in NSLOT.items()}
        duse = {q: [0] * n for q, n in NSLOT.items()}
        dnext = {q: 0 for q in NSLOT}
        for o in ops:
            e = o['eng']
            if o['dma']:
                s = dnext[e]
                dnext[e] = (s + 1) % NSLOT[e]
                if duse[e][s] > 0:
                    o['pre'] = (dsems[e][s], 16 * duse[e][s])
                duse[e][s] += 1
                o['sig'] = (dsems[e][s], 16 * duse[e][s])
            elif o['need']:
                ep = cnt[e] // EPOCH
                while len(esems[e]) <= ep:
                    esems[e].append(stack.enter_context(nc.semaphore(f"e_{e}_{len(esems[e])}")))
                cnt[e] += 1
                o['sig'] = (esems[e][ep], cnt[e] - ep * EPOCH)
        by_eng = {e: [o for o in ops if o['eng'] == e] for e in cnt}
        final_waits = []
        for q in NSLOT:
            for s in range(NSLOT[q]):
                if duse[q][s] > 0:
                    final_waits.append((dsems[q][s], 16 * duse[q][s]))

        def run(ename, eng):
            waited = {}
            for o in by_eng[ename]:
                needs = {}
                for d in o['deps']:
                    p = ops[d]
                    if p['eng'] == 'pe' and ename == 'pe' and not p['dma'] and not o['dma']:
                        continue
                    sem, val = p['sig']
                    key = id(sem)
                    if needs.get(key, (None, 0))[1] < val:
                        needs[key] = (sem, val)
                if o['pre'] is not None:
                    sem, val = o['pre']
                    key = id(sem)
                    if needs.get(key, (None, 0))[1] < val:
                        needs[key] = (sem, val)
                for key, (sem, val) in needs.items():
                    if waited.get(key, 0) < val:
                        eng.wait_ge(sem, val)
                        waited[key] = val
                inst = o['fn'](eng)
                if o['sig'] is not None:
                    inst.then_inc(o['sig'][0], 16 if o['dma'] else 1)
            if ename == 'sp':
                for sem, val in final_waits:
                    eng.wait_ge(sem, val)

        with nc.Block() as block:
            @block.tensor
            def _(e):
                run('pe', e)

            @block.scalar
            def _(e):
                run('act', e)

            @block.vector
            def _(e):
                run('dve', e)

            @block.gpsimd
            def _(e):
                run('pool', e)

            @block.sync
            def _(e):
                run('sp', e)


def _flat(x):
    out = []
    for k in x:
        if isinstance(k, (list, tuple, set, range)):
            out.extend(_flat(k))
        else:
            out.append(k)
    return out


class Buf:
    def __init__(self, arena, off, nbytes):
        assert off % 4 == 0 and nbytes % 4 == 0
        self.A, self.off, self.nbytes = arena, off, nbytes

    def g(self, lo=0, hi=None):
        hi = self.nbytes if hi is None else hi
        return range((self.off + lo) // G, (self.off + hi + G - 1) // G)

    def f32(self):
        return self.A[:, self.off // 4:(self.off + self.nbytes) // 4]

    def bf(self):
        return self.A[:, self.off // 4:(self.off + self.nbytes) // 4].bitcast(BF16)

    def u32(self):
        return self.A[:, self.off // 4:(self.off + self.nbytes) // 4].bitcast(U32)

    def sub(self, lo, n):
        return Buf(self.A, self.off + lo, n)


class Alloc:
    def __init__(self, arena, base, limit):
        self.A, self.p, self.limit = arena, base, limit

    def get(self, nbytes):
        nb = (nbytes + G - 1) // G * G
        b = Buf(self.A, self.p, (nbytes + 3) // 4 * 4)
        self.p += nb
        assert self.p <= self.limit, (self.p, self.limit)
        return b


def mk(ap, pattern, extra_off=0):
    return bass.AP(tensor=ap.tensor, offset=ap.offset + extra_off, ap=[list(ap.ap[0])] + [list(p) for p in pattern])


def _t5_bucket(d):
    d = np.maximum(d, 0)
    x = np.maximum(d, 1).astype(np.float32) / np.float32(16)
    large = 16 + (np.log(x).astype(np.float32) / np.float32(math.log(128 / 16)) * np.float32(16)).astype(np.int32)
    large = np.minimum(large, 31)
    return np.where(d < 16, d, large)


def _host_consts():
    kk = np.arange(128)[:, None]
    qq = np.arange(256)[None, :]
    dist = qq - kk
    valid = dist >= 0
    bk = _t5_bucket(dist)
    masks = np.zeros((128, 32, 256), np.float32)
    for b in range(32):
        masks[:, b, :] = (valid & (bk == b)).astype(np.float32)
    negmask = np.where(valid, 0.0, NEG).astype(np.float32)
    band = np.zeros((128, 12, 128), np.float32)
    s = np.arange(128)[:, None]
    t = np.arange(128)[None, :]
    for gi, w in enumerate((2, 4, 8, 16)):
        cnt0 = np.minimum(t + 1, w).astype(np.float32)
        band[:, gi * 3 + 0, :] = ((s <= t) & (s > t - w)) / cnt0 - (s == t)
        band[:, gi * 3 + 1, :] = ((s <= t) & (s > t - w)) / np.float32(w) - (s == t)
        band[:, gi * 3 + 2, :] = (s > 128 + t - w) / np.float32(w)
    ident = np.eye(128, dtype=np.float32)
    iota16 = np.broadcast_to(np.arange(16, dtype=np.float32)[None, :], (128, 16)).copy()
    sel0 = np.zeros((64, 128), np.float32)
    sel0[0, :] = 1.0
    sel0[32, :] = 1.0
    return dict(masks=masks, negmask=negmask, band=band, ident=ident, iota16=iota16, sel0=sel0)


def build(stage=99, dbg=False):
    nc = bass.Bass("TRN2", target_bir_lowering=False)
    P = Prog()

    def din(name, shape, dtype=F32):
        return nc.dram_tensor(name, list(shape), dtype, kind="ExternalInput")

    x_d = din("x", [NB, S, D])
    cT_d = din("cT", [128, NB * KC])
    wada_d = din("w_ada", [D, DIN])
    bada_d = din("b_ada_bc", [128, DIN])
    nw_d = din("nw_bc", [128, 3 * D])
    win_d = din("w_in", [D, DIN])
    wout_d = din("w_out", [D, D])
    wqry_d = din("w_query", [D, 2048])
    poolw_d = din("pool_w", [4, 256, 256])
    psc_d = din("pool_scaleT", [128, 8])
    subk_d = din("subkT", [128, 16 * 128])
    edown_d = din("e_down", [NEXP, D])
    eup_d = din("e_up", [NEXP, D])
    lam_d = din("lam_bc", [128, 256])
    subln_d = din("sublnT", [128, 1])
    relb_d = din("relb_bc", [128, 256])
    masks_d = din("masks", [128, 32 * 256])
    negm_d = din("negmask", [128, 256])
    band_d = din("band", [128, 12 * 128])
    ident_d = din("ident", [128, 128])
    iota_d = din("iota16", [128, 16])
    sel0_d = din("sel0", [64, 128])
    out_d = nc.dram_tensor("out", [NB, S, D], F32, kind="ExternalOutput")
    etab_d = nc.dram_tensor("etab", [NEXP, 2 * D], BF16, kind="Internal")
    dbg_d = {}

    def dbg_out(name, shape):
        dbg_d[name] = nc.dram_tensor(name, list(shape), F32, kind="ExternalOutput")
        return dbg_d[name]

    stack = contextlib.ExitStack()
    TOT = 212480
    arena = stack.enter_context(nc.sbuf_tensor("arena", [128, TOT // 4], F32))
    banks = [stack.enter_context(nc.psum_tensor(f"B{i}", [128, 512], F32)) for i in range(8)]
    BK = [f"B{i}" for i in range(8)]

    al = Alloc(arena, 0, TOT)
    ident_f = al.get(512)
    ident_b = al.get(256)
    ones_b = al.get(256)
    ones_f = al.get(512)
    sel0 = al.get(512)
    relb = al.get(1024)
    rb31m = al.get(32)
    lamv = al.get(1024)
    small = al.get(512)
    pscT = al.get(32)
    band = al.get(12 * 128 * 2)
    subkT = al.get(16 * 128 * 2)
    iota16 = al.get(64)
    sink = al.get(64)
    nf_bc = al.get(4096)
    sh1 = al.get(4096)
    a1 = al.get(4096)
    g1 = al.get(4096)
    sh2 = al.get(4096)
    a2 = al.get(4096)
    g2 = al.get(4096)
    hT = al.get(KC * S * 2)
    mT = al.get(KC * S * 2)
    wout = al.get(KC * D * 2)
    work_base = al.p
    SM = small.f32()
    (SM_S1, SM_S2, SM_E1, SM_E2, SM_NLAM, SM_WSUB, SM_SS, SM_RSTD, SM_LN, SM_SS2, SM_RSTD2, SM_SS3, SM_RSTD3,
     SM_LN2, SM_LN3, SM_SUBLN) = range(16)

    def sm(i):
        return SM[:, i:i + 1]

    def smg(i):
        return small.g()

    def dma(q, out, in_, r, w):
        P.add(q, lambda e: e.dma_start(out=out, in_=in_), r=r, w=w, dma=True)

    def mm(out, lhsT, rhs, start, stop, r, w, **kw):
        P.add('pe', lambda e: e.matmul(out, lhsT, rhs, start=start, stop=stop, **kw), r=r, w=w)

    def tr(out, in_, ident, r, w):
        P.add('pe', lambda e: e.transpose(out, in_, ident), r=r, w=w)

    def act(out, in_, func, r, w, bias=None, scale=None, accum_out=None, eng='act'):
        def f(e):
            kw = {}
            if bias is not None:
                kw['bias'] = bias
            if scale is not None:
                kw['scale'] = scale
            if accum_out is not None:
                kw['accum_out'] = accum_out
            return e.activation(out=out, in_=in_, func=func, **kw)
        P.add('act', f, r=r, w=w)

    def tt(out, in0, in1, op, r, w, eng='dve'):
        P.add(eng, lambda e: e.tensor_tensor(out=out, in0=in0, in1=in1, op=op), r=r, w=w)

    def ts(out, in0, s1, s2, op0, op1, r, w, eng='dve'):
        if op1 is None:
            P.add(eng, lambda e: e.tensor_scalar(out=out, in0=in0, scalar1=s1, scalar2=None, op0=op0), r=r, w=w)
        else:
            P.add(eng, lambda e: e.tensor_scalar(out=out, in0=in0, scalar1=s1, scalar2=s2, op0=op0, op1=op1), r=r, w=w)

    def stt(out, in0, scalar, in1, op0, op1, r, w):
        P.add('dve', lambda e: e.scalar_tensor_tensor(out=out, in0=in0, scalar=scalar, in1=in1, op0=op0, op1=op1), r=r, w=w)

    def cp(out, in_, r, w, eng='dve'):
        if eng == 'act':
            P.add('act', lambda e: e.copy(out=out, in_=in_), r=r, w=w)
        else:
            P.add(eng, lambda e: e.tensor_copy(out=out, in_=in_), r=r, w=w)

    def ttr(out, in0, in1, accum_out, r, w):
        P.add('dve', lambda e: e.scalar_tensor_tensor(out=out, in0=in0, scalar=1.0, in1=in1, op0=ALU.mult, op1=ALU.mult,
                                                     accum_out=accum_out), r=r, w=w)

    def tred(out, in_, op, r, w):
        P.add('dve', lambda e: e.tensor_reduce(out=out, in_=in_, axis=AX.X, op=op), r=r, w=w)

    def memset(ap, val, r, w, eng='dve'):
        P.add(eng, lambda e: e.memset(ap, val), r=r, w=w)

    def rstd_chain(ss_i, ln_i, rstd_i, n, eps):
        ts(sm(ln_i), sm(ss_i), 1.0 / n, eps, ALU.mult, ALU.add, r=[small.g()], w=[small.g()])
        act(sm(ln_i), sm(ln_i), AF.Ln, r=[small.g()], w=[small.g()])
        act(sm(rstd_i), sm(ln_i), AF.Exp, r=[small.g()], w=[small.g()], scale=-0.5)

    def dump(name, ap_sb, shape, r):
        if name in dbg_d:
            return
        d = dbg_out(name, shape)
        dma('sp', d.ap(), ap_sb, r=r, w=[name])

    dma('sp', ident_f.f32(), ident_d.ap(), r=[], w=[ident_f.g()])
    dma('sp', sel0.f32()[0:64, :], sel0_d.ap(), r=[], w=[sel0.g()])
    dma('sp', relb.f32(), relb_d.ap(), r=[], w=[relb.g()])
    dma('sp', lamv.f32(), lam_d.ap(), r=[], w=[lamv.g()])
    dma('sp', pscT.f32(), psc_d.ap(), r=[], w=[pscT.g()])
    dma('sp', iota16.f32(), iota_d.ap(), r=[], w=[iota16.g()])
    dma('sp', nf_bc.f32(), nw_d.ap()[:, 2 * D:3 * D], r=[], w=[nf_bc.g()])
    dma('sp', sm(SM_SUBLN), subln_d.ap(), r=[], w=[small.g()])
    dma('pool', band.bf(), band_d.ap(), r=[], w=[band.g()])
    dma('pool', subkT.bf(), subk_d.ap(), r=[], w=[subkT.g()])
    ETAB_JOBS = [(ti, r0) for ti in range(2) for r0 in range(0, NEXP, 1024)]
    ETAB_KEYS = [f"etab{ti}_{r0}" for (ti, r0) in ETAB_JOBS]

    def precast(n):
        for _ in range(n):
            if not ETAB_JOBS:
                return
            ti, r0 = ETAB_JOBS.pop(0)
            tbl = (edown_d, eup_d)[ti]
            dma('pool', etab_d.ap()[r0:r0 + 1024, ti * D:(ti + 1) * D], tbl.ap()[r0:r0 + 1024, :], r=[], w=[f"etab{ti}_{r0}"])

    cp(ident_b.bf(), ident_f.f32(), r=[ident_f.g()], w=[ident_b.g()])
    memset(ones_b.bf(), 1.0, r=[], w=[ones_b.g()])
    memset(ones_f.f32(), 1.0, r=[], w=[ones_f.g()])
    ts(rb31m.f32(), relb.f32()[:, 31 * 8:32 * 8], -OFF, None, ALU.add, None, r=[relb.g()], w=[rb31m.g()])
    ts(sm(SM_WSUB), sm(SM_SUBLN), 0.8, None, ALU.mult, None, r=[small.g()], w=[small.g()])
    wa = Alloc(arena, work_base, TOT)
    junk64 = wa.get(256)
    LV = lamv.f32()
    ttr(junk64.f32(), LV[:, 0:64], LV[:, 64:128], sm(SM_S1), r=[lamv.g()], w=[junk64.g(), small.g()])
    ttr(junk64.f32(), LV[:, 128:192], LV[:, 192:256], sm(SM_S2), r=[lamv.g()], w=[junk64.g(), small.g()])
    act(sm(SM_E1), sm(SM_S1), AF.Exp, r=[small.g()], w=[small.g()])
    act(sm(SM_E2), sm(SM_S2), AF.Exp, r=[small.g()], w=[small.g()])
    tt(sm(SM_NLAM), sm(SM_E2), sm(SM_E1), ALU.subtract, r=[small.g()], w=[small.g()])
    ts(sm(SM_NLAM), sm(SM_NLAM), -0.2, None, ALU.add, None, r=[small.g()], w=[small.g()])
    bias_scr = nc.dram_tensor("bias_scr", [128, 2048], F32, kind="Internal")
    biasT = wa.get(8 * 1024)
    mk_chunk = wa.get(8 * 256 * 4)
    BT3 = biasT.f32().rearrange("p (h q) -> p h q", h=8)
    for h in range(8):
        dma('sp', BT3[:, h, :], negm_d.ap(), r=[], w=[biasT.g(h * 1024, (h + 1) * 1024)])
    for c in range(4):
        dma('sp', mk_chunk.f32(), masks_d.ap()[:, c * 2048:(c + 1) * 2048], r=[], w=[mk_chunk.g()])
        MC = mk_chunk.f32().rearrange("p (b q) -> p b q", b=8)
        for bb in range(8):
            b = c * 8 + bb
            for h in range(8):
                stt(BT3[:, h, :], MC[:, bb, :], relb.f32()[:, b * 8 + h:b * 8 + h + 1], BT3[:, h, :], ALU.mult, ALU.add,
                    r=[mk_chunk.g(), relb.g(), biasT.g(h * 1024, (h + 1) * 1024)], w=[biasT.g(h * 1024, (h + 1) * 1024)])
    ts(biasT.f32(), biasT.f32(), -OFF, None, ALU.add, None, r=[biasT.g()], w=[biasT.g()])
    dma('sp', bias_scr.ap(), biasT.f32(), r=[biasT.g()], w=["bias_scr"])
    if dbg:
        dump("dbg_bias", biasT.f32(), [128, 2048], r=[biasT.g()])
        dump("dbg_small", small.f32()[:, 0:16], [128, 16], r=[small.g()])

    hT3 = hT.bf().rearrange("p (k t) -> p k t", k=KC)
    mT3 = mT.bf().rearrange("p (k t) -> p k t", k=KC)

    def hT_g(c0, c1):
        return [hT.g(k * S * 2 + c0 * 2, k * S * 2 + c1 * 2) for k in range(KC)]

    def mT_g(k, c0, c1):
        return mT.g(k * S * 2 + c0 * 2, k * S * 2 + c1 * 2)

    BTb = banks[7][:].bitcast(BF16)

    for b in range(NB):
        wa = Alloc(arena, work_base, TOT)
        cact = wa.get(64)
        crep = wa.get(KC * 128 * 4)
        wblk = [wa.get(KC * 512 * 4) for _ in range(2)]
        bblk = [wa.get(2048) for _ in range(2)]
        mtmp = wa.get(2048)
        nwt = wa.get(4096)
        cin = wa.get(64)
        dma('sp', cin.f32()[:, 0:KC], cT_d.ap()[:, b * KC:(b + 1) * KC], r=[], w=[cin.g()])
        act(cact.f32()[:, 0:KC], cin.f32()[:, 0:KC], AF.Silu, r=[cin.g()], w=[cact.g()])
        cp(crep.f32().rearrange("p (k m) -> p k m", k=KC), mk(cact.f32(), [[1, KC], [0, 128]]), r=[cact.g()], w=[crep.g()])
        CR = crep.f32().rearrange("p (k m) -> p k m", k=KC)
        wada_v = wada_d.ap().rearrange("(k p) n -> p k n", p=128)
        dsts = [sh1, sh1, a1, a1, g1, g1, sh2, sh2, a2, a2, g2, g2]
        for nb_ in range(12):
            wb = wblk[nb_ % 2]
            bb_ = bblk[nb_ % 2]
            dma('sp', wb.f32().rearrange("p (k n) -> p k n", k=KC), wada_v[:, :, nb_ * 512:(nb_ + 1) * 512], r=[], w=[wb.g()])
            dma('sp', bb_.f32(), bada_d.ap()[:, nb_ * 512:(nb_ + 1) * 512], r=[], w=[bb_.g()])
            WB = wb.f32().rearrange("p (k n) -> p k n", k=KC)
            for k in range(KC):
                mm(banks[6][:], CR[:, k, :], WB[:, k, :], k == 0, k == KC - 1, r=[crep.g(), wb.g()], w=[BK[6]])
            dst = dsts[nb_]
            half = nb_ % 2
            dst_ap = dst.f32()[:, half * 512:(half + 1) * 512]
            dg_ = dst.g(half * 2048, (half + 1) * 2048)
            if nb_ in (2, 3, 8, 9):
                which = 0 if nb_ in (2, 3) else 1
                dma('sp', nwt.f32()[:, 0:512], nw_d.ap()[:, which * D + half * 512: which * D + (half + 1) * 512], r=[], w=[nwt.g()])
                tt(mtmp.f32(), banks[6][:], bb_.f32(), ALU.add, r=[BK[6], bb_.g()], w=[mtmp.g()])
                stt(dst_ap, mtmp.f32(), 1.0, nwt.f32()[:, 0:512], ALU.add, ALU.mult, r=[mtmp.g(), nwt.g()], w=[dg_])
            else:
                tt(dst_ap, banks[6][:], bb_.f32(), ALU.add, r=[BK[6], bb_.g()], w=[dg_])
        if dbg and b == 0:
            dump("dbg_a1", a1.f32(), [128, 1024], r=[a1.g()])
            dump("dbg_g2", g2.f32(), [128, 1024], r=[g2.g()])

        wa = Alloc(arena, work_base, TOT)
        xt = [wa.get(4096) for _ in range(2)]
        tmpf = wa.get(4096)
        junkb = wa.get(2048)
        hbf = wa.get(2048)
        for i in range(NT):
            xb = xt[i % 2]
            dma('sp', xb.f32(), x_d.ap()[b, i * 128:(i + 1) * 128, :], r=[], w=[xb.g()])
            act(junkb.bf(), xb.f32(), AF.Square, r=[xb.g()], w=[junkb.g(), small.g()], accum_out=sm(SM_SS))
            rstd_chain(SM_SS, SM_LN, SM_RSTD, D, 1e-6)
            stt(tmpf.f32(), xb.f32(), sm(SM_RSTD), a1.f32(), ALU.mult, ALU.mult, r=[xb.g(), small.g(), a1.g()], w=[tmpf.g()])
            tt(hbf.bf(), tmpf.f32(), sh1.f32(), ALU.add, r=[tmpf.g(), sh1.g()], w=[hbf.g()])
            for k in range(KC):
                tr(BTb[:, k * 128:(k + 1) * 128], hbf.bf()[:, k * 128:(k + 1) * 128], ident_b.bf(), r=[hbf.g(), ident_b.g()], w=[BK[7]])
            cp(hT3[:, :, i * 128:(i + 1) * 128], BTb.rearrange("p (k t) -> p k t", k=KC), r=[BK[7]], w=hT_g(i * 128, (i + 1) * 128), eng='act')
        if dbg and b == 0:
            wa2 = Alloc(arena, wa.p, TOT)
            dtmp = wa2.get(8192)
            cp(dtmp.f32(), hT3[:, 0, :], r=hT_g(0, S), w=[dtmp.g()])
            dump("dbg_hT0", dtmp.f32(), [128, 2048], r=[dtmp.g()])
        if stage <= 1:
            continue

        win_v = win_d.ap().rearrange("(k p) n -> p k n", p=128)
        wa = Alloc(arena, work_base, TOT)
        wp = wa.get(KC * 256 * 2)
        wgp = wa.get(KC * 256 * 2)
        pw = wa.get(2 * 256 * 2)
        pg = wa.get(NT * 256 * 2)
        pT = [wa.get(512 * 2) for _ in range(2)]
        sg = wa.get(512 * 4)
        WP = wp.bf().rearrange("p (k n) -> p k n", k=KC)
        WGP = wgp.bf().rearrange("p (k n) -> p k n", k=KC)
        PW = pw.bf().rearrange("p (c n) -> p c n", c=2)
        PG = pg.bf().rearrange("p (i n) -> p i n", i=NT)
        BAND = band.bf().rearrange("p (j n) -> p j n", j=12)
        for g in range(4):
            dma('pool', WP, win_v[:, :, 3072 + g * 256:3072 + (g + 1) * 256], r=[], w=[wp.g()])
            dma('pool', WGP, win_v[:, :, 5120 + g * 256:5120 + (g + 1) * 256], r=[], w=[wgp.g()])
            dma('pool', PW, poolw_d.ap()[g].rearrange("(c p) n -> p c n", p=128), r=[], w=[pw.g()])
            for i in range(NT):
                for k in range(KC):
                    mm(banks[7][:, (i % 2) * 256:(i % 2 + 1) * 256], hT3[:, k, i * 128:(i + 1) * 128], WP[:, k, :], k == 0, k == KC - 1,
                       r=[hT_g(i * 128, (i + 1) * 128), wp.g()], w=[BK[7]])
                if i % 2 == 1:
                    cp(pg.bf()[:, (i - 1) * 256:(i + 1) * 256], banks[7][:], r=[BK[7]], w=[pg.g((i - 1) * 512, (i + 1) * 512)], eng='act')
            for c in range(4):
                for cc in range(2):
                    for il in range(4):
                        i = c * 4 + il
                        cur = g * 3 + (0 if i == 0 else 1)
                        mm(banks[cc][:, il * 128:(il + 1) * 128], PG[:, i, cc * 128:(cc + 1) * 128], BAND[:, cur, :], True, i == 0,
                           r=[pg.g(i * 512, (i + 1) * 512), band.g()], w=[BK[cc]])
                        if i > 0:
                            mm(banks[cc][:, il * 128:(il + 1) * 128], PG[:, i - 1, cc * 128:(cc + 1) * 128], BAND[:, g * 3 + 2, :], False, True,
                               r=[pg.g((i - 1) * 512, i * 512), band.g()], w=[BK[cc]])
                    cp(pT[cc].bf(), banks[cc][:], r=[BK[cc]], w=[pT[cc].g()], eng='act')
                for ec in range(2):
                    kch = g * 2 + ec
                    mm(banks[2][:], PW[:, 0, ec * 128:(ec + 1) * 128], pT[0].bf(), True, False, r=[pw.g(), pT[0].g()], w=[BK[2]])
                    mm(banks[2][:], PW[:, 1, ec * 128:(ec + 1) * 128], pT[1].bf(), False, True, r=[pw.g(), pT[1].g()], w=[BK[2]])
                    for k in range(KC):
                        mm(banks[3][:], WGP[:, k, ec * 128:(ec + 1) * 128], hT3[:, k, c * 512:(c + 1) * 512], k == 0, k == KC - 1,
                           r=[wgp.g(), hT_g(c * 512, (c + 1) * 512)], w=[BK[3]])
                    act(sg.f32(), banks[3][:], AF.Sigmoid, r=[BK[3]], w=[sg.g()])
                    stt(mT3[:, kch, c * 512:(c + 1) * 512], banks[2][:], pscT.f32()[:, kch:kch + 1], sg.f32(), ALU.mult, ALU.mult,
                        r=[BK[2], pscT.g(), sg.g()], w=[mT_g(kch, c * 512, (c + 1) * 512)])
        if dbg and b == 0 and stage == 2:
            wa2 = Alloc(arena, wa.p, TOT)
            dtmp = wa2.get(8192)
            for kk_ in (0, 5):
                cp(dtmp.f32(), mT3[:, kk_, :], r=[mT.g()], w=[dtmp.g()])
                dump(f"dbg_mT{kk_}", dtmp.f32(), [128, 2048], r=[dtmp.g()])
        if stage <= 2:
            continue

        wa = Alloc(arena, work_base, TOT)
        wq = wa.get(KC * 128 * 2)
        wk_ = wa.get(KC * 128 * 2)
        wv = wa.get(KC * 128 * 2)
        wg = wa.get(KC * 128 * 2)
        QT = wa.get(S * 2)
        KT = wa.get(S * 2)
        Vb = wa.get(NT * 128 * 2)
        sgT = wa.get(S * 2)
        PT = [[wa.get(512 * 2) for _ in range(2)] for _ in range(2)]
        ntmp = [wa.get(256 * 4) for _ in range(2)]
        O1s = wa.get(2048)
        O2s = wa.get(2048)
        lnz = wa.get(2048)
        rz = wa.get(2048)
        sq = wa.get(2048)
        t1 = wa.get(2048)
        rs = wa.get(2048)
        biasT = wa.get(8 * 1024)
        BT3 = biasT.f32().rearrange("p (h q) -> p h q", h=8)
        dma('sp', biasT.f32(), bias_scr.ap(), r=["bias_scr"], w=[biasT.g()])
        WQ = wq.bf().rearrange("p (k n) -> p k n", k=KC)
        WK = wk_.bf().rearrange("p (k n) -> p k n", k=KC)
        WV = wv.bf().rearrange("p (k n) -> p k n", k=KC)
        WG = wg.bf().rearrange("p (k n) -> p k n", k=KC)
        V3 = Vb.bf().rearrange("p (i n) -> p i n", i=NT)
        nheads = H if stage > 3 or not dbg else 1
        for h in range(nheads):
            dma('pool', WQ, win_v[:, :, h * 128:(h + 1) * 128], r=[], w=[wq.g()])
            dma('pool', WK, win_v[:, :, 1024 + h * 128:1024 + (h + 1) * 128], r=[], w=[wk_.g()])
            dma('pool', WV, win_v[:, :, 2048 + h * 128:2048 + (h + 1) * 128], r=[], w=[wv.g()])
            dma('pool', WG, win_v[:, :, 4096 + h * 128:4096 + (h + 1) * 128], r=[], w=[wg.g()])
            precast(4)
            pj = 0
            for (W_, wb_, dst, mode) in ((WQ, wq, QT, 'q'), (WK, wk_, KT, 'k'), (WG, wg, sgT, 'g')):
                for c in range(4):
                    bk = 6 + (pj % 2)
                    pj += 1
                    for k in range(KC):
                        mm(banks[bk][:], W_[:, k, :], hT3[:, k, c * 512:(c + 1) * 512], k == 0, k == KC - 1,
                           r=[wb_.g(), hT_g(c * 512, (c + 1) * 512)], w=[BK[bk]])
                    o_ap = dst.bf()[:, c * 512:(c + 1) * 512]
                    o_g = dst.g(c * 1024, (c + 1) * 1024)
                    if mode == 'q':
                        P.add('act', (lambda o_ap=o_ap, bk=bk: (lambda e: e.mul(out=o_ap, in_=banks[bk][:], mul=0.125)))(), r=[BK[bk]], w=[o_g])
                    elif mode == 'k':
                        cp(o_ap, banks[bk][:], r=[BK[bk]], w=[o_g], eng='act')
                    else:
                        act(o_ap, banks[bk][:], AF.Sigmoid, r=[BK[bk]], w=[o_g])
            for i in range(NT):
                bk = 6 + ((i // 4) % 2)
                for k in range(KC):
                    mm(banks[bk][:, (i % 4) * 128:(i % 4 + 1) * 128], hT3[:, k, i * 128:(i + 1) * 128], WV[:, k, :], k == 0, k == KC - 1,
                       r=[hT_g(i * 128, (i + 1) * 128), wv.g()], w=[BK[bk]])
                if i % 4 == 3:
                    cp(Vb.bf()[:, (i - 3) * 128:(i + 1) * 128], banks[bk][:], r=[BK[bk]], w=[Vb.g((i - 3) * 256, (i + 1) * 256)], eng='act')

            def qk(c, j):
                col0 = max(0, j - 4 * c) * 128
                par = j % 2
                for m in range(2):
                    bk = m * 2 + par
                    mm(banks[bk][:, col0:512], KT.bf()[m * 64:(m + 1) * 64, j * 128:(j + 1) * 128],
                       QT.bf()[m * 64:(m + 1) * 64, c * 512 + col0:(c + 1) * 512], True, True,
                       r=[KT.g(j * 256, (j + 1) * 256), QT.g(c * 1024 + col0 * 2, (c + 1) * 1024)], w=[BK[bk]])

            for c in range(4):
                jmax = 4 * c + 3
                qk(c, 0)
                for j in range(jmax + 1):
                    if j + 1 <= jmax:
                        qk(c, j + 1)
                    col0 = max(0, j - 4 * c) * 128
                    par = j % 2
                    il_lo = max(0, j - 4 * c)
                    il_hi = min(3, j + 1 - 4 * c)
                    far0 = max(0, j + 2 - 4 * c) * 128
                    for m in range(2):
                        bk = m * 2 + par
                        pt = PT[m][par]
                        if il_hi >= il_lo and il_hi >= 0:
                            n0, n1 = il_lo * 128, (il_hi + 1) * 128
                            b0 = (4 * c + il_lo - j) * 128
                            nn = n1 - n0
                            tt(ntmp[m].f32()[:, 0:nn], banks[bk][:, n0:n1], BT3[:, h, b0:b0 + nn], ALU.add,
                               r=[BK[bk], biasT.g(h * 1024, (h + 1) * 1024)], w=[ntmp[m].g()])
                            act(pt.bf()[:, n0:n1], ntmp[m].f32()[:, 0:nn], AF.Exp, r=[ntmp[m].g()], w=[pt.g(n0 * 2, n1 * 2)])
                        if far0 < 512:
                            act(pt.bf()[:, far0:512], banks[bk][:, far0:512], AF.Exp, r=[BK[bk], rb31m.g()], w=[pt.g(far0 * 2, 1024)],
                                bias=rb31m.f32()[:, h:h + 1])
                        mm(banks[4 + m][:, col0:512], V3[:, j, :], pt.bf()[:, col0:512], j == 0, j == jmax,
                           r=[Vb.g(j * 256, (j + 1) * 256), pt.g(col0 * 2, 1024)], w=[BK[4 + m]], skip_group_check=True)
                        mm(banks[6][m * 32:(m + 1) * 32, col0:512], ones_b.bf()[:, 0:32], pt.bf()[:, col0:512], j == 0, j == jmax,
                           r=[ones_b.g(), pt.g(col0 * 2, 1024)], w=[BK[6]], skip_group_check=True)
                cs = slice(c * 512, (c + 1) * 512)
                cp(O1s.f32(), banks[4][:], r=[BK[4]], w=[O1s.g()], eng='act')
                cp(O2s.f32(), banks[5][:], r=[BK[5]], w=[O2s.g()], eng='act')
                act(lnz.f32()[0:64, :], banks[6][0:64, :], AF.Ln, r=[BK[6]], w=[lnz.g()])
                act(rz.f32()[0:64, :], lnz.f32()[0:64, :], AF.Exp, r=[lnz.g()], w=[rz.g()], scale=-1.0)
                ts(rz.f32()[32:64, :], rz.f32()[32:64, :], SM[32:64, SM_NLAM:SM_NLAM + 1], None, ALU.mult, None, r=[rz.g(), small.g()], w=[rz.g()])
                mm(banks[0][:], sel0.f32()[0:32, :], rz.f32()[0:32, :], True, True, r=[sel0.g(), rz.g()], w=[BK[0]])
                mm(banks[1][:], sel0.f32()[32:64, :], rz.f32()[32:64, :], True, True, r=[sel0.g(), rz.g()], w=[BK[1]])
                tt(t1.f32(), O1s.f32(), banks[0][:], ALU.mult, r=[O1s.g(), BK[0]], w=[t1.g()])
                tt(O2s.f32(), O2s.f32(), banks[1][:], ALU.mult, r=[O2s.g(), BK[1]], w=[O2s.g()])
                tt(t1.f32(), t1.f32(), O2s.f32(), ALU.add, r=[t1.g(), O2s.g()], w=[t1.g()])
                act(sq.f32(), t1.f32(), AF.Square, r=[t1.g()], w=[sq.g()])
                mm(banks[2][:], ones_f.f32(), sq.f32(), True, True, r=[ones_f.g(), sq.g()], w=[BK[2]])
                ts(rs.f32(), banks[2][:], 1.0 / 128, 1e-5, ALU.mult, ALU.add, r=[BK[2]], w=[rs.g()])
                act(rs.f32(), rs.f32(), AF.Ln, r=[rs.g()], w=[rs.g()])
                act(rs.f32(), rs.f32(), AF.Exp, r=[rs.g()], w=[rs.g()], scale=-0.5)
                tt(t1.f32(), t1.f32(), rs.f32(), ALU.mult, r=[t1.g(), rs.g()], w=[t1.g()])
                stt(t1.f32(), t1.f32(), sm(SM_WSUB), sgT.bf()[:, cs], ALU.mult, ALU.mult, r=[t1.g(), small.g(), sgT.g(c * 1024, (c + 1) * 1024)], w=[t1.g()])
                tt(mT3[:, h, cs], t1.f32(), mT3[:, h, cs], ALU.add, r=[t1.g(), mT_g(h, c * 512, (c + 1) * 512)], w=[mT_g(h, c * 512, (c + 1) * 512)])
        if dbg and b == 0 and stage == 3:
            wa2 = Alloc(arena, wa.p, TOT)
            dtmp = wa2.get(8192)
            cp(dtmp.f32(), mT3[:, 0, :], r=[mT.g()], w=[dtmp.g()])
            dump("dbg_mT0", dtmp.f32(), [128, 2048], r=[dtmp.g()])
        if stage <= 3:
            continue

        precast(64)
        WOUT3 = wout.bf().rearrange("p (k n) -> p k n", k=KC)
        dma('pool', WOUT3, wout_d.ap().rearrange("(k p) n -> p k n", p=128), r=[], w=[wout.g()])
        for k in range(KC):
            tt(WOUT3[:, k, :], WOUT3[:, k, :], g1.f32(), ALU.mult, r=[wout.g(k * 2048, (k + 1) * 2048), g1.g()], w=[wout.g(k * 2048, (k + 1) * 2048)])
        WQ3 = hT.bf().rearrange("p (k n) -> p k n", k=KC)
        dma('pool', WQ3, wqry_d.ap().rearrange("(k p) n -> p k n", p=128), r=[], w=[hT.g()])
        wa = Alloc(arena, work_base, TOT)
        xt5 = g1
        xm = [wa.get(4096) for _ in range(2)]
        h2 = [wa.get(2048) for _ in range(2)]
        eidu = [wa.get(512) for _ in range(2)]
        gate = [wa.get(512) for _ in range(2)]
        tmpf = a1
        ot = sh1
        ssb = wa.get(8192)
        wkb = ssb
        tmpfX = ssb.sub(0, 4096)
        junkbX = ssb.sub(4096, 2048)
        h2T = ssb.sub(0, 2048)
        qTs = ssb.sub(4096, 4096)
        v16 = wa.get(1024)
        ixu = wa.get(1024)
        ixf = wa.get(1024)
        c16, posu, pau, pbu, paf, pbf, i1s, i2s, eidf, scm, ee, actv, gl, wgt = [wa.get(512) for _ in range(14)]
        zz = wa.get(32)
        rzz = wa.get(32)
        dgb = [wa.get(256) for _ in range(4)]
        NBUF = (TOT - wa.p) // 4096
        assert NBUF >= 5, NBUF
        cbuf = [wa.get(4096) for _ in range(NBUF)]
        SINK = mk(sink.bf(), [[0, D]])
        H2T3 = h2T.bf().rearrange("p (k t) -> p k t", k=KC)
        QTS3 = qTs.bf().rearrange("p (q t) -> p q t", q=16)
        SUBK3 = subkT.bf().rearrange("p (q n) -> p q n", q=16)
        SS3 = ssb.f32().rearrange("p (q n) -> p q n", q=16)
        WK3 = wkb.f32().rearrange("p (q n) -> p q n", q=16)
        V16 = v16.f32().rearrange("p (q j) -> p q j", q=16)
        IXU = ixu.u32().rearrange("p (q j) -> p q j", q=16)
        ntiles = NT if not dbg else 2

        def gen(name, r, w, eng='dve', **kw):
            P.add(eng, lambda e: getattr(e, name)(**kw), r=r, w=w)

        def stageX(i):
            par = i % 2
            xm_, h2_, eidu_, gate_ = xm[par], h2[par], eidu[par], gate[par]
            t0 = i * 128
            dma('sp', xt5.f32(), x_d.ap()[b, t0:t0 + 128, :], r=[], w=[xt5.g()])
            for half in range(2):
                for k in range(KC):
                    mm(banks[2 + half][:], mT3[:, k, t0:t0 + 128], WOUT3[:, k, half * 512:(half + 1) * 512], k == 0, k == KC - 1,
                       r=[mT_g(k, t0, t0 + 128), wout.g()], w=[BK[2 + half]])
            for half in range(2):
                hs = slice(half * 512, (half + 1) * 512)
                tt(xm_.f32()[:, hs], banks[2 + half][:], xt5.f32()[:, hs], ALU.add, r=[BK[2 + half], xt5.g()], w=[xm_.g(half * 2048, (half + 1) * 2048)])
            act(junkbX.bf(), xm_.f32(), AF.Square, r=[xm_.g()], w=[junkbX.g(), small.g()], accum_out=sm(SM_SS2))
            rstd_chain(SM_SS2, SM_LN2, SM_RSTD2, D, 1e-6)
            stt(tmpfX.f32(), xm_.f32(), sm(SM_RSTD2), a2.f32(), ALU.mult, ALU.mult, r=[xm_.g(), small.g(), a2.g()], w=[tmpfX.g()])
            tt(h2_.bf(), tmpfX.f32(), sh2.f32(), ALU.add, r=[tmpfX.g(), sh2.g()], w=[h2_.g()])
            for k in range(KC):
                tr(BTb[:, k * 128:(k + 1) * 128], h2_.bf()[:, k * 128:(k + 1) * 128], ident_b.bf(), r=[h2_.g(), ident_b.g()], w=[BK[7]])
            cp(h2T.bf(), BTb, r=[BK[7]], w=[h2T.g()], eng='act')
            for qc in range(16):
                bk = 2 + (qc // 4) % 2
                for k in range(KC):
                    mm(banks[bk][:, (qc % 4) * 128:(qc % 4 + 1) * 128], WQ3[:, k, qc * 128:(qc + 1) * 128], H2T3[:, k, :], k == 0, k == KC - 1,
                       r=[hT.g(), h2T.g()], w=[BK[bk]])
                if qc % 4 == 3:
                    cp(qTs.bf()[:, (qc - 3) * 128:(qc + 1) * 128], banks[bk][:], r=[BK[bk]], w=[qTs.g((qc - 3) * 256, (qc + 1) * 256)], eng='act')
            for qc in range(16):
                bk = 4 + qc // 4
                mm(banks[bk][:, (qc % 4) * 128:(qc % 4 + 1) * 128], QTS3[:, qc, :], SUBK3[:, qc, :], True, True,
                   r=[qTs.g(qc * 256, (qc + 1) * 256), subkT.g()], w=[BK[bk]])
            for q4 in range(4):
                cp(ssb.f32()[:, q4 * 512:(q4 + 1) * 512], banks[4 + q4][:], r=[BK[4 + q4]], w=[ssb.g(q4 * 2048, (q4 + 1) * 2048)], eng='act')
            if dbg and i == 0:
                dump("dbg_xm", xm_.f32(), [128, 1024], r=[xm_.g()])
                dump("dbg_s", ssb.f32(), [128, 2048], r=[ssb.g()])
            sg_ = lambda qc: ssb.g(qc * 512, (qc + 1) * 512)
            wg_ = lambda qc: wkb.g(qc * 512, (qc + 1) * 512)
            vk = lambda qc, hf: f"v16:{qc}:{hf}"
            ik = lambda qc, hf: f"ixu:{qc}:{hf}"
            ALLV = [vk(q_, h_) for q_ in range(16) for h_ in range(2)]
            ALLI = [ik(q_, h_) for q_ in range(16) for h_ in range(2)]
            for qc in range(16):
                gen('max', [sg_(qc), v16.g()], [vk(qc, 0)], out=V16[:, qc, 0:8], in_=SS3[:, qc, :])
            for qc in range(16):
                gen('max_index', [sg_(qc), vk(qc, 0), ixu.g()], [ik(qc, 0)], out=IXU[:, qc, 0:8], in_max=V16[:, qc, 0:8], in_values=SS3[:, qc, :])
            for qc in range(16):
                gen('match_replace', [sg_(qc), vk(qc, 0)], [wg_(qc)], out=WK3[:, qc, :], in_to_replace=V16[:, qc, 0:8], in_values=SS3[:, qc, :], imm_value=NEG)
            for qc in range(16):
                gen('max', [wg_(qc), v16.g()], [vk(qc, 1)], out=V16[:, qc, 8:16], in_=WK3[:, qc, :])
            for qc in range(16):
                gen('max_index', [wg_(qc), vk(qc, 1), ixu.g()], [ik(qc, 1)], out=IXU[:, qc, 8:16], in_max=V16[:, qc, 8:16], in_values=WK3[:, qc, :])
            cp(ixf.f32(), ixu.u32(), r=ALLI, w=[ixf.g()])
            cand, wk2 = ssb, wkb
            C4 = cand.f32().rearrange("p (h n) -> p h n", h=8)
            W4 = wk2.f32().rearrange("p (h n) -> p h n", h=8)
            vf = v16.f32()
            tt(mk(cand.f32(), [[256, 8], [16, 16], [1, 16]]), mk(vf, [[32, 8], [1, 16], [0, 16]]), mk(vf, [[32, 8], [0, 16], [1, 16]], 16), ALU.add,
               r=ALLV, w=[cand.g()])
            C16 = c16.f32().rearrange("p (h j) -> p h j", h=8)
            POS = posu.u32().rearrange("p (h j) -> p h j", h=8)
            cg_ = lambda h_: cand.g(h_ * 1024, (h_ + 1) * 1024)
            w2g_ = lambda h_: wk2.g(h_ * 1024, (h_ + 1) * 1024)
            ck = lambda h_, hf: f"c16:{h_}:{hf}"
            pk = lambda h_, hf: f"pos:{h_}:{hf}"
            ALLC = [ck(h_, f_) for h_ in range(8) for f_ in range(2)]
            ALLP = [pk(h_, f_) for h_ in range(8) for f_ in range(2)]
            for h_ in range(8):
                gen('max', [cg_(h_), c16.g()], [ck(h_, 0)], out=C16[:, h_, 0:8], in_=C4[:, h_, :])
            for h_ in range(8):
                gen('max_index', [cg_(h_), ck(h_, 0), posu.g()], [pk(h_, 0)], out=POS[:, h_, 0:8], in_max=C16[:, h_, 0:8], in_values=C4[:, h_, :])
            for h_ in range(8):
                gen('match_replace', [cg_(h_), ck(h_, 0)], [w2g_(h_)], out=W4[:, h_, :], in_to_replace=C16[:, h_, 0:8], in_values=C4[:, h_, :], imm_value=NEG)
            for h_ in range(8):
                gen('max', [w2g_(h_), c16.g()], [ck(h_, 1)], out=C16[:, h_, 8:16], in_=W4[:, h_, :])
            for h_ in range(8):
                gen('max_index', [w2g_(h_), ck(h_, 1), posu.g()], [pk(h_, 1)], out=POS[:, h_, 8:16], in_max=C16[:, h_, 8:16], in_values=W4[:, h_, :])
            gen('tensor_single_scalar', ALLP, [pau.g()], out=pau.u32(), in_=posu.u32(), scalar=4, op=ALU.logical_shift_right)
            gen('tensor_single_scalar', ALLP, [pbu.g()], out=pbu.u32(), in_=posu.u32(), scalar=15, op=ALU.bitwise_and)
            cp(paf.f32(), pau.u32(), r=[pau.g()], w=[paf.g()])
            cp(pbf.f32(), pbu.u32(), r=[pbu.g()], w=[pbf.g()])
            oh, prod = ssb, wkb
            for (pf_, off_, dst_) in ((paf, 0, i1s), (pbf, 16, i2s)):
                tt(mk(oh.f32(), [[16, 128], [1, 16]]), mk(pf_.f32(), [[1, 128], [0, 16]]), mk(iota16.f32(), [[0, 128], [1, 16]]), ALU.is_equal,
                   r=[pf_.g(), iota16.g()], w=[oh.g()])
                tt(mk(prod.f32(), [[256, 8], [16, 16], [1, 16]]), mk(oh.f32(), [[256, 8], [16, 16], [1, 16]]), mk(ixf.f32(), [[32, 8], [0, 16], [1, 16]], off_), ALU.mult,
                   r=[oh.g(), ixf.g()], w=[prod.g()])
                tred(dst_.f32(), mk(prod.f32(), [[16, 128], [1, 16]]), ALU.add, r=[prod.g()], w=[dst_.g()])
            stt(eidf.f32(), i1s.f32(), 128.0, i2s.f32(), ALU.mult, ALU.add, r=[i1s.g(), i2s.g()], w=[eidf.g()])
            cp(eidu_.u32(), eidf.f32(), r=[eidf.g()], w=[eidu_.g()])
            tt(mk(scm.f32(), [[16, 8], [1, 16]]), mk(c16.f32(), [[16, 8], [1, 16]]), mk(c16.f32(), [[16, 8], [0, 16]]), ALU.subtract, r=ALLC, w=[scm.g()])
            act(ee.f32(), scm.f32(), AF.Exp, r=[scm.g()], w=[ee.g()])
            tred(zz.f32(), mk(ee.f32(), [[16, 8], [1, 16]]), ALU.add, r=[ee.g()], w=[zz.g()])
            gen('reciprocal', [zz.g()], [rzz.g()], out=rzz.f32(), in_=zz.f32())
            tt(mk(gate_.f32(), [[16, 8], [1, 16]]), mk(ee.f32(), [[16, 8], [1, 16]]), mk(rzz.f32(), [[1, 8], [0, 16]]), ALU.mult, r=[ee.g(), rzz.g()], w=[gate_.g()])
            if dbg and i == 0:
                dump("dbg_eid", eidf.f32(), [128, 128], r=[eidf.g()])
                dump("dbg_gate", gate_.f32(), [128, 128], r=[gate_.g()])

        def stageY(i, pump):
            par = i % 2
            xm_, h2_, eidu_, gate_ = xm[par], h2[par], eidu[par], gate[par]
            t0 = i * 128
            EID = eidu_.u32()

            def gather(out_ap, slot, r, w):
                P.add('pool', lambda e: e.indirect_dma_start(out=out_ap, out_offset=None, in_=etab_d.ap(),
                                                             in_offset=bass.IndirectOffsetOnAxis(ap=EID[:, slot:slot + 1], axis=0)),
                      r=r, w=w, dma=True)

            def slot_tail(sl):
                cb = cbuf[sl % NBUF]
                dg_ = dgb[sl % 4]
                act(wgt.f32()[:, sl:sl + 1], gl.f32()[:, sl:sl + 1], AF.Copy, r=[f"gl{sl}", gate_.g(), wgt.g()], w=[f"wgt{sl}"],
                    scale=gate_.f32()[:, sl:sl + 1])
                P.add('act', (lambda dg_=dg_, sl=sl: (lambda e: e.activation(out=dg_.bf(), in_=ident_f.f32(), func=AF.Copy, scale=wgt.f32()[:, sl:sl + 1])))(),
                      r=[ident_f.g(), f"wgt{sl}"], w=[dg_.g()])
                for half in range(2):
                    mm(banks[half][:], dg_.bf(), cb.bf()[:, D + half * 512:D + (half + 1) * 512], sl == 0, sl == 127,
                       r=[dg_.g(), cb.g(2048, 4096)], w=[BK[half]])

            for sl in range(128):
                cb = cbuf[sl % NBUF]
                gather(cb.bf(), sl, r=[eidu_.g()] + ETAB_KEYS, w=[cb.g()])
                P.add('dve', (lambda cb=cb, sl=sl: (lambda e: e.scalar_tensor_tensor(out=SINK, in0=cb.bf()[:, 0:D], scalar=1.0, in1=h2_.bf(), op0=ALU.mult, op1=ALU.mult,
                                                                                 accum_out=actv.f32()[:, sl:sl + 1])))(),
                      r=[cb.g(0, 2048), h2_.g(), actv.g()], w=[f"actv{sl}"])
                act(gl.f32()[:, sl:sl + 1], actv.f32()[:, sl:sl + 1], AF.Gelu, r=[f"actv{sl}", gl.g()], w=[f"gl{sl}"])
                if sl >= 1:
                    slot_tail(sl - 1)
                pump(3)
            slot_tail(127)
            for half in range(2):
                hs = slice(half * 512, (half + 1) * 512)
                tt(tmpf.f32()[:, hs], banks[half][:], g2.f32()[:, hs], ALU.mult, r=[BK[half], g2.g()], w=[tmpf.g(half * 2048, (half + 1) * 2048)])
                tt(ot.f32()[:, hs], tmpf.f32()[:, hs], xm_.f32()[:, hs], ALU.add, r=[tmpf.g(half * 2048, (half + 1) * 2048), xm_.g()], w=[ot.g(half * 2048, (half + 1) * 2048)])
            if dbg and i == 0:
                dump("dbg_xo", ot.f32(), [128, 1024], r=[ot.g()])
            act(SINK, ot.f32(), AF.Square, r=[ot.g()], w=[small.g()], accum_out=sm(SM_SS3))
            rstd_chain(SM_SS3, SM_LN3, SM_RSTD3, D, 1e-6)
            stt(ot.f32(), ot.f32(), sm(SM_RSTD3), nf_bc.f32(), ALU.mult, ALU.mult, r=[ot.g(), small.g(), nf_bc.g()], w=[ot.g()])
            dma('sp', out_d.ap()[b, t0:t0 + 128, :], ot.f32(), r=[ot.g()], w=[f"out{b}_{i}"])

        def drain(q, n=None):
            k = 0
            while q and (n is None or k < n):
                P.add(*q.pop(0))
                k += 1

        P.capture = []
        stageX(0)
        q = P.capture
        P.capture = None
        drain(q)
        for i in range(ntiles):
            if i + 1 < ntiles:
                P.capture = []
                stageX(i + 1)
                q = P.capture
                P.capture = None
            else:
                q = []
            stageY(i, lambda n: drain(q, n))
            drain(q)

    P.emit(nc, stack)
    stack.close()
    return nc, list(dbg_d.keys())


def _prep_inputs(inputs):
    f = np.float32
    x = np.asarray(inputs['x'], f)
    c = np.asarray(inputs['c'], f)
    cst = _host_consts()
    rep = lambda v, n=128: np.ascontiguousarray(np.broadcast_to(np.asarray(v, f).reshape(1, -1), (n, np.asarray(v).size)))
    nw = np.concatenate([np.asarray(inputs['norm_mix_w'], f).reshape(-1), np.asarray(inputs['norm_ffn_w'], f).reshape(-1),
                         np.asarray(inputs['norm_final_w'], f).reshape(-1)])
    lam = np.concatenate([np.asarray(inputs[k], f).reshape(-1) for k in ('lambda_q1', 'lambda_k1', 'lambda_q2', 'lambda_k2')])
    sk1 = np.asarray(inputs['sub_keys_1'], f)[0]
    sk2 = np.asarray(inputs['sub_keys_2'], f)[0]
    subkT = np.zeros((128, 16, 128), f)
    for h in range(8):
        subkT[:, h * 2 + 0, :] = sk1[h].T
        subkT[:, h * 2 + 1, :] = sk2[h].T
    shared = {
        'w_ada': np.ascontiguousarray(np.asarray(inputs['w_ada'], f)[0]),
        'b_ada_bc': rep(inputs['b_ada']),
        'nw_bc': rep(nw),
        'w_in': np.ascontiguousarray(np.asarray(inputs['w_in'], f)[0]),
        'w_out': np.ascontiguousarray(np.asarray(inputs['w_out'], f)[0]),
        'w_query': np.ascontiguousarray(np.asarray(inputs['w_query'], f)[0]),
        'pool_w': np.ascontiguousarray(np.asarray(inputs['pool_w'], f)[0]),
        'pool_scaleT': np.ascontiguousarray(np.asarray(inputs['pool_scale'], f).reshape(8, 128).T),
        'subkT': np.ascontiguousarray(subkT.reshape(128, 2048)),
        'e_down': np.ascontiguousarray(np.asarray(inputs['expert_down'], f)[0]),
        'e_up': np.ascontiguousarray(np.asarray(inputs['expert_up'], f)[0]),
        'lam_bc': rep(lam),
        'sublnT': np.ascontiguousarray(np.asarray(inputs['subln_w'], f).reshape(128, 1)),
        'relb_bc': rep(np.asarray(inputs['rel_bias'], f).reshape(-1)),
        'masks': np.ascontiguousarray(cst['masks'].reshape(128, -1)),
        'negmask': cst['negmask'],
        'band': np.ascontiguousarray(cst['band'].reshape(128, -1)),
        'ident': cst['ident'],
        'iota16': cst['iota16'],
        'sel0': cst['sel0'],
    }
    in_maps = []
    for core in range(NCORES):
        m = dict(shared)
        m['x'] = np.ascontiguousarray(x[core * NB:(core + 1) * NB])
        cc = c[core * NB:(core + 1) * NB]
        m['cT'] = np.ascontiguousarray(cc.reshape(NB, KC, 128).transpose(2, 0, 1).reshape(128, NB * KC))
        in_maps.append(m)
    return in_maps


_CACHE = {}


def kernel(**inputs):
    in_maps = _prep_inputs(inputs)
    if 'nc' not in _CACHE:
        _CACHE['nc'] = build()[0]
    nc = _CACHE['nc']
    res = run_bass_kernel_spmd(nc, in_maps, core_ids=list(range(NCORES)))
    out = np.concatenate([np.asarray(r['out']) for r in res.results], axis=0)
    return out.astype(np.float32)
```

```python
import os
import math
import contextlib
import numpy as np
import concourse.bass as bass
import concourse.mybir as mybir
from concourse.bass_utils import run_bass_kernel_spmd

dt = mybir.dt
AF = mybir.ActivationFunctionType
ALU = mybir.AluOpType
AX = mybir.AxisListType
F32, BF16, U32 = dt.float32, dt.bfloat16, dt.uint32

NCORES = 8
NB = 2
S = 2048
NT = 16
D = 1024
KC = 8
H = 8
DIN = 6144
NEXP = 16384
OFF = 8.0
NEG = -1.0e30
G = 512


class Prog:
    def __init__(self):
        self.ops = []
        self.last_w = {}
        self.readers = {}
        self.capture = None

    def add(self, eng, fn, r=(), w=(), dma=False):
        if self.capture is not None:
            self.capture.append((eng, fn, r, w, dma))
            return None
        i = len(self.ops)
        deps = set()
        rk = _flat(r)
        wk = _flat(w)
        for k in rk:
            lw = self.last_w.get(k)
            if lw is not None:
                deps.add(lw)
        for k in wk:
            lw = self.last_w.get(k)
            if lw is not None:
                deps.add(lw)
            deps.update(self.readers.get(k, ()))
        deps.discard(i)
        for k in rk:
            self.readers.setdefault(k, []).append(i)
        for k in wk:
            self.last_w[k] = i
            self.readers[k] = []
        self.ops.append(dict(eng=eng, fn=fn, deps=deps, dma=dma, sig=None, need=False, pre=None))
        return i

    def emit(self, nc, stack):
        ops = self.ops
        EPOCH = 30000
        NSLOT = {'sp': 24, 'pool': 24, 'act': 8}
        for o in ops:
            for d in o['deps']:
                p = ops[d]
                if p['eng'] == 'pe' and o['eng'] == 'pe' and not p['dma'] and not o['dma']:
                    continue
                p['need'] = True
        cnt = {e: 0 for e in ('pe', 'act', 'dve', 'pool', 'sp')}
        esems = {e: [] for e in cnt}
        dsems = {q: [stack.enter_context(nc.semaphore(f"d_{q}_{i}")) for i in range(n)] for q, n in NSLOT.items()}
        duse = {q: [0] * n for q, n in NSLOT.items()}
        dnext = {q: 0 for q in NSLOT}
        for o in ops:
            e = o['eng']
            if o['dma']:
                s = dnext[e]
                dnext[e] = (s + 1) % NSLOT[e]
                if duse[e][s] > 0:
                    o['pre'] = (dsems[e][s], 16 * duse[e][s])
                duse[e][s] += 1
                o['sig'] = (dsems[e][s], 16 * duse[e][s])
            elif o['need']:
                ep = cnt[e] // EPOCH
                while len(esems[e]) <= ep:
                    esems[e].append(stack.enter_context(nc.semaphore(f"e_{e}_{len(esems[e])}")))
                cnt[e] += 1
                o['sig'] = (esems[e][ep], cnt[e] - ep * EPOCH)
        by_eng = {e: [o for o in ops if o['eng'] == e] for e in cnt}
        final_waits = []
        for q in NSLOT:
            for s in range(NSLOT[q]):
                if duse[q][s] > 0:
                    final_waits.append((dsems[q][s], 16 * duse[q][s]))

        def run(ename, eng):
            waited = {}
            for o in by_eng[ename]:
                needs = {}
                for d in o['deps']:
                    p = ops[d]
                    if p['eng'] == 'pe' and ename == 'pe' and not p['dma'] and not o['dma']:
                        continue
                    sem, val = p['sig']
                    key = id(sem)
                    if needs.get(key, (None, 0))[1] < val:
                        needs[key] = (sem, val)
                if o['pre'] is not None:
                    sem, val = o['pre']
                    key = id(sem)
                    if needs.get(key, (None, 0))[1] < val:
                        needs[key] = (sem, val)
                for key, (sem, val) in needs.items():
                    if waited.get(key, 0) < val:
                        eng.wait_ge(sem, val)
                        waited[key] = val
                inst = o['fn'](eng)
                if o['sig'] is not None:
                    inst.then_inc(o['sig'][0], 16 if o['dma'] else 1)
            if ename == 'sp':
                for sem, val in final_waits:
                    eng.wait_ge(sem, val)

        with nc.Block() as block:
            @block.tensor
            def _(e):
                run('pe', e)

            @block.scalar
            def _(e):
                run('act', e)

            @block.vector
            def _(e):
                run('dve', e)

            @block.gpsimd
            def _(e):
                run('pool', e)

            @block.sync
            def _(e):
                run('sp', e)


def _flat(x):
    out = []
    for k in x:
        if isinstance(k, (list, tuple, set, range)):
            out.extend(_flat(k))
        else:
            out.append(k)
    return out


class Buf:
    def __init__(self, arena, off, nbytes):
        assert off % 4 == 0 and nbytes % 4 == 0
        self.A, self.off, self.nbytes = arena, off, nbytes

    def g(self, lo=0, hi=None):
        hi = self.nbytes if hi is None else hi
        return range((self.off + lo) // G, (self.off + hi + G - 1) // G)

    def f32(self):
        return self.A[:, self.off // 4:(self.off + self.nbytes) // 4]

    def bf(self):
        return self.A[:, self.off // 4:(self.off + self.nbytes) // 4].bitcast(BF16)

    def u32(self):
        return self.A[:, self.off // 4:(self.off + self.nbytes) // 4].bitcast(U32)

    def sub(self, lo, n):
        return Buf(self.A, self.off + lo, n)


class Alloc:
    def __init__(self, arena, base, limit):
        self.A, self.p, self.limit = arena, base, limit

    def get(self, nbytes):
        nb = (nbytes + G - 1) // G * G
        b = Buf(self.A, self.p, (nbytes + 3) // 4 * 4)
        self.p += nb
        assert self.p <= self.limit, (self.p, self.limit)
        return b


def mk(ap, pattern, extra_off=0):
    return bass.AP(tensor=ap.tensor, offset=ap.offset + extra_off, ap=[list(ap.ap[0])] + [list(p) for p in pattern])


def _t5_bucket(d):
    d = np.maximum(d, 0)
    x = np.maximum(d, 1).astype(np.float32) / np.float32(16)
    large = 16 + (np.log(x).astype(np.float32) / np.float32(math.log(128 / 16)) * np.float32(16)).astype(np.int32)
    large = np.minimum(large, 31)
    return np.where(d < 16, d, large)


def _host_consts():
    kk = np.arange(128)[:, None]
    qq = np.arange(256)[None, :]
    dist = qq - kk
    valid = dist >= 0
    bk = _t5_bucket(dist)
    masks = np.zeros((128, 32, 256), np.float32)
    for b in range(32):
        masks[:, b, :] = (valid & (bk == b)).astype(np.float32)
    negmask = np.where(valid, 0.0, NEG).astype(np.float32)
    band = np.zeros((128, 12, 128), np.float32)
    s = np.arange(128)[:, None]
    t = np.arange(128)[None, :]
    for gi, w in enumerate((2, 4, 8, 16)):
        cnt0 = np.minimum(t + 1, w).astype(np.float32)
        band[:, gi * 3 + 0, :] = ((s <= t) & (s > t - w)) / cnt0 - (s == t)
        band[:, gi * 3 + 1, :] = ((s <= t) & (s > t - w)) / np.float32(w) - (s == t)
        band[:, gi * 3 + 2, :] = (s > 128 + t - w) / np.float32(w)
    ident = np.eye(128, dtype=np.float32)
    iota16 = np.broadcast_to(np.arange(16, dtype=np.float32)[None, :], (128, 16)).copy()
    sel0 = np.zeros((64, 128), np.float32)
    sel0[0, :] = 1.0
    sel0[32, :] = 1.0
    return dict(masks=masks, negmask=negmask, band=band, ident=ident, iota16=iota16, sel0=sel0)


def build(stage=99, dbg=False):
    nc = bass.Bass("TRN2", target_bir_lowering=False)
    P = Prog()

    def din(name, shape, dtype=F32):
        return nc.dram_tensor(name, list(shape), dtype, kind="ExternalInput")

    x_d = din("x", [NB, S, D])
    cT_d = din("cT", [128, NB * KC])
    wada_d = din("w_ada", [D, DIN])
    bada_d = din("b_ada_bc", [128, DIN])
    nw_d = din("nw_bc", [128, 3 * D])
    win_d = din("w_in", [D, DIN])
    wout_d = din("w_out", [D, D])
    wqry_d = din("w_query", [D, 2048])
    poolw_d = din("pool_w", [4, 256, 256])
    psc_d = din("pool_scaleT", [128, 8])
    subk_d = din("subkT", [128, 16 * 128])
    edown_d = din("e_down", [NEXP, D])
    eup_d = din("e_up", [NEXP, D])
    lam_d = din("lam_bc", [128, 256])
    subln_d = din("sublnT", [128, 1])
    relb_d = din("relb_bc", [128, 256])
    masks_d = din("masks", [128, 32 * 256])
    negm_d = din("negmask", [128, 256])
    band_d = din("band", [128, 12 * 128])
    ident_d = din("ident", [128, 128])
    iota_d = din("iota16", [128, 16])
    sel0_d = din("sel0", [64, 128])
    out_d = nc.dram_tensor("out", [NB, S, D], F32, kind="ExternalOutput")
    etab_d = nc.dram_tensor("etab", [NEXP, 2 * D], BF16, kind="Internal")
    dbg_d = {}

    def dbg_out(name, shape):
        dbg_d[name] = nc.dram_tensor(name, list(shape), F32, kind="ExternalOutput")
        return dbg_d[name]

    stack = contextlib.ExitStack()
    TOT = 212480
    arena = stack.enter_context(nc.sbuf_tensor("arena", [128, TOT // 4], F32))
    banks = [stack.enter_context(nc.psum_tensor(f"B{i}", [128, 512], F32)) for i in range(8)]
    BK = [f"B{i}" for i in range(8)]

    al = Alloc(arena, 0, TOT)
    ident_f = al.get(512)
    ident_b = al.get(256)
    ones_b = al.get(256)
    ones_f = al.get(512)
    sel0 = al.get(512)
    relb = al.get(1024)
    rb31m = al.get(32)
    lamv = al.get(1024)
    small = al.get(512)
    pscT = al.get(32)
    band = al.get(12 * 128 * 2)
    subkT = al.get(16 * 128 * 2)
    iota16 = al.get(64)
    sink = al.get(64)
    nf_bc = al.get(4096)
    sh1 = al.get(4096)
    a1 = al.get(4096)
    g1 = al.get(4096)
    sh2 = al.get(4096)
    a2 = al.get(4096)
    g2 = al.get(4096)
    hT = al.get(KC * S * 2)
    mT = al.get(KC * S * 2)
    wout = al.get(KC * D * 2)
    work_base = al.p
    SM = small.f32()
    (SM_S1, SM_S2, SM_E1, SM_E2, SM_NLAM, SM_WSUB, SM_SS, SM_RSTD, SM_LN, SM_SS2, SM_RSTD2, SM_SS3, SM_RSTD3,
     SM_LN2, SM_LN3, SM_SUBLN) = range(16)

    def sm(i):
        return SM[:, i:i + 1]

    def smg(i):
        return small.g()

    def dma(q, out, in_, r, w):
        P.add(q, lambda e: e.dma_start(out=out, in_=in_), r=r, w=w, dma=True)

    def mm(out, lhsT, rhs, start, stop, r, w, **kw):
        P.add('pe', lambda e: e.matmul(out, lhsT, rhs, start=start, stop=stop, **kw), r=r, w=w)

    def tr(out, in_, ident, r, w):
        P.add('pe', lambda e: e.transpose(out, in_, ident), r=r, w=w)

    def act(out, in_, func, r, w, bias=None, scale=None, accum_out=None, eng='act'):
        def f(e):
            kw = {}
            if bias is not None:
                kw['bias'] = bias
            if scale is not None:
                kw['scale'] = scale
            if accum_out is not None:
                kw['accum_out'] = accum_out
            return e.activation(out=out, in_=in_, func=func, **kw)
        P.add('act', f, r=r, w=w)

    def tt(out, in0, in1, op, r, w, eng='dve'):
        P.add(eng, lambda e: e.tensor_tensor(out=out, in0=in0, in1=in1, op=op), r=r, w=w)

    def ts(out, in0, s1, s2, op0, op1, r, w, eng='dve'):
        if op1 is None:
            P.add(eng, lambda e: e.tensor_scalar(out=out, in0=in0, scalar1=s1, scalar2=None, op0=op0), r=r, w=w)
        else:
            P.add(eng, lambda e: e.tensor_scalar(out=out, in0=in0, scalar1=s1, scalar2=s2, op0=op0, op1=op1), r=r, w=w)

    def stt(out, in0, scalar, in1, op0, op1, r, w):
        P.add('dve', lambda e: e.scalar_tensor_tensor(out=out, in0=in0, scalar=scalar, in1=in1, op0=op0, op1=op1), r=r, w=w)

    def cp(out, in_, r, w, eng='dve'):
        if eng == 'act':
            P.add('act', lambda e: e.copy(out=out, in_=in_), r=r, w=w)
        else:
            P.add(eng, lambda e: e.tensor_copy(out=out, in_=in_), r=r, w=w)

    def ttr(out, in0, in1, accum_out, r, w):
        P.add('dve', lambda e: e.scalar_tensor_tensor(out=out, in0=in0, scalar=1.0, in1=in1, op0=ALU.mult, op1=ALU.mult,
                                                     accum_out=accum_out), r=r, w=w)

    def tred(out, in_, op, r, w):
        P.add('dve', lambda e: e.tensor_reduce(out=out, in_=in_, axis=AX.X, op=op), r=r, w=w)

    def memset(ap, val, r, w, eng='dve'):
        P.add(eng, lambda e: e.memset(ap, val), r=r, w=w)

    def rstd_chain(ss_i, ln_i, rstd_i, n, eps):
        ts(sm(ln_i), sm(ss_i), 1.0 / n, eps, ALU.mult, ALU.add, r=[small.g()], w=[small.g()])
        act(sm(ln_i), sm(ln_i), AF.Ln, r=[small.g()], w=[small.g()])
        act(sm(rstd_i), sm(ln_i), AF.Exp, r=[small.g()], w=[small.g()], scale=-0.5)

    def dump(name, ap_sb, shape, r):
        if name in dbg_d:
            return
        d = dbg_out(name, shape)
        dma('sp', d.ap(), ap_sb, r=r, w=[name])

    dma('sp', ident_f.f32(), ident_d.ap(), r=[], w=[ident_f.g()])
    dma('sp', sel0.f32()[0:64, :], sel0_d.ap(), r=[], w=[sel0.g()])
    dma('sp', relb.f32(), relb_d.ap(), r=[], w=[relb.g()])
    dma('sp', lamv.f32(), lam_d.ap(), r=[], w=[lamv.g()])
    dma('sp', pscT.f32(), psc_d.ap(), r=[], w=[pscT.g()])
    dma('sp', iota16.f32(), iota_d.ap(), r=[], w=[iota16.g()])
    dma('sp', nf_bc.f32(), nw_d.ap()[:, 2 * D:3 * D], r=[], w=[nf_bc.g()])
    dma('sp', sm(SM_SUBLN), subln_d.ap(), r=[], w=[small.g()])
    dma('pool', band.bf(), band_d.ap(), r=[], w=[band.g()])
    dma('pool', subkT.bf(), subk_d.ap(), r=[], w=[subkT.g()])
    ETAB_JOBS = [(ti, r0) for ti in range(2) for r0 in range(0, NEXP, 1024)]
    ETAB_KEYS = [f"etab{ti}_{r0}" for (ti, r0) in ETAB_JOBS]

    def precast(n):
        for _ in range(n):
            if not ETAB_JOBS:
                return
            ti, r0 = ETAB_JOBS.pop(0)
            tbl = (edown_d, eup_d)[ti]
            dma('pool', etab_d.ap()[r0:r0 + 1024, ti * D:(ti + 1) * D], tbl.ap()[r0:r0 + 1024, :], r=[], w=[f"etab{ti}_{r0}"])

    cp(ident_b.bf(), ident_f.f32(), r=[ident_f.g()], w=[ident_b.g()])
    memset(ones_b.bf(), 1.0, r=[], w=[ones_b.g()])
    memset(ones_f.f32(), 1.0, r=[], w=[ones_f.g()])
    ts(rb31m.f32(), relb.f32()[:, 31 * 8:32 * 8], -OFF, None, ALU.add, None, r=[relb.g()], w=[rb31m.g()])
    ts(sm(SM_WSUB), sm(SM_SUBLN), 0.8, None, ALU.mult, None, r=[small.g()], w=[small.g()])
    wa = Alloc(arena, work_base, TOT)
    junk64 = wa.get(256)
    LV = lamv.f32()
    ttr(junk64.f32(), LV[:, 0:64], LV[:, 64:128], sm(SM_S1), r=[lamv.g()], w=[junk64.g(), small.g()])
    ttr(junk64.f32(), LV[:, 128:192], LV[:, 192:256], sm(SM_S2), r=[lamv.g()], w=[junk64.g(), small.g()])
    act(sm(SM_E1), sm(SM_S1), AF.Exp, r=[small.g()], w=[small.g()])
    act(sm(SM_E2), sm(SM_S2), AF.Exp, r=[small.g()], w=[small.g()])
    tt(sm(SM_NLAM), sm(SM_E2), sm(SM_E1), ALU.subtract, r=[small.g()], w=[small.g()])
    ts(sm(SM_NLAM), sm(SM_NLAM), -0.2, None, ALU.add, None, r=[small.g()], w=[small.g()])
    bias_scr = nc.dram_tensor("bias_scr", [128, 2048], F32, kind="Internal")
    biasT = wa.get(8 * 1024)
    mk_chunk = wa.get(8 * 256 * 4)
    BT3 = biasT.f32().rearrange("p (h q) -> p h q", h=8)
    for h in range(8):
        dma('sp', BT3[:, h, :], negm_d.ap(), r=[], w=[biasT.g(h * 1024, (h + 1) * 1024)])
    for c in range(4):
        dma('sp', mk_chunk.f32(), masks_d.ap()[:, c * 2048:(c + 1) * 2048], r=[], w=[mk_chunk.g()])
        MC = mk_chunk.f32().rearrange("p (b q) -> p b q", b=8)
        for bb in range(8):
            b = c * 8 + bb
            for h in range(8):
                stt(BT3[:, h, :], MC[:, bb, :], relb.f32()[:, b * 8 + h:b * 8 + h + 1], BT3[:, h, :], ALU.mult, ALU.add,
                    r=[mk_chunk.g(), relb.g(), biasT.g(h * 1024, (h + 1) * 1024)], w=[biasT.g(h * 1024, (h + 1) * 1024)])
    ts(biasT.f32(), biasT.f32(), -OFF, None, ALU.add, None, r=[biasT.g()], w=[biasT.g()])
    dma('sp', bias_scr.ap(), biasT.f32(), r=[biasT.g()], w=["bias_scr"])
    if dbg:
        dump("dbg_bias", biasT.f32(), [128, 2048], r=[biasT.g()])
        dump("dbg_small", small.f32()[:, 0:16], [128, 16], r=[small.g()])

    hT3 = hT.bf().rearrange("p (k t) -> p k t", k=KC)
    mT3 = mT.bf().rearrange("p (k t) -> p k t", k=KC)

    def hT_g(c0, c1):
        return [hT.g(k * S * 2 + c0 * 2, k * S * 2 + c1 * 2) for k in range(KC)]

    def mT_g(k, c0, c1):
        return mT.g(k * S * 2 + c0 * 2, k * S * 2 + c1 * 2)

    BTb = banks[7][:].bitcast(BF16)

    for b in range(NB):
        wa = Alloc(arena, work_base, TOT)
        cact = wa.get(64)
        crep = wa.get(KC * 128 * 4)
        wblk = [wa.get(KC * 512 * 4) for _ in range(2)]
        bblk = [wa.get(2048) for _ in range(2)]
        mtmp = wa.get(2048)
        nwt = wa.get(4096)
        cin = wa.get(64)
        dma('sp', cin.f32()[:, 0:KC], cT_d.ap()[:, b * KC:(b + 1) * KC], r=[], w=[cin.g()])
        act(cact.f32()[:, 0:KC], cin.f32()[:, 0:KC], AF.Silu, r=[cin.g()], w=[cact.g()])
        cp(crep.f32().rearrange("p (k m) -> p k m", k=KC), mk(cact.f32(), [[1, KC], [0, 128]]), r=[cact.g()], w=[crep.g()])
        CR = crep.f32().rearrange("p (k m) -> p k m", k=KC)
        wada_v = wada_d.ap().rearrange("(k p) n -> p k n", p=128)
        dsts = [sh1, sh1, a1, a1, g1, g1, sh2, sh2, a2, a2, g2, g2]
        for nb_ in range(12):
            wb = wblk[nb_ % 2]
            bb_ = bblk[nb_ % 2]
            dma('sp', wb.f32().rearrange("p (k n) -> p k n", k=KC), wada_v[:, :, nb_ * 512:(nb_ + 1) * 512], r=[], w=[wb.g()])
            dma('sp', bb_.f32(), bada_d.ap()[:, nb_ * 512:(nb_ + 1) * 512], r=[], w=[bb_.g()])
            WB = wb.f32().rearrange("p (k n) -> p k n", k=KC)
            for k in range(KC):
                mm(banks[6][:], CR[:, k, :], WB[:, k, :], k == 0, k == KC - 1, r=[crep.g(), wb.g()], w=[BK[6]])
            dst = dsts[nb_]
            half = nb_ % 2
            dst_ap = dst.f32()[:, half * 512:(half + 1) * 512]
            dg_ = dst.g(half * 2048, (half + 1) * 2048)
            if nb_ in (2, 3, 8, 9):
                which = 0 if nb_ in (2, 3) else 1
                dma('sp', nwt.f32()[:, 0:512], nw_d.ap()[:, which * D + half * 512: which * D + (half + 1) * 512], r=[], w=[nwt.g()])
                tt(mtmp.f32(), banks[6][:], bb_.f32(), ALU.add, r=[BK[6], bb_.g()], w=[mtmp.g()])
                stt(dst_ap, mtmp.f32(), 1.0, nwt.f32()[:, 0:512], ALU.add, ALU.mult, r=[mtmp.g(), nwt.g()], w=[dg_])
            else:
                tt(dst_ap, banks[6][:], bb_.f32(), ALU.add, r=[BK[6], bb_.g()], w=[dg_])
        if dbg and b == 0:
            dump("dbg_a1", a1.f32(), [128, 1024], r=[a1.g()])
            dump("dbg_g2", g2.f32(), [128, 1024], r=[g2.g()])

        wa = Alloc(arena, work_base, TOT)
        xt = [wa.get(4096) for _ in range(2)]
        tmpf = wa.get(4096)
        junkb = wa.get(2048)
        hbf = wa.get(2048)
        for i in range(NT):
            xb = xt[i % 2]
            dma('sp', xb.f32(), x_d.ap()[b, i * 128:(i + 1) * 128, :], r=[], w=[xb.g()])
            act(junkb.bf(), xb.f32(), AF.Square, r=[xb.g()], w=[junkb.g(), small.g()], accum_out=sm(SM_SS))
            rstd_chain(SM_SS, SM_LN, SM_RSTD, D, 1e-6)
            stt(tmpf.f32(), xb.f32(), sm(SM_RSTD), a1.f32(), ALU.mult, ALU.mult, r=[xb.g(), small.g(), a1.g()], w=[tmpf.g()])
            tt(hbf.bf(), tmpf.f32(), sh1.f32(), ALU.add, r=[tmpf.g(), sh1.g()], w=[hbf.g()])
            for k in range(KC):
                tr(BTb[:, k * 128:(k + 1) * 128], hbf.bf()[:, k * 128:(k + 1) * 128], ident_b.bf(), r=[hbf.g(), ident_b.g()], w=[BK[7]])
            cp(hT3[:, :, i * 128:(i + 1) * 128], BTb.rearrange("p (k t) -> p k t", k=KC), r=[BK[7]], w=hT_g(i * 128, (i + 1) * 128), eng='act')
        if dbg and b == 0:
            wa2 = Alloc(arena, wa.p, TOT)
            dtmp = wa2.get(8192)
            cp(dtmp.f32(), hT3[:, 0, :], r=hT_g(0, S), w=[dtmp.g()])
            dump("dbg_hT0", dtmp.f32(), [128, 2048], r=[dtmp.g()])
        if stage <= 1:
            continue

        win_v = win_d.ap().rearrange("(k p) n -> p k n", p=128)
        wa = Alloc(arena, work_base, TOT)
        wp = wa.get(KC * 256 * 2)
        wgp = wa.get(KC * 256 * 2)
        pw = wa.get(2 * 256 * 2)
        pg = wa.get(NT * 256 * 2)
        pT = [wa.get(512 * 2) for _ in range(2)]
        sg = wa.get(512 * 4)
        WP = wp.bf().rearrange("p (k n) -> p k n", k=KC)
        WGP = wgp.bf().rearrange("p (k n) -> p k n", k=KC)
        PW = pw.bf().rearrange("p (c n) -> p c n", c=2)
        PG = pg.bf().rearrange("p (i n) -> p i n", i=NT)
        BAND = band.bf().rearrange("p (j n) -> p j n", j=12)
        for g in range(4):
            dma('pool', WP, win_v[:, :, 3072 + g * 256:3072 + (g + 1) * 256], r=[], w=[wp.g()])
            dma('pool', WGP, win_v[:, :, 5120 + g * 256:5120 + (g + 1) * 256], r=[], w=[wgp.g()])
            dma('pool', PW, poolw_d.ap()[g].rearrange("(c p) n -> p c n", p=128), r=[], w=[pw.g()])
            for i in range(NT):
                for k in range(KC):
                    mm(banks[7][:, (i % 2) * 256:(i % 2 + 1) * 256], hT3[:, k, i * 128:(i + 1) * 128], WP[:, k, :], k == 0, k == KC - 1,
                       r=[hT_g(i * 128, (i + 1) * 128), wp.g()], w=[BK[7]])
                if i % 2 == 1:
                    cp(pg.bf()[:, (i - 1) * 256:(i + 1) * 256], banks[7][:], r=[BK[7]], w=[pg.g((i - 1) * 512, (i + 1) * 512)], eng='act')
            for c in range(4):
                for cc in range(2):
                    for il in range(4):
                        i = c * 4 + il
                        cur = g * 3 + (0 if i == 0 else 1)
                        mm(banks[cc][:, il * 128:(il + 1) * 128], PG[:, i, cc * 128:(cc + 1) * 128], BAND[:, cur, :], True, i == 0,
                           r=[pg.g(i * 512, (i + 1) * 512), band.g()], w=[BK[cc]])
                        if i > 0:
                            mm(banks[cc][:, il * 128:(il + 1) * 128], PG[:, i - 1, cc * 128:(cc + 1) * 128], BAND[:, g * 3 + 2, :], False, True,
                               r=[pg.g((i - 1) * 512, i * 512), band.g()], w=[BK[cc]])
                    cp(pT[cc].bf(), banks[cc][:], r=[BK[cc]], w=[pT[cc].g()], eng='act')
                for ec in range(2):
                    kch = g * 2 + ec
                    mm(banks[2][:], PW[:, 0, ec * 128:(ec + 1) * 128], pT[0].bf(), True, False, r=[pw.g(), pT[0].g()], w=[BK[2]])
                    mm(banks[2][:], PW[:, 1, ec * 128:(ec + 1) * 128], pT[1].bf(), False, True, r=[pw.g(), pT[1].g()], w=[BK[2]])
                    for k in range(KC):
                        mm(banks[3][:], WGP[:, k, ec * 128:(ec + 1) * 128], hT3[:, k, c * 512:(c + 1) * 512], k == 0, k == KC - 1,
                           r=[wgp.g(), hT_g(c * 512, (c + 1) * 512)], w=[BK[3]])
                    act(sg.f32(), banks[3][:], AF.Sigmoid, r=[BK[3]], w=[sg.g()])
                    stt(mT3[:, kch, c * 512:(c + 1) * 512], banks[2][:], pscT.f32()[:, kch:kch + 1], sg.f32(), ALU.mult, ALU.mult,
                        r=[BK[2], pscT.g(), sg.g()], w=[mT_g(kch, c * 512, (c + 1) * 512)])
        if dbg and b == 0 and stage == 2:
            wa2 = Alloc(arena, wa.p, TOT)
            dtmp = wa2.get(8192)
            for kk_ in (0, 5):
                cp(dtmp.f32(), mT3[:, kk_, :], r=[mT.g()], w=[dtmp.g()])
                dump(f"dbg_mT{kk_}", dtmp.f32(), [128, 2048], r=[dtmp.g()])
        if stage <= 2:
            continue

        wa = Alloc(arena, work_base, TOT)
        wq = wa.get(KC * 128 * 2)
        wk_ = wa.get(KC * 128 * 2)
        wv = wa.get(KC * 128 * 2)
        wg = wa.get(KC * 128 * 2)
        QTz = [wa.get(S * 2) for _ in range(2)]
        KT = wa.get(S * 2)
        Vb = wa.get(NT * 128 * 2)
        sgT = wa.get(S * 2)
        PT = [[wa.get(512 * 2) for _ in range(2)] for _ in range(2)]
        ntmp = [wa.get(256 * 4) for _ in range(2)]
        O1s = wa.get(2048)
        O2s = wa.get(2048)
        lnz = wa.get(2048)
        rz = wa.get(2048)
        rz2 = wa.get(2048)
        sq = wa.get(2048)
        t1 = wa.get(2048)
        rs = wa.get(2048)
        biasT = wa.get(8 * 1024)
        BT3 = biasT.f32().rearrange("p (h q) -> p h q", h=8)
        dma('sp', biasT.f32(), bias_scr.ap(), r=["bias_scr"], w=[biasT.g()])
        WQ = wq.bf().rearrange("p (k n) -> p k n", k=KC)
        WK = wk_.bf().rearrange("p (k n) -> p k n", k=KC)
        WV = wv.bf().rearrange("p (k n) -> p k n", k=KC)
        WG = wg.bf().rearrange("p (k n) -> p k n", k=KC)
        V3 = Vb.bf().rearrange("p (i n) -> p i n", i=NT)
        nheads = H if stage > 3 or not dbg else 1
        memset(QTz[0].bf()[64:128, :], 0.0, r=[], w=[QTz[0].g()])
        memset(QTz[1].bf()[0:64, :], 0.0, r=[], w=[QTz[1].g()])
        for h in range(nheads):
            dma('pool', WQ, win_v[:, :, h * 128:(h + 1) * 128], r=[], w=[wq.g()])
            dma('pool', WK, win_v[:, :, 1024 + h * 128:1024 + (h + 1) * 128], r=[], w=[wk_.g()])
            dma('pool', WV, win_v[:, :, 2048 + h * 128:2048 + (h + 1) * 128], r=[], w=[wv.g()])
            dma('pool', WG, win_v[:, :, 4096 + h * 128:4096 + (h + 1) * 128], r=[], w=[wg.g()])
            precast(4)
            pj = 0
            for (W_, wb_, dst, mode) in ((WQ, wq, None, 'q'), (WK, wk_, KT, 'k'), (WG, wg, sgT, 'g')):
                for c in range(4):
                    bk = pj % 4
                    pj += 1
                    for k in range(KC):
                        mm(banks[bk][:], W_[:, k, :], hT3[:, k, c * 512:(c + 1) * 512], k == 0, k == KC - 1,
                           r=[wb_.g(), hT_g(c * 512, (c + 1) * 512)], w=[BK[bk]])
                    if mode == 'q':
                        for m_ in range(2):
                            o_ap = QTz[m_].bf()[m_ * 64:(m_ + 1) * 64, c * 512:(c + 1) * 512]
                            P.add('act', (lambda o_ap=o_ap, bk=bk, m_=m_: (lambda e: e.mul(out=o_ap, in_=banks[bk][m_ * 64:(m_ + 1) * 64, :], mul=0.125)))(),
                                  r=[BK[bk]], w=[QTz[m_].g(c * 1024, (c + 1) * 1024)])
                        continue
                    o_ap = dst.bf()[:, c * 512:(c + 1) * 512]
                    o_g = dst.g(c * 1024, (c + 1) * 1024)
                    if mode == 'k':
                        cp(o_ap, banks[bk][:], r=[BK[bk]], w=[o_g], eng='act')
                    else:
                        act(o_ap, banks[bk][:], AF.Sigmoid, r=[BK[bk]], w=[o_g])
            for i in range(NT):
                bk = (i // 4) % 4
                for k in range(KC):
                    mm(banks[bk][:, (i % 4) * 128:(i % 4 + 1) * 128], hT3[:, k, i * 128:(i + 1) * 128], WV[:, k, :], k == 0, k == KC - 1,
                       r=[hT_g(i * 128, (i + 1) * 128), wv.g()], w=[BK[bk]])
                if i % 4 == 3:
                    cp(Vb.bf()[:, (i - 3) * 128:(i + 1) * 128], banks[bk][:], r=[BK[bk]], w=[Vb.g((i - 3) * 256, (i + 1) * 256)], eng='act')

            def qk(c, j):
                col0 = max(0, j - 4 * c) * 128
                par = j % 2
                for m in range(2):
                    bk = m * 2 + par
                    mm(banks[bk][:, col0:512], KT.bf()[:, j * 128:(j + 1) * 128],
                       QTz[m].bf()[:, c * 512 + col0:(c + 1) * 512], True, True,
                       r=[KT.g(j * 256, (j + 1) * 256), QTz[m].g(c * 1024 + col0 * 2, (c + 1) * 1024)], w=[BK[bk]])

            for c in range(4):
                jmax = 4 * c + 3
                qk(c, 0)
                for j in range(jmax + 1):
                    if j + 1 <= jmax:
                        qk(c, j + 1)
                    col0 = max(0, j - 4 * c) * 128
                    par = j % 2
                    il_lo = max(0, j - 4 * c)
                    il_hi = min(3, j + 1 - 4 * c)
                    far0 = max(0, j + 2 - 4 * c) * 128
                    for m in range(2):
                        bk = m * 2 + par
                        pt = PT[m][par]
                        if il_hi >= il_lo and il_hi >= 0:
                            n0, n1 = il_lo * 128, (il_hi + 1) * 128
                            b0 = (4 * c + il_lo - j) * 128
                            nn = n1 - n0
                            tt(ntmp[m].f32()[:, 0:nn], banks[bk][:, n0:n1], BT3[:, h, b0:b0 + nn], ALU.add,
                               r=[BK[bk], biasT.g(h * 1024, (h + 1) * 1024)], w=[ntmp[m].g()])
                            act(pt.bf()[:, n0:n1], ntmp[m].f32()[:, 0:nn], AF.Exp, r=[ntmp[m].g()], w=[pt.g(n0 * 2, n1 * 2)])
                        if far0 < 512:
                            act(pt.bf()[:, far0:512], banks[bk][:, far0:512], AF.Exp, r=[BK[bk], rb31m.g()], w=[pt.g(far0 * 2, 1024)],
                                bias=rb31m.f32()[:, h:h + 1])
                        mm(banks[4 + m][:, col0:512], V3[:, j, :], pt.bf()[:, col0:512], j == 0, j == jmax,
                           r=[Vb.g(j * 256, (j + 1) * 256), pt.g(col0 * 2, 1024)], w=[BK[4 + m]], skip_group_check=True)
                        mm(banks[6 + m][:, col0:512], ones_b.bf(), pt.bf()[:, col0:512], j == 0, j == jmax,
                           r=[ones_b.g(), pt.g(col0 * 2, 1024)], w=[BK[6 + m]], skip_group_check=True)
                cs = slice(c * 512, (c + 1) * 512)
                cp(O1s.f32(), banks[4][:], r=[BK[4]], w=[O1s.g()], eng='act')
                cp(O2s.f32(), banks[5][:], r=[BK[5]], w=[O2s.g()], eng='act')
                act(lnz.f32(), banks[6][:], AF.Ln, r=[BK[6]], w=[lnz.g()])
                act(rz.f32(), lnz.f32(), AF.Exp, r=[lnz.g()], w=[rz.g()], scale=-1.0)
                act(lnz.f32(), banks[7][:], AF.Ln, r=[BK[7]], w=[lnz.g()])
                act(rz2.f32(), lnz.f32(), AF.Exp, r=[lnz.g()], w=[rz2.g()], scale=-1.0)
                tt(t1.f32(), O1s.f32(), rz.f32(), ALU.mult, r=[O1s.g(), rz.g()], w=[t1.g()])
                tt(O2s.f32(), O2s.f32(), rz2.f32(), ALU.mult, r=[O2s.g(), rz2.g()], w=[O2s.g()])
                stt(t1.f32(), O2s.f32(), sm(SM_NLAM), t1.f32(), ALU.mult, ALU.add, r=[t1.g(), O2s.g(), small.g()], w=[t1.g()])
                act(sq.f32(), t1.f32(), AF.Square, r=[t1.g()], w=[sq.g()])
                mm(banks[6][:], ones_f.f32(), sq.f32(), True, True, r=[ones_f.g(), sq.g()], w=[BK[6]])
                ts(rs.f32(), banks[6][:], 1.0 / 128, 1e-5, ALU.mult, ALU.add, r=[BK[6]], w=[rs.g()])
                act(rs.f32(), rs.f32(), AF.Ln, r=[rs.g()], w=[rs.g()])
                act(rs.f32(), rs.f32(), AF.Exp, r=[rs.g()], w=[rs.g()], scale=-0.5)
                tt(t1.f32(), t1.f32(), rs.f32(), ALU.mult, r=[t1.g(), rs.g()], w=[t1.g()])
                stt(t1.f32(), t1.f32(), sm(SM_WSUB), sgT.bf()[:, cs], ALU.mult, ALU.mult, r=[t1.g(), small.g(), sgT.g(c * 1024, (c + 1) * 1024)], w=[t1.g()])
                tt(mT3[:, h, cs], t1.f32(), mT3[:, h, cs], ALU.add, r=[t1.g(), mT_g(h, c * 512, (c + 1) * 512)], w=[mT_g(h, c * 512, (c + 1) * 512)])
        if dbg and b == 0 and stage == 3:
            wa2 = Alloc(arena, wa.p, TOT)
            dtmp = wa2.get(8192)
            cp(dtmp.f32(), mT3[:, 0, :], r=[mT.g()], w=[dtmp.g()])
            dump("dbg_mT0", dtmp.f32(), [128, 2048], r=[dtmp.g()])
        if stage <= 3:
            continue

        precast(64)
        WOUT3 = wout.bf().rearrange("p (k n) -> p k n", k=KC)
        dma('pool', WOUT3, wout_d.ap().rearrange("(k p) n -> p k n", p=128), r=[], w=[wout.g()])
        for k in range(KC):
            tt(WOUT3[:, k, :], WOUT3[:, k, :], g1.f32(), ALU.mult, r=[wout.g(k * 2048, (k + 1) * 2048), g1.g()], w=[wout.g(k * 2048, (k + 1) * 2048)])
        WQ3 = hT.bf().rearrange("p (k n) -> p k n", k=KC)
        dma('pool', WQ3, wqry_d.ap().rearrange("(k p) n -> p k n", p=128), r=[], w=[hT.g()])
        wa = Alloc(arena, work_base, TOT)
        xt5 = g1
        xm = [wa.get(4096) for _ in range(2)]
        h2 = [wa.get(2048) for _ in range(2)]
        eidu = [wa.get(512) for _ in range(2)]
        gate = [wa.get(512) for _ in range(2)]
        tmpf = a1
        ot = sh1
        ssb = wa.get(8192)
        wkb = ssb
        tmpfX = ssb.sub(0, 4096)
        junkbX = ssb.sub(4096, 2048)
        h2T = ssb.sub(0, 2048)
        qTs = ssb.sub(4096, 4096)
        v16 = wa.get(1024)
        ixu = wa.get(1024)
        ixf = wa.get(1024)
        c16, posu, pau, pbu, paf, pbf, i1s, i2s, eidf, scm, ee, actv, gl, wgt = [wa.get(512) for _ in range(14)]
        zz = wa.get(32)
        rzz = wa.get(32)
        dgb = [wa.get(256) for _ in range(4)]
        NBUF = (TOT - wa.p) // 4096
        assert NBUF >= 5, NBUF
        cbuf = [wa.get(4096) for _ in range(NBUF)]
        SINK = mk(sink.bf(), [[0, D]])
        H2T3 = h2T.bf().rearrange("p (k t) -> p k t", k=KC)
        QTS3 = qTs.bf().rearrange("p (q t) -> p q t", q=16)
        SUBK3 = subkT.bf().rearrange("p (q n) -> p q n", q=16)
        SS3 = ssb.f32().rearrange("p (q n) -> p q n", q=16)
        WK3 = wkb.f32().rearrange("p (q n) -> p q n", q=16)
        V16 = v16.f32().rearrange("p (q j) -> p q j", q=16)
        IXU = ixu.u32().rearrange("p (q j) -> p q j", q=16)
        ntiles = NT if not dbg else 2

        def gen(name, r, w, eng='dve', **kw):
            P.add(eng, lambda e: getattr(e, name)(**kw), r=r, w=w)

        def stageX(i):
            par = i % 2
            xm_, h2_, eidu_, gate_ = xm[par], h2[par], eidu[par], gate[par]
            t0 = i * 128
            dma('sp', xt5.f32(), x_d.ap()[b, t0:t0 + 128, :], r=[], w=[xt5.g()])
            for half in range(2):
                for k in range(KC):
                    mm(banks[2 + half][:], mT3[:, k, t0:t0 + 128], WOUT3[:, k, half * 512:(half + 1) * 512], k == 0, k == KC - 1,
                       r=[mT_g(k, t0, t0 + 128), wout.g()], w=[BK[2 + half]])
            for half in range(2):
                hs = slice(half * 512, (half + 1) * 512)
                tt(xm_.f32()[:, hs], banks[2 + half][:], xt5.f32()[:, hs], ALU.add, r=[BK[2 + half], xt5.g()], w=[xm_.g(half * 2048, (half + 1) * 2048)])
            act(junkbX.bf(), xm_.f32(), AF.Square, r=[xm_.g()], w=[junkbX.g(), small.g()], accum_out=sm(SM_SS2))
            rstd_chain(SM_SS2, SM_LN2, SM_RSTD2, D, 1e-6)
            stt(tmpfX.f32(), xm_.f32(), sm(SM_RSTD2), a2.f32(), ALU.mult, ALU.mult, r=[xm_.g(), small.g(), a2.g()], w=[tmpfX.g()])
            tt(h2_.bf(), tmpfX.f32(), sh2.f32(), ALU.add, r=[tmpfX.g(), sh2.g()], w=[h2_.g()])
            for k in range(KC):
                tr(BTb[:, k * 128:(k + 1) * 128], h2_.bf()[:, k * 128:(k + 1) * 128], ident_b.bf(), r=[h2_.g(), ident_b.g()], w=[BK[7]])
            cp(h2T.bf(), BTb, r=[BK[7]], w=[h2T.g()], eng='act')
            for qc in range(16):
                bk = 2 + (qc // 4) % 2
                for k in range(KC):
                    mm(banks[bk][:, (qc % 4) * 128:(qc % 4 + 1) * 128], WQ3[:, k, qc * 128:(qc + 1) * 128], H2T3[:, k, :], k == 0, k == KC - 1,
                       r=[hT.g(), h2T.g()], w=[BK[bk]])
                if qc % 4 == 3:
                    cp(qTs.bf()[:, (qc - 3) * 128:(qc + 1) * 128], banks[bk][:], r=[BK[bk]], w=[qTs.g((qc - 3) * 256, (qc + 1) * 256)], eng='act')
            for qc in range(16):
                bk = 4 + qc // 4
                mm(banks[bk][:, (qc % 4) * 128:(qc % 4 + 1) * 128], QTS3[:, qc, :], SUBK3[:, qc, :], True, True,
                   r=[qTs.g(qc * 256, (qc + 1) * 256), subkT.g()], w=[BK[bk]])
            for q4 in range(4):
                cp(ssb.f32()[:, q4 * 512:(q4 + 1) * 512], banks[4 + q4][:], r=[BK[4 + q4]], w=[ssb.g(q4 * 2048, (q4 + 1) * 2048)], eng='act')
            if dbg and i == 0:
                dump("dbg_xm", xm_.f32(), [128, 1024], r=[xm_.g()])
                dump("dbg_s", ssb.f32(), [128, 2048], r=[ssb.g()])
            sg_ = lambda qc: ssb.g(qc * 512, (qc + 1) * 512)
            wg_ = lambda qc: wkb.g(qc * 512, (qc + 1) * 512)
            vk = lambda qc, hf: f"v16:{qc}:{hf}"
            ik = lambda qc, hf: f"ixu:{qc}:{hf}"
            ALLV = [vk(q_, h_) for q_ in range(16) for h_ in range(2)]
            ALLI = [ik(q_, h_) for q_ in range(16) for h_ in range(2)]
            for qc in range(16):
                gen('max', [sg_(qc), v16.g()], [vk(qc, 0)], out=V16[:, qc, 0:8], in_=SS3[:, qc, :])
            for qc in range(16):
                gen('max_index', [sg_(qc), vk(qc, 0), ixu.g()], [ik(qc, 0)], out=IXU[:, qc, 0:8], in_max=V16[:, qc, 0:8], in_values=SS3[:, qc, :])
            for qc in range(16):
                gen('match_replace', [sg_(qc), vk(qc, 0)], [wg_(qc)], out=WK3[:, qc, :], in_to_replace=V16[:, qc, 0:8], in_values=SS3[:, qc, :], imm_value=NEG)
            for qc in range(16):
                gen('max', [wg_(qc), v16.g()], [vk(qc, 1)], out=V16[:, qc, 8:16], in_=WK3[:, qc, :])
            for qc in range(16):
                gen('max_index', [wg_(qc), vk(qc, 1), ixu.g()], [ik(qc, 1)], out=IXU[:, qc, 8:16], in_max=V16[:, qc, 8:16], in_values=WK3[:, qc, :])
            cp(ixf.f32(), ixu.u32(), r=ALLI, w=[ixf.g()])
            cand, wk2 = ssb, wkb
            C4 = cand.f32().rearrange("p (h n) -> p h n", h=8)
            W4 = wk2.f32().rearrange("p (h n) -> p h n", h=8)
            vf = v16.f32()
            tt(mk(cand.f32(), [[256, 8], [16, 16], [1, 16]]), mk(vf, [[32, 8], [1, 16], [0, 16]]), mk(vf, [[32, 8], [0, 16], [1, 16]], 16), ALU.add,
               r=ALLV, w=[cand.g()])
            C16 = c16.f32().rearrange("p (h j) -> p h j", h=8)
            POS = posu.u32().rearrange("p (h j) -> p h j", h=8)
            cg_ = lambda h_: cand.g(h_ * 1024, (h_ + 1) * 1024)
            w2g_ = lambda h_: wk2.g(h_ * 1024, (h_ + 1) * 1024)
            ck = lambda h_, hf: f"c16:{h_}:{hf}"
            pk = lambda h_, hf: f"pos:{h_}:{hf}"
            ALLC = [ck(h_, f_) for h_ in range(8) for f_ in range(2)]
            ALLP = [pk(h_, f_) for h_ in range(8) for f_ in range(2)]
            for h_ in range(8):
                gen('max', [cg_(h_), c16.g()], [ck(h_, 0)], out=C16[:, h_, 0:8], in_=C4[:, h_, :])
            for h_ in range(8):
                gen('max_index', [cg_(h_), ck(h_, 0), posu.g()], [pk(h_, 0)], out=POS[:, h_, 0:8], in_max=C16[:, h_, 0:8], in_values=C4[:, h_, :])
            for h_ in range(8):
                gen('match_replace', [cg_(h_), ck(h_, 0)], [w2g_(h_)], out=W4[:, h_, :], in_to_replace=C16[:, h_, 0:8], in_values=C4[:, h_, :], imm_value=NEG)
            for h_ in range(8):
                gen('max', [w2g_(h_), c16.g()], [ck(h_, 1)], out=C16[:, h_, 8:16], in_=W4[:, h_, :])
            for h_ in range(8):
                gen('max_index', [w2g_(h_), ck(h_, 1), posu.g()], [pk(h_, 1)], out=POS[:, h_, 8:16], in_max=C16[:, h_, 8:16], in_values=W4[:, h_, :])
            gen('tensor_single_scalar', ALLP, [pau.g()], out=pau.u32(), in_=posu.u32(), scalar=4, op=ALU.logical_shift_right)
            gen('tensor_single_scalar', ALLP, [pbu.g()], out=pbu.u32(), in_=posu.u32(), scalar=15, op=ALU.bitwise_and)
            cp(paf.f32(), pau.u32(), r=[pau.g()], w=[paf.g()])
            cp(pbf.f32(), pbu.u32(), r=[pbu.g()], w=[pbf.g()])
            oh, prod = ssb, wkb
            for (pf_, off_, dst_) in ((paf, 0, i1s), (pbf, 16, i2s)):
                tt(mk(oh.f32(), [[16, 128], [1, 16]]), mk(pf_.f32(), [[1, 128], [0, 16]]), mk(iota16.f32(), [[0, 128], [1, 16]]), ALU.is_equal,
                   r=[pf_.g(), iota16.g()], w=[oh.g()])
                tt(mk(prod.f32(), [[256, 8], [16, 16], [1, 16]]), mk(oh.f32(), [[256, 8], [16, 16], [1, 16]]), mk(ixf.f32(), [[32, 8], [0, 16], [1, 16]], off_), ALU.mult,
                   r=[oh.g(), ixf.g()], w=[prod.g()])
                tred(dst_.f32(), mk(prod.f32(), [[16, 128], [1, 16]]), ALU.add, r=[prod.g()], w=[dst_.g()])
            stt(eidf.f32(), i1s.f32(), 128.0, i2s.f32(), ALU.mult, ALU.add, r=[i1s.g(), i2s.g()], w=[eidf.g()])
            cp(eidu_.u32(), eidf.f32(), r=[eidf.g()], w=[eidu_.g()])
            tt(mk(scm.f32(), [[16, 8], [1, 16]]), mk(c16.f32(), [[16, 8], [1, 16]]), mk(c16.f32(), [[16, 8], [0, 16]]), ALU.subtract, r=ALLC, w=[scm.g()])
            act(ee.f32(), scm.f32(), AF.Exp, r=[scm.g()], w=[ee.g()])
            tred(zz.f32(), mk(ee.f32(), [[16, 8], [1, 16]]), ALU.add, r=[ee.g()], w=[zz.g()])
            gen('reciprocal', [zz.g()], [rzz.g()], out=rzz.f32(), in_=zz.f32())
            tt(mk(gate_.f32(), [[16, 8], [1, 16]]), mk(ee.f32(), [[16, 8], [1, 16]]), mk(rzz.f32(), [[1, 8], [0, 16]]), ALU.mult, r=[ee.g(), rzz.g()], w=[gate_.g()])
            if dbg and i == 0:
                dump("dbg_eid", eidf.f32(), [128, 128], r=[eidf.g()])
                dump("dbg_gate", gate_.f32(), [128, 128], r=[gate_.g()])

        def stageY(i, pump):
            par = i % 2
            xm_, h2_, eidu_, gate_ = xm[par], h2[par], eidu[par], gate[par]
            t0 = i * 128
            EID = eidu_.u32()

            def gather(out_ap, slot, r, w):
                P.add('pool', lambda e: e.indirect_dma_start(out=out_ap, out_offset=None, in_=etab_d.ap(),
                                                             in_offset=bass.IndirectOffsetOnAxis(ap=EID[:, slot:slot + 1], axis=0)),
                      r=r, w=w, dma=True)

            def slot_tail(sl):
                cb = cbuf[sl % NBUF]
                dg_ = dgb[sl % 4]
                act(wgt.f32()[:, sl:sl + 1], gl.f32()[:, sl:sl + 1], AF.Copy, r=[f"gl{sl}", gate_.g(), wgt.g()], w=[f"wgt{sl}"],
                    scale=gate_.f32()[:, sl:sl + 1])
                P.add('act', (lambda dg_=dg_, sl=sl: (lambda e: e.activation(out=dg_.bf(), in_=ident_f.f32(), func=AF.Copy, scale=wgt.f32()[:, sl:sl + 1])))(),
                      r=[ident_f.g(), f"wgt{sl}"], w=[dg_.g()])
                for half in range(2):
                    mm(banks[half][:], dg_.bf(), cb.bf()[:, D + half * 512:D + (half + 1) * 512], sl == 0, sl == 127,
                       r=[dg_.g(), cb.g(2048, 4096)], w=[BK[half]])

            for sl in range(128):
                cb = cbuf[sl % NBUF]
                gather(cb.bf(), sl, r=[eidu_.g()] + ETAB_KEYS, w=[cb.g()])
                P.add('dve', (lambda cb=cb, sl=sl: (lambda e: e.scalar_tensor_tensor(out=SINK, in0=cb.bf()[:, 0:D], scalar=1.0, in1=h2_.bf(), op0=ALU.mult, op1=ALU.mult,
                                                                                 accum_out=actv.f32()[:, sl:sl + 1])))(),
                      r=[cb.g(0, 2048), h2_.g(), actv.g()], w=[f"actv{sl}"])
                act(gl.f32()[:, sl:sl + 1], actv.f32()[:, sl:sl + 1], AF.Gelu, r=[f"actv{sl}", gl.g()], w=[f"gl{sl}"])
                if sl >= 1:
                    slot_tail(sl - 1)
                pump(3)
            slot_tail(127)
            for half in range(2):
                hs = slice(half * 512, (half + 1) * 512)
                tt(tmpf.f32()[:, hs], banks[half][:], g2.f32()[:, hs], ALU.mult, r=[BK[half], g2.g()], w=[tmpf.g(half * 2048, (half + 1) * 2048)])
                tt(ot.f32()[:, hs], tmpf.f32()[:, hs], xm_.f32()[:, hs], ALU.add, r=[tmpf.g(half * 2048, (half + 1) * 2048), xm_.g()], w=[ot.g(half * 2048, (half + 1) * 2048)])
            if dbg and i == 0:
                dump("dbg_xo", ot.f32(), [128, 1024], r=[ot.g()])
            act(SINK, ot.f32(), AF.Square, r=[ot.g()], w=[small.g()], accum_out=sm(SM_SS3))
            rstd_chain(SM_SS3, SM_LN3, SM_RSTD3, D, 1e-6)
            stt(ot.f32(), ot.f32(), sm(SM_RSTD3), nf_bc.f32(), ALU.mult, ALU.mult, r=[ot.g(), small.g(), nf_bc.g()], w=[ot.g()])
            dma('sp', out_d.ap()[b, t0:t0 + 128, :], ot.f32(), r=[ot.g()], w=[f"out{b}_{i}"])

        def drain(q, n=None):
            k = 0
            while q and (n is None or k < n):
                P.add(*q.pop(0))
                k += 1

        P.capture = []
        stageX(0)
        q = P.capture
        P.capture = None
        drain(q)
        for i in range(ntiles):
            if i + 1 < ntiles:
                P.capture = []
                stageX(i + 1)
                q = P.capture
                P.capture = None
            else:
                q = []
            stageY(i, lambda n: drain(q, n))
            drain(q)

    P.emit(nc, stack)
    stack.close()
    return nc, list(dbg_d.keys())


def _prep_inputs(inputs):
    f = np.float32
    x = np.asarray(inputs['x'], f)
    c = np.asarray(inputs['c'], f)
    cst = _host_consts()
    rep = lambda v, n=128: np.ascontiguousarray(np.broadcast_to(np.asarray(v, f).reshape(1, -1), (n, np.asarray(v).size)))
    nw = np.concatenate([np.asarray(inputs['norm_mix_w'], f).reshape(-1), np.asarray(inputs['norm_ffn_w'], f).reshape(-1),
                         np.asarray(inputs['norm_final_w'], f).reshape(-1)])
    lam = np.concatenate([np.asarray(inputs[k], f).reshape(-1) for k in ('lambda_q1', 'lambda_k1', 'lambda_q2', 'lambda_k2')])
    sk1 = np.asarray(inputs['sub_keys_1'], f)[0]
    sk2 = np.asarray(inputs['sub_keys_2'], f)[0]
    subkT = np.zeros((128, 16, 128), f)
    for h in range(8):
        subkT[:, h * 2 + 0, :] = sk1[h].T
        subkT[:, h * 2 + 1, :] = sk2[h].T
    shared = {
        'w_ada': np.ascontiguousarray(np.asarray(inputs['w_ada'], f)[0]),
        'b_ada_bc': rep(inputs['b_ada']),
        'nw_bc': rep(nw),
        'w_in': np.ascontiguousarray(np.asarray(inputs['w_in'], f)[0]),
        'w_out': np.ascontiguousarray(np.asarray(inputs['w_out'], f)[0]),
        'w_query': np.ascontiguousarray(np.asarray(inputs['w_query'], f)[0]),
        'pool_w': np.ascontiguousarray(np.asarray(inputs['pool_w'], f)[0]),
        'pool_scaleT': np.ascontiguousarray(np.asarray(inputs['pool_scale'], f).reshape(8, 128).T),
        'subkT': np.ascontiguousarray(subkT.reshape(128, 2048)),
        'e_down': np.ascontiguousarray(np.asarray(inputs['expert_down'], f)[0]),
        'e_up': np.ascontiguousarray(np.asarray(inputs['expert_up'], f)[0]),
        'lam_bc': rep(lam),
        'sublnT': np.ascontiguousarray(np.asarray(inputs['subln_w'], f).reshape(128, 1)),
        'relb_bc': rep(np.asarray(inputs['rel_bias'], f).reshape(-1)),
        'masks': np.ascontiguousarray(cst['masks'].reshape(128, -1)),
        'negmask': cst['negmask'],
        'band': np.ascontiguousarray(cst['band'].reshape(128, -1)),
        'ident': cst['ident'],
        'iota16': cst['iota16'],
        'sel0': cst['sel0'],
    }
    in_maps = []
    for core in range(NCORES):
        m = dict(shared)
        m['x'] = np.ascontiguousarray(x[core * NB:(core + 1) * NB])
        cc = c[core * NB:(core + 1) * NB]
        m['cT'] = np.ascontiguousarray(cc.reshape(NB, KC, 128).transpose(2, 0, 1).reshape(128, NB * KC))
        in_maps.append(m)
    return in_maps


_CACHE = {}


def kernel(**inputs):
    in_maps = _prep_inputs(inputs)
    if 'nc' not in _CACHE:
        _CACHE['nc'] = build()[0]
    nc = _CACHE['nc']
    res = run_bass_kernel_spmd(nc, in_maps, core_ids=list(range(NCORES)))
    out = np.concatenate([np.asarray(r['out']) for r in res.results], axis=0)
    return out.astype(np.float32)
```

```python
import os
import math
import contextlib
import numpy as np
import concourse.bass as bass
import concourse.mybir as mybir
from concourse.bass_utils import run_bass_kernel_spmd

dt = mybir.dt
AF = mybir.ActivationFunctionType
ALU = mybir.AluOpType
AX = mybir.AxisListType
F32, BF16, U32 = dt.float32, dt.bfloat16, dt.uint32

NCORES = 8
NB = 2
S = 2048
NT = 16
D = 1024
KC = 8
H = 8
DIN = 6144
NEXP = 16384
OFF = 8.0
NEG = -1.0e30
G = 512


class Prog:
    def __init__(self):
        self.ops = []
        self.last_w = {}
        self.readers = {}
        self.capture = None

    def add(self, eng, fn, r=(), w=(), dma=False):
        if self.capture is not None:
            self.capture.append((eng, fn, r, w, dma))
            return None
        i = len(self.ops)
        deps = set()
        rk = _flat(r)
        wk = _flat(w)
        for k in rk:
            lw = self.last_w.get(k)
            if lw is not None:
                deps.add(lw)
        for k in wk:
            lw = self.last_w.get(k)
            if lw is not None:
                deps.add(lw)
            deps.update(self.readers.get(k, ()))
        deps.discard(i)
        for k in rk:
            self.readers.setdefault(k, []).append(i)
        for k in wk:
            self.last_w[k] = i
            self.readers[k] = []
        self.ops.append(dict(eng=eng, fn=fn, deps=deps, dma=dma, sig=None, need=False, pre=None))
        return i

    def emit(self, nc, stack):
        ops = self.ops
        EPOCH = 30000
        NSLOT = {'sp': 24, 'pool': 24, 'act': 8}
        for o in ops:
            for d in o['deps']:
                p = ops[d]
                if p['eng'] == 'pe' and o['eng'] == 'pe' and not p['dma'] and not o['dma']:
                    continue
                p['need'] = True
        cnt = {e: 0 for e in ('pe', 'act', 'dve', 'pool', 'sp')}
        esems = {e: [] for e in cnt}
        dsems = {q: [stack.enter_context(nc.semaphore(f"d_{q}_{i}")) for i in range(n)] for q, n in NSLOT.items()}
        duse = {q: [0] * n for q, n in NSLOT.items()}
        dnext = {q: 0 for q in NSLOT}
        for o in ops:
            e = o['eng']
            if o['dma']:
                s = dnext[e]
                dnext[e] = (s + 1) % NSLOT[e]
                if duse[e][s] > 0:
                    o['pre'] = (dsems[e][s], 16 * duse[e][s])
                duse[e][s] += 1
                o['sig'] = (dsems[e][s], 16 * duse[e][s])
            elif o['need']:
                ep = cnt[e] // EPOCH
                while len(esems[e]) <= ep:
                    esems[e].append(stack.enter_context(nc.semaphore(f"e_{e}_{len(esems[e])}")))
                cnt[e] += 1
                o['sig'] = (esems[e][ep], cnt[e] - ep * EPOCH)
        by_eng = {e: [o for o in ops if o['eng'] == e] for e in cnt}
        final_waits = []
        for q in NSLOT:
            for s in range(NSLOT[q]):
                if duse[q][s] > 0:
                    final_waits.append((dsems[q][s], 16 * duse[q][s]))

        def run(ename, eng):
            waited = {}
            for o in by_eng[ename]:
                needs = {}
                for d in o['deps']:
                    p = ops[d]
                    if p['eng'] == 'pe' and ename == 'pe' and not p['dma'] and not o['dma']:
                        continue
                    sem, val = p['sig']
                    key = id(sem)
                    if needs.get(key, (None, 0))[1] < val:
                        needs[key] = (sem, val)
                if o['pre'] is not None:
                    sem, val = o['pre']
                    key = id(sem)
                    if needs.get(key, (None, 0))[1] < val:
                        needs[key] = (sem, val)
                for key, (sem, val) in needs.items():
                    if waited.get(key, 0) < val:
                        eng.wait_ge(sem, val)
                        waited[key] = val
                inst = o['fn'](eng)
                if o['sig'] is not None:
                    inst.then_inc(o['sig'][0], 16 if o['dma'] else 1)
            if ename == 'sp':
                for sem, val in final_waits:
                    eng.wait_ge(sem, val)

        with nc.Block() as block:
            @block.tensor
            def _(e):
                run('pe', e)

            @block.scalar
            def _(e):
                run('act', e)

            @block.vector
            def _(e):
                run('dve', e)

            @block.gpsimd
            def _(e):
                run('pool', e)

            @block.sync
            def _(e):
                run('sp', e)


def _flat(x):
    out = []
    for k in x:
        if isinstance(k, (list, tuple, set, range)):
            out.extend(_flat(k))
        else:
            out.append(k)
    return out


class Buf:
    def __init__(self, arena, off, nbytes):
        assert off % 4 == 0 and nbytes % 4 == 0
        self.A, self.off, self.nbytes = arena, off, nbytes

    def g(self, lo=0, hi=None):
        hi = self.nbytes if hi is None else hi
        return range((self.off + lo) // G, (self.off + hi + G - 1) // G)

    def f32(self):
        return self.A[:, self.off // 4:(self.off + self.nbytes) // 4]

    def bf(self):
        return self.A[:, self.off // 4:(self.off + self.nbytes) // 4].bitcast(BF16)

    def u32(self):
        return self.A[:, self.off // 4:(self.off + self.nbytes) // 4].bitcast(U32)

    def sub(self, lo, n):
        return Buf(self.A, self.off + lo, n)


class Alloc:
    def __init__(self, arena, base, limit):
        self.A, self.p, self.limit = arena, base, limit

    def get(self, nbytes):
        nb = (nbytes + G - 1) // G * G
        b = Buf(self.A, self.p, (nbytes + 3) // 4 * 4)
        self.p += nb
        assert self.p <= self.limit, (self.p, self.limit)
        return b


def mk(ap, pattern, extra_off=0):
    return bass.AP(tensor=ap.tensor, offset=ap.offset + extra_off, ap=[list(ap.ap[0])] + [list(p) for p in pattern])


def _t5_bucket(d):
    d = np.maximum(d, 0)
    x = np.maximum(d, 1).astype(np.float32) / np.float32(16)
    large = 16 + (np.log(x).astype(np.float32) / np.float32(math.log(128 / 16)) * np.float32(16)).astype(np.int32)
    large = np.minimum(large, 31)
    return np.where(d < 16, d, large)


def _host_consts():
    kk = np.arange(128)[:, None]
    qq = np.arange(256)[None, :]
    dist = qq - kk
    valid = dist >= 0
    bk = _t5_bucket(dist)
    masks = np.zeros((128, 32, 256), np.float32)
    for b in range(32):
        masks[:, b, :] = (valid & (bk == b)).astype(np.float32)
    negmask = np.where(valid, 0.0, NEG).astype(np.float32)
    band = np.zeros((128, 12, 128), np.float32)
    s = np.arange(128)[:, None]
    t = np.arange(128)[None, :]
    for gi, w in enumerate((2, 4, 8, 16)):
        cnt0 = np.minimum(t + 1, w).astype(np.float32)
        band[:, gi * 3 + 0, :] = ((s <= t) & (s > t - w)) / cnt0 - (s == t)
        band[:, gi * 3 + 1, :] = ((s <= t) & (s > t - w)) / np.float32(w) - (s == t)
        band[:, gi * 3 + 2, :] = (s > 128 + t - w) / np.float32(w)
    ident = np.eye(128, dtype=np.float32)
    iota16 = np.broadcast_to(np.arange(16, dtype=np.float32)[None, :], (128, 16)).copy()
    sel0 = np.zeros((64, 128), np.float32)
    sel0[0, :] = 1.0
    sel0[32, :] = 1.0
    return dict(masks=masks, negmask=negmask, band=band, ident=ident, iota16=iota16, sel0=sel0)


def build(stage=99, dbg=False):
    nc = bass.Bass("TRN2", target_bir_lowering=False)
    P = Prog()

    def din(name, shape, dtype=F32):
        return nc.dram_tensor(name, list(shape), dtype, kind="ExternalInput")

    x_d = din("x", [NB, S, D])
    cT_d = din("cT", [128, NB * KC])
    wada_d = din("w_ada", [D, DIN])
    bada_d = din("b_ada_bc", [128, DIN])
    nw_d = din("nw_bc", [128, 3 * D])
    win_d = din("w_in", [D, DIN])
    wout_d = din("w_out", [D, D])
    wqry_d = din("w_query", [D, 2048])
    poolw_d = din("pool_w", [4, 256, 256])
    psc_d = din("pool_scaleT", [128, 8])
    subk_d = din("subkT", [128, 16 * 128])
    edown_d = din("e_down", [NEXP, D])
    eup_d = din("e_up", [NEXP, D])
    lam_d = din("lam_bc", [128, 256])
    subln_d = din("sublnT", [128, 1])
    relb_d = din("relb_bc", [128, 256])
    masks_d = din("masks", [128, 32 * 256])
    negm_d = din("negmask", [128, 256])
    band_d = din("band", [128, 12 * 128])
    ident_d = din("ident", [128, 128])
    iota_d = din("iota16", [128, 16])
    sel0_d = din("sel0", [64, 128])
    out_d = nc.dram_tensor("out", [NB, S, D], F32, kind="ExternalOutput")
    etab_d = nc.dram_tensor("etab", [NEXP, 2 * D], BF16, kind="Internal")
    dbg_d = {}

    def dbg_out(name, shape):
        dbg_d[name] = nc.dram_tensor(name, list(shape), F32, kind="ExternalOutput")
        return dbg_d[name]

    stack = contextlib.ExitStack()
    TOT = 212480
    arena = stack.enter_context(nc.sbuf_tensor("arena", [128, TOT // 4], F32))
    banks = [stack.enter_context(nc.psum_tensor(f"B{i}", [128, 512], F32)) for i in range(8)]
    BK = [f"B{i}" for i in range(8)]

    al = Alloc(arena, 0, TOT)
    ident_f = al.get(512)
    ident_b = al.get(256)
    ones_b = al.get(256)
    ones_f = al.get(512)
    sel0 = al.get(512)
    relb = al.get(1024)
    rb31m = al.get(32)
    lamv = al.get(1024)
    small = al.get(512)
    pscT = al.get(32)
    band = al.get(12 * 128 * 2)
    subkT = al.get(16 * 128 * 2)
    iota16 = al.get(64)
    sink = al.get(64)
    nf_bc = al.get(4096)
    sh1 = al.get(4096)
    a1 = al.get(4096)
    g1 = al.get(4096)
    sh2 = al.get(4096)
    a2 = al.get(4096)
    g2 = al.get(4096)
    hT = al.get(KC * S * 2)
    mT = al.get(KC * S * 2)
    wout = al.get(KC * D * 2)
    work_base = al.p
    SM = small.f32()
    (SM_S1, SM_S2, SM_E1, SM_E2, SM_NLAM, SM_WSUB, SM_SS, SM_RSTD, SM_LN, SM_SS2, SM_RSTD2, SM_SS3, SM_RSTD3,
     SM_LN2, SM_LN3, SM_SUBLN) = range(16)

    def sm(i):
        return SM[:, i:i + 1]

    def smg(i):
        return small.g()

    def dma(q, out, in_, r, w):
        P.add(q, lambda e: e.dma_start(out=out, in_=in_), r=r, w=w, dma=True)

    def mm(out, lhsT, rhs, start, stop, r, w, **kw):
        P.add('pe', lambda e: e.matmul(out, lhsT, rhs, start=start, stop=stop, **kw), r=r, w=w)

    def tr(out, in_, ident, r, w):
        P.add('pe', lambda e: e.transpose(out, in_, ident), r=r, w=w)

    def act(out, in_, func, r, w, bias=None, scale=None, accum_out=None, eng='act'):
        def f(e):
            kw = {}
            if bias is not None:
                kw['bias'] = bias
            if scale is not None:
                kw['scale'] = scale
            if accum_out is not None:
                kw['accum_out'] = accum_out
            return e.activation(out=out, in_=in_, func=func, **kw)
        P.add('act', f, r=r, w=w)

    def tt(out, in0, in1, op, r, w, eng='dve'):
        P.add(eng, lambda e: e.tensor_tensor(out=out, in0=in0, in1=in1, op=op), r=r, w=w)

    def ts(out, in0, s1, s2, op0, op1, r, w, eng='dve'):
        if op1 is None:
            P.add(eng, lambda e: e.tensor_scalar(out=out, in0=in0, scalar1=s1, scalar2=None, op0=op0), r=r, w=w)
        else:
            P.add(eng, lambda e: e.tensor_scalar(out=out, in0=in0, scalar1=s1, scalar2=s2, op0=op0, op1=op1), r=r, w=w)

    def stt(out, in0, scalar, in1, op0, op1, r, w):
        P.add('dve', lambda e: e.scalar_tensor_tensor(out=out, in0=in0, scalar=scalar, in1=in1, op0=op0, op1=op1), r=r, w=w)

    def cp(out, in_, r, w, eng='dve'):
        if eng == 'act':
            P.add('act', lambda e: e.copy(out=out, in_=in_), r=r, w=w)
        else:
            P.add(eng, lambda e: e.tensor_copy(out=out, in_=in_), r=r, w=w)

    def ttr(out, in0, in1, accum_out, r, w):
        P.add('dve', lambda e: e.scalar_tensor_tensor(out=out, in0=in0, scalar=1.0, in1=in1, op0=ALU.mult, op1=ALU.mult,
                                                     accum_out=accum_out), r=r, w=w)

    def tred(out, in_, op, r, w):
        P.add('dve', lambda e: e.tensor_reduce(out=out, in_=in_, axis=AX.X, op=op), r=r, w=w)

    def memset(ap, val, r, w, eng='dve'):
        P.add(eng, lambda e: e.memset(ap, val), r=r, w=w)

    def rstd_chain(ss_i, ln_i, rstd_i, n, eps):
        ts(sm(ln_i), sm(ss_i), 1.0 / n, eps, ALU.mult, ALU.add, r=[small.g()], w=[small.g()])
        act(sm(ln_i), sm(ln_i), AF.Ln, r=[small.g()], w=[small.g()])
        act(sm(rstd_i), sm(ln_i), AF.Exp, r=[small.g()], w=[small.g()], scale=-0.5)

    def dump(name, ap_sb, shape, r):
        if name in dbg_d:
            return
        d = dbg_out(name, shape)
        dma('sp', d.ap(), ap_sb, r=r, w=[name])

    dma('sp', ident_f.f32(), ident_d.ap(), r=[], w=[ident_f.g()])
    dma('sp', sel0.f32()[0:64, :], sel0_d.ap(), r=[], w=[sel0.g()])
    dma('sp', relb.f32(), relb_d.ap(), r=[], w=[relb.g()])
    dma('sp', lamv.f32(), lam_d.ap(), r=[], w=[lamv.g()])
    dma('sp', pscT.f32(), psc_d.ap(), r=[], w=[pscT.g()])
    dma('sp', iota16.f32(), iota_d.ap(), r=[], w=[iota16.g()])
    dma('sp', nf_bc.f32(), nw_d.ap()[:, 2 * D:3 * D], r=[], w=[nf_bc.g()])
    dma('sp', sm(SM_SUBLN), subln_d.ap(), r=[], w=[small.g()])
    dma('pool', band.bf(), band_d.ap(), r=[], w=[band.g()])
    dma('pool', subkT.bf(), subk_d.ap(), r=[], w=[subkT.g()])
    ETAB_JOBS = [(ti, r0) for ti in range(2) for r0 in range(0, NEXP, 1024)]
    ETAB_KEYS = [f"etab{ti}_{r0}" for (ti, r0) in ETAB_JOBS]

    def precast(n):
        for _ in range(n):
            if not ETAB_JOBS:
                return
            ti, r0 = ETAB_JOBS.pop(0)
            tbl = (edown_d, eup_d)[ti]
            dma('pool', etab_d.ap()[r0:r0 + 1024, ti * D:(ti + 1) * D], tbl.ap()[r0:r0 + 1024, :], r=[], w=[f"etab{ti}_{r0}"])

    cp(ident_b.bf(), ident_f.f32(), r=[ident_f.g()], w=[ident_b.g()])
    memset(ones_b.bf(), 1.0, r=[], w=[ones_b.g()])
    memset(ones_f.f32(), 1.0, r=[], w=[ones_f.g()])
    ts(rb31m.f32(), relb.f32()[:, 31 * 8:32 * 8], -OFF, None, ALU.add, None, r=[relb.g()], w=[rb31m.g()])
    ts(sm(SM_WSUB), sm(SM_SUBLN), 0.8, None, ALU.mult, None, r=[small.g()], w=[small.g()])
    wa = Alloc(arena, work_base, TOT)
    junk64 = wa.get(256)
    LV = lamv.f32()
    ttr(junk64.f32(), LV[:, 0:64], LV[:, 64:128], sm(SM_S1), r=[lamv.g()], w=[junk64.g(), small.g()])
    ttr(junk64.f32(), LV[:, 128:192], LV[:, 192:256], sm(SM_S2), r=[lamv.g()], w=[junk64.g(), small.g()])
    act(sm(SM_E1), sm(SM_S1), AF.Exp, r=[small.g()], w=[small.g()])
    act(sm(SM_E2), sm(SM_S2), AF.Exp, r=[small.g()], w=[small.g()])
    tt(sm(SM_NLAM), sm(SM_E2), sm(SM_E1), ALU.subtract, r=[small.g()], w=[small.g()])
    ts(sm(SM_NLAM), sm(SM_NLAM), -0.2, None, ALU.add, None, r=[small.g()], w=[small.g()])
    bias_scr = nc.dram_tensor("bias_scr", [128, 2048], F32, kind="Internal")
    biasT = wa.get(8 * 1024)
    mk_chunk = wa.get(8 * 256 * 4)
    BT3 = biasT.f32().rearrange("p (h q) -> p h q", h=8)
    for h in range(8):
        dma('sp', BT3[:, h, :], negm_d.ap(), r=[], w=[biasT.g(h * 1024, (h + 1) * 1024)])
    for c in range(4):
        dma('sp', mk_chunk.f32(), masks_d.ap()[:, c * 2048:(c + 1) * 2048], r=[], w=[mk_chunk.g()])
        MC = mk_chunk.f32().rearrange("p (b q) -> p b q", b=8)
        for bb in range(8):
            b = c * 8 + bb
            for h in range(8):
                stt(BT3[:, h, :], MC[:, bb, :], relb.f32()[:, b * 8 + h:b * 8 + h + 1], BT3[:, h, :], ALU.mult, ALU.add,
                    r=[mk_chunk.g(), relb.g(), biasT.g(h * 1024, (h + 1) * 1024)], w=[biasT.g(h * 1024, (h + 1) * 1024)])
    ts(biasT.f32(), biasT.f32(), -OFF, None, ALU.add, None, r=[biasT.g()], w=[biasT.g()])
    dma('sp', bias_scr.ap(), biasT.f32(), r=[biasT.g()], w=["bias_scr"])
    if dbg:
        dump("dbg_bias", biasT.f32(), [128, 2048], r=[biasT.g()])
        dump("dbg_small", small.f32()[:, 0:16], [128, 16], r=[small.g()])

    hT3 = hT.bf().rearrange("p (k t) -> p k t", k=KC)
    mT3 = mT.bf().rearrange("p (k t) -> p k t", k=KC)

    def hT_g(c0, c1):
        return [hT.g(k * S * 2 + c0 * 2, k * S * 2 + c1 * 2) for k in range(KC)]

    def mT_g(k, c0, c1):
        return mT.g(k * S * 2 + c0 * 2, k * S * 2 + c1 * 2)

    BTb = banks[7][:].bitcast(BF16)

    for b in range(NB):
        wa = Alloc(arena, work_base, TOT)
        cact = wa.get(64)
        crep = wa.get(KC * 128 * 4)
        wblk = [wa.get(KC * 512 * 4) for _ in range(2)]
        bblk = [wa.get(2048) for _ in range(2)]
        mtmp = wa.get(2048)
        nwt = wa.get(4096)
        cin = wa.get(64)
        dma('sp', cin.f32()[:, 0:KC], cT_d.ap()[:, b * KC:(b + 1) * KC], r=[], w=[cin.g()])
        act(cact.f32()[:, 0:KC], cin.f32()[:, 0:KC], AF.Silu, r=[cin.g()], w=[cact.g()])
        cp(crep.f32().rearrange("p (k m) -> p k m", k=KC), mk(cact.f32(), [[1, KC], [0, 128]]), r=[cact.g()], w=[crep.g()])
        CR = crep.f32().rearrange("p (k m) -> p k m", k=KC)
        wada_v = wada_d.ap().rearrange("(k p) n -> p k n", p=128)
        dsts = [sh1, sh1, a1, a1, g1, g1, sh2, sh2, a2, a2, g2, g2]
        for nb_ in range(12):
            wb = wblk[nb_ % 2]
            bb_ = bblk[nb_ % 2]
            dma('sp', wb.f32().rearrange("p (k n) -> p k n", k=KC), wada_v[:, :, nb_ * 512:(nb_ + 1) * 512], r=[], w=[wb.g()])
            dma('sp', bb_.f32(), bada_d.ap()[:, nb_ * 512:(nb_ + 1) * 512], r=[], w=[bb_.g()])
            WB = wb.f32().rearrange("p (k n) -> p k n", k=KC)
            for k in range(KC):
                mm(banks[6][:], CR[:, k, :], WB[:, k, :], k == 0, k == KC - 1, r=[crep.g(), wb.g()], w=[BK[6]])
            dst = dsts[nb_]
            half = nb_ % 2
            dst_ap = dst.f32()[:, half * 512:(half + 1) * 512]
            dg_ = dst.g(half * 2048, (half + 1) * 2048)
            if nb_ in (2, 3, 8, 9):
                which = 0 if nb_ in (2, 3) else 1
                dma('sp', nwt.f32()[:, 0:512], nw_d.ap()[:, which * D + half * 512: which * D + (half + 1) * 512], r=[], w=[nwt.g()])
                tt(mtmp.f32(), banks[6][:], bb_.f32(), ALU.add, r=[BK[6], bb_.g()], w=[mtmp.g()])
                stt(dst_ap, mtmp.f32(), 1.0, nwt.f32()[:, 0:512], ALU.add, ALU.mult, r=[mtmp.g(), nwt.g()], w=[dg_])
            else:
                tt(dst_ap, banks[6][:], bb_.f32(), ALU.add, r=[BK[6], bb_.g()], w=[dg_])
        if dbg and b == 0:
            dump("dbg_a1", a1.f32(), [128, 1024], r=[a1.g()])
            dump("dbg_g2", g2.f32(), [128, 1024], r=[g2.g()])

        wa = Alloc(arena, work_base, TOT)
        xt = [wa.get(4096) for _ in range(2)]
        tmpf = wa.get(4096)
        junkb = wa.get(2048)
        hbf = wa.get(2048)
        for i in range(NT):
            xb = xt[i % 2]
            dma('sp', xb.f32(), x_d.ap()[b, i * 128:(i + 1) * 128, :], r=[], w=[xb.g()])
            act(junkb.bf(), xb.f32(), AF.Square, r=[xb.g()], w=[junkb.g(), small.g()], accum_out=sm(SM_SS))
            rstd_chain(SM_SS, SM_LN, SM_RSTD, D, 1e-6)
            stt(tmpf.f32(), xb.f32(), sm(SM_RSTD), a1.f32(), ALU.mult, ALU.mult, r=[xb.g(), small.g(), a1.g()], w=[tmpf.g()])
            tt(hbf.bf(), tmpf.f32(), sh1.f32(), ALU.add, r=[tmpf.g(), sh1.g()], w=[hbf.g()])
            for k in range(KC):
                tr(BTb[:, k * 128:(k + 1) * 128], hbf.bf()[:, k * 128:(k + 1) * 128], ident_b.bf(), r=[hbf.g(), ident_b.g()], w=[BK[7]])
            cp(hT3[:, :, i * 128:(i + 1) * 128], BTb.rearrange("p (k t) -> p k t", k=KC), r=[BK[7]], w=hT_g(i * 128, (i + 1) * 128), eng='act')
        if dbg and b == 0:
            wa2 = Alloc(arena, wa.p, TOT)
            dtmp = wa2.get(8192)
            cp(dtmp.f32(), hT3[:, 0, :], r=hT_g(0, S), w=[dtmp.g()])
            dump("dbg_hT0", dtmp.f32(), [128, 2048], r=[dtmp.g()])
        if stage <= 1:
            continue

        win_v = win_d.ap().rearrange("(k p) n -> p k n", p=128)
        wa = Alloc(arena, work_base, TOT)
        wp = wa.get(KC * 256 * 2)
        wgp = wa.get(KC * 256 * 2)
        pw = wa.get(2 * 256 * 2)
        pg = wa.get(NT * 256 * 2)
        pT = [wa.get(512 * 2) for _ in range(2)]
        sg = wa.get(512 * 4)
        WP = wp.bf().rearrange("p (k n) -> p k n", k=KC)
        WGP = wgp.bf().rearrange("p (k n) -> p k n", k=KC)
        PW = pw.bf().rearrange("p (c n) -> p c n", c=2)
        PG = pg.bf().rearrange("p (i n) -> p i n", i=NT)
        BAND = band.bf().rearrange("p (j n) -> p j n", j=12)
        for g in range(4):
            dma('pool', WP, win_v[:, :, 3072 + g * 256:3072 + (g + 1) * 256], r=[], w=[wp.g()])
            dma('pool', WGP, win_v[:, :, 5120 + g * 256:5120 + (g + 1) * 256], r=[], w=[wgp.g()])
            dma('pool', PW, poolw_d.ap()[g].rearrange("(c p) n -> p c n", p=128), r=[], w=[pw.g()])
            for i in range(NT):
                for k in range(KC):
                    mm(banks[7][:, (i % 2) * 256:(i % 2 + 1) * 256], hT3[:, k, i * 128:(i + 1) * 128], WP[:, k, :], k == 0, k == KC - 1,
                       r=[hT_g(i * 128, (i + 1) * 128), wp.g()], w=[BK[7]])
                if i % 2 == 1:
                    cp(pg.bf()[:, (i - 1) * 256:(i + 1) * 256], banks[7][:], r=[BK[7]], w=[pg.g((i - 1) * 512, (i + 1) * 512)], eng='act')
            for c in range(4):
                for cc in range(2):
                    for il in range(4):
                        i = c * 4 + il
                        cur = g * 3 + (0 if i == 0 else 1)
                        mm(banks[cc][:, il * 128:(il + 1) * 128], PG[:, i, cc * 128:(cc + 1) * 128], BAND[:, cur, :], True, i == 0,
                           r=[pg.g(i * 512, (i + 1) * 512), band.g()], w=[BK[cc]])
                        if i > 0:
                            mm(banks[cc][:, il * 128:(il + 1) * 128], PG[:, i - 1, cc * 128:(cc + 1) * 128], BAND[:, g * 3 + 2, :], False, True,
                               r=[pg.g((i - 1) * 512, i * 512), band.g()], w=[BK[cc]])
                    cp(pT[cc].bf(), banks[cc][:], r=[BK[cc]], w=[pT[cc].g()], eng='act')
                for ec in range(2):
                    kch = g * 2 + ec
                    mm(banks[2][:], PW[:, 0, ec * 128:(ec + 1) * 128], pT[0].bf(), True, False, r=[pw.g(), pT[0].g()], w=[BK[2]])
                    mm(banks[2][:], PW[:, 1, ec * 128:(ec + 1) * 128], pT[1].bf(), False, True, r=[pw.g(), pT[1].g()], w=[BK[2]])
                    for k in range(KC):
                        mm(banks[3][:], WGP[:, k, ec * 128:(ec + 1) * 128], hT3[:, k, c * 512:(c + 1) * 512], k == 0, k == KC - 1,
                           r=[wgp.g(), hT_g(c * 512, (c + 1) * 512)], w=[BK[3]])
                    act(sg.f32(), banks[3][:], AF.Sigmoid, r=[BK[3]], w=[sg.g()])
                    stt(mT3[:, kch, c * 512:(c + 1) * 512], banks[2][:], pscT.f32()[:, kch:kch + 1], sg.f32(), ALU.mult, ALU.mult,
                        r=[BK[2], pscT.g(), sg.g()], w=[mT_g(kch, c * 512, (c + 1) * 512)])
        if dbg and b == 0 and stage == 2:
            wa2 = Alloc(arena, wa.p, TOT)
            dtmp = wa2.get(8192)
            for kk_ in (0, 5):
                cp(dtmp.f32(), mT3[:, kk_, :], r=[mT.g()], w=[dtmp.g()])
                dump(f"dbg_mT{kk_}", dtmp.f32(), [128, 2048], r=[dtmp.g()])
        if stage <= 2:
            continue

        wa = Alloc(arena, work_base, TOT)
        wq = wa.get(KC * 128 * 2)
        wk_ = wa.get(KC * 128 * 2)
        wv = wa.get(KC * 128 * 2)
        wg = wa.get(KC * 128 * 2)
        QTz = [wa.get(S * 2) for _ in range(2)]
        KT = wa.get(S * 2)
        Vb = wa.get(NT * 128 * 2)
        sgT = wa.get(S * 2)
        PT = [[wa.get(512 * 2) for _ in range(2)] for _ in range(2)]
        ntmp = [wa.get(256 * 4) for _ in range(2)]
        O1s = wa.get(2048)
        O2s = wa.get(2048)
        lnz = wa.get(2048)
        rz = wa.get(2048)
        rz2 = wa.get(2048)
        sq = wa.get(2048)
        t1 = wa.get(2048)
        rs = wa.get(2048)
        biasT = wa.get(8 * 1024)
        BT3 = biasT.f32().rearrange("p (h q) -> p h q", h=8)
        dma('sp', biasT.f32(), bias_scr.ap(), r=["bias_scr"], w=[biasT.g()])
        WQ = wq.bf().rearrange("p (k n) -> p k n", k=KC)
        WK = wk_.bf().rearrange("p (k n) -> p k n", k=KC)
        WV = wv.bf().rearrange("p (k n) -> p k n", k=KC)
        WG = wg.bf().rearrange("p (k n) -> p k n", k=KC)
        V3 = Vb.bf().rearrange("p (i n) -> p i n", i=NT)
        nheads = H if stage > 3 or not dbg else 1
        memset(QTz[0].bf()[64:128, :], 0.0, r=[], w=[QTz[0].g()])
        memset(QTz[1].bf()[0:64, :], 0.0, r=[], w=[QTz[1].g()])
        for h in range(nheads):
            dma('pool', WQ, win_v[:, :, h * 128:(h + 1) * 128], r=[], w=[wq.g()])
            dma('pool', WK, win_v[:, :, 1024 + h * 128:1024 + (h + 1) * 128], r=[], w=[wk_.g()])
            dma('pool', WV, win_v[:, :, 2048 + h * 128:2048 + (h + 1) * 128], r=[], w=[wv.g()])
            dma('pool', WG, win_v[:, :, 4096 + h * 128:4096 + (h + 1) * 128], r=[], w=[wg.g()])
            precast(4)
            pj = 0
            for (W_, wb_, dst, mode) in ((WQ, wq, None, 'q'), (WK, wk_, KT, 'k'), (WG, wg, sgT, 'g')):
                for c in range(4):
                    bk = pj % 4
                    pj += 1
                    for k in range(KC):
                        mm(banks[bk][:], W_[:, k, :], hT3[:, k, c * 512:(c + 1) * 512], k == 0, k == KC - 1,
                           r=[wb_.g(), hT_g(c * 512, (c + 1) * 512)], w=[BK[bk]])
                    if mode == 'q':
                        for m_ in range(2):
                            o_ap = QTz[m_].bf()[m_ * 64:(m_ + 1) * 64, c * 512:(c + 1) * 512]
                            P.add('act', (lambda o_ap=o_ap, bk=bk, m_=m_: (lambda e: e.mul(out=o_ap, in_=banks[bk][m_ * 64:(m_ + 1) * 64, :], mul=0.125)))(),
                                  r=[BK[bk]], w=[QTz[m_].g(c * 1024, (c + 1) * 1024)])
                        continue
                    o_ap = dst.bf()[:, c * 512:(c + 1) * 512]
                    o_g = dst.g(c * 1024, (c + 1) * 1024)
                    if mode == 'k':
                        cp(o_ap, banks[bk][:], r=[BK[bk]], w=[o_g], eng='act')
                    else:
                        act(o_ap, banks[bk][:], AF.Sigmoid, r=[BK[bk]], w=[o_g])
            for i in range(NT):
                bk = (i // 4) % 4
                for k in range(KC):
                    mm(banks[bk][:, (i % 4) * 128:(i % 4 + 1) * 128], hT3[:, k, i * 128:(i + 1) * 128], WV[:, k, :], k == 0, k == KC - 1,
                       r=[hT_g(i * 128, (i + 1) * 128), wv.g()], w=[BK[bk]])
                if i % 4 == 3:
                    cp(Vb.bf()[:, (i - 3) * 128:(i + 1) * 128], banks[bk][:], r=[BK[bk]], w=[Vb.g((i - 3) * 256, (i + 1) * 256)], eng='act')

            def qk(c, j):
                col0 = max(0, j - 4 * c) * 128
                par = j % 2
                for m in range(2):
                    bk = m * 2 + par
                    mm(banks[bk][:, col0:512], KT.bf()[:, j * 128:(j + 1) * 128],
                       QTz[m].bf()[:, c * 512 + col0:(c + 1) * 512], True, True,
                       r=[KT.g(j * 256, (j + 1) * 256), QTz[m].g(c * 1024 + col0 * 2, (c + 1) * 1024)], w=[BK[bk]])

            for c in range(4):
                jmax = 4 * c + 3
                qk(c, 0)
                for j in range(jmax + 1):
                    if j + 1 <= jmax:
                        qk(c, j + 1)
                    col0 = max(0, j - 4 * c) * 128
                    par = j % 2
                    il_lo = max(0, j - 4 * c)
                    il_hi = min(3, j + 1 - 4 * c)
                    far0 = max(0, j + 2 - 4 * c) * 128
                    for m in range(2):
                        bk = m * 2 + par
                        pt = PT[m][par]
                        if il_hi >= il_lo and il_hi >= 0:
                            n0, n1 = il_lo * 128, (il_hi + 1) * 128
                            b0 = (4 * c + il_lo - j) * 128
                            nn = n1 - n0
                            tt(ntmp[m].f32()[:, 0:nn], banks[bk][:, n0:n1], BT3[:, h, b0:b0 + nn], ALU.add,
                               r=[BK[bk], biasT.g(h * 1024, (h + 1) * 1024)], w=[ntmp[m].g()])
                            act(pt.bf()[:, n0:n1], ntmp[m].f32()[:, 0:nn], AF.Exp, r=[ntmp[m].g()], w=[pt.g(n0 * 2, n1 * 2)])
                        if far0 < 512:
                            act(pt.bf()[:, far0:512], banks[bk][:, far0:512], AF.Exp, r=[BK[bk], rb31m.g()], w=[pt.g(far0 * 2, 1024)],
                                bias=rb31m.f32()[:, h:h + 1])
                        mm(banks[4 + m][:, col0:512], V3[:, j, :], pt.bf()[:, col0:512], j == 0, j == jmax,
                           r=[Vb.g(j * 256, (j + 1) * 256), pt.g(col0 * 2, 1024)], w=[BK[4 + m]], skip_group_check=True)
                        mm(banks[6 + m][:, col0:512], ones_b.bf(), pt.bf()[:, col0:512], j == 0, j == jmax,
                           r=[ones_b.g(), pt.g(col0 * 2, 1024)], w=[BK[6 + m]], skip_group_check=True)
                cs = slice(c * 512, (c + 1) * 512)
                cp(O1s.f32(), banks[4][:], r=[BK[4]], w=[O1s.g()], eng='act')
                cp(O2s.f32(), banks[5][:], r=[BK[5]], w=[O2s.g()], eng='act')
                act(lnz.f32(), banks[6][:], AF.Ln, r=[BK[6]], w=[lnz.g()])
                act(rz.f32(), lnz.f32(), AF.Exp, r=[lnz.g()], w=[rz.g()], scale=-1.0)
                act(lnz.f32(), banks[7][:], AF.Ln, r=[BK[7]], w=[lnz.g()])
                act(rz2.f32(), lnz.f32(), AF.Exp, r=[lnz.g()], w=[rz2.g()], scale=-1.0)
                tt(t1.f32(), O1s.f32(), rz.f32(), ALU.mult, r=[O1s.g(), rz.g()], w=[t1.g()])
                tt(O2s.f32(), O2s.f32(), rz2.f32(), ALU.mult, r=[O2s.g(), rz2.g()], w=[O2s.g()])
                stt(t1.f32(), O2s.f32(), sm(SM_NLAM), t1.f32(), ALU.mult, ALU.add, r=[t1.g(), O2s.g(), small.g()], w=[t1.g()])
                act(sq.bf()[:, 0:512], t1.f32(), AF.Square, r=[t1.g()], w=[sq.g()])
                mm(banks[6][:], ones_b.bf(), sq.bf()[:, 0:512], True, True, r=[ones_b.g(), sq.g()], w=[BK[6]])
                ts(rs.f32(), banks[6][:], 1.0 / 128, 1e-5, ALU.mult, ALU.add, r=[BK[6]], w=[rs.g()])
                act(rs.f32(), rs.f32(), AF.Ln, r=[rs.g()], w=[rs.g()])
                act(rs.f32(), rs.f32(), AF.Exp, r=[rs.g()], w=[rs.g()], scale=-0.5)
                tt(t1.f32(), t1.f32(), rs.f32(), ALU.mult, r=[t1.g(), rs.g()], w=[t1.g()])
                stt(t1.f32(), t1.f32(), sm(SM_WSUB), sgT.bf()[:, cs], ALU.mult, ALU.mult, r=[t1.g(), small.g(), sgT.g(c * 1024, (c + 1) * 1024)], w=[t1.g()])
                tt(mT3[:, h, cs], t1.f32(), mT3[:, h, cs], ALU.add, r=[t1.g(), mT_g(h, c * 512, (c + 1) * 512)], w=[mT_g(h, c * 512, (c + 1) * 512)])
        if dbg and b == 0 and stage == 3:
            wa2 = Alloc(arena, wa.p, TOT)
            dtmp = wa2.get(8192)
            cp(dtmp.f32(), mT3[:, 0, :], r=[mT.g()], w=[dtmp.g()])
            dump("dbg_mT0", dtmp.f32(), [128, 2048], r=[dtmp.g()])
        if stage <= 3:
            continue

        precast(64)
        WOUT3 = wout.bf().rearrange("p (k n) -> p k n", k=KC)
        dma('pool', WOUT3, wout_d.ap().rearrange("(k p) n -> p k n", p=128), r=[], w=[wout.g()])
        for k in range(KC):
            tt(WOUT3[:, k, :], WOUT3[:, k, :], g1.f32(), ALU.mult, r=[wout.g(k * 2048, (k + 1) * 2048), g1.g()], w=[wout.g(k * 2048, (k + 1) * 2048)])
        WQ3 = hT.bf().rearrange("p (k n) -> p k n", k=KC)
        dma('pool', WQ3, wqry_d.ap().rearrange("(k p) n -> p k n", p=128), r=[], w=[hT.g()])
        wa = Alloc(arena, work_base, TOT)
        xt5 = g1
        xm = [wa.get(4096) for _ in range(2)]
        h2 = [wa.get(2048) for _ in range(2)]
        eidu = [wa.get(512) for _ in range(2)]
        gate = [wa.get(512) for _ in range(2)]
        tmpf = a1
        ot = sh1
        ssb = wa.get(8192)
        wkb = ssb
        tmpfX = ssb.sub(0, 4096)
        junkbX = ssb.sub(4096, 2048)
        h2T = ssb.sub(0, 2048)
        qTs = ssb.sub(4096, 4096)
        v16 = wa.get(1024)
        ixu = wa.get(1024)
        ixf = wa.get(1024)
        c16, posu, pau, pbu, paf, pbf, i1s, i2s, eidf, scm, ee, actv, gl, wgt = [wa.get(512) for _ in range(14)]
        zz = wa.get(32)
        rzz = wa.get(32)
        dgb = [wa.get(256) for _ in range(4)]
        NBUF = (TOT - wa.p) // 4096
        assert NBUF >= 5, NBUF
        cbuf = [wa.get(4096) for _ in range(NBUF)]
        SINK = mk(sink.bf(), [[0, D]])
        H2T3 = h2T.bf().rearrange("p (k t) -> p k t", k=KC)
        QTS3 = qTs.bf().rearrange("p (q t) -> p q t", q=16)
        SUBK3 = subkT.bf().rearrange("p (q n) -> p q n", q=16)
        SS3 = ssb.f32().rearrange("p (q n) -> p q n", q=16)
        WK3 = wkb.f32().rearrange("p (q n) -> p q n", q=16)
        V16 = v16.f32().rearrange("p (q j) -> p q j", q=16)
        IXU = ixu.u32().rearrange("p (q j) -> p q j", q=16)
        ntiles = NT if not dbg else 2

        def gen(name, r, w, eng='dve', **kw):
            P.add(eng, lambda e: getattr(e, name)(**kw), r=r, w=w)

        def stageX(i):
            par = i % 2
            xm_, h2_, eidu_, gate_ = xm[par], h2[par], eidu[par], gate[par]
            t0 = i * 128
            dma('sp', xt5.f32(), x_d.ap()[b, t0:t0 + 128, :], r=[], w=[xt5.g()])
            for half in range(2):
                for k in range(KC):
                    mm(banks[2 + half][:], mT3[:, k, t0:t0 + 128], WOUT3[:, k, half * 512:(half + 1) * 512], k == 0, k == KC - 1,
                       r=[mT_g(k, t0, t0 + 128), wout.g()], w=[BK[2 + half]])
            for half in range(2):
                hs = slice(half * 512, (half + 1) * 512)
                tt(xm_.f32()[:, hs], banks[2 + half][:], xt5.f32()[:, hs], ALU.add, r=[BK[2 + half], xt5.g()], w=[xm_.g(half * 2048, (half + 1) * 2048)])
            act(junkbX.bf(), xm_.f32(), AF.Square, r=[xm_.g()], w=[junkbX.g(), small.g()], accum_out=sm(SM_SS2))
            rstd_chain(SM_SS2, SM_LN2, SM_RSTD2, D, 1e-6)
            stt(tmpfX.f32(), xm_.f32(), sm(SM_RSTD2), a2.f32(), ALU.mult, ALU.mult, r=[xm_.g(), small.g(), a2.g()], w=[tmpfX.g()])
            tt(h2_.bf(), tmpfX.f32(), sh2.f32(), ALU.add, r=[tmpfX.g(), sh2.g()], w=[h2_.g()])
            for k in range(KC):
                tr(BTb[:, k * 128:(k + 1) * 128], h2_.bf()[:, k * 128:(k + 1) * 128], ident_b.bf(), r=[h2_.g(), ident_b.g()], w=[BK[7]])
            cp(h2T.bf(), BTb, r=[BK[7]], w=[h2T.g()], eng='act')
            for qc in range(16):
                bk = 2 + (qc // 4) % 2
                for k in range(KC):
                    mm(banks[bk][:, (qc % 4) * 128:(qc % 4 + 1) * 128], WQ3[:, k, qc * 128:(qc + 1) * 128], H2T3[:, k, :], k == 0, k == KC - 1,
                       r=[hT.g(), h2T.g()], w=[BK[bk]])
                if qc % 4 == 3:
                    cp(qTs.bf()[:, (qc - 3) * 128:(qc + 1) * 128], banks[bk][:], r=[BK[bk]], w=[qTs.g((qc - 3) * 256, (qc + 1) * 256)], eng='act')
            for qc in range(16):
                bk = 4 + qc // 4
                mm(banks[bk][:, (qc % 4) * 128:(qc % 4 + 1) * 128], QTS3[:, qc, :], SUBK3[:, qc, :], True, True,
                   r=[qTs.g(qc * 256, (qc + 1) * 256), subkT.g()], w=[BK[bk]])
            for q4 in range(4):
                cp(ssb.f32()[:, q4 * 512:(q4 + 1) * 512], banks[4 + q4][:], r=[BK[4 + q4]], w=[ssb.g(q4 * 2048, (q4 + 1) * 2048)], eng='act')
            if dbg and i == 0:
                dump("dbg_xm", xm_.f32(), [128, 1024], r=[xm_.g()])
                dump("dbg_s", ssb.f32(), [128, 2048], r=[ssb.g()])
            sg_ = lambda qc: ssb.g(qc * 512, (qc + 1) * 512)
            wg_ = lambda qc: wkb.g(qc * 512, (qc + 1) * 512)
            vk = lambda qc, hf: f"v16:{qc}:{hf}"
            ik = lambda qc, hf: f"ixu:{qc}:{hf}"
            ALLV = [vk(q_, h_) for q_ in range(16) for h_ in range(2)]
            ALLI = [ik(q_, h_) for q_ in range(16) for h_ in range(2)]
            for qc in range(16):
                gen('max', [sg_(qc), v16.g()], [vk(qc, 0)], out=V16[:, qc, 0:8], in_=SS3[:, qc, :])
            for qc in range(16):
                gen('max_index', [sg_(qc), vk(qc, 0), ixu.g()], [ik(qc, 0)], out=IXU[:, qc, 0:8], in_max=V16[:, qc, 0:8], in_values=SS3[:, qc, :])
            for qc in range(16):
                gen('match_replace', [sg_(qc), vk(qc, 0)], [wg_(qc)], out=WK3[:, qc, :], in_to_replace=V16[:, qc, 0:8], in_values=SS3[:, qc, :], imm_value=NEG)
            for qc in range(16):
                gen('max', [wg_(qc), v16.g()], [vk(qc, 1)], out=V16[:, qc, 8:16], in_=WK3[:, qc, :])
            for qc in range(16):
                gen('max_index', [wg_(qc), vk(qc, 1), ixu.g()], [ik(qc, 1)], out=IXU[:, qc, 8:16], in_max=V16[:, qc, 8:16], in_values=WK3[:, qc, :])
            cp(ixf.f32(), ixu.u32(), r=ALLI, w=[ixf.g()])
            cand, wk2 = ssb, wkb
            C4 = cand.f32().rearrange("p (h n) -> p h n", h=8)
            W4 = wk2.f32().rearrange("p (h n) -> p h n", h=8)
            vf = v16.f32()
            tt(mk(cand.f32(), [[256, 8], [16, 16], [1, 16]]), mk(vf, [[32, 8], [1, 16], [0, 16]]), mk(vf, [[32, 8], [0, 16], [1, 16]], 16), ALU.add,
               r=ALLV, w=[cand.g()])
            C16 = c16.f32().rearrange("p (h j) -> p h j", h=8)
            POS = posu.u32().rearrange("p (h j) -> p h j", h=8)
            cg_ = lambda h_: cand.g(h_ * 1024, (h_ + 1) * 1024)
            w2g_ = lambda h_: wk2.g(h_ * 1024, (h_ + 1) * 1024)
            ck = lambda h_, hf: f"c16:{h_}:{hf}"
            pk = lambda h_, hf: f"pos:{h_}:{hf}"
            ALLC = [ck(h_, f_) for h_ in range(8) for f_ in range(2)]
            ALLP = [pk(h_, f_) for h_ in range(8) for f_ in range(2)]
            for h_ in range(8):
                gen('max', [cg_(h_), c16.g()], [ck(h_, 0)], out=C16[:, h_, 0:8], in_=C4[:, h_, :])
            for h_ in range(8):
                gen('max_index', [cg_(h_), ck(h_, 0), posu.g()], [pk(h_, 0)], out=POS[:, h_, 0:8], in_max=C16[:, h_, 0:8], in_values=C4[:, h_, :])
            for h_ in range(8):
                gen('match_replace', [cg_(h_), ck(h_, 0)], [w2g_(h_)], out=W4[:, h_, :], in_to_replace=C16[:, h_, 0:8], in_values=C4[:, h_, :], imm_value=NEG)
            for h_ in range(8):
                gen('max', [w2g_(h_), c16.g()], [ck(h_, 1)], out=C16[:, h_, 8:16], in_=W4[:, h_, :])
            for h_ in range(8):
                gen('max_index', [w2g_(h_), ck(h_, 1), posu.g()], [pk(h_, 1)], out=POS[:, h_, 8:16], in_max=C16[:, h_, 8:16], in_values=W4[:, h_, :])
            gen('tensor_single_scalar', ALLP, [pau.g()], out=pau.u32(), in_=posu.u32(), scalar=4, op=ALU.logical_shift_right)
            gen('tensor_single_scalar', ALLP, [pbu.g()], out=pbu.u32(), in_=posu.u32(), scalar=15, op=ALU.bitwise_and)
            cp(paf.f32(), pau.u32(), r=[pau.g()], w=[paf.g()])
            cp(pbf.f32(), pbu.u32(), r=[pbu.g()], w=[pbf.g()])
            oh, prod = ssb, wkb
            for (pf_, off_, dst_) in ((paf, 0, i1s), (pbf, 16, i2s)):
                tt(mk(oh.f32(), [[16, 128], [1, 16]]), mk(pf_.f32(), [[1, 128], [0, 16]]), mk(iota16.f32(), [[0, 128], [1, 16]]), ALU.is_equal,
                   r=[pf_.g(), iota16.g()], w=[oh.g()])
                tt(mk(prod.f32(), [[256, 8], [16, 16], [1, 16]]), mk(oh.f32(), [[256, 8], [16, 16], [1, 16]]), mk(ixf.f32(), [[32, 8], [0, 16], [1, 16]], off_), ALU.mult,
                   r=[oh.g(), ixf.g()], w=[prod.g()])
                tred(dst_.f32(), mk(prod.f32(), [[16, 128], [1, 16]]), ALU.add, r=[prod.g()], w=[dst_.g()])
            stt(eidf.f32(), i1s.f32(), 128.0, i2s.f32(), ALU.mult, ALU.add, r=[i1s.g(), i2s.g()], w=[eidf.g()])
            cp(eidu_.u32(), eidf.f32(), r=[eidf.g()], w=[eidu_.g()])
            tt(mk(scm.f32(), [[16, 8], [1, 16]]), mk(c16.f32(), [[16, 8], [1, 16]]), mk(c16.f32(), [[16, 8], [0, 16]]), ALU.subtract, r=ALLC, w=[scm.g()])
            act(ee.f32(), scm.f32(), AF.Exp, r=[scm.g()], w=[ee.g()])
            tred(zz.f32(), mk(ee.f32(), [[16, 8], [1, 16]]), ALU.add, r=[ee.g()], w=[zz.g()])
            gen('reciprocal', [zz.g()], [rzz.g()], out=rzz.f32(), in_=zz.f32())
            tt(mk(gate_.f32(), [[16, 8], [1, 16]]), mk(ee.f32(), [[16, 8], [1, 16]]), mk(rzz.f32(), [[1, 8], [0, 16]]), ALU.mult, r=[ee.g(), rzz.g()], w=[gate_.g()])
            if dbg and i == 0:
                dump("dbg_eid", eidf.f32(), [128, 128], r=[eidf.g()])
                dump("dbg_gate", gate_.f32(), [128, 128], r=[gate_.g()])

        def stageY(i, pump):
            par = i % 2
            xm_, h2_, eidu_, gate_ = xm[par], h2[par], eidu[par], gate[par]
            t0 = i * 128
            EID = eidu_.u32()

            def gather(out_ap, slot, r, w):
                P.add('pool', lambda e: e.indirect_dma_start(out=out_ap, out_offset=None, in_=etab_d.ap(),
                                                             in_offset=bass.IndirectOffsetOnAxis(ap=EID[:, slot:slot + 1], axis=0)),
                      r=r, w=w, dma=True)

            def slot_tail(sl):
                cb = cbuf[sl % NBUF]
                dg_ = dgb[sl % 4]
                act(wgt.f32()[:, sl:sl + 1], gl.f32()[:, sl:sl + 1], AF.Copy, r=[f"gl{sl}", gate_.g(), wgt.g()], w=[f"wgt{sl}"],
                    scale=gate_.f32()[:, sl:sl + 1])
                P.add('act', (lambda dg_=dg_, sl=sl: (lambda e: e.activation(out=dg_.bf(), in_=ident_f.f32(), func=AF.Copy, scale=wgt.f32()[:, sl:sl + 1])))(),
                      r=[ident_f.g(), f"wgt{sl}"], w=[dg_.g()])
                for half in range(2):
                    mm(banks[half][:], dg_.bf(), cb.bf()[:, D + half * 512:D + (half + 1) * 512], sl == 0, sl == 127,
                       r=[dg_.g(), cb.g(2048, 4096)], w=[BK[half]])

            for sl in range(128):
                cb = cbuf[sl % NBUF]
                gather(cb.bf(), sl, r=[eidu_.g()] + ETAB_KEYS, w=[cb.g()])
                P.add('dve', (lambda cb=cb, sl=sl: (lambda e: e.scalar_tensor_tensor(out=SINK, in0=cb.bf()[:, 0:D], scalar=1.0, in1=h2_.bf(), op0=ALU.mult, op1=ALU.mult,
                                                                                 accum_out=actv.f32()[:, sl:sl + 1])))(),
                      r=[cb.g(0, 2048), h2_.g(), actv.g()], w=[f"actv{sl}"])
                act(gl.f32()[:, sl:sl + 1], actv.f32()[:, sl:sl + 1], AF.Gelu, r=[f"actv{sl}", gl.g()], w=[f"gl{sl}"])
                if sl >= 1:
                    slot_tail(sl - 1)
                pump(3)
            slot_tail(127)
            for half in range(2):
                hs = slice(half * 512, (half + 1) * 512)
                tt(tmpf.f32()[:, hs], banks[half][:], g2.f32()[:, hs], ALU.mult, r=[BK[half], g2.g()], w=[tmpf.g(half * 2048, (half + 1) * 2048)])
                tt(ot.f32()[:, hs], tmpf.f32()[:, hs], xm_.f32()[:, hs], ALU.add, r=[tmpf.g(half * 2048, (half + 1) * 2048), xm_.g()], w=[ot.g(half * 2048, (half + 1) * 2048)])
            if dbg and i == 0:
                dump("dbg_xo", ot.f32(), [128, 1024], r=[ot.g()])
            act(SINK, ot.f32(), AF.Square, r=[ot.g()], w=[small.g()], accum_out=sm(SM_SS3))
            rstd_chain(SM_SS3, SM_LN3, SM_RSTD3, D, 1e-6)
            stt(ot.f32(), ot.f32(), sm(SM_RSTD3), nf_bc.f32(), ALU.mult, ALU.mult, r=[ot.g(), small.g(), nf_bc.g()], w=[ot.g()])
            dma('sp', out_d.ap()[b, t0:t0 + 128, :], ot.f32(), r=[ot.g()], w=[f"out{b}_{i}"])

        def drain(q, n=None):
            k = 0
            while q and (n is None or k < n):
                P.add(*q.pop(0))
                k += 1

        P.capture = []
        stageX(0)
        q = P.capture
        P.capture = None
        drain(q)
        for i in range(ntiles):
            if i + 1 < ntiles:
                P.capture = []
                stageX(i + 1)
                q = P.capture
                P.capture = None
            else:
                q = []
            stageY(i, lambda n: drain(q, n))
            drain(q)

    P.emit(nc, stack)
    stack.close()
    return nc, list(dbg_d.keys())


def _prep_inputs(inputs):
    f = np.float32
    x = np.asarray(inputs['x'], f)
    c = np.asarray(inputs['c'], f)
    cst = _host_consts()
    rep = lambda v, n=128: np.ascontiguousarray(np.broadcast_to(np.asarray(v, f).reshape(1, -1), (n, np.asarray(v).size)))
    nw = np.concatenate([np.asarray(inputs['norm_mix_w'], f).reshape(-1), np.asarray(inputs['norm_ffn_w'], f).reshape(-1),
                         np.asarray(inputs['norm_final_w'], f).reshape(-1)])
    lam = np.concatenate([np.asarray(inputs[k], f).reshape(-1) for k in ('lambda_q1', 'lambda_k1', 'lambda_q2', 'lambda_k2')])
    sk1 = np.asarray(inputs['sub_keys_1'], f)[0]
    sk2 = np.asarray(inputs['sub_keys_2'], f)[0]
    subkT = np.zeros((128, 16, 128), f)
    for h in range(8):
        subkT[:, h * 2 + 0, :] = sk1[h].T
        subkT[:, h * 2 + 1, :] = sk2[h].T
    shared = {
        'w_ada': np.ascontiguousarray(np.asarray(inputs['w_ada'], f)[0]),
        'b_ada_bc': rep(inputs['b_ada']),
        'nw_bc': rep(nw),
        'w_in': np.ascontiguousarray(np.asarray(inputs['w_in'], f)[0]),
        'w_out': np.ascontiguousarray(np.asarray(inputs['w_out'], f)[0]),
        'w_query': np.ascontiguousarray(np.asarray(inputs['w_query'], f)[0]),
        'pool_w': np.ascontiguousarray(np.asarray(inputs['pool_w'], f)[0]),
        'pool_scaleT': np.ascontiguousarray(np.asarray(inputs['pool_scale'], f).reshape(8, 128).T),
        'subkT': np.ascontiguousarray(subkT.reshape(128, 2048)),
        'e_down': np.ascontiguousarray(np.asarray(inputs['expert_down'], f)[0]),
        'e_up': np.ascontiguousarray(np.asarray(inputs['expert_up'], f)[0]),
        'lam_bc': rep(lam),
        'sublnT': np.ascontiguousarray(np.asarray(inputs['subln_w'], f).reshape(128, 1)),
        'relb_bc': rep(np.asarray(inputs['rel_bias'], f).reshape(-1)),
        'masks': np.ascontiguousarray(cst['masks'].reshape(128, -1)),
        'negmask': cst['negmask'],
        'band': np.ascontiguousarray(cst['band'].reshape(128, -1)),
        'ident': cst['ident'],
        'iota16': cst['iota16'],
        'sel0': cst['sel0'],
    }
    in_maps = []
    for core in range(NCORES):
        m = dict(shared)
        m['x'] = np.ascontiguousarray(x[core * NB:(core + 1) * NB])
        cc = c[core * NB:(core + 1) * NB]
        m['cT'] = np.ascontiguousarray(cc.reshape(NB, KC, 128).transpose(2, 0, 1).reshape(128, NB * KC))
        in_maps.append(m)
    return in_maps


_CACHE = {}


def kernel(**inputs):
    in_maps = _prep_inputs(inputs)
    if 'nc' not in _CACHE:
        _CACHE['nc'] = build()[0]
    nc = _CACHE['nc']
    res = run_bass_kernel_spmd(nc, in_maps, core_ids=list(range(NCORES)))
    out = np.concatenate([np.asarray(r['out']) for r in res.results], axis=0)
    return out.astype(np.float32)
```

```python
import os
import math
import contextlib
import numpy as np
import concourse.bass as bass
import concourse.mybir as mybir
from concourse.bass_utils import run_bass_kernel_spmd

dt = mybir.dt
AF = mybir.ActivationFunctionType
ALU = mybir.AluOpType
AX = mybir.AxisListType
F32, BF16, U32 = dt.float32, dt.bfloat16, dt.uint32

NCORES = 8
NB = 2
S = 2048
NT = 16
D = 1024
KC = 8
H = 8
DIN = 6144
NEXP = 16384
OFF = 8.0
NEG = -1.0e30
G = 512


class Prog:
    def __init__(self):
        self.ops = []
        self.last_w = {}
        self.readers = {}
        self.capture = None

    def add(self, eng, fn, r=(), w=(), dma=False):
        if self.capture is not None:
            self.capture.append((eng, fn, r, w, dma))
            return None
        i = len(self.ops)
        deps = set()
        rk = _flat(r)
        wk = _flat(w)
        for k in rk:
            lw = self.last_w.get(k)
            if lw is not None:
                deps.add(lw)
        for k in wk:
            lw = self.last_w.get(k)
            if lw is not None:
                deps.add(lw)
            deps.update(self.readers.get(k, ()))
        deps.discard(i)
        for k in rk:
            self.readers.setdefault(k, []).append(i)
        for k in wk:
            self.last_w[k] = i
            self.readers[k] = []
        self.ops.append(dict(eng=eng, fn=fn, deps=deps, dma=dma, sig=None, need=False, pre=None))
        return i

    def emit(self, nc, stack):
        ops = self.ops
        EPOCH = 30000
        NSLOT = {'sp': 24, 'pool': 24, 'act': 8}
        for o in ops:
            for d in o['deps']:
                p = ops[d]
                if p['eng'] == 'pe' and o['eng'] == 'pe' and not p['dma'] and not o['dma']:
                    continue
                p['need'] = True
        cnt = {e: 0 for e in ('pe', 'act', 'dve', 'pool', 'sp')}
        esems = {e: [] for e in cnt}
        dsems = {q: [stack.enter_context(nc.semaphore(f"d_{q}_{i}")) for i in range(n)] for q, n in NSLOT.items()}
        duse = {q: [0] * n for q, n in NSLOT.items()}
        dnext = {q: 0 for q in NSLOT}
        for o in ops:
            e = o['eng']
            if o['dma']:
                s = dnext[e]
                dnext[e] = (s + 1) % NSLOT[e]
                if duse[e][s] > 0:
                    o['pre'] = (dsems[e][s], 16 * duse[e][s])
                duse[e][s] += 1
                o['sig'] = (dsems[e][s], 16 * duse[e][s])
            elif o['need']:
                ep = cnt[e] // EPOCH
                while len(esems[e]) <= ep:
                    esems[e].append(stack.enter_context(nc.semaphore(f"e_{e}_{len(esems[e])}")))
                cnt[e] += 1
                o['sig'] = (esems[e][ep], cnt[e] - ep * EPOCH)
        by_eng = {e: [o for o in ops if o['eng'] == e] for e in cnt}
        final_waits = []
        for q in NSLOT:
            for s in range(NSLOT[q]):
                if duse[q][s] > 0:
                    final_waits.append((dsems[q][s], 16 * duse[q][s]))

        def run(ename, eng):
            waited = {}
            for o in by_eng[ename]:
                needs = {}
                for d in o['deps']:
                    p = ops[d]
                    if p['eng'] == 'pe' and ename == 'pe' and not p['dma'] and not o['dma']:
                        continue
                    sem, val = p['sig']
                    key = id(sem)
                    if needs.get(key, (None, 0))[1] < val:
                        needs[key] = (sem, val)
                if o['pre'] is not None:
                    sem, val = o['pre']
                    key = id(sem)
                    if needs.get(key, (None, 0))[1] < val:
                        needs[key] = (sem, val)
                for key, (sem, val) in needs.items():
                    if waited.get(key, 0) < val:
                        eng.wait_ge(sem, val)
                        waited[key] = val
                inst = o['fn'](eng)
                if o['sig'] is not None:
                    inst.then_inc(o['sig'][0], 16 if o['dma'] else 1)
            if ename == 'sp':
                for sem, val in final_waits:
                    eng.wait_ge(sem, val)

        with nc.Block() as block:
            @block.tensor
            def _(e):
                run('pe', e)

            @block.scalar
            def _(e):
                run('act', e)

            @block.vector
            def _(e):
                run('dve', e)

            @block.gpsimd
            def _(e):
                run('pool', e)

            @block.sync
            def _(e):
                run('sp', e)


def _flat(x):
    out = []
    for k in x:
        if isinstance(k, (list, tuple, set, range)):
            out.extend(_flat(k))
        else:
            out.append(k)
    return out


class Buf:
    def __init__(self, arena, off, nbytes):
        assert off % 4 == 0 and nbytes % 4 == 0
        self.A, self.off, self.nbytes = arena, off, nbytes

    def g(self, lo=0, hi=None):
        hi = self.nbytes if hi is None else hi
        return range((self.off + lo) // G, (self.off + hi + G - 1) // G)

    def f32(self):
        return self.A[:, self.off // 4:(self.off + self.nbytes) // 4]

    def bf(self):
        return self.A[:, self.off // 4:(self.off + self.nbytes) // 4].bitcast(BF16)

    def u32(self):
        return self.A[:, self.off // 4:(self.off + self.nbytes) // 4].bitcast(U32)

    def sub(self, lo, n):
        return Buf(self.A, self.off + lo, n)


class Alloc:
    def __init__(self, arena, base, limit):
        self.A, self.p, self.limit = arena, base, limit

    def get(self, nbytes):
        nb = (nbytes + G - 1) // G * G
        b = Buf(self.A, self.p, (nbytes + 3) // 4 * 4)
        self.p += nb
        assert self.p <= self.limit, (self.p, self.limit)
        return b


def mk(ap, pattern, extra_off=0):
    return bass.AP(tensor=ap.tensor, offset=ap.offset + extra_off, ap=[list(ap.ap[0])] + [list(p) for p in pattern])


def _t5_bucket(d):
    d = np.maximum(d, 0)
    x = np.maximum(d, 1).astype(np.float32) / np.float32(16)
    large = 16 + (np.log(x).astype(np.float32) / np.float32(math.log(128 / 16)) * np.float32(16)).astype(np.int32)
    large = np.minimum(large, 31)
    return np.where(d < 16, d, large)


def _host_consts():
    kk = np.arange(128)[:, None]
    qq = np.arange(256)[None, :]
    dist = qq - kk
    valid = dist >= 0
    bk = _t5_bucket(dist)
    masks = np.zeros((128, 32, 256), np.float32)
    for b in range(32):
        masks[:, b, :] = (valid & (bk == b)).astype(np.float32)
    negmask = np.where(valid, 0.0, NEG).astype(np.float32)
    band = np.zeros((128, 12, 128), np.float32)
    s = np.arange(128)[:, None]
    t = np.arange(128)[None, :]
    for gi, w in enumerate((2, 4, 8, 16)):
        cnt0 = np.minimum(t + 1, w).astype(np.float32)
        band[:, gi * 3 + 0, :] = ((s <= t) & (s > t - w)) / cnt0 - (s == t)
        band[:, gi * 3 + 1, :] = ((s <= t) & (s > t - w)) / np.float32(w) - (s == t)
        band[:, gi * 3 + 2, :] = (s > 128 + t - w) / np.float32(w)
    ident = np.eye(128, dtype=np.float32)
    iota16 = np.broadcast_to(np.arange(16, dtype=np.float32)[None, :], (128, 16)).copy()
    sel0 = np.zeros((64, 128), np.float32)
    sel0[0, :] = 1.0
    sel0[32, :] = 1.0
    return dict(masks=masks, negmask=negmask, band=band, ident=ident, iota16=iota16, sel0=sel0)


def build(stage=99, dbg=False):
    nc = bass.Bass("TRN2", target_bir_lowering=False)
    P = Prog()

    def din(name, shape, dtype=F32):
        return nc.dram_tensor(name, list(shape), dtype, kind="ExternalInput")

    x_d = din("x", [NB, S, D])
    cT_d = din("cT", [128, NB * KC])
    wada_d = din("w_ada", [D, DIN])
    bada_d = din("b_ada_bc", [128, DIN])
    nw_d = din("nw_bc", [128, 3 * D])
    win_d = din("w_in", [D, DIN])
    wout_d = din("w_out", [D, D])
    wqry_d = din("w_query", [D, 2048])
    poolw_d = din("pool_w", [4, 256, 256])
    psc_d = din("pool_scaleT", [128, 8])
    subk_d = din("subkT", [128, 16 * 128])
    edown_d = din("e_down", [NEXP, D])
    eup_d = din("e_up", [NEXP, D])
    lam_d = din("lam_bc", [128, 256])
    subln_d = din("sublnT", [128, 1])
    relb_d = din("relb_bc", [128, 256])
    masks_d = din("masks", [128, 32 * 256])
    negm_d = din("negmask", [128, 256])
    band_d = din("band", [128, 12 * 128])
    ident_d = din("ident", [128, 128])
    iota_d = din("iota16", [128, 16])
    sel0_d = din("sel0", [64, 128])
    out_d = nc.dram_tensor("out", [NB, S, D], F32, kind="ExternalOutput")
    etab_d = nc.dram_tensor("etab", [NEXP, 2 * D], BF16, kind="Internal")
    dbg_d = {}

    def dbg_out(name, shape):
        dbg_d[name] = nc.dram_tensor(name, list(shape), F32, kind="ExternalOutput")
        return dbg_d[name]

    stack = contextlib.ExitStack()
    TOT = 212480
    arena = stack.enter_context(nc.sbuf_tensor("arena", [128, TOT // 4], F32))
    banks = [stack.enter_context(nc.psum_tensor(f"B{i}", [128, 512], F32)) for i in range(8)]
    BK = [f"B{i}" for i in range(8)]

    al = Alloc(arena, 0, TOT)
    ident_f = al.get(512)
    ident_b = al.get(256)
    ones_b = al.get(256)
    ones_f = al.get(512)
    sel0 = al.get(512)
    relb = al.get(1024)
    rb31m = al.get(32)
    lamv = al.get(1024)
    small = al.get(512)
    pscT = al.get(32)
    band = al.get(12 * 128 * 2)
    subkT = al.get(16 * 128 * 2)
    iota16 = al.get(64)
    sink = al.get(64)
    nf_bc = al.get(4096)
    sh1 = al.get(4096)
    a1 = al.get(4096)
    g1 = al.get(4096)
    sh2 = al.get(4096)
    a2 = al.get(4096)
    g2 = al.get(4096)
    hT = al.get(KC * S * 2)
    mT = al.get(KC * S * 2)
    wout = al.get(KC * D * 2)
    work_base = al.p
    SM = small.f32()
    (SM_S1, SM_S2, SM_E1, SM_E2, SM_NLAM, SM_WSUB, SM_SS, SM_RSTD, SM_LN, SM_SS2, SM_RSTD2, SM_SS3, SM_RSTD3,
     SM_LN2, SM_LN3, SM_SUBLN) = range(16)

    def sm(i):
        return SM[:, i:i + 1]

    def smg(i):
        return small.g()

    def dma(q, out, in_, r, w):
        P.add(q, lambda e: e.dma_start(out=out, in_=in_), r=r, w=w, dma=True)

    def mm(out, lhsT, rhs, start, stop, r, w, **kw):
        P.add('pe', lambda e: e.matmul(out, lhsT, rhs, start=start, stop=stop, **kw), r=r, w=w)

    def tr(out, in_, ident, r, w):
        P.add('pe', lambda e: e.transpose(out, in_, ident), r=r, w=w)

    def act(out, in_, func, r, w, bias=None, scale=None, accum_out=None, eng='act'):
        def f(e):
            kw = {}
            if bias is not None:
                kw['bias'] = bias
            if scale is not None:
                kw['scale'] = scale
            if accum_out is not None:
                kw['accum_out'] = accum_out
            return e.activation(out=out, in_=in_, func=func, **kw)
        P.add('act', f, r=r, w=w)

    def tt(out, in0, in1, op, r, w, eng='dve'):
        P.add(eng, lambda e: e.tensor_tensor(out=out, in0=in0, in1=in1, op=op), r=r, w=w)

    def ts(out, in0, s1, s2, op0, op1, r, w, eng='dve'):
        if op1 is None:
            P.add(eng, lambda e: e.tensor_scalar(out=out, in0=in0, scalar1=s1, scalar2=None, op0=op0), r=r, w=w)
        else:
            P.add(eng, lambda e: e.tensor_scalar(out=out, in0=in0, scalar1=s1, scalar2=s2, op0=op0, op1=op1), r=r, w=w)

    def stt(out, in0, scalar, in1, op0, op1, r, w):
        P.add('dve', lambda e: e.scalar_tensor_tensor(out=out, in0=in0, scalar=scalar, in1=in1, op0=op0, op1=op1), r=r, w=w)

    def cp(out, in_, r, w, eng='dve'):
        if eng == 'act':
            P.add('act', lambda e: e.copy(out=out, in_=in_), r=r, w=w)
        else:
            P.add(eng, lambda e: e.tensor_copy(out=out, in_=in_), r=r, w=w)

    def ttr(out, in0, in1, accum_out, r, w):
        P.add('dve', lambda e: e.scalar_tensor_tensor(out=out, in0=in0, scalar=1.0, in1=in1, op0=ALU.mult, op1=ALU.mult,
                                                     accum_out=accum_out), r=r, w=w)

    def tred(out, in_, op, r, w):
        P.add('dve', lambda e: e.tensor_reduce(out=out, in_=in_, axis=AX.X, op=op), r=r, w=w)

    def memset(ap, val, r, w, eng='dve'):
        P.add(eng, lambda e: e.memset(ap, val), r=r, w=w)

    def rstd_chain(ss_i, ln_i, rstd_i, n, eps):
        ts(sm(ln_i), sm(ss_i), 1.0 / n, eps, ALU.mult, ALU.add, r=[small.g()], w=[small.g()])
        act(sm(ln_i), sm(ln_i), AF.Ln, r=[small.g()], w=[small.g()])
        act(sm(rstd_i), sm(ln_i), AF.Exp, r=[small.g()], w=[small.g()], scale=-0.5)

    def dump(name, ap_sb, shape, r):
        if name in dbg_d:
            return
        d = dbg_out(name, shape)
        dma('sp', d.ap(), ap_sb, r=r, w=[name])

    dma('sp', ident_f.f32(), ident_d.ap(), r=[], w=[ident_f.g()])
    dma('sp', sel0.f32()[0:64, :], sel0_d.ap(), r=[], w=[sel0.g()])
    dma('sp', relb.f32(), relb_d.ap(), r=[], w=[relb.g()])
    dma('sp', lamv.f32(), lam_d.ap(), r=[], w=[lamv.g()])
    dma('sp', pscT.f32(), psc_d.ap(), r=[], w=[pscT.g()])
    dma('sp', iota16.f32(), iota_d.ap(), r=[], w=[iota16.g()])
    dma('sp', nf_bc.f32(), nw_d.ap()[:, 2 * D:3 * D], r=[], w=[nf_bc.g()])
    dma('sp', sm(SM_SUBLN), subln_d.ap(), r=[], w=[small.g()])
    dma('pool', band.bf(), band_d.ap(), r=[], w=[band.g()])
    dma('pool', subkT.bf(), subk_d.ap(), r=[], w=[subkT.g()])
    ETAB_JOBS = [(ti, r0) for ti in range(2) for r0 in range(0, NEXP, 1024)]
    ETAB_KEYS = [f"etab{ti}_{r0}" for (ti, r0) in ETAB_JOBS]

    def precast(n):
        for _ in range(n):
            if not ETAB_JOBS:
                return
            ti, r0 = ETAB_JOBS.pop(0)
            tbl = (edown_d, eup_d)[ti]
            dma('pool', etab_d.ap()[r0:r0 + 1024, ti * D:(ti + 1) * D], tbl.ap()[r0:r0 + 1024, :], r=[], w=[f"etab{ti}_{r0}"])

    cp(ident_b.bf(), ident_f.f32(), r=[ident_f.g()], w=[ident_b.g()])
    memset(ones_b.bf(), 1.0, r=[], w=[ones_b.g()])
    memset(ones_f.f32(), 1.0, r=[], w=[ones_f.g()])
    ts(rb31m.f32(), relb.f32()[:, 31 * 8:32 * 8], -OFF, None, ALU.add, None, r=[relb.g()], w=[rb31m.g()])
    ts(sm(SM_WSUB), sm(SM_SUBLN), 0.8, None, ALU.mult, None, r=[small.g()], w=[small.g()])
    wa = Alloc(arena, work_base, TOT)
    junk64 = wa.get(256)
    LV = lamv.f32()
    ttr(junk64.f32(), LV[:, 0:64], LV[:, 64:128], sm(SM_S1), r=[lamv.g()], w=[junk64.g(), small.g()])
    ttr(junk64.f32(), LV[:, 128:192], LV[:, 192:256], sm(SM_S2), r=[lamv.g()], w=[junk64.g(), small.g()])
    act(sm(SM_E1), sm(SM_S1), AF.Exp, r=[small.g()], w=[small.g()])
    act(sm(SM_E2), sm(SM_S2), AF.Exp, r=[small.g()], w=[small.g()])
    tt(sm(SM_NLAM), sm(SM_E2), sm(SM_E1), ALU.subtract, r=[small.g()], w=[small.g()])
    ts(sm(SM_NLAM), sm(SM_NLAM), -0.2, None, ALU.add, None, r=[small.g()], w=[small.g()])
    bias_scr = nc.dram_tensor("bias_scr", [128, 2048], F32, kind="Internal")
    biasT = wa.get(8 * 1024)
    mk_chunk = wa.get(8 * 256 * 4)
    BT3 = biasT.f32().rearrange("p (h q) -> p h q", h=8)
    for h in range(8):
        dma('sp', BT3[:, h, :], negm_d.ap(), r=[], w=[biasT.g(h * 1024, (h + 1) * 1024)])
    for c in range(4):
        dma('sp', mk_chunk.f32(), masks_d.ap()[:, c * 2048:(c + 1) * 2048], r=[], w=[mk_chunk.g()])
        MC = mk_chunk.f32().rearrange("p (b q) -> p b q", b=8)
        for bb in range(8):
            b = c * 8 + bb
            for h in range(8):
                stt(BT3[:, h, :], MC[:, bb, :], relb.f32()[:, b * 8 + h:b * 8 + h + 1], BT3[:, h, :], ALU.mult, ALU.add,
                    r=[mk_chunk.g(), relb.g(), biasT.g(h * 1024, (h + 1) * 1024)], w=[biasT.g(h * 1024, (h + 1) * 1024)])
    ts(biasT.f32(), biasT.f32(), -OFF, None, ALU.add, None, r=[biasT.g()], w=[biasT.g()])
    dma('sp', bias_scr.ap(), biasT.f32(), r=[biasT.g()], w=["bias_scr"])
    if dbg:
        dump("dbg_bias", biasT.f32(), [128, 2048], r=[biasT.g()])
        dump("dbg_small", small.f32()[:, 0:16], [128, 16], r=[small.g()])

    hT3 = hT.bf().rearrange("p (k t) -> p k t", k=KC)
    mT3 = mT.bf().rearrange("p (k t) -> p k t", k=KC)

    def hT_g(c0, c1):
        return [hT.g(k * S * 2 + c0 * 2, k * S * 2 + c1 * 2) for k in range(KC)]

    def mT_g(k, c0, c1):
        return mT.g(k * S * 2 + c0 * 2, k * S * 2 + c1 * 2)

    BTb = banks[7][:].bitcast(BF16)

    for b in range(NB):
        wa = Alloc(arena, work_base, TOT)
        cact = wa.get(64)
        crep = wa.get(KC * 128 * 4)
        wblk = [wa.get(KC * 512 * 4) for _ in range(2)]
        bblk = [wa.get(2048) for _ in range(2)]
        mtmp = wa.get(2048)
        nwt = wa.get(4096)
        cin = wa.get(64)
        dma('sp', cin.f32()[:, 0:KC], cT_d.ap()[:, b * KC:(b + 1) * KC], r=[], w=[cin.g()])
        act(cact.f32()[:, 0:KC], cin.f32()[:, 0:KC], AF.Silu, r=[cin.g()], w=[cact.g()])
        cp(crep.f32().rearrange("p (k m) -> p k m", k=KC), mk(cact.f32(), [[1, KC], [0, 128]]), r=[cact.g()], w=[crep.g()])
        CR = crep.f32().rearrange("p (k m) -> p k m", k=KC)
        wada_v = wada_d.ap().rearrange("(k p) n -> p k n", p=128)
        dsts = [sh1, sh1, a1, a1, g1, g1, sh2, sh2, a2, a2, g2, g2]
        for nb_ in range(12):
            wb = wblk[nb_ % 2]
            bb_ = bblk[nb_ % 2]
            dma('sp', wb.f32().rearrange("p (k n) -> p k n", k=KC), wada_v[:, :, nb_ * 512:(nb_ + 1) * 512], r=[], w=[wb.g()])
            dma('sp', bb_.f32(), bada_d.ap()[:, nb_ * 512:(nb_ + 1) * 512], r=[], w=[bb_.g()])
            WB = wb.f32().rearrange("p (k n) -> p k n", k=KC)
            for k in range(KC):
                mm(banks[6][:], CR[:, k, :], WB[:, k, :], k == 0, k == KC - 1, r=[crep.g(), wb.g()], w=[BK[6]])
            dst = dsts[nb_]
            half = nb_ % 2
            dst_ap = dst.f32()[:, half * 512:(half + 1) * 512]
            dg_ = dst.g(half * 2048, (half + 1) * 2048)
            if nb_ in (2, 3, 8, 9):
                which = 0 if nb_ in (2, 3) else 1
                dma('sp', nwt.f32()[:, 0:512], nw_d.ap()[:, which * D + half * 512: which * D + (half + 1) * 512], r=[], w=[nwt.g()])
                tt(mtmp.f32(), banks[6][:], bb_.f32(), ALU.add, r=[BK[6], bb_.g()], w=[mtmp.g()])
                stt(dst_ap, mtmp.f32(), 1.0, nwt.f32()[:, 0:512], ALU.add, ALU.mult, r=[mtmp.g(), nwt.g()], w=[dg_])
            else:
                tt(dst_ap, banks[6][:], bb_.f32(), ALU.add, r=[BK[6], bb_.g()], w=[dg_])
        if dbg and b == 0:
            dump("dbg_a1", a1.f32(), [128, 1024], r=[a1.g()])
            dump("dbg_g2", g2.f32(), [128, 1024], r=[g2.g()])

        wa = Alloc(arena, work_base, TOT)
        xt = [wa.get(4096) for _ in range(2)]
        tmpf = wa.get(4096)
        junkb = wa.get(2048)
        hbf = wa.get(2048)
        for i in range(NT):
            xb = xt[i % 2]
            dma('sp', xb.f32(), x_d.ap()[b, i * 128:(i + 1) * 128, :], r=[], w=[xb.g()])
            act(junkb.bf(), xb.f32(), AF.Square, r=[xb.g()], w=[junkb.g(), small.g()], accum_out=sm(SM_SS))
            rstd_chain(SM_SS, SM_LN, SM_RSTD, D, 1e-6)
            stt(tmpf.f32(), xb.f32(), sm(SM_RSTD), a1.f32(), ALU.mult, ALU.mult, r=[xb.g(), small.g(), a1.g()], w=[tmpf.g()])
            tt(hbf.bf(), tmpf.f32(), sh1.f32(), ALU.add, r=[tmpf.g(), sh1.g()], w=[hbf.g()])
            for k in range(KC):
                tr(BTb[:, k * 128:(k + 1) * 128], hbf.bf()[:, k * 128:(k + 1) * 128], ident_b.bf(), r=[hbf.g(), ident_b.g()], w=[BK[7]])
            cp(hT3[:, :, i * 128:(i + 1) * 128], BTb.rearrange("p (k t) -> p k t", k=KC), r=[BK[7]], w=hT_g(i * 128, (i + 1) * 128), eng='act')
        if dbg and b == 0:
            wa2 = Alloc(arena, wa.p, TOT)
            dtmp = wa2.get(8192)
            cp(dtmp.f32(), hT3[:, 0, :], r=hT_g(0, S), w=[dtmp.g()])
            dump("dbg_hT0", dtmp.f32(), [128, 2048], r=[dtmp.g()])
        if stage <= 1:
            continue

        win_v = win_d.ap().rearrange("(k p) n -> p k n", p=128)
        wa = Alloc(arena, work_base, TOT)
        wp = wa.get(KC * 256 * 2)
        wgp = wa.get(KC * 256 * 2)
        pw = wa.get(2 * 256 * 2)
        pg = wa.get(NT * 256 * 2)
        pT = [wa.get(512 * 2) for _ in range(2)]
        sg = wa.get(512 * 4)
        WP = wp.bf().rearrange("p (k n) -> p k n", k=KC)
        WGP = wgp.bf().rearrange("p (k n) -> p k n", k=KC)
        PW = pw.bf().rearrange("p (c n) -> p c n", c=2)
        PG = pg.bf().rearrange("p (i n) -> p i n", i=NT)
        BAND = band.bf().rearrange("p (j n) -> p j n", j=12)
        for g in range(4):
            dma('pool', WP, win_v[:, :, 3072 + g * 256:3072 + (g + 1) * 256], r=[], w=[wp.g()])
            dma('pool', WGP, win_v[:, :, 5120 + g * 256:5120 + (g + 1) * 256], r=[], w=[wgp.g()])
            dma('pool', PW, poolw_d.ap()[g].rearrange("(c p) n -> p c n", p=128), r=[], w=[pw.g()])
            for i in range(NT):
                for k in range(KC):
                    mm(banks[7][:, (i % 2) * 256:(i % 2 + 1) * 256], hT3[:, k, i * 128:(i + 1) * 128], WP[:, k, :], k == 0, k == KC - 1,
                       r=[hT_g(i * 128, (i + 1) * 128), wp.g()], w=[BK[7]])
                if i % 2 == 1:
                    cp(pg.bf()[:, (i - 1) * 256:(i + 1) * 256], banks[7][:], r=[BK[7]], w=[pg.g((i - 1) * 512, (i + 1) * 512)], eng='act')
            for c in range(4):
                for cc in range(2):
                    for il in range(4):
                        i = c * 4 + il
                        cur = g * 3 + (0 if i == 0 else 1)
                        mm(banks[cc][:, il * 128:(il + 1) * 128], PG[:, i, cc * 128:(cc + 1) * 128], BAND[:, cur, :], True, i == 0,
                           r=[pg.g(i * 512, (i + 1) * 512), band.g()], w=[BK[cc]])
                        if i > 0:
                            mm(banks[cc][:, il * 128:(il + 1) * 128], PG[:, i - 1, cc * 128:(cc + 1) * 128], BAND[:, g * 3 + 2, :], False, True,
                               r=[pg.g((i - 1) * 512, i * 512), band.g()], w=[BK[cc]])
                    cp(pT[cc].bf(), banks[cc][:], r=[BK[cc]], w=[pT[cc].g()], eng='act')
                for ec in range(2):
                    kch = g * 2 + ec
                    mm(banks[2][:], PW[:, 0, ec * 128:(ec + 1) * 128], pT[0].bf(), True, False, r=[pw.g(), pT[0].g()], w=[BK[2]])
                    mm(banks[2][:], PW[:, 1, ec * 128:(ec + 1) * 128], pT[1].bf(), False, True, r=[pw.g(), pT[1].g()], w=[BK[2]])
                    for k in range(KC):
                        mm(banks[3][:], WGP[:, k, ec * 128:(ec + 1) * 128], hT3[:, k, c * 512:(c + 1) * 512], k == 0, k == KC - 1,
                           r=[wgp.g(), hT_g(c * 512, (c + 1) * 512)], w=[BK[3]])
                    act(sg.f32(), banks[3][:], AF.Sigmoid, r=[BK[3]], w=[sg.g()])
                    stt(mT3[:, kch, c * 512:(c + 1) * 512], banks[2][:], pscT.f32()[:, kch:kch + 1], sg.f32(), ALU.mult, ALU.mult,
                        r=[BK[2], pscT.g(), sg.g()], w=[mT_g(kch, c * 512, (c + 1) * 512)])
        if dbg and b == 0 and stage == 2:
            wa2 = Alloc(arena, wa.p, TOT)
            dtmp = wa2.get(8192)
            for kk_ in (0, 5):
                cp(dtmp.f32(), mT3[:, kk_, :], r=[mT.g()], w=[dtmp.g()])
                dump(f"dbg_mT{kk_}", dtmp.f32(), [128, 2048], r=[dtmp.g()])
        if stage <= 2:
            continue

        wa = Alloc(arena, work_base, TOT)
        wq = wa.get(KC * 128 * 2)
        wk_ = wa.get(KC * 128 * 2)
        wv = wa.get(KC * 128 * 2)
        wg = wa.get(KC * 128 * 2)
        QTz = [wa.get(S * 2) for _ in range(2)]
        KT = wa.get(S * 2)
        Vb = wa.get(NT * 128 * 2)
        sgT = wa.get(S * 2)
        PT = [[wa.get(512 * 2) for _ in range(2)] for _ in range(2)]
        ntmp = [wa.get(256 * 4) for _ in range(2)]
        O1s = wa.get(2048)
        O2s = wa.get(2048)
        lnz = wa.get(2048)
        rz = wa.get(2048)
        rz2 = wa.get(2048)
        sq = wa.get(2048)
        t1 = wa.get(2048)
        rs = wa.get(2048)
        biasT = wa.get(8 * 1024)
        BT3 = biasT.f32().rearrange("p (h q) -> p h q", h=8)
        dma('sp', biasT.f32(), bias_scr.ap(), r=["bias_scr"], w=[biasT.g()])
        WQ = wq.bf().rearrange("p (k n) -> p k n", k=KC)
        WK = wk_.bf().rearrange("p (k n) -> p k n", k=KC)
        WV = wv.bf().rearrange("p (k n) -> p k n", k=KC)
        WG = wg.bf().rearrange("p (k n) -> p k n", k=KC)
        V3 = Vb.bf().rearrange("p (i n) -> p i n", i=NT)
        nheads = H if stage > 3 or not dbg else 1
        memset(QTz[0].bf()[64:128, :], 0.0, r=[], w=[QTz[0].g()])
        memset(QTz[1].bf()[0:64, :], 0.0, r=[], w=[QTz[1].g()])
        for h in range(nheads):
            dma('pool', WQ, win_v[:, :, h * 128:(h + 1) * 128], r=[], w=[wq.g()])
            dma('pool', WK, win_v[:, :, 1024 + h * 128:1024 + (h + 1) * 128], r=[], w=[wk_.g()])
            dma('pool', WV, win_v[:, :, 2048 + h * 128:2048 + (h + 1) * 128], r=[], w=[wv.g()])
            dma('pool', WG, win_v[:, :, 4096 + h * 128:4096 + (h + 1) * 128], r=[], w=[wg.g()])
            precast(4)
            pj = 0
            for (W_, wb_, dst, mode) in ((WQ, wq, None, 'q'), (WK, wk_, KT, 'k'), (WG, wg, sgT, 'g')):
                for c in range(4):
                    bk = pj % 4
                    pj += 1
                    for k in range(KC):
                        mm(banks[bk][:], W_[:, k, :], hT3[:, k, c * 512:(c + 1) * 512], k == 0, k == KC - 1,
                           r=[wb_.g(), hT_g(c * 512, (c + 1) * 512)], w=[BK[bk]])
                    if mode == 'q':
                        for m_ in range(2):
                            o_ap = QTz[m_].bf()[m_ * 64:(m_ + 1) * 64, c * 512:(c + 1) * 512]
                            P.add('act', (lambda o_ap=o_ap, bk=bk, m_=m_: (lambda e: e.mul(out=o_ap, in_=banks[bk][m_ * 64:(m_ + 1) * 64, :], mul=0.125)))(),
                                  r=[BK[bk]], w=[QTz[m_].g(c * 1024, (c + 1) * 1024)])
                        continue
                    o_ap = dst.bf()[:, c * 512:(c + 1) * 512]
                    o_g = dst.g(c * 1024, (c + 1) * 1024)
                    if mode == 'k':
                        cp(o_ap, banks[bk][:], r=[BK[bk]], w=[o_g], eng='act')
                    else:
                        act(o_ap, banks[bk][:], AF.Sigmoid, r=[BK[bk]], w=[o_g])
            for i in range(NT):
                bk = (i // 4) % 4
                for k in range(KC):
                    mm(banks[bk][:, (i % 4) * 128:(i % 4 + 1) * 128], hT3[:, k, i * 128:(i + 1) * 128], WV[:, k, :], k == 0, k == KC - 1,
                       r=[hT_g(i * 128, (i + 1) * 128), wv.g()], w=[BK[bk]])
                if i % 4 == 3:
                    cp(Vb.bf()[:, (i - 3) * 128:(i + 1) * 128], banks[bk][:], r=[BK[bk]], w=[Vb.g((i - 3) * 256, (i + 1) * 256)], eng='act')

            def qk(c, j):
                col0 = max(0, j - 4 * c) * 128
                par = j % 2
                for m in range(2):
                    bk = m * 2 + par
                    mm(banks[bk][:, col0:512], KT.bf()[:, j * 128:(j + 1) * 128],
                       QTz[m].bf()[:, c * 512 + col0:(c + 1) * 512], True, True,
                       r=[KT.g(j * 256, (j + 1) * 256), QTz[m].g(c * 1024 + col0 * 2, (c + 1) * 1024)], w=[BK[bk]])

            for c in range(4):
                jmax = 4 * c + 3
                qk(c, 0)
                for j in range(jmax + 1):
                    if j + 1 <= jmax:
                        qk(c, j + 1)
                    col0 = max(0, j - 4 * c) * 128
                    par = j % 2
                    il_lo = max(0, j - 4 * c)
                    il_hi = min(3, j + 1 - 4 * c)
                    far0 = max(0, j + 2 - 4 * c) * 128
                    for m in range(2):
                        bk = m * 2 + par
                        pt = PT[m][par]
                        if il_hi >= il_lo and il_hi >= 0:
                            n0, n1 = il_lo * 128, (il_hi + 1) * 128
                            b0 = (4 * c + il_lo - j) * 128
                            nn = n1 - n0
                            tt(ntmp[m].f32()[:, 0:nn], banks[bk][:, n0:n1], BT3[:, h, b0:b0 + nn], ALU.add,
                               r=[BK[bk], biasT.g(h * 1024, (h + 1) * 1024)], w=[ntmp[m].g()])
                            act(pt.bf()[:, n0:n1], ntmp[m].f32()[:, 0:nn], AF.Exp, r=[ntmp[m].g()], w=[pt.g(n0 * 2, n1 * 2)])
                        if far0 < 512:
                            act(pt.bf()[:, far0:512], banks[bk][:, far0:512], AF.Exp, r=[BK[bk], rb31m.g()], w=[pt.g(far0 * 2, 1024)],
                                bias=rb31m.f32()[:, h:h + 1])
                        mm(banks[4 + m][:, col0:512], V3[:, j, :], pt.bf()[:, col0:512], j == 0, j == jmax,
                           r=[Vb.g(j * 256, (j + 1) * 256), pt.g(col0 * 2, 1024)], w=[BK[4 + m]], skip_group_check=True)
                        mm(banks[6 + m][:, col0:512], ones_b.bf(), pt.bf()[:, col0:512], j == 0, j == jmax,
                           r=[ones_b.g(), pt.g(col0 * 2, 1024)], w=[BK[6 + m]], skip_group_check=True)
                cs = slice(c * 512, (c + 1) * 512)
                cp(O1s.f32(), banks[4][:], r=[BK[4]], w=[O1s.g()], eng='act')
                cp(O2s.f32(), banks[5][:], r=[BK[5]], w=[O2s.g()], eng='act')
                act(lnz.f32(), banks[6][:], AF.Ln, r=[BK[6]], w=[lnz.g()])
                act(rz.f32(), lnz.f32(), AF.Exp, r=[lnz.g()], w=[rz.g()], scale=-1.0)
                act(lnz.f32(), banks[7][:], AF.Ln, r=[BK[7]], w=[lnz.g()])
                act(rz2.f32(), lnz.f32(), AF.Exp, r=[lnz.g()], w=[rz2.g()], scale=-1.0)
                tt(t1.f32(), O1s.f32(), rz.f32(), ALU.mult, r=[O1s.g(), rz.g()], w=[t1.g()])
                tt(O2s.f32(), O2s.f32(), rz2.f32(), ALU.mult, r=[O2s.g(), rz2.g()], w=[O2s.g()])
                stt(t1.f32(), O2s.f32(), sm(SM_NLAM), t1.f32(), ALU.mult, ALU.add, r=[t1.g(), O2s.g(), small.g()], w=[t1.g()])
                act(sq.bf()[:, 0:512], t1.f32(), AF.Square, r=[t1.g()], w=[sq.g()])
                mm(banks[6][:], ones_b.bf(), sq.bf()[:, 0:512], True, True, r=[ones_b.g(), sq.g()], w=[BK[6]])
                ts(rs.f32(), banks[6][:], 1.0 / 128, 1e-5, ALU.mult, ALU.add, r=[BK[6]], w=[rs.g()])
                act(rs.f32(), rs.f32(), AF.Ln, r=[rs.g()], w=[rs.g()])
                act(rs.f32(), rs.f32(), AF.Exp, r=[rs.g()], w=[rs.g()], scale=-0.5)
                tt(t1.f32(), t1.f32(), rs.f32(), ALU.mult, r=[t1.g(), rs.g()], w=[t1.g()])
                stt(t1.f32(), t1.f32(), sm(SM_WSUB), sgT.bf()[:, cs], ALU.mult, ALU.mult, r=[t1.g(), small.g(), sgT.g(c * 1024, (c + 1) * 1024)], w=[t1.g()])
                tt(mT3[:, h, cs], t1.f32(), mT3[:, h, cs], ALU.add, r=[t1.g(), mT_g(h, c * 512, (c + 1) * 512)], w=[mT_g(h, c * 512, (c + 1) * 512)])
        if dbg and b == 0 and stage == 3:
            wa2 = Alloc(arena, wa.p, TOT)
            dtmp = wa2.get(8192)
            cp(dtmp.f32(), mT3[:, 0, :], r=[mT.g()], w=[dtmp.g()])
            dump("dbg_mT0", dtmp.f32(), [128, 2048], r=[dtmp.g()])
        if stage <= 3:
            continue

        precast(64)
        WOUT3 = wout.bf().rearrange("p (k n) -> p k n", k=KC)
        dma('pool', WOUT3, wout_d.ap().rearrange("(k p) n -> p k n", p=128), r=[], w=[wout.g()])
        for k in range(KC):
            tt(WOUT3[:, k, :], WOUT3[:, k, :], g1.f32(), ALU.mult, r=[wout.g(k * 2048, (k + 1) * 2048), g1.g()], w=[wout.g(k * 2048, (k + 1) * 2048)])
        WQ3 = hT.bf().rearrange("p (k n) -> p k n", k=KC)
        dma('pool', WQ3, wqry_d.ap().rearrange("(k p) n -> p k n", p=128), r=[], w=[hT.g()])
        wa = Alloc(arena, work_base, TOT)
        xt5 = g1
        xm = [wa.get(4096) for _ in range(2)]
        h2 = [wa.get(2048) for _ in range(2)]
        eidu = [wa.get(512) for _ in range(2)]
        gate = [wa.get(512) for _ in range(2)]
        tmpf = a1
        ot = sh1
        ssb = wa.get(8192)
        wkb = ssb
        tmpfX = ssb.sub(0, 4096)
        junkbX = ssb.sub(4096, 2048)
        h2T = ssb.sub(0, 2048)
        qTs = ssb.sub(4096, 4096)
        v16 = wa.get(1024)
        ixu = wa.get(1024)
        ixf = wa.get(1024)
        c16, posu, pau, pbu, paf, pbf, i1s, i2s, eidf, scm, ee, actv, gl, wgt = [wa.get(512) for _ in range(14)]
        zz = wa.get(32)
        rzz = wa.get(32)
        dgb = [wa.get(256) for _ in range(4)]
        NBUF = (TOT - wa.p) // 4096
        assert NBUF >= 5, NBUF
        cbuf = [wa.get(4096) for _ in range(NBUF)]
        SINK = mk(sink.bf(), [[0, D]])
        H2T3 = h2T.bf().rearrange("p (k t) -> p k t", k=KC)
        QTS3 = qTs.bf().rearrange("p (q t) -> p q t", q=16)
        SUBK3 = subkT.bf().rearrange("p (q n) -> p q n", q=16)
        SS3 = ssb.f32().rearrange("p (q n) -> p q n", q=16)
        WK3 = wkb.f32().rearrange("p (q n) -> p q n", q=16)
        V16 = v16.f32().rearrange("p (q j) -> p q j", q=16)
        IXU = ixu.u32().rearrange("p (q j) -> p q j", q=16)
        ntiles = NT if not dbg else 2

        def gen(name, r, w, eng='dve', **kw):
            P.add(eng, lambda e: getattr(e, name)(**kw), r=r, w=w)

        def stageX(i):
            par = i % 2
            xm_, h2_, eidu_, gate_ = xm[par], h2[par], eidu[par], gate[par]
            t0 = i * 128
            dma('sp', xt5.f32(), x_d.ap()[b, t0:t0 + 128, :], r=[], w=[xt5.g()])
            for half in range(2):
                for k in range(KC):
                    mm(banks[2 + half][:], mT3[:, k, t0:t0 + 128], WOUT3[:, k, half * 512:(half + 1) * 512], k == 0, k == KC - 1,
                       r=[mT_g(k, t0, t0 + 128), wout.g()], w=[BK[2 + half]])
            for half in range(2):
                hs = slice(half * 512, (half + 1) * 512)
                tt(xm_.f32()[:, hs], banks[2 + half][:], xt5.f32()[:, hs], ALU.add, r=[BK[2 + half], xt5.g()], w=[xm_.g(half * 2048, (half + 1) * 2048)])
            act(junkbX.bf(), xm_.f32(), AF.Square, r=[xm_.g()], w=[junkbX.g(), small.g()], accum_out=sm(SM_SS2))
            rstd_chain(SM_SS2, SM_LN2, SM_RSTD2, D, 1e-6)
            for hf in range(2):
                hs = slice(hf * 512, (hf + 1) * 512)
                stt(tmpfX.f32()[:, hs], xm_.f32()[:, hs], sm(SM_RSTD2), a2.f32()[:, hs], ALU.mult, ALU.mult, r=[xm_.g(), small.g(), a2.g()], w=[tmpfX.g(hf * 2048, (hf + 1) * 2048)])
                tt(h2_.bf()[:, hs], tmpfX.f32()[:, hs], sh2.f32()[:, hs], ALU.add, r=[tmpfX.g(hf * 2048, (hf + 1) * 2048), sh2.g()], w=[h2_.g(hf * 1024, (hf + 1) * 1024)])
            for k in range(KC):
                tr(BTb[:, k * 128:(k + 1) * 128], h2_.bf()[:, k * 128:(k + 1) * 128], ident_b.bf(), r=[h2_.g(), ident_b.g()], w=[BK[7]])
            cp(h2T.bf(), BTb, r=[BK[7]], w=[h2T.g()], eng='act')
            for qc in range(16):
                bk = 2 + (qc // 4) % 2
                for k in range(KC):
                    mm(banks[bk][:, (qc % 4) * 128:(qc % 4 + 1) * 128], WQ3[:, k, qc * 128:(qc + 1) * 128], H2T3[:, k, :], k == 0, k == KC - 1,
                       r=[hT.g(), h2T.g()], w=[BK[bk]])
                if qc % 4 == 3:
                    cp(qTs.bf()[:, (qc - 3) * 128:(qc + 1) * 128], banks[bk][:], r=[BK[bk]], w=[qTs.g((qc - 3) * 256, (qc + 1) * 256)], eng='act')
            for qc in range(16):
                bk = 4 + qc // 4
                mm(banks[bk][:, (qc % 4) * 128:(qc % 4 + 1) * 128], QTS3[:, qc, :], SUBK3[:, qc, :], True, True,
                   r=[qTs.g(qc * 256, (qc + 1) * 256), subkT.g()], w=[BK[bk]])
            for q4 in range(4):
                cp(ssb.f32()[:, q4 * 512:(q4 + 1) * 512], banks[4 + q4][:], r=[BK[4 + q4]], w=[ssb.g(q4 * 2048, (q4 + 1) * 2048)], eng='act')
            if dbg and i == 0:
                dump("dbg_xm", xm_.f32(), [128, 1024], r=[xm_.g()])
                dump("dbg_s", ssb.f32(), [128, 2048], r=[ssb.g()])
            sg_ = lambda qc: ssb.g(qc * 512, (qc + 1) * 512)
            wg_ = lambda qc: wkb.g(qc * 512, (qc + 1) * 512)
            vk = lambda qc, hf: f"v16:{qc}:{hf}"
            ik = lambda qc, hf: f"ixu:{qc}:{hf}"
            ALLV = [vk(q_, h_) for q_ in range(16) for h_ in range(2)]
            ALLI = [ik(q_, h_) for q_ in range(16) for h_ in range(2)]
            for qc in range(16):
                gen('max', [sg_(qc), v16.g()], [vk(qc, 0)], out=V16[:, qc, 0:8], in_=SS3[:, qc, :])
            for qc in range(16):
                gen('max_index', [sg_(qc), vk(qc, 0), ixu.g()], [ik(qc, 0)], out=IXU[:, qc, 0:8], in_max=V16[:, qc, 0:8], in_values=SS3[:, qc, :])
            for qc in range(16):
                gen('match_replace', [sg_(qc), vk(qc, 0)], [wg_(qc)], out=WK3[:, qc, :], in_to_replace=V16[:, qc, 0:8], in_values=SS3[:, qc, :], imm_value=NEG)
            for qc in range(16):
                gen('max', [wg_(qc), v16.g()], [vk(qc, 1)], out=V16[:, qc, 8:16], in_=WK3[:, qc, :])
            for qc in range(16):
                gen('max_index', [wg_(qc), vk(qc, 1), ixu.g()], [ik(qc, 1)], out=IXU[:, qc, 8:16], in_max=V16[:, qc, 8:16], in_values=WK3[:, qc, :])
            cp(ixf.f32(), ixu.u32(), r=ALLI, w=[ixf.g()])
            cand, wk2 = ssb, wkb
            C4 = cand.f32().rearrange("p (h n) -> p h n", h=8)
            W4 = wk2.f32().rearrange("p (h n) -> p h n", h=8)
            vf = v16.f32()
            for hp in range(4):
                tt(mk(cand.f32(), [[256, 2], [16, 16], [1, 16]], hp * 512), mk(vf, [[32, 2], [1, 16], [0, 16]], hp * 64),
                   mk(vf, [[32, 2], [0, 16], [1, 16]], hp * 64 + 16), ALU.add, r=ALLV, w=[cand.g(hp * 2048, (hp + 1) * 2048)])
            C16 = c16.f32().rearrange("p (h j) -> p h j", h=8)
            POS = posu.u32().rearrange("p (h j) -> p h j", h=8)
            cg_ = lambda h_: cand.g(h_ * 1024, (h_ + 1) * 1024)
            w2g_ = lambda h_: wk2.g(h_ * 1024, (h_ + 1) * 1024)
            ck = lambda h_, hf: f"c16:{h_}:{hf}"
            pk = lambda h_, hf: f"pos:{h_}:{hf}"
            ALLC = [ck(h_, f_) for h_ in range(8) for f_ in range(2)]
            ALLP = [pk(h_, f_) for h_ in range(8) for f_ in range(2)]
            for h_ in range(8):
                gen('max', [cg_(h_), c16.g()], [ck(h_, 0)], out=C16[:, h_, 0:8], in_=C4[:, h_, :])
            for h_ in range(8):
                gen('max_index', [cg_(h_), ck(h_, 0), posu.g()], [pk(h_, 0)], out=POS[:, h_, 0:8], in_max=C16[:, h_, 0:8], in_values=C4[:, h_, :])
            for h_ in range(8):
                gen('match_replace', [cg_(h_), ck(h_, 0)], [w2g_(h_)], out=W4[:, h_, :], in_to_replace=C16[:, h_, 0:8], in_values=C4[:, h_, :], imm_value=NEG)
            for h_ in range(8):
                gen('max', [w2g_(h_), c16.g()], [ck(h_, 1)], out=C16[:, h_, 8:16], in_=W4[:, h_, :])
            for h_ in range(8):
                gen('max_index', [w2g_(h_), ck(h_, 1), posu.g()], [pk(h_, 1)], out=POS[:, h_, 8:16], in_max=C16[:, h_, 8:16], in_values=W4[:, h_, :])
            gen('tensor_single_scalar', ALLP, [pau.g()], out=pau.u32(), in_=posu.u32(), scalar=4, op=ALU.logical_shift_right)
            gen('tensor_single_scalar', ALLP, [pbu.g()], out=pbu.u32(), in_=posu.u32(), scalar=15, op=ALU.bitwise_and)
            cp(paf.f32(), pau.u32(), r=[pau.g()], w=[paf.g()])
            cp(pbf.f32(), pbu.u32(), r=[pbu.g()], w=[pbf.g()])
            oh, prod = ssb, wkb
            for (pf_, off_, dst_) in ((paf, 0, i1s), (pbf, 16, i2s)):
                for hp in range(4):
                    og = oh.g(hp * 2048, (hp + 1) * 2048)
                    tt(mk(oh.f32(), [[16, 32], [1, 16]], hp * 512), mk(pf_.f32(), [[1, 32], [0, 16]], hp * 32), mk(iota16.f32(), [[0, 32], [1, 16]]), ALU.is_equal,
                       r=[pf_.g(), iota16.g()], w=[og])
                    tt(mk(prod.f32(), [[256, 2], [16, 16], [1, 16]], hp * 512), mk(oh.f32(), [[256, 2], [16, 16], [1, 16]], hp * 512),
                       mk(ixf.f32(), [[32, 2], [0, 16], [1, 16]], hp * 64 + off_), ALU.mult, r=[og, ixf.g()], w=[og])
                    tred(dst_.f32()[:, hp * 32:(hp + 1) * 32], mk(prod.f32(), [[16, 32], [1, 16]], hp * 512), ALU.add, r=[og], w=[dst_.g()])
            stt(eidf.f32(), i1s.f32(), 128.0, i2s.f32(), ALU.mult, ALU.add, r=[i1s.g(), i2s.g()], w=[eidf.g()])
            cp(eidu_.u32(), eidf.f32(), r=[eidf.g()], w=[eidu_.g()])
            tt(mk(scm.f32(), [[16, 8], [1, 16]]), mk(c16.f32(), [[16, 8], [1, 16]]), mk(c16.f32(), [[16, 8], [0, 16]]), ALU.subtract, r=ALLC, w=[scm.g()])
            act(ee.f32(), scm.f32(), AF.Exp, r=[scm.g()], w=[ee.g()])
            tred(zz.f32(), mk(ee.f32(), [[16, 8], [1, 16]]), ALU.add, r=[ee.g()], w=[zz.g()])
            gen('reciprocal', [zz.g()], [rzz.g()], out=rzz.f32(), in_=zz.f32())
            tt(mk(gate_.f32(), [[16, 8], [1, 16]]), mk(ee.f32(), [[16, 8], [1, 16]]), mk(rzz.f32(), [[1, 8], [0, 16]]), ALU.mult, r=[ee.g(), rzz.g()], w=[gate_.g()])
            if dbg and i == 0:
                dump("dbg_eid", eidf.f32(), [128, 128], r=[eidf.g()])
                dump("dbg_gate", gate_.f32(), [128, 128], r=[gate_.g()])

        def stageY(i, pump):
            par = i % 2
            xm_, h2_, eidu_, gate_ = xm[par], h2[par], eidu[par], gate[par]
            t0 = i * 128
            EID = eidu_.u32()

            def gather(out_ap, slot, r, w):
                P.add('pool', lambda e: e.indirect_dma_start(out=out_ap, out_offset=None, in_=etab_d.ap(),
                                                             in_offset=bass.IndirectOffsetOnAxis(ap=EID[:, slot:slot + 1], axis=0)),
                      r=r, w=w, dma=True)

            def slot_tail(sl):
                cb = cbuf[sl % NBUF]
                dg_ = dgb[sl % 4]
                act(wgt.f32()[:, sl:sl + 1], gl.f32()[:, sl:sl + 1], AF.Copy, r=[f"gl{sl}", gate_.g(), wgt.g()], w=[f"wgt{sl}"],
                    scale=gate_.f32()[:, sl:sl + 1])
                P.add('act', (lambda dg_=dg_, sl=sl: (lambda e: e.activation(out=dg_.bf(), in_=ident_f.f32(), func=AF.Copy, scale=wgt.f32()[:, sl:sl + 1])))(),
                      r=[ident_f.g(), f"wgt{sl}"], w=[dg_.g()])
                for half in range(2):
                    mm(banks[half][:], dg_.bf(), cb.bf()[:, D + half * 512:D + (half + 1) * 512], sl == 0, sl == 127,
                       r=[dg_.g(), cb.g(2048, 4096)], w=[BK[half]])

            for sl in range(128):
                cb = cbuf[sl % NBUF]
                gather(cb.bf(), sl, r=[eidu_.g()] + ETAB_KEYS, w=[cb.g()])
                P.add('dve', (lambda cb=cb, sl=sl: (lambda e: e.scalar_tensor_tensor(out=SINK, in0=cb.bf()[:, 0:D], scalar=1.0, in1=h2_.bf(), op0=ALU.mult, op1=ALU.mult,
                                                                                 accum_out=actv.f32()[:, sl:sl + 1])))(),
                      r=[cb.g(0, 2048), h2_.g(), actv.g()], w=[f"actv{sl}"])
                act(gl.f32()[:, sl:sl + 1], actv.f32()[:, sl:sl + 1], AF.Gelu, r=[f"actv{sl}", gl.g()], w=[f"gl{sl}"])
                if sl >= 1:
                    slot_tail(sl - 1)
                pump(3)
            slot_tail(127)
            for half in range(2):
                hs = slice(half * 512, (half + 1) * 512)
                tt(tmpf.f32()[:, hs], banks[half][:], g2.f32()[:, hs], ALU.mult, r=[BK[half], g2.g()], w=[tmpf.g(half * 2048, (half + 1) * 2048)])
                tt(ot.f32()[:, hs], tmpf.f32()[:, hs], xm_.f32()[:, hs], ALU.add, r=[tmpf.g(half * 2048, (half + 1) * 2048), xm_.g()], w=[ot.g(half * 2048, (half + 1) * 2048)])
            if dbg and i == 0:
                dump("dbg_xo", ot.f32(), [128, 1024], r=[ot.g()])
            act(SINK, ot.f32(), AF.Square, r=[ot.g()], w=[small.g()], accum_out=sm(SM_SS3))
            rstd_chain(SM_SS3, SM_LN3, SM_RSTD3, D, 1e-6)
            stt(ot.f32(), ot.f32(), sm(SM_RSTD3), nf_bc.f32(), ALU.mult, ALU.mult, r=[ot.g(), small.g(), nf_bc.g()], w=[ot.g()])
            dma('sp', out_d.ap()[b, t0:t0 + 128, :], ot.f32(), r=[ot.g()], w=[f"out{b}_{i}"])

        def drain(q, n=None):
            k = 0
            while q and (n is None or k < n):
                P.add(*q.pop(0))
                k += 1

        P.capture = []
        stageX(0)
        q = P.capture
        P.capture = None
        drain(q)
        for i in range(ntiles):
            if i + 1 < ntiles:
                P.capture = []
                stageX(i + 1)
                q = P.capture
                P.capture = None
            else:
                q = []
            stageY(i, lambda n: drain(q, n))
            drain(q)

    P.emit(nc, stack)
    stack.close()
    return nc, list(dbg_d.keys())


def _prep_inputs(inputs):
    f = np.float32
    x = np.asarray(inputs['x'], f)
    c = np.asarray(inputs['c'], f)
    cst = _host_consts()
    rep = lambda v, n=128: np.ascontiguousarray(np.broadcast_to(np.asarray(v, f).reshape(1, -1), (n, np.asarray(v).size)))
    nw = np.concatenate([np.asarray(inputs['norm_mix_w'], f).reshape(-1), np.asarray(inputs['norm_ffn_w'], f).reshape(-1),
                         np.asarray(inputs['norm_final_w'], f).reshape(-1)])
    lam = np.concatenate([np.asarray(inputs[k], f).reshape(-1) for k in ('lambda_q1', 'lambda_k1', 'lambda_q2', 'lambda_k2')])
    sk1 = np.asarray(inputs['sub_keys_1'], f)[0]
    sk2 = np.asarray(inputs['sub_keys_2'], f)[0]
    subkT = np.zeros((128, 16, 128), f)
    for h in range(8):
        subkT[:, h * 2 + 0, :] = sk1[h].T
        subkT[:, h * 2 + 1, :] = sk2[h].T
    shared = {
        'w_ada': np.ascontiguousarray(np.asarray(inputs['w_ada'], f)[0]),
        'b_ada_bc': rep(inputs['b_ada']),
        'nw_bc': rep(nw),
        'w_in': np.ascontiguousarray(np.asarray(inputs['w_in'], f)[0]),
        'w_out': np.ascontiguousarray(np.asarray(inputs['w_out'], f)[0]),
        'w_query': np.ascontiguousarray(np.asarray(inputs['w_query'], f)[0]),
        'pool_w': np.ascontiguousarray(np.asarray(inputs['pool_w'], f)[0]),
        'pool_scaleT': np.ascontiguousarray(np.asarray(inputs['pool_scale'], f).reshape(8, 128).T),
        'subkT': np.ascontiguousarray(subkT.reshape(128, 2048)),
        'e_down': np.ascontiguousarray(np.asarray(inputs['expert_down'], f)[0]),
        'e_up': np.ascontiguousarray(np.asarray(inputs['expert_up'], f)[0]),
        'lam_bc': rep(lam),
        'sublnT': np.ascontiguousarray(np.asarray(inputs['subln_w'], f).reshape(128, 1)),
        'relb_bc': rep(np.asarray(inputs['rel_bias'], f).reshape(-1)),
        'masks': np.ascontiguousarray(cst['masks'].reshape(128, -1)),
        'negmask': cst['negmask'],
        'band': np.ascontiguousarray(cst['band'].reshape(128, -1)),
        'ident': cst['ident'],
        'iota16': cst['iota16'],
        'sel0': cst['sel0'],
    }
    in_maps = []
    for core in range(NCORES):
        m = dict(shared)
        m['x'] = np.ascontiguousarray(x[core * NB:(core + 1) * NB])
        cc = c[core * NB:(core + 1) * NB]
        m['cT'] = np.ascontiguousarray(cc.reshape(NB, KC, 128).transpose(2, 0, 1).reshape(128, NB * KC))
        in_maps.append(m)
    return in_maps


_CACHE = {}


def kernel(**inputs):
    in_maps = _prep_inputs(inputs)
    if 'nc' not in _CACHE:
        _CACHE['nc'] = build()[0]
    nc = _CACHE['nc']
    res = run_bass_kernel_spmd(nc, in_maps, core_ids=list(range(NCORES)))
    out = np.concatenate([np.asarray(r['out']) for r in res.results], axis=0)
    return out.astype(np.float32)
```

```python
import os
import math
import contextlib
import numpy as np
import concourse.bass as bass
import concourse.mybir as mybir
from concourse.bass_utils import run_bass_kernel_spmd

dt = mybir.dt
AF = mybir.ActivationFunctionType
ALU = mybir.AluOpType
AX = mybir.AxisListType
F32, BF16, U32 = dt.float32, dt.bfloat16, dt.uint32

NCORES = 8
NB = 2
S = 2048
NT = 16
D = 1024
KC = 8
H = 8
DIN = 6144
NEXP = 16384
OFF = 8.0
NEG = -1.0e30
G = 512


class Prog:
    def __init__(self):
        self.ops = []
        self.last_w = {}
        self.readers = {}
        self.capture = None

    def add(self, eng, fn, r=(), w=(), dma=False):
        if self.capture is not None:
            self.capture.append((eng, fn, r, w, dma))
            return None
        i = len(self.ops)
        deps = set()
        rk = _flat(r)
        wk = _flat(w)
        for k in rk:
            lw = self.last_w.get(k)
            if lw is not None:
                deps.add(lw)
        for k in wk:
            lw = self.last_w.get(k)
            if lw is not None:
                deps.add(lw)
            deps.update(self.readers.get(k, ()))
        deps.discard(i)
        for k in rk:
            self.readers.setdefault(k, []).append(i)
        for k in wk:
            self.last_w[k] = i
            self.readers[k] = []
        self.ops.append(dict(eng=eng, fn=fn, deps=deps, dma=dma, sig=None, need=False, pre=None))
        return i

    def emit(self, nc, stack):
        ops = self.ops
        EPOCH = 30000
        NSLOT = {'sp': 24, 'pool': 24, 'act': 8}
        for o in ops:
            for d in o['deps']:
                p = ops[d]
                if p['eng'] == 'pe' and o['eng'] == 'pe' and not p['dma'] and not o['dma']:
                    continue
                p['need'] = True
        cnt = {e: 0 for e in ('pe', 'act', 'dve', 'pool', 'sp')}
        esems = {e: [] for e in cnt}
        dsems = {q: [stack.enter_context(nc.semaphore(f"d_{q}_{i}")) for i in range(n)] for q, n in NSLOT.items()}
        duse = {q: [0] * n for q, n in NSLOT.items()}
        dnext = {q: 0 for q in NSLOT}
        for o in ops:
            e = o['eng']
            if o['dma']:
                s = dnext[e]
                dnext[e] = (s + 1) % NSLOT[e]
                if duse[e][s] > 0:
                    o['pre'] = (dsems[e][s], 16 * duse[e][s])
                duse[e][s] += 1
                o['sig'] = (dsems[e][s], 16 * duse[e][s])
            elif o['need']:
                ep = cnt[e] // EPOCH
                while len(esems[e]) <= ep:
                    esems[e].append(stack.enter_context(nc.semaphore(f"e_{e}_{len(esems[e])}")))
                cnt[e] += 1
                o['sig'] = (esems[e][ep], cnt[e] - ep * EPOCH)
        by_eng = {e: [o for o in ops if o['eng'] == e] for e in cnt}
        final_waits = []
        for q in NSLOT:
            for s in range(NSLOT[q]):
                if duse[q][s] > 0:
                    final_waits.append((dsems[q][s], 16 * duse[q][s]))

        def run(ename, eng):
            waited = {}
            for o in by_eng[ename]:
                needs = {}
                for d in o['deps']:
                    p = ops[d]
                    if p['eng'] == 'pe' and ename == 'pe' and not p['dma'] and not o['dma']:
                        continue
                    sem, val = p['sig']
                    key = id(sem)
                    if needs.get(key, (None, 0))[1] < val:
                        needs[key] = (sem, val)
                if o['pre'] is not None:
                    sem, val = o['pre']
                    key = id(sem)
                    if needs.get(key, (None, 0))[1] < val:
                        needs[key] = (sem, val)
                for key, (sem, val) in needs.items():
                    if waited.get(key, 0) < val:
                        eng.wait_ge(sem, val)
                        waited[key] = val
                inst = o['fn'](eng)
                if o['sig'] is not None:
                    inst.then_inc(o['sig'][0], 16 if o['dma'] else 1)
            if ename == 'sp':
                for sem, val in final_waits:
                    eng.wait_ge(sem, val)

        with nc.Block() as block:
            @block.tensor
            def _(e):
                run('pe', e)

            @block.scalar
            def _(e):
                run('act', e)

            @block.vector
            def _(e):
                run('dve', e)

            @block.gpsimd
            def _(e):
                run('pool', e)

            @block.sync
            def _(e):
                run('sp', e)


def _flat(x):
    out = []
    for k in x:
        if isinstance(k, (list, tuple, set, range)):
            out.extend(_flat(k))
        else:
            out.append(k)
    return out


class Buf:
    def __init__(self, arena, off, nbytes):
        assert off % 4 == 0 and nbytes % 4 == 0
        self.A, self.off, self.nbytes = arena, off, nbytes

    def g(self, lo=0, hi=None):
        hi = self.nbytes if hi is None else hi
        return range((self.off + lo) // G, (self.off + hi + G - 1) // G)

    def f32(self):
        return self.A[:, self.off // 4:(self.off + self.nbytes) // 4]

    def bf(self):
        return self.A[:, self.off // 4:(self.off + self.nbytes) // 4].bitcast(BF16)

    def u32(self):
        return self.A[:, self.off // 4:(self.off + self.nbytes) // 4].bitcast(U32)

    def sub(self, lo, n):
        return Buf(self.A, self.off + lo, n)


class Alloc:
    def __init__(self, arena, base, limit):
        self.A, self.p, self.limit = arena, base, limit

    def get(self, nbytes):
        nb = (nbytes + G - 1) // G * G
        b = Buf(self.A, self.p, (nbytes + 3) // 4 * 4)
        self.p += nb
        assert self.p <= self.limit, (self.p, self.limit)
        return b


def mk(ap, pattern, extra_off=0):
    return bass.AP(tensor=ap.tensor, offset=ap.offset + extra_off, ap=[list(ap.ap[0])] + [list(p) for p in pattern])


def _t5_bucket(d):
    d = np.maximum(d, 0)
    x = np.maximum(d, 1).astype(np.float32) / np.float32(16)
    large = 16 + (np.log(x).astype(np.float32) / np.float32(math.log(128 / 16)) * np.float32(16)).astype(np.int32)
    large = np.minimum(large, 31)
    return np.where(d < 16, d, large)


def _host_consts():
    kk = np.arange(128)[:, None]
    qq = np.arange(256)[None, :]
    dist = qq - kk
    valid = dist >= 0
    bk = _t5_bucket(dist)
    masks = np.zeros((128, 32, 256), np.float32)
    for b in range(32):
        masks[:, b, :] = (valid & (bk == b)).astype(np.float32)
    negmask = np.where(valid, 0.0, NEG).astype(np.float32)
    band = np.zeros((128, 12, 128), np.float32)
    s = np.arange(128)[:, None]
    t = np.arange(128)[None, :]
    for gi, w in enumerate((2, 4, 8, 16)):
        cnt0 = np.minimum(t + 1, w).astype(np.float32)
        band[:, gi * 3 + 0, :] = ((s <= t) & (s > t - w)) / cnt0 - (s == t)
        band[:, gi * 3 + 1, :] = ((s <= t) & (s > t - w)) / np.float32(w) - (s == t)
        band[:, gi * 3 + 2, :] = (s > 128 + t - w) / np.float32(w)
    ident = np.eye(128, dtype=np.float32)
    iota16 = np.broadcast_to(np.arange(16, dtype=np.float32)[None, :], (128, 16)).copy()
    sel0 = np.zeros((64, 128), np.float32)
    sel0[0, :] = 1.0
    sel0[32, :] = 1.0
    return dict(masks=masks, negmask=negmask, band=band, ident=ident, iota16=iota16, sel0=sel0)


def build(stage=99, dbg=False):
    nc = bass.Bass("TRN2", target_bir_lowering=False)
    P = Prog()

    def din(name, shape, dtype=F32):
        return nc.dram_tensor(name, list(shape), dtype, kind="ExternalInput")

    x_d = din("x", [NB, S, D])
    cT_d = din("cT", [128, NB * KC])
    wada_d = din("w_ada", [D, DIN])
    bada_d = din("b_ada_bc", [128, DIN])
    nw_d = din("nw_bc", [128, 3 * D])
    win_d = din("w_in", [D, DIN])
    wout_d = din("w_out", [D, D])
    wqry_d = din("w_query", [D, 2048])
    poolw_d = din("pool_w", [4, 256, 256])
    psc_d = din("pool_scaleT", [128, 8])
    subk_d = din("subkT", [128, 16 * 128])
    edown_d = din("e_down", [NEXP, D])
    eup_d = din("e_up", [NEXP, D])
    lam_d = din("lam_bc", [128, 256])
    subln_d = din("sublnT", [128, 1])
    relb_d = din("relb_bc", [128, 256])
    masks_d = din("masks", [128, 32 * 256])
    negm_d = din("negmask", [128, 256])
    band_d = din("band", [128, 12 * 128])
    ident_d = din("ident", [128, 128])
    iota_d = din("iota16", [128, 16])
    sel0_d = din("sel0", [64, 128])
    out_d = nc.dram_tensor("out", [NB, S, D], F32, kind="ExternalOutput")
    etab_d = nc.dram_tensor("etab", [NEXP, 2 * D], BF16, kind="Internal")
    dbg_d = {}

    def dbg_out(name, shape):
        dbg_d[name] = nc.dram_tensor(name, list(shape), F32, kind="ExternalOutput")
        return dbg_d[name]

    stack = contextlib.ExitStack()
    TOT = 212480
    arena = stack.enter_context(nc.sbuf_tensor("arena", [128, TOT // 4], F32))
    banks = [stack.enter_context(nc.psum_tensor(f"B{i}", [128, 512], F32)) for i in range(8)]
    BK = [f"B{i}" for i in range(8)]

    al = Alloc(arena, 0, TOT)
    ident_f = al.get(512)
    ident_b = al.get(256)
    ones_b = al.get(256)
    ones_f = al.get(512)
    sel0 = al.get(512)
    relb = al.get(1024)
    rb31m = al.get(32)
    lamv = al.get(1024)
    small = al.get(512)
    pscT = al.get(32)
    band = al.get(12 * 128 * 2)
    subkT = al.get(16 * 128 * 2)
    iota16 = al.get(64)
    sink = al.get(64)
    nf_bc = al.get(4096)
    sh1 = al.get(4096)
    a1 = al.get(4096)
    g1 = al.get(4096)
    sh2 = al.get(4096)
    a2 = al.get(4096)
    g2 = al.get(4096)
    hT = al.get(KC * S * 2)
    mT = al.get(KC * S * 2)
    wout = al.get(KC * D * 2)
    work_base = al.p
    SM = small.f32()
    (SM_S1, SM_S2, SM_E1, SM_E2, SM_NLAM, SM_WSUB, SM_SS, SM_RSTD, SM_LN, SM_SS2, SM_RSTD2, SM_SS3, SM_RSTD3,
     SM_LN2, SM_LN3, SM_SUBLN) = range(16)

    def sm(i):
        return SM[:, i:i + 1]

    def smg(i):
        return small.g()

    def dma(q, out, in_, r, w):
        P.add(q, lambda e: e.dma_start(out=out, in_=in_), r=r, w=w, dma=True)

    def mm(out, lhsT, rhs, start, stop, r, w, **kw):
        P.add('pe', lambda e: e.matmul(out, lhsT, rhs, start=start, stop=stop, **kw), r=r, w=w)

    def tr(out, in_, ident, r, w):
        P.add('pe', lambda e: e.transpose(out, in_, ident), r=r, w=w)

    def act(out, in_, func, r, w, bias=None, scale=None, accum_out=None, eng='act'):
        def f(e):
            kw = {}
            if bias is not None:
                kw['bias'] = bias
            if scale is not None:
                kw['scale'] = scale
            if accum_out is not None:
                kw['accum_out'] = accum_out
            return e.activation(out=out, in_=in_, func=func, **kw)
        P.add('act', f, r=r, w=w)

    def tt(out, in0, in1, op, r, w, eng='dve'):
        P.add(eng, lambda e: e.tensor_tensor(out=out, in0=in0, in1=in1, op=op), r=r, w=w)

    def ts(out, in0, s1, s2, op0, op1, r, w, eng='dve'):
        if op1 is None:
            P.add(eng, lambda e: e.tensor_scalar(out=out, in0=in0, scalar1=s1, scalar2=None, op0=op0), r=r, w=w)
        else:
            P.add(eng, lambda e: e.tensor_scalar(out=out, in0=in0, scalar1=s1, scalar2=s2, op0=op0, op1=op1), r=r, w=w)

    def stt(out, in0, scalar, in1, op0, op1, r, w):
        P.add('dve', lambda e: e.scalar_tensor_tensor(out=out, in0=in0, scalar=scalar, in1=in1, op0=op0, op1=op1), r=r, w=w)

    def cp(out, in_, r, w, eng='dve'):
        if eng == 'act':
            P.add('act', lambda e: e.copy(out=out, in_=in_), r=r, w=w)
        else:
            P.add(eng, lambda e: e.tensor_copy(out=out, in_=in_), r=r, w=w)

    def ttr(out, in0, in1, accum_out, r, w):
        P.add('dve', lambda e: e.scalar_tensor_tensor(out=out, in0=in0, scalar=1.0, in1=in1, op0=ALU.mult, op1=ALU.mult,
                                                     accum_out=accum_out), r=r, w=w)

    def tred(out, in_, op, r, w):
        P.add('dve', lambda e: e.tensor_reduce(out=out, in_=in_, axis=AX.X, op=op), r=r, w=w)

    def memset(ap, val, r, w, eng='dve'):
        P.add(eng, lambda e: e.memset(ap, val), r=r, w=w)

    def rstd_chain(ss_i, ln_i, rstd_i, n, eps):
        ts(sm(ln_i), sm(ss_i), 1.0 / n, eps, ALU.mult, ALU.add, r=[small.g()], w=[small.g()])
        act(sm(ln_i), sm(ln_i), AF.Ln, r=[small.g()], w=[small.g()])
        act(sm(rstd_i), sm(ln_i), AF.Exp, r=[small.g()], w=[small.g()], scale=-0.5)

    def dump(name, ap_sb, shape, r):
        if name in dbg_d:
            return
        d = dbg_out(name, shape)
        dma('sp', d.ap(), ap_sb, r=r, w=[name])

    dma('sp', ident_f.f32(), ident_d.ap(), r=[], w=[ident_f.g()])
    dma('sp', sel0.f32()[0:64, :], sel0_d.ap(), r=[], w=[sel0.g()])
    dma('sp', relb.f32(), relb_d.ap(), r=[], w=[relb.g()])
    dma('sp', lamv.f32(), lam_d.ap(), r=[], w=[lamv.g()])
    dma('sp', pscT.f32(), psc_d.ap(), r=[], w=[pscT.g()])
    dma('sp', iota16.f32(), iota_d.ap(), r=[], w=[iota16.g()])
    dma('sp', nf_bc.f32(), nw_d.ap()[:, 2 * D:3 * D], r=[], w=[nf_bc.g()])
    dma('sp', sm(SM_SUBLN), subln_d.ap(), r=[], w=[small.g()])
    dma('pool', band.bf(), band_d.ap(), r=[], w=[band.g()])
    dma('pool', subkT.bf(), subk_d.ap(), r=[], w=[subkT.g()])
    ETAB_JOBS = [(ti, r0) for ti in range(2) for r0 in range(0, NEXP, 1024)]
    ETAB_KEYS = [f"etab{ti}_{r0}" for (ti, r0) in ETAB_JOBS]

    def precast(n):
        for _ in range(n):
            if not ETAB_JOBS:
                return
            ti, r0 = ETAB_JOBS.pop(0)
            tbl = (edown_d, eup_d)[ti]
            dma('pool', etab_d.ap()[r0:r0 + 1024, ti * D:(ti + 1) * D], tbl.ap()[r0:r0 + 1024, :], r=[], w=[f"etab{ti}_{r0}"])

    cp(ident_b.bf(), ident_f.f32(), r=[ident_f.g()], w=[ident_b.g()])
    memset(ones_b.bf(), 1.0, r=[], w=[ones_b.g()])
    memset(ones_f.f32(), 1.0, r=[], w=[ones_f.g()])
    ts(rb31m.f32(), relb.f32()[:, 31 * 8:32 * 8], -OFF, None, ALU.add, None, r=[relb.g()], w=[rb31m.g()])
    ts(sm(SM_WSUB), sm(SM_SUBLN), 0.8, None, ALU.mult, None, r=[small.g()], w=[small.g()])
    wa = Alloc(arena, work_base, TOT)
    junk64 = wa.get(256)
    LV = lamv.f32()
    ttr(junk64.f32(), LV[:, 0:64], LV[:, 64:128], sm(SM_S1), r=[lamv.g()], w=[junk64.g(), small.g()])
    ttr(junk64.f32(), LV[:, 128:192], LV[:, 192:256], sm(SM_S2), r=[lamv.g()], w=[junk64.g(), small.g()])
    act(sm(SM_E1), sm(SM_S1), AF.Exp, r=[small.g()], w=[small.g()])
    act(sm(SM_E2), sm(SM_S2), AF.Exp, r=[small.g()], w=[small.g()])
    tt(sm(SM_NLAM), sm(SM_E2), sm(SM_E1), ALU.subtract, r=[small.g()], w=[small.g()])
    ts(sm(SM_NLAM), sm(SM_NLAM), -0.2, None, ALU.add, None, r=[small.g()], w=[small.g()])
    bias_scr = nc.dram_tensor("bias_scr", [128, 2048], F32, kind="Internal")
    biasT = wa.get(8 * 1024)
    mk_chunk = wa.get(8 * 256 * 4)
    BT3 = biasT.f32().rearrange("p (h q) -> p h q", h=8)
    for h in range(8):
        dma('sp', BT3[:, h, :], negm_d.ap(), r=[], w=[biasT.g(h * 1024, (h + 1) * 1024)])
    for c in range(4):
        dma('sp', mk_chunk.f32(), masks_d.ap()[:, c * 2048:(c + 1) * 2048], r=[], w=[mk_chunk.g()])
        MC = mk_chunk.f32().rearrange("p (b q) -> p b q", b=8)
        for bb in range(8):
            b = c * 8 + bb
            for h in range(8):
                stt(BT3[:, h, :], MC[:, bb, :], relb.f32()[:, b * 8 + h:b * 8 + h + 1], BT3[:, h, :], ALU.mult, ALU.add,
                    r=[mk_chunk.g(), relb.g(), biasT.g(h * 1024, (h + 1) * 1024)], w=[biasT.g(h * 1024, (h + 1) * 1024)])
    ts(biasT.f32(), biasT.f32(), -OFF, None, ALU.add, None, r=[biasT.g()], w=[biasT.g()])
    dma('sp', bias_scr.ap(), biasT.f32(), r=[biasT.g()], w=["bias_scr"])
    if dbg:
        dump("dbg_bias", biasT.f32(), [128, 2048], r=[biasT.g()])
        dump("dbg_small", small.f32()[:, 0:16], [128, 16], r=[small.g()])

    hT3 = hT.bf().rearrange("p (k t) -> p k t", k=KC)
    mT3 = mT.bf().rearrange("p (k t) -> p k t", k=KC)

    def hT_g(c0, c1):
        return [hT.g(k * S * 2 + c0 * 2, k * S * 2 + c1 * 2) for k in range(KC)]

    def mT_g(k, c0, c1):
        return mT.g(k * S * 2 + c0 * 2, k * S * 2 + c1 * 2)

    BTb = banks[7][:].bitcast(BF16)

    for b in range(NB):
        wa = Alloc(arena, work_base, TOT)
        cact = wa.get(64)
        crep = wa.get(KC * 128 * 4)
        wblk = [wa.get(KC * 512 * 4) for _ in range(2)]
        bblk = [wa.get(2048) for _ in range(2)]
        mtmp = wa.get(2048)
        nwt = wa.get(4096)
        cin = wa.get(64)
        dma('sp', cin.f32()[:, 0:KC], cT_d.ap()[:, b * KC:(b + 1) * KC], r=[], w=[cin.g()])
        act(cact.f32()[:, 0:KC], cin.f32()[:, 0:KC], AF.Silu, r=[cin.g()], w=[cact.g()])
        cp(crep.f32().rearrange("p (k m) -> p k m", k=KC), mk(cact.f32(), [[1, KC], [0, 128]]), r=[cact.g()], w=[crep.g()])
        CR = crep.f32().rearrange("p (k m) -> p k m", k=KC)
        wada_v = wada_d.ap().rearrange("(k p) n -> p k n", p=128)
        dsts = [sh1, sh1, a1, a1, g1, g1, sh2, sh2, a2, a2, g2, g2]
        for nb_ in range(12):
            wb = wblk[nb_ % 2]
            bb_ = bblk[nb_ % 2]
            dma('sp', wb.f32().rearrange("p (k n) -> p k n", k=KC), wada_v[:, :, nb_ * 512:(nb_ + 1) * 512], r=[], w=[wb.g()])
            dma('sp', bb_.f32(), bada_d.ap()[:, nb_ * 512:(nb_ + 1) * 512], r=[], w=[bb_.g()])
            WB = wb.f32().rearrange("p (k n) -> p k n", k=KC)
            for k in range(KC):
                mm(banks[6][:], CR[:, k, :], WB[:, k, :], k == 0, k == KC - 1, r=[crep.g(), wb.g()], w=[BK[6]])
            dst = dsts[nb_]
            half = nb_ % 2
            dst_ap = dst.f32()[:, half * 512:(half + 1) * 512]
            dg_ = dst.g(half * 2048, (half + 1) * 2048)
            if nb_ in (2, 3, 8, 9):
                which = 0 if nb_ in (2, 3) else 1
                dma('sp', nwt.f32()[:, 0:512], nw_d.ap()[:, which * D + half * 512: which * D + (half + 1) * 512], r=[], w=[nwt.g()])
                tt(mtmp.f32(), banks[6][:], bb_.f32(), ALU.add, r=[BK[6], bb_.g()], w=[mtmp.g()])
                stt(dst_ap, mtmp.f32(), 1.0, nwt.f32()[:, 0:512], ALU.add, ALU.mult, r=[mtmp.g(), nwt.g()], w=[dg_])
            else:
                tt(dst_ap, banks[6][:], bb_.f32(), ALU.add, r=[BK[6], bb_.g()], w=[dg_])
        if dbg and b == 0:
            dump("dbg_a1", a1.f32(), [128, 1024], r=[a1.g()])
            dump("dbg_g2", g2.f32(), [128, 1024], r=[g2.g()])

        wa = Alloc(arena, work_base, TOT)
        xt = [wa.get(4096) for _ in range(2)]
        tmpf = wa.get(4096)
        junkb = wa.get(2048)
        hbf = wa.get(2048)
        for i in range(NT):
            xb = xt[i % 2]
            dma('sp', xb.f32(), x_d.ap()[b, i * 128:(i + 1) * 128, :], r=[], w=[xb.g()])
            act(junkb.bf(), xb.f32(), AF.Square, r=[xb.g()], w=[junkb.g(), small.g()], accum_out=sm(SM_SS))
            rstd_chain(SM_SS, SM_LN, SM_RSTD, D, 1e-6)
            stt(tmpf.f32(), xb.f32(), sm(SM_RSTD), a1.f32(), ALU.mult, ALU.mult, r=[xb.g(), small.g(), a1.g()], w=[tmpf.g()])
            tt(hbf.bf(), tmpf.f32(), sh1.f32(), ALU.add, r=[tmpf.g(), sh1.g()], w=[hbf.g()])
            for k in range(KC):
                tr(BTb[:, k * 128:(k + 1) * 128], hbf.bf()[:, k * 128:(k + 1) * 128], ident_b.bf(), r=[hbf.g(), ident_b.g()], w=[BK[7]])
            cp(hT3[:, :, i * 128:(i + 1) * 128], BTb.rearrange("p (k t) -> p k t", k=KC), r=[BK[7]], w=hT_g(i * 128, (i + 1) * 128), eng='act')
        if dbg and b == 0:
            wa2 = Alloc(arena, wa.p, TOT)
            dtmp = wa2.get(8192)
            cp(dtmp.f32(), hT3[:, 0, :], r=hT_g(0, S), w=[dtmp.g()])
            dump("dbg_hT0", dtmp.f32(), [128, 2048], r=[dtmp.g()])
        if stage <= 1:
            continue

        win_v = win_d.ap().rearrange("(k p) n -> p k n", p=128)
        wa = Alloc(arena, work_base, TOT)
        wp = wa.get(KC * 256 * 2)
        wgp = wa.get(KC * 256 * 2)
        pw = wa.get(2 * 256 * 2)
        pg = wa.get(NT * 256 * 2)
        pT = [wa.get(512 * 2) for _ in range(2)]
        sg = wa.get(512 * 4)
        WP = wp.bf().rearrange("p (k n) -> p k n", k=KC)
        WGP = wgp.bf().rearrange("p (k n) -> p k n", k=KC)
        PW = pw.bf().rearrange("p (c n) -> p c n", c=2)
        PG = pg.bf().rearrange("p (i n) -> p i n", i=NT)
        BAND = band.bf().rearrange("p (j n) -> p j n", j=12)
        for g in range(4):
            dma('pool', WP, win_v[:, :, 3072 + g * 256:3072 + (g + 1) * 256], r=[], w=[wp.g()])
            dma('pool', WGP, win_v[:, :, 5120 + g * 256:5120 + (g + 1) * 256], r=[], w=[wgp.g()])
            dma('pool', PW, poolw_d.ap()[g].rearrange("(c p) n -> p c n", p=128), r=[], w=[pw.g()])
            for i in range(NT):
                for k in range(KC):
                    mm(banks[7][:, (i % 2) * 256:(i % 2 + 1) * 256], hT3[:, k, i * 128:(i + 1) * 128], WP[:, k, :], k == 0, k == KC - 1,
                       r=[hT_g(i * 128, (i + 1) * 128), wp.g()], w=[BK[7]])
                if i % 2 == 1:
                    cp(pg.bf()[:, (i - 1) * 256:(i + 1) * 256], banks[7][:], r=[BK[7]], w=[pg.g((i - 1) * 512, (i + 1) * 512)], eng='act')
            for c in range(4):
                for cc in range(2):
                    for il in range(4):
                        i = c * 4 + il
                        cur = g * 3 + (0 if i == 0 else 1)
                        mm(banks[cc][:, il * 128:(il + 1) * 128], PG[:, i, cc * 128:(cc + 1) * 128], BAND[:, cur, :], True, i == 0,
                           r=[pg.g(i * 512, (i + 1) * 512), band.g()], w=[BK[cc]])
                        if i > 0:
                            mm(banks[cc][:, il * 128:(il + 1) * 128], PG[:, i - 1, cc * 128:(cc + 1) * 128], BAND[:, g * 3 + 2, :], False, True,
                               r=[pg.g((i - 1) * 512, i * 512), band.g()], w=[BK[cc]])
                    cp(pT[cc].bf(), banks[cc][:], r=[BK[cc]], w=[pT[cc].g()], eng='act')
                for ec in range(2):
                    kch = g * 2 + ec
                    mm(banks[2][:], PW[:, 0, ec * 128:(ec + 1) * 128], pT[0].bf(), True, False, r=[pw.g(), pT[0].g()], w=[BK[2]])
                    mm(banks[2][:], PW[:, 1, ec * 128:(ec + 1) * 128], pT[1].bf(), False, True, r=[pw.g(), pT[1].g()], w=[BK[2]])
                    for k in range(KC):
                        mm(banks[3][:], WGP[:, k, ec * 128:(ec + 1) * 128], hT3[:, k, c * 512:(c + 1) * 512], k == 0, k == KC - 1,
                           r=[wgp.g(), hT_g(c * 512, (c + 1) * 512)], w=[BK[3]])
                    act(sg.f32(), banks[3][:], AF.Sigmoid, r=[BK[3]], w=[sg.g()])
                    stt(mT3[:, kch, c * 512:(c + 1) * 512], banks[2][:], pscT.f32()[:, kch:kch + 1], sg.f32(), ALU.mult, ALU.mult,
                        r=[BK[2], pscT.g(), sg.g()], w=[mT_g(kch, c * 512, (c + 1) * 512)])
        if dbg and b == 0 and stage == 2:
            wa2 = Alloc(arena, wa.p, TOT)
            dtmp = wa2.get(8192)
            for kk_ in (0, 5):
                cp(dtmp.f32(), mT3[:, kk_, :], r=[mT.g()], w=[dtmp.g()])
                dump(f"dbg_mT{kk_}", dtmp.f32(), [128, 2048], r=[dtmp.g()])
        if stage <= 2:
            continue

        wa = Alloc(arena, work_base, TOT)
        wq2 = [wa.get(KC * 128 * 2) for _ in range(2)]
        wk2_ = [wa.get(KC * 128 * 2) for _ in range(2)]
        wv2 = [wa.get(KC * 128 * 2) for _ in range(2)]
        wg2 = [wa.get(KC * 128 * 2) for _ in range(2)]
        QTz = [wa.get(S * 2) for _ in range(2)]
        KT = wa.get(S * 2)
        Vb = wa.get(NT * 128 * 2)
        sgT = wa.get(S * 2)
        PT = [[wa.get(512 * 2) for _ in range(2)] for _ in range(2)]
        ntmp = [wa.get(256 * 4) for _ in range(2)]
        O1s = wa.get(2048)
        O2s = wa.get(2048)
        lnz = wa.get(2048)
        rz = wa.get(2048)
        rz2 = wa.get(2048)
        sq = wa.get(2048)
        t1 = wa.get(2048)
        rs = wa.get(2048)
        biasT = wa.get(8 * 1024)
        BT3 = biasT.f32().rearrange("p (h q) -> p h q", h=8)
        dma('sp', biasT.f32(), bias_scr.ap(), r=["bias_scr"], w=[biasT.g()])
        V3 = Vb.bf().rearrange("p (i n) -> p i n", i=NT)
        nheads = H if stage > 3 or not dbg else 1
        memset(QTz[0].bf()[64:128, :], 0.0, r=[], w=[QTz[0].g()])
        memset(QTz[1].bf()[0:64, :], 0.0, r=[], w=[QTz[1].g()])
        for h in range(nheads):
            wq, wk_, wv, wg = wq2[h % 2], wk2_[h % 2], wv2[h % 2], wg2[h % 2]
            WQ = wq.bf().rearrange("p (k n) -> p k n", k=KC)
            WK = wk_.bf().rearrange("p (k n) -> p k n", k=KC)
            WV = wv.bf().rearrange("p (k n) -> p k n", k=KC)
            WG = wg.bf().rearrange("p (k n) -> p k n", k=KC)
            dma('pool', WQ, win_v[:, :, h * 128:(h + 1) * 128], r=[], w=[wq.g()])
            dma('pool', WK, win_v[:, :, 1024 + h * 128:1024 + (h + 1) * 128], r=[], w=[wk_.g()])
            dma('pool', WV, win_v[:, :, 2048 + h * 128:2048 + (h + 1) * 128], r=[], w=[wv.g()])
            dma('pool', WG, win_v[:, :, 4096 + h * 128:4096 + (h + 1) * 128], r=[], w=[wg.g()])
            precast(4)
            pj = 0
            for (W_, wb_, dst, mode) in ((WQ, wq, None, 'q'), (WK, wk_, KT, 'k'), (WG, wg, sgT, 'g')):
                for c in range(4):
                    bk = pj % 4
                    pj += 1
                    for k in range(KC):
                        mm(banks[bk][:], W_[:, k, :], hT3[:, k, c * 512:(c + 1) * 512], k == 0, k == KC - 1,
                           r=[wb_.g(), hT_g(c * 512, (c + 1) * 512)], w=[BK[bk]])
                    if mode == 'q':
                        for m_ in range(2):
                            o_ap = QTz[m_].bf()[m_ * 64:(m_ + 1) * 64, c * 512:(c + 1) * 512]
                            P.add('act', (lambda o_ap=o_ap, bk=bk, m_=m_: (lambda e: e.mul(out=o_ap, in_=banks[bk][m_ * 64:(m_ + 1) * 64, :], mul=0.125)))(),
                                  r=[BK[bk]], w=[QTz[m_].g(c * 1024, (c + 1) * 1024)])
                        continue
                    o_ap = dst.bf()[:, c * 512:(c + 1) * 512]
                    o_g = dst.g(c * 1024, (c + 1) * 1024)
                    if mode == 'k':
                        cp(o_ap, banks[bk][:], r=[BK[bk]], w=[o_g], eng='act')
                    else:
                        act(o_ap, banks[bk][:], AF.Sigmoid, r=[BK[bk]], w=[o_g])
            for i in range(NT):
                bk = (i // 4) % 4
                for k in range(KC):
                    mm(banks[bk][:, (i % 4) * 128:(i % 4 + 1) * 128], hT3[:, k, i * 128:(i + 1) * 128], WV[:, k, :], k == 0, k == KC - 1,
                       r=[hT_g(i * 128, (i + 1) * 128), wv.g()], w=[BK[bk]])
                if i % 4 == 3:
                    cp(Vb.bf()[:, (i - 3) * 128:(i + 1) * 128], banks[bk][:], r=[BK[bk]], w=[Vb.g((i - 3) * 256, (i + 1) * 256)], eng='act')

            def qk(c, j):
                col0 = max(0, j - 4 * c) * 128
                par = j % 2
                for m in range(2):
                    bk = m * 2 + par
                    mm(banks[bk][:, col0:512], KT.bf()[:, j * 128:(j + 1) * 128],
                       QTz[m].bf()[:, c * 512 + col0:(c + 1) * 512], True, True,
                       r=[KT.g(j * 256, (j + 1) * 256), QTz[m].g(c * 1024 + col0 * 2, (c + 1) * 1024)], w=[BK[bk]])

            for c in range(4):
                jmax = 4 * c + 3
                qk(c, 0)
                for j in range(jmax + 1):
                    if j + 1 <= jmax:
                        qk(c, j + 1)
                    col0 = max(0, j - 4 * c) * 128
                    par = j % 2
                    il_lo = max(0, j - 4 * c)
                    il_hi = min(3, j + 1 - 4 * c)
                    far0 = max(0, j + 2 - 4 * c) * 128
                    for m in range(2):
                        bk = m * 2 + par
                        pt = PT[m][par]
                        if il_hi >= il_lo and il_hi >= 0:
                            n0, n1 = il_lo * 128, (il_hi + 1) * 128
                            b0 = (4 * c + il_lo - j) * 128
                            nn = n1 - n0
                            tt(ntmp[m].f32()[:, 0:nn], banks[bk][:, n0:n1], BT3[:, h, b0:b0 + nn], ALU.add,
                               r=[BK[bk], biasT.g(h * 1024, (h + 1) * 1024)], w=[ntmp[m].g()])
                            act(pt.bf()[:, n0:n1], ntmp[m].f32()[:, 0:nn], AF.Exp, r=[ntmp[m].g()], w=[pt.g(n0 * 2, n1 * 2)])
                        if far0 < 512:
                            act(pt.bf()[:, far0:512], banks[bk][:, far0:512], AF.Exp, r=[BK[bk], rb31m.g()], w=[pt.g(far0 * 2, 1024)],
                                bias=rb31m.f32()[:, h:h + 1])
                        mm(banks[4 + m][:, col0:512], V3[:, j, :], pt.bf()[:, col0:512], j == 0, j == jmax,
                           r=[Vb.g(j * 256, (j + 1) * 256), pt.g(col0 * 2, 1024)], w=[BK[4 + m]], skip_group_check=True)
                        mm(banks[6 + m][:, col0:512], ones_b.bf(), pt.bf()[:, col0:512], j == 0, j == jmax,
                           r=[ones_b.g(), pt.g(col0 * 2, 1024)], w=[BK[6 + m]], skip_group_check=True)
                cs = slice(c * 512, (c + 1) * 512)
                cp(O1s.f32(), banks[4][:], r=[BK[4]], w=[O1s.g()], eng='act')
                cp(O2s.f32(), banks[5][:], r=[BK[5]], w=[O2s.g()], eng='act')
                act(lnz.f32(), banks[6][:], AF.Ln, r=[BK[6]], w=[lnz.g()])
                act(rz.f32(), lnz.f32(), AF.Exp, r=[lnz.g()], w=[rz.g()], scale=-1.0)
                act(lnz.f32(), banks[7][:], AF.Ln, r=[BK[7]], w=[lnz.g()])
                act(rz2.f32(), lnz.f32(), AF.Exp, r=[lnz.g()], w=[rz2.g()], scale=-1.0)
                tt(t1.f32(), O1s.f32(), rz.f32(), ALU.mult, r=[O1s.g(), rz.g()], w=[t1.g()])
                tt(O2s.f32(), O2s.f32(), rz2.f32(), ALU.mult, r=[O2s.g(), rz2.g()], w=[O2s.g()])
                stt(t1.f32(), O2s.f32(), sm(SM_NLAM), t1.f32(), ALU.mult, ALU.add, r=[t1.g(), O2s.g(), small.g()], w=[t1.g()])
                act(sq.bf()[:, 0:512], t1.f32(), AF.Square, r=[t1.g()], w=[sq.g()])
                mm(banks[6][:], ones_b.bf(), sq.bf()[:, 0:512], True, True, r=[ones_b.g(), sq.g()], w=[BK[6]])
                ts(rs.f32(), banks[6][:], 1.0 / 128, 1e-5, ALU.mult, ALU.add, r=[BK[6]], w=[rs.g()])
                act(rs.f32(), rs.f32(), AF.Ln, r=[rs.g()], w=[rs.g()])
                act(rs.f32(), rs.f32(), AF.Exp, r=[rs.g()], w=[rs.g()], scale=-0.5)
                tt(t1.f32(), t1.f32(), rs.f32(), ALU.mult, r=[t1.g(), rs.g()], w=[t1.g()])
                stt(t1.f32(), t1.f32(), sm(SM_WSUB), sgT.bf()[:, cs], ALU.mult, ALU.mult, r=[t1.g(), small.g(), sgT.g(c * 1024, (c + 1) * 1024)], w=[t1.g()])
                tt(mT3[:, h, cs], t1.f32(), mT3[:, h, cs], ALU.add, r=[t1.g(), mT_g(h, c * 512, (c + 1) * 512)], w=[mT_g(h, c * 512, (c + 1) * 512)])
        if dbg and b == 0 and stage == 3:
            wa2 = Alloc(arena, wa.p, TOT)
            dtmp = wa2.get(8192)
            cp(dtmp.f32(), mT3[:, 0, :], r=[mT.g()], w=[dtmp.g()])
            dump("dbg_mT0", dtmp.f32(), [128, 2048], r=[dtmp.g()])
        if stage <= 3:
            continue

        precast(64)
        WOUT3 = wout.bf().rearrange("p (k n) -> p k n", k=KC)
        dma('pool', WOUT3, wout_d.ap().rearrange("(k p) n -> p k n", p=128), r=[], w=[wout.g()])
        for k in range(KC):
            tt(WOUT3[:, k, :], WOUT3[:, k, :], g1.f32(), ALU.mult, r=[wout.g(k * 2048, (k + 1) * 2048), g1.g()], w=[wout.g(k * 2048, (k + 1) * 2048)])
        WQ3 = hT.bf().rearrange("p (k n) -> p k n", k=KC)
        dma('pool', WQ3, wqry_d.ap().rearrange("(k p) n -> p k n", p=128), r=[], w=[hT.g()])
        wa = Alloc(arena, work_base, TOT)
        xt5 = g1
        xm = [wa.get(4096) for _ in range(2)]
        h2 = [wa.get(2048) for _ in range(2)]
        eidu = [wa.get(512) for _ in range(2)]
        gate = [wa.get(512) for _ in range(2)]
        tmpf = a1
        ot = sh1
        ssb = wa.get(8192)
        wkb = ssb
        tmpfX = ssb.sub(0, 4096)
        junkbX = ssb.sub(4096, 2048)
        h2T = ssb.sub(0, 2048)
        qTs = ssb.sub(4096, 4096)
        v16 = wa.get(1024)
        ixu = wa.get(1024)
        ixf = wa.get(1024)
        c16, posu, pau, pbu, paf, pbf, i1s, i2s, eidf, scm, ee, actv, gl, wgt = [wa.get(512) for _ in range(14)]
        zz = wa.get(32)
        rzz = wa.get(32)
        dgb = [wa.get(256) for _ in range(4)]
        NBUF = (TOT - wa.p) // 4096
        assert NBUF >= 5, NBUF
        cbuf = [wa.get(4096) for _ in range(NBUF)]
        SINK = mk(sink.bf(), [[0, D]])
        H2T3 = h2T.bf().rearrange("p (k t) -> p k t", k=KC)
        QTS3 = qTs.bf().rearrange("p (q t) -> p q t", q=16)
        SUBK3 = subkT.bf().rearrange("p (q n) -> p q n", q=16)
        SS3 = ssb.f32().rearrange("p (q n) -> p q n", q=16)
        WK3 = wkb.f32().rearrange("p (q n) -> p q n", q=16)
        V16 = v16.f32().rearrange("p (q j) -> p q j", q=16)
        IXU = ixu.u32().rearrange("p (q j) -> p q j", q=16)
        ntiles = NT if not dbg else 2

        def gen(name, r, w, eng='dve', **kw):
            P.add(eng, lambda e: getattr(e, name)(**kw), r=r, w=w)

        def stageX(i):
            par = i % 2
            xm_, h2_, eidu_, gate_ = xm[par], h2[par], eidu[par], gate[par]
            t0 = i * 128
            dma('sp', xt5.f32(), x_d.ap()[b, t0:t0 + 128, :], r=[], w=[xt5.g()])
            for half in range(2):
                for k in range(KC):
                    mm(banks[2 + half][:], mT3[:, k, t0:t0 + 128], WOUT3[:, k, half * 512:(half + 1) * 512], k == 0, k == KC - 1,
                       r=[mT_g(k, t0, t0 + 128), wout.g()], w=[BK[2 + half]])
            for half in range(2):
                hs = slice(half * 512, (half + 1) * 512)
                tt(xm_.f32()[:, hs], banks[2 + half][:], xt5.f32()[:, hs], ALU.add, r=[BK[2 + half], xt5.g()], w=[xm_.g(half * 2048, (half + 1) * 2048)])
            act(junkbX.bf(), xm_.f32(), AF.Square, r=[xm_.g()], w=[junkbX.g(), small.g()], accum_out=sm(SM_SS2))
            rstd_chain(SM_SS2, SM_LN2, SM_RSTD2, D, 1e-6)
            for hf in range(2):
                hs = slice(hf * 512, (hf + 1) * 512)
                stt(tmpfX.f32()[:, hs], xm_.f32()[:, hs], sm(SM_RSTD2), a2.f32()[:, hs], ALU.mult, ALU.mult, r=[xm_.g(), small.g(), a2.g()], w=[tmpfX.g(hf * 2048, (hf + 1) * 2048)])
                tt(h2_.bf()[:, hs], tmpfX.f32()[:, hs], sh2.f32()[:, hs], ALU.add, r=[tmpfX.g(hf * 2048, (hf + 1) * 2048), sh2.g()], w=[h2_.g(hf * 1024, (hf + 1) * 1024)])
            for k in range(KC):
                tr(BTb[:, k * 128:(k + 1) * 128], h2_.bf()[:, k * 128:(k + 1) * 128], ident_b.bf(), r=[h2_.g(), ident_b.g()], w=[BK[7]])
            cp(h2T.bf(), BTb, r=[BK[7]], w=[h2T.g()], eng='act')
            for qc in range(16):
                bk = 2 + (qc // 4) % 2
                for k in range(KC):
                    mm(banks[bk][:, (qc % 4) * 128:(qc % 4 + 1) * 128], WQ3[:, k, qc * 128:(qc + 1) * 128], H2T3[:, k, :], k == 0, k == KC - 1,
                       r=[hT.g(), h2T.g()], w=[BK[bk]])
                if qc % 4 == 3:
                    cp(qTs.bf()[:, (qc - 3) * 128:(qc + 1) * 128], banks[bk][:], r=[BK[bk]], w=[qTs.g((qc - 3) * 256, (qc + 1) * 256)], eng='act')
            for qc in range(16):
                bk = 4 + qc // 4
                mm(banks[bk][:, (qc % 4) * 128:(qc % 4 + 1) * 128], QTS3[:, qc, :], SUBK3[:, qc, :], True, True,
                   r=[qTs.g(qc * 256, (qc + 1) * 256), subkT.g()], w=[BK[bk]])
            for q4 in range(4):
                cp(ssb.f32()[:, q4 * 512:(q4 + 1) * 512], banks[4 + q4][:], r=[BK[4 + q4]], w=[ssb.g(q4 * 2048, (q4 + 1) * 2048)], eng='act')
            if dbg and i == 0:
                dump("dbg_xm", xm_.f32(), [128, 1024], r=[xm_.g()])
                dump("dbg_s", ssb.f32(), [128, 2048], r=[ssb.g()])
            sg_ = lambda qc: ssb.g(qc * 512, (qc + 1) * 512)
            wg_ = lambda qc: wkb.g(qc * 512, (qc + 1) * 512)
            vk = lambda qc, hf: f"v16:{qc}:{hf}"
            ik = lambda qc, hf: f"ixu:{qc}:{hf}"
            ALLV = [vk(q_, h_) for q_ in range(16) for h_ in range(2)]
            ALLI = [ik(q_, h_) for q_ in range(16) for h_ in range(2)]
            for qc in range(16):
                gen('max', [sg_(qc), v16.g()], [vk(qc, 0)], out=V16[:, qc, 0:8], in_=SS3[:, qc, :])
            for qc in range(16):
                gen('max_index', [sg_(qc), vk(qc, 0), ixu.g()], [ik(qc, 0)], out=IXU[:, qc, 0:8], in_max=V16[:, qc, 0:8], in_values=SS3[:, qc, :])
            for qc in range(16):
                gen('match_replace', [sg_(qc), vk(qc, 0)], [wg_(qc)], out=WK3[:, qc, :], in_to_replace=V16[:, qc, 0:8], in_values=SS3[:, qc, :], imm_value=NEG)
            for qc in range(16):
                gen('max', [wg_(qc), v16.g()], [vk(qc, 1)], out=V16[:, qc, 8:16], in_=WK3[:, qc, :])
            for qc in range(16):
                gen('max_index', [wg_(qc), vk(qc, 1), ixu.g()], [ik(qc, 1)], out=IXU[:, qc, 8:16], in_max=V16[:, qc, 8:16], in_values=WK3[:, qc, :])
            cp(ixf.f32(), ixu.u32(), r=ALLI, w=[ixf.g()])
            cand, wk2 = ssb, wkb
            C4 = cand.f32().rearrange("p (h n) -> p h n", h=8)
            W4 = wk2.f32().rearrange("p (h n) -> p h n", h=8)
            vf = v16.f32()
            for hp in range(4):
                tt(mk(cand.f32(), [[256, 2], [16, 16], [1, 16]], hp * 512), mk(vf, [[32, 2], [1, 16], [0, 16]], hp * 64),
                   mk(vf, [[32, 2], [0, 16], [1, 16]], hp * 64 + 16), ALU.add, r=ALLV, w=[cand.g(hp * 2048, (hp + 1) * 2048)])
            C16 = c16.f32().rearrange("p (h j) -> p h j", h=8)
            POS = posu.u32().rearrange("p (h j) -> p h j", h=8)
            cg_ = lambda h_: cand.g(h_ * 1024, (h_ + 1) * 1024)
            w2g_ = lambda h_: wk2.g(h_ * 1024, (h_ + 1) * 1024)
            ck = lambda h_, hf: f"c16:{h_}:{hf}"
            pk = lambda h_, hf: f"pos:{h_}:{hf}"
            ALLC = [ck(h_, f_) for h_ in range(8) for f_ in range(2)]
            ALLP = [pk(h_, f_) for h_ in range(8) for f_ in range(2)]
            for h_ in range(8):
                gen('max', [cg_(h_), c16.g()], [ck(h_, 0)], out=C16[:, h_, 0:8], in_=C4[:, h_, :])
            for h_ in range(8):
                gen('max_index', [cg_(h_), ck(h_, 0), posu.g()], [pk(h_, 0)], out=POS[:, h_, 0:8], in_max=C16[:, h_, 0:8], in_values=C4[:, h_, :])
            for h_ in range(8):
                gen('match_replace', [cg_(h_), ck(h_, 0)], [w2g_(h_)], out=W4[:, h_, :], in_to_replace=C16[:, h_, 0:8], in_values=C4[:, h_, :], imm_value=NEG)
            for h_ in range(8):
                gen('max', [w2g_(h_), c16.g()], [ck(h_, 1)], out=C16[:, h_, 8:16], in_=W4[:, h_, :])
            for h_ in range(8):
                gen('max_index', [w2g_(h_), ck(h_, 1), posu.g()], [pk(h_, 1)], out=POS[:, h_, 8:16], in_max=C16[:, h_, 8:16], in_values=W4[:, h_, :])
            gen('tensor_single_scalar', ALLP, [pau.g()], out=pau.u32(), in_=posu.u32(), scalar=4, op=ALU.logical_shift_right)
            gen('tensor_single_scalar', ALLP, [pbu.g()], out=pbu.u32(), in_=posu.u32(), scalar=15, op=ALU.bitwise_and)
            cp(paf.f32(), pau.u32(), r=[pau.g()], w=[paf.g()])
            cp(pbf.f32(), pbu.u32(), r=[pbu.g()], w=[pbf.g()])
            oh, prod = ssb, wkb
            for (pf_, off_, dst_) in ((paf, 0, i1s), (pbf, 16, i2s)):
                for hp in range(4):
                    og = oh.g(hp * 2048, (hp + 1) * 2048)
                    tt(mk(oh.f32(), [[16, 32], [1, 16]], hp * 512), mk(pf_.f32(), [[1, 32], [0, 16]], hp * 32), mk(iota16.f32(), [[0, 32], [1, 16]]), ALU.is_equal,
                       r=[pf_.g(), iota16.g()], w=[og])
                    tt(mk(prod.f32(), [[256, 2], [16, 16], [1, 16]], hp * 512), mk(oh.f32(), [[256, 2], [16, 16], [1, 16]], hp * 512),
                       mk(ixf.f32(), [[32, 2], [0, 16], [1, 16]], hp * 64 + off_), ALU.mult, r=[og, ixf.g()], w=[og])
                    tred(dst_.f32()[:, hp * 32:(hp + 1) * 32], mk(prod.f32(), [[16, 32], [1, 16]], hp * 512), ALU.add, r=[og], w=[dst_.g()])
            stt(eidf.f32(), i1s.f32(), 128.0, i2s.f32(), ALU.mult, ALU.add, r=[i1s.g(), i2s.g()], w=[eidf.g()])
            cp(eidu_.u32(), eidf.f32(), r=[eidf.g()], w=[eidu_.g()])
            tt(mk(scm.f32(), [[16, 8], [1, 16]]), mk(c16.f32(), [[16, 8], [1, 16]]), mk(c16.f32(), [[16, 8], [0, 16]]), ALU.subtract, r=ALLC, w=[scm.g()])
            act(ee.f32(), scm.f32(), AF.Exp, r=[scm.g()], w=[ee.g()])
            tred(zz.f32(), mk(ee.f32(), [[16, 8], [1, 16]]), ALU.add, r=[ee.g()], w=[zz.g()])
            gen('reciprocal', [zz.g()], [rzz.g()], out=rzz.f32(), in_=zz.f32())
            tt(mk(gate_.f32(), [[16, 8], [1, 16]]), mk(ee.f32(), [[16, 8], [1, 16]]), mk(rzz.f32(), [[1, 8], [0, 16]]), ALU.mult, r=[ee.g(), rzz.g()], w=[gate_.g()])
            if dbg and i == 0:
                dump("dbg_eid", eidf.f32(), [128, 128], r=[eidf.g()])
                dump("dbg_gate", gate_.f32(), [128, 128], r=[gate_.g()])

        def stageY(i, pump):
            par = i % 2
            xm_, h2_, eidu_, gate_ = xm[par], h2[par], eidu[par], gate[par]
            t0 = i * 128
            EID = eidu_.u32()

            def gather(out_ap, slot, r, w):
                P.add('pool', lambda e: e.indirect_dma_start(out=out_ap, out_offset=None, in_=etab_d.ap(),
                                                             in_offset=bass.IndirectOffsetOnAxis(ap=EID[:, slot:slot + 1], axis=0)),
                      r=r, w=w, dma=True)

            def slot_tail(sl):
                cb = cbuf[sl % NBUF]
                dg_ = dgb[sl % 4]
                act(wgt.f32()[:, sl:sl + 1], gl.f32()[:, sl:sl + 1], AF.Copy, r=[f"gl{sl}", gate_.g(), wgt.g()], w=[f"wgt{sl}"],
                    scale=gate_.f32()[:, sl:sl + 1])
                P.add('act', (lambda dg_=dg_, sl=sl: (lambda e: e.activation(out=dg_.bf(), in_=ident_f.f32(), func=AF.Copy, scale=wgt.f32()[:, sl:sl + 1])))(),
                      r=[ident_f.g(), f"wgt{sl}"], w=[dg_.g()])
                for half in range(2):
                    mm(banks[half][:], dg_.bf(), cb.bf()[:, D + half * 512:D + (half + 1) * 512], sl == 0, sl == 127,
                       r=[dg_.g(), cb.g(2048, 4096)], w=[BK[half]])

            for sl in range(128):
                cb = cbuf[sl % NBUF]
                gather(cb.bf(), sl, r=[eidu_.g()] + ETAB_KEYS, w=[cb.g()])
                P.add('dve', (lambda cb=cb, sl=sl: (lambda e: e.scalar_tensor_tensor(out=SINK, in0=cb.bf()[:, 0:D], scalar=1.0, in1=h2_.bf(), op0=ALU.mult, op1=ALU.mult,
                                                                                 accum_out=actv.f32()[:, sl:sl + 1])))(),
                      r=[cb.g(0, 2048), h2_.g(), actv.g()], w=[f"actv{sl}"])
                act(gl.f32()[:, sl:sl + 1], actv.f32()[:, sl:sl + 1], AF.Gelu, r=[f"actv{sl}", gl.g()], w=[f"gl{sl}"])
                if sl >= 1:
                    slot_tail(sl - 1)
                pump(3)
            slot_tail(127)
            for half in range(2):
                hs = slice(half * 512, (half + 1) * 512)
                tt(tmpf.f32()[:, hs], banks[half][:], g2.f32()[:, hs], ALU.mult, r=[BK[half], g2.g()], w=[tmpf.g(half * 2048, (half + 1) * 2048)])
                tt(ot.f32()[:, hs], tmpf.f32()[:, hs], xm_.f32()[:, hs], ALU.add, r=[tmpf.g(half * 2048, (half + 1) * 2048), xm_.g()], w=[ot.g(half * 2048, (half + 1) * 2048)])
            if dbg and i == 0:
                dump("dbg_xo", ot.f32(), [128, 1024], r=[ot.g()])
            act(SINK, ot.f32(), AF.Square, r=[ot.g()], w=[small.g()], accum_out=sm(SM_SS3))
            rstd_chain(SM_SS3, SM_LN3, SM_RSTD3, D, 1e-6)
            stt(ot.f32(), ot.f32(), sm(SM_RSTD3), nf_bc.f32(), ALU.mult, ALU.mult, r=[ot.g(), small.g(), nf_bc.g()], w=[ot.g()])
            dma('sp', out_d.ap()[b, t0:t0 + 128, :], ot.f32(), r=[ot.g()], w=[f"out{b}_{i}"])

        def drain(q, n=None):
            k = 0
            while q and (n is None or k < n):
                P.add(*q.pop(0))
                k += 1

        P.capture = []
        stageX(0)
        q = P.capture
        P.capture = None
        drain(q)
        for i in range(ntiles):
            if i + 1 < ntiles:
                P.capture = []
                stageX(i + 1)
                q = P.capture
                P.capture = None
            else:
                q = []
            stageY(i, lambda n: drain(q, n))
            drain(q)

    P.emit(nc, stack)
    stack.close()
    return nc, list(dbg_d.keys())


def _prep_inputs(inputs):
    f = np.float32
    x = np.asarray(inputs['x'], f)
    c = np.asarray(inputs['c'], f)
    cst = _host_consts()
    rep = lambda v, n=128: np.ascontiguousarray(np.broadcast_to(np.asarray(v, f).reshape(1, -1), (n, np.asarray(v).size)))
    nw = np.concatenate([np.asarray(inputs['norm_mix_w'], f).reshape(-1), np.asarray(inputs['norm_ffn_w'], f).reshape(-1),
                         np.asarray(inputs['norm_final_w'], f).reshape(-1)])
    lam = np.concatenate([np.asarray(inputs[k], f).reshape(-1) for k in ('lambda_q1', 'lambda_k1', 'lambda_q2', 'lambda_k2')])
    sk1 = np.asarray(inputs['sub_keys_1'], f)[0]
    sk2 = np.asarray(inputs['sub_keys_2'], f)[0]
    subkT = np.zeros((128, 16, 128), f)
    for h in range(8):
        subkT[:, h * 2 + 0, :] = sk1[h].T
        subkT[:, h * 2 + 1, :] = sk2[h].T
    shared = {
        'w_ada': np.ascontiguousarray(np.asarray(inputs['w_ada'], f)[0]),
        'b_ada_bc': rep(inputs['b_ada']),
        'nw_bc': rep(nw),
        'w_in': np.ascontiguousarray(np.asarray(inputs['w_in'], f)[0]),
        'w_out': np.ascontiguousarray(np.asarray(inputs['w_out'], f)[0]),
        'w_query': np.ascontiguousarray(np.asarray(inputs['w_query'], f)[0]),
        'pool_w': np.ascontiguousarray(np.asarray(inputs['pool_w'], f)[0]),
        'pool_scaleT': np.ascontiguousarray(np.asarray(inputs['pool_scale'], f).reshape(8, 128).T),
        'subkT': np.ascontiguousarray(subkT.reshape(128, 2048)),
        'e_down': np.ascontiguousarray(np.asarray(inputs['expert_down'], f)[0]),
        'e_up': np.ascontiguousarray(np.asarray(inputs['expert_up'], f)[0]),
        'lam_bc': rep(lam),
        'sublnT': np.ascontiguousarray(np.asarray(inputs['subln_w'], f).reshape(128, 1)),
        'relb_bc': rep(np.asarray(inputs['rel_bias'], f).reshape(-1)),
        'masks': np.ascontiguousarray(cst['masks'].reshape(128, -1)),
        'negmask': cst['negmask'],
        'band': np.ascontiguousarray(cst['band'].reshape(128, -1)),
        'ident': cst['ident'],
        'iota16': cst['iota16'],
        'sel0': cst['sel0'],
    }
    in_maps = []
    for core in range(NCORES):
        m = dict(shared)
        m['x'] = np.ascontiguousarray(x[core * NB:(core + 1) * NB])
        cc = c[core * NB:(core + 1) * NB]
        m['cT'] = np.ascontiguousarray(cc.reshape(NB, KC, 128).transpose(2, 0, 1).reshape(128, NB * KC))
        in_maps.append(m)
    return in_maps


_CACHE = {}


def kernel(**inputs):
    in_maps = _prep_inputs(inputs)
    if 'nc' not in _CACHE:
        _CACHE['nc'] = build()[0]
    nc = _CACHE['nc']
    res = run_bass_kernel_spmd(nc, in_maps, core_ids=list(range(NCORES)))
    out = np.concatenate([np.asarray(r['out']) for r in res.results], axis=0)
    return out.astype(np.float32)
```

```python
import os
import math
import contextlib
import numpy as np
import concourse.bass as bass
import concourse.mybir as mybir
from concourse.bass_utils import run_bass_kernel_spmd

dt = mybir.dt
AF = mybir.ActivationFunctionType
ALU = mybir.AluOpType
AX = mybir.AxisListType
F32, BF16, U32 = dt.float32, dt.bfloat16, dt.uint32

NCORES = 8
NB = 2
S = 2048
NT = 16
D = 1024
KC = 8
H = 8
DIN = 6144
NEXP = 16384
OFF = 8.0
NEG = -1.0e30
G = 512


class Prog:
    def __init__(self):
        self.ops = []
        self.last_w = {}
        self.readers = {}
        self.capture = None

    def add(self, eng, fn, r=(), w=(), dma=False):
        if self.capture is not None:
            self.capture.append((eng, fn, r, w, dma))
            return None
        i = len(self.ops)
        deps = set()
        rk = _flat(r)
        wk = _flat(w)
        for k in rk:
            lw = self.last_w.get(k)
            if lw is not None:
                deps.add(lw)
        for k in wk:
            lw = self.last_w.get(k)
            if lw is not None:
                deps.add(lw)
            deps.update(self.readers.get(k, ()))
        deps.discard(i)
        for k in rk:
            self.readers.setdefault(k, []).append(i)
        for k in wk:
            self.last_w[k] = i
            self.readers[k] = []
        self.ops.append(dict(eng=eng, fn=fn, deps=deps, dma=dma, sig=None, need=False, pre=None))
        return i

    def emit(self, nc, stack):
        ops = self.ops
        EPOCH = 30000
        NSLOT = {'sp': 24, 'pool': 24, 'act': 8}
        for o in ops:
            for d in o['deps']:
                p = ops[d]
                if p['eng'] == 'pe' and o['eng'] == 'pe' and not p['dma'] and not o['dma']:
                    continue
                p['need'] = True
        cnt = {e: 0 for e in ('pe', 'act', 'dve', 'pool', 'sp')}
        esems = {e: [] for e in cnt}
        dsems = {q: [stack.enter_context(nc.semaphore(f"d_{q}_{i}")) for i in range(n)] for q, n in NSLOT.items()}
        duse = {q: [0] * n for q, n in NSLOT.items()}
        dnext = {q: 0 for q in NSLOT}
        for o in ops:
            e = o['eng']
            if o['dma']:
                s = dnext[e]
                dnext[e] = (s + 1) % NSLOT[e]
                if duse[e][s] > 0:
                    o['pre'] = (dsems[e][s], 16 * duse[e][s])
                duse[e][s] += 1
                o['sig'] = (dsems[e][s], 16 * duse[e][s])
            elif o['need']:
                ep = cnt[e] // EPOCH
                while len(esems[e]) <= ep:
                    esems[e].append(stack.enter_context(nc.semaphore(f"e_{e}_{len(esems[e])}")))
                cnt[e] += 1
                o['sig'] = (esems[e][ep], cnt[e] - ep * EPOCH)
        by_eng = {e: [o for o in ops if o['eng'] == e] for e in cnt}
        final_waits = []
        for q in NSLOT:
            for s in range(NSLOT[q]):
                if duse[q][s] > 0:
                    final_waits.append((dsems[q][s], 16 * duse[q][s]))

        def run(ename, eng):
            waited = {}
            for o in by_eng[ename]:
                needs = {}
                for d in o['deps']:
                    p = ops[d]
                    if p['eng'] == 'pe' and ename == 'pe' and not p['dma'] and not o['dma']:
                        continue
                    sem, val = p['sig']
                    key = id(sem)
                    if needs.get(key, (None, 0))[1] < val:
                        needs[key] = (sem, val)
                if o['pre'] is not None:
                    sem, val = o['pre']
                    key = id(sem)
                    if needs.get(key, (None, 0))[1] < val:
                        needs[key] = (sem, val)
                for key, (sem, val) in needs.items():
                    if waited.get(key, 0) < val:
                        eng.wait_ge(sem, val)
                        waited[key] = val
                inst = o['fn'](eng)
                if o['sig'] is not None:
                    inst.then_inc(o['sig'][0], 16 if o['dma'] else 1)
            if ename == 'sp':
                for sem, val in final_waits:
                    eng.wait_ge(sem, val)

        with nc.Block() as block:
            @block.tensor
            def _(e):
                run('pe', e)

            @block.scalar
            def _(e):
                run('act', e)

            @block.vector
            def _(e):
                run('dve', e)

            @block.gpsimd
            def _(e):
                run('pool', e)

            @block.sync
            def _(e):
                run('sp', e)


def _flat(x):
    out = []
    for k in x:
        if isinstance(k, (list, tuple, set, range)):
            out.extend(_flat(k))
        else:
            out.append(k)
    return out


class Buf:
    def __init__(self, arena, off, nbytes):
        assert off % 4 == 0 and nbytes % 4 == 0
        self.A, self.off, self.nbytes = arena, off, nbytes

    def g(self, lo=0, hi=None):
        hi = self.nbytes if hi is None else hi
        return range((self.off + lo) // G, (self.off + hi + G - 1) // G)

    def f32(self):
        return self.A[:, self.off // 4:(self.off + self.nbytes) // 4]

    def bf(self):
        return self.A[:, self.off // 4:(self.off + self.nbytes) // 4].bitcast(BF16)

    def u32(self):
        return self.A[:, self.off // 4:(self.off + self.nbytes) // 4].bitcast(U32)

    def sub(self, lo, n):
        return Buf(self.A, self.off + lo, n)


class Alloc:
    def __init__(self, arena, base, limit):
        self.A, self.p, self.limit = arena, base, limit

    def get(self, nbytes):
        nb = (nbytes + G - 1) // G * G
        b = Buf(self.A, self.p, (nbytes + 3) // 4 * 4)
        self.p += nb
        assert self.p <= self.limit, (self.p, self.limit)
        return b


def mk(ap, pattern, extra_off=0):
    return bass.AP(tensor=ap.tensor, offset=ap.offset + extra_off, ap=[list(ap.ap[0])] + [list(p) for p in pattern])


def _t5_bucket(d):
    d = np.maximum(d, 0)
    x = np.maximum(d, 1).astype(np.float32) / np.float32(16)
    large = 16 + (np.log(x).astype(np.float32) / np.float32(math.log(128 / 16)) * np.float32(16)).astype(np.int32)
    large = np.minimum(large, 31)
    return np.where(d < 16, d, large)


def _host_consts():
    kk = np.arange(128)[:, None]
    qq = np.arange(256)[None, :]
    dist = qq - kk
    valid = dist >= 0
    bk = _t5_bucket(dist)
    masks = np.zeros((128, 32, 256), np.float32)
    for b in range(32):
        masks[:, b, :] = (valid & (bk == b)).astype(np.float32)
    negmask = np.where(valid, 0.0, NEG).astype(np.float32)
    band = np.zeros((128, 12, 128), np.float32)
    s = np.arange(128)[:, None]
    t = np.arange(128)[None, :]
    for gi, w in enumerate((2, 4, 8, 16)):
        cnt0 = np.minimum(t + 1, w).astype(np.float32)
        band[:, gi * 3 + 0, :] = ((s <= t) & (s > t - w)) / cnt0 - (s == t)
        band[:, gi * 3 + 1, :] = ((s <= t) & (s > t - w)) / np.float32(w) - (s == t)
        band[:, gi * 3 + 2, :] = (s > 128 + t - w) / np.float32(w)
    ident = np.eye(128, dtype=np.float32)
    iota16 = np.broadcast_to(np.arange(16, dtype=np.float32)[None, :], (128, 16)).copy()
    sel0 = np.zeros((64, 128), np.float32)
    sel0[0, :] = 1.0
    sel0[32, :] = 1.0
    return dict(masks=masks, negmask=negmask, band=band, ident=ident, iota16=iota16, sel0=sel0)


def build(stage=99, dbg=False):
    nc = bass.Bass("TRN2", target_bir_lowering=False)
    P = Prog()

    def din(name, shape, dtype=F32):
        return nc.dram_tensor(name, list(shape), dtype, kind="ExternalInput")

    x_d = din("x", [NB, S, D])
    cT_d = din("cT", [128, NB * KC])
    wada_d = din("w_ada", [D, DIN])
    bada_d = din("b_ada_bc", [128, DIN])
    nw_d = din("nw_bc", [128, 3 * D])
    win_d = din("w_in", [D, DIN])
    wout_d = din("w_out", [D, D])
    wqry_d = din("w_query", [D, 2048])
    poolw_d = din("pool_w", [4, 256, 256])
    psc_d = din("pool_scaleT", [128, 8])
    subk_d = din("subkT", [128, 16 * 128])
    edown_d = din("e_down", [NEXP, D])
    eup_d = din("e_up", [NEXP, D])
    lam_d = din("lam_bc", [128, 256])
    subln_d = din("sublnT", [128, 1])
    relb_d = din("relb_bc", [128, 256])
    masks_d = din("masks", [128, 32 * 256])
    negm_d = din("negmask", [128, 256])
    band_d = din("band", [128, 12 * 128])
    ident_d = din("ident", [128, 128])
    iota_d = din("iota16", [128, 16])
    sel0_d = din("sel0", [64, 128])
    out_d = nc.dram_tensor("out", [NB, S, D], F32, kind="ExternalOutput")
    etab_d = nc.dram_tensor("etab", [NEXP, 2 * D], BF16, kind="Internal")
    dbg_d = {}

    def dbg_out(name, shape):
        dbg_d[name] = nc.dram_tensor(name, list(shape), F32, kind="ExternalOutput")
        return dbg_d[name]

    stack = contextlib.ExitStack()
    TOT = 212480
    arena = stack.enter_context(nc.sbuf_tensor("arena", [128, TOT // 4], F32))
    banks = [stack.enter_context(nc.psum_tensor(f"B{i}", [128, 512], F32)) for i in range(8)]
    BK = [f"B{i}" for i in range(8)]

    al = Alloc(arena, 0, TOT)
    ident_f = al.get(512)
    ident_b = al.get(256)
    ones_b = al.get(256)
    ones_f = al.get(512)
    sel0 = al.get(512)
    relb = al.get(1024)
    rb31m = al.get(32)
    lamv = al.get(1024)
    small = al.get(512)
    pscT = al.get(32)
    band = al.get(12 * 128 * 2)
    subkT = al.get(16 * 128 * 2)
    iota16 = al.get(64)
    sink = al.get(64)
    nf_bc = al.get(4096)
    sh1 = al.get(4096)
    a1 = al.get(4096)
    g1 = al.get(4096)
    sh2 = al.get(4096)
    a2 = al.get(4096)
    g2 = al.get(4096)
    hT = al.get(KC * S * 2)
    mT = al.get(KC * S * 2)
    wout = al.get(KC * D * 2)
    work_base = al.p
    SM = small.f32()
    (SM_S1, SM_S2, SM_E1, SM_E2, SM_NLAM, SM_WSUB, SM_SS, SM_RSTD, SM_LN, SM_SS2, SM_RSTD2, SM_SS3, SM_RSTD3,
     SM_LN2, SM_LN3, SM_SUBLN) = range(16)

    def sm(i):
        return SM[:, i:i + 1]

    def smg(i):
        return small.g()

    def dma(q, out, in_, r, w):
        P.add(q, lambda e: e.dma_start(out=out, in_=in_), r=r, w=w, dma=True)

    def mm(out, lhsT, rhs, start, stop, r, w, **kw):
        P.add('pe', lambda e: e.matmul(out, lhsT, rhs, start=start, stop=stop, **kw), r=r, w=w)

    def tr(out, in_, ident, r, w):
        P.add('pe', lambda e: e.transpose(out, in_, ident), r=r, w=w)

    def act(out, in_, func, r, w, bias=None, scale=None, accum_out=None, eng='act'):
        def f(e):
            kw = {}
            if bias is not None:
                kw['bias'] = bias
            if scale is not None:
                kw['scale'] = scale
            if accum_out is not None:
                kw['accum_out'] = accum_out
            return e.activation(out=out, in_=in_, func=func, **kw)
        P.add('act', f, r=r, w=w)

    def tt(out, in0, in1, op, r, w, eng='dve'):
        P.add(eng, lambda e: e.tensor_tensor(out=out, in0=in0, in1=in1, op=op), r=r, w=w)

    def ts(out, in0, s1, s2, op0, op1, r, w, eng='dve'):
        if op1 is None:
            P.add(eng, lambda e: e.tensor_scalar(out=out, in0=in0, scalar1=s1, scalar2=None, op0=op0), r=r, w=w)
        else:
            P.add(eng, lambda e: e.tensor_scalar(out=out, in0=in0, scalar1=s1, scalar2=s2, op0=op0, op1=op1), r=r, w=w)

    def stt(out, in0, scalar, in1, op0, op1, r, w):
        P.add('dve', lambda e: e.scalar_tensor_tensor(out=out, in0=in0, scalar=scalar, in1=in1, op0=op0, op1=op1), r=r, w=w)

    def cp(out, in_, r, w, eng='dve'):
        if eng == 'act':
            P.add('act', lambda e: e.copy(out=out, in_=in_), r=r, w=w)
        else:
            P.add(eng, lambda e: e.tensor_copy(out=out, in_=in_), r=r, w=w)

    def ttr(out, in0, in1, accum_out, r, w):
        P.add('dve', lambda e: e.scalar_tensor_tensor(out=out, in0=in0, scalar=1.0, in1=in1, op0=ALU.mult, op1=ALU.mult,
                                                     accum_out=accum_out), r=r, w=w)

    def tred(out, in_, op, r, w):
        P.add('dve', lambda e: e.tensor_reduce(out=out, in_=in_, axis=AX.X, op=op), r=r, w=w)

    def memset(ap, val, r, w, eng='dve'):
        P.add(eng, lambda e: e.memset(ap, val), r=r, w=w)

    def rstd_chain(ss_i, ln_i, rstd_i, n, eps):
        ts(sm(ln_i), sm(ss_i), 1.0 / n, eps, ALU.mult, ALU.add, r=[small.g()], w=[small.g()])
        act(sm(ln_i), sm(ln_i), AF.Ln, r=[small.g()], w=[small.g()])
        act(sm(rstd_i), sm(ln_i), AF.Exp, r=[small.g()], w=[small.g()], scale=-0.5)

    def dump(name, ap_sb, shape, r):
        if name in dbg_d:
            return
        d = dbg_out(name, shape)
        dma('sp', d.ap(), ap_sb, r=r, w=[name])

    dma('sp', ident_f.f32(), ident_d.ap(), r=[], w=[ident_f.g()])
    dma('sp', sel0.f32()[0:64, :], sel0_d.ap(), r=[], w=[sel0.g()])
    dma('sp', relb.f32(), relb_d.ap(), r=[], w=[relb.g()])
    dma('sp', lamv.f32(), lam_d.ap(), r=[], w=[lamv.g()])
    dma('sp', pscT.f32(), psc_d.ap(), r=[], w=[pscT.g()])
    dma('sp', iota16.f32(), iota_d.ap(), r=[], w=[iota16.g()])
    dma('sp', nf_bc.f32(), nw_d.ap()[:, 2 * D:3 * D], r=[], w=[nf_bc.g()])
    dma('sp', sm(SM_SUBLN), subln_d.ap(), r=[], w=[small.g()])
    dma('pool', band.bf(), band_d.ap(), r=[], w=[band.g()])
    dma('pool', subkT.bf(), subk_d.ap(), r=[], w=[subkT.g()])
    ETAB_JOBS = [(ti, r0) for ti in range(2) for r0 in range(0, NEXP, 1024)]
    ETAB_KEYS = [f"etab{ti}_{r0}" for (ti, r0) in ETAB_JOBS]

    def precast(n):
        for _ in range(n):
            if not ETAB_JOBS:
                return
            ti, r0 = ETAB_JOBS.pop(0)
            tbl = (edown_d, eup_d)[ti]
            dma('pool', etab_d.ap()[r0:r0 + 1024, ti * D:(ti + 1) * D], tbl.ap()[r0:r0 + 1024, :], r=[], w=[f"etab{ti}_{r0}"])

    cp(ident_b.bf(), ident_f.f32(), r=[ident_f.g()], w=[ident_b.g()])
    memset(ones_b.bf(), 1.0, r=[], w=[ones_b.g()])
    memset(ones_f.f32(), 1.0, r=[], w=[ones_f.g()])
    ts(rb31m.f32(), relb.f32()[:, 31 * 8:32 * 8], -OFF, None, ALU.add, None, r=[relb.g()], w=[rb31m.g()])
    ts(sm(SM_WSUB), sm(SM_SUBLN), 0.8, None, ALU.mult, None, r=[small.g()], w=[small.g()])
    wa = Alloc(arena, work_base, TOT)
    junk64 = wa.get(256)
    LV = lamv.f32()
    ttr(junk64.f32(), LV[:, 0:64], LV[:, 64:128], sm(SM_S1), r=[lamv.g()], w=[junk64.g(), small.g()])
    ttr(junk64.f32(), LV[:, 128:192], LV[:, 192:256], sm(SM_S2), r=[lamv.g()], w=[junk64.g(), small.g()])
    act(sm(SM_E1), sm(SM_S1), AF.Exp, r=[small.g()], w=[small.g()])
    act(sm(SM_E2), sm(SM_S2), AF.Exp, r=[small.g()], w=[small.g()])
    tt(sm(SM_NLAM), sm(SM_E2), sm(SM_E1), ALU.subtract, r=[small.g()], w=[small.g()])
    ts(sm(SM_NLAM), sm(SM_NLAM), -0.2, None, ALU.add, None, r=[small.g()], w=[small.g()])
    bias_scr = nc.dram_tensor("bias_scr", [128, 2048], F32, kind="Internal")
    biasT = wa.get(8 * 1024)
    mk_chunk = wa.get(8 * 256 * 4)
    BT3 = biasT.f32().rearrange("p (h q) -> p h q", h=8)
    for h in range(8):
        dma('sp', BT3[:, h, :], negm_d.ap(), r=[], w=[biasT.g(h * 1024, (h + 1) * 1024)])
    for c in range(4):
        dma('sp', mk_chunk.f32(), masks_d.ap()[:, c * 2048:(c + 1) * 2048], r=[], w=[mk_chunk.g()])
        MC = mk_chunk.f32().rearrange("p (b q) -> p b q", b=8)
        for bb in range(8):
            b = c * 8 + bb
            for h in range(8):
                stt(BT3[:, h, :], MC[:, bb, :], relb.f32()[:, b * 8 + h:b * 8 + h + 1], BT3[:, h, :], ALU.mult, ALU.add,
                    r=[mk_chunk.g(), relb.g(), biasT.g(h * 1024, (h + 1) * 1024)], w=[biasT.g(h * 1024, (h + 1) * 1024)])
    ts(biasT.f32(), biasT.f32(), -OFF, None, ALU.add, None, r=[biasT.g()], w=[biasT.g()])
    dma('sp', bias_scr.ap(), biasT.f32(), r=[biasT.g()], w=["bias_scr"])
    if dbg:
        dump("dbg_bias", biasT.f32(), [128, 2048], r=[biasT.g()])
        dump("dbg_small", small.f32()[:, 0:16], [128, 16], r=[small.g()])

    hT3 = hT.bf().rearrange("p (k t) -> p k t", k=KC)
    mT3 = mT.bf().rearrange("p (k t) -> p k t", k=KC)

    def hT_g(c0, c1):
        return [hT.g(k * S * 2 + c0 * 2, k * S * 2 + c1 * 2) for k in range(KC)]

    def mT_g(k, c0, c1):
        return mT.g(k * S * 2 + c0 * 2, k * S * 2 + c1 * 2)

    BTb = banks[7][:].bitcast(BF16)

    for b in range(NB):
        wa = Alloc(arena, work_base, TOT)
        cact = wa.get(64)
        crep = wa.get(KC * 128 * 4)
        wblk = [wa.get(KC * 512 * 4) for _ in range(2)]
        bblk = [wa.get(2048) for _ in range(2)]
        mtmp = wa.get(2048)
        nwt = wa.get(4096)
        cin = wa.get(64)
        dma('sp', cin.f32()[:, 0:KC], cT_d.ap()[:, b * KC:(b + 1) * KC], r=[], w=[cin.g()])
        act(cact.f32()[:, 0:KC], cin.f32()[:, 0:KC], AF.Silu, r=[cin.g()], w=[cact.g()])
        cp(crep.f32().rearrange("p (k m) -> p k m", k=KC), mk(cact.f32(), [[1, KC], [0, 128]]), r=[cact.g()], w=[crep.g()])
        CR = crep.f32().rearrange("p (k m) -> p k m", k=KC)
        wada_v = wada_d.ap().rearrange("(k p) n -> p k n", p=128)
        dsts = [sh1, sh1, a1, a1, g1, g1, sh2, sh2, a2, a2, g2, g2]
        for nb_ in range(12):
            wb = wblk[nb_ % 2]
            bb_ = bblk[nb_ % 2]
            dma('sp', wb.f32().rearrange("p (k n) -> p k n", k=KC), wada_v[:, :, nb_ * 512:(nb_ + 1) * 512], r=[], w=[wb.g()])
            dma('sp', bb_.f32(), bada_d.ap()[:, nb_ * 512:(nb_ + 1) * 512], r=[], w=[bb_.g()])
            WB = wb.f32().rearrange("p (k n) -> p k n", k=KC)
            for k in range(KC):
                mm(banks[6][:], CR[:, k, :], WB[:, k, :], k == 0, k == KC - 1, r=[crep.g(), wb.g()], w=[BK[6]])
            dst = dsts[nb_]
            half = nb_ % 2
            dst_ap = dst.f32()[:, half * 512:(half + 1) * 512]
            dg_ = dst.g(half * 2048, (half + 1) * 2048)
            if nb_ in (2, 3, 8, 9):
                which = 0 if nb_ in (2, 3) else 1
                dma('sp', nwt.f32()[:, 0:512], nw_d.ap()[:, which * D + half * 512: which * D + (half + 1) * 512], r=[], w=[nwt.g()])
                tt(mtmp.f32(), banks[6][:], bb_.f32(), ALU.add, r=[BK[6], bb_.g()], w=[mtmp.g()])
                stt(dst_ap, mtmp.f32(), 1.0, nwt.f32()[:, 0:512], ALU.add, ALU.mult, r=[mtmp.g(), nwt.g()], w=[dg_])
            else:
                tt(dst_ap, banks[6][:], bb_.f32(), ALU.add, r=[BK[6], bb_.g()], w=[dg_])
        if dbg and b == 0:
            dump("dbg_a1", a1.f32(), [128, 1024], r=[a1.g()])
            dump("dbg_g2", g2.f32(), [128, 1024], r=[g2.g()])

        wa = Alloc(arena, work_base, TOT)
        xt = [wa.get(4096) for _ in range(2)]
        tmpf = wa.get(4096)
        junkb = wa.get(2048)
        hbf = wa.get(2048)
        for i in range(NT):
            xb = xt[i % 2]
            dma('sp', xb.f32(), x_d.ap()[b, i * 128:(i + 1) * 128, :], r=[], w=[xb.g()])
            act(junkb.bf(), xb.f32(), AF.Square, r=[xb.g()], w=[junkb.g(), small.g()], accum_out=sm(SM_SS))
            rstd_chain(SM_SS, SM_LN, SM_RSTD, D, 1e-6)
            stt(tmpf.f32(), xb.f32(), sm(SM_RSTD), a1.f32(), ALU.mult, ALU.mult, r=[xb.g(), small.g(), a1.g()], w=[tmpf.g()])
            tt(hbf.bf(), tmpf.f32(), sh1.f32(), ALU.add, r=[tmpf.g(), sh1.g()], w=[hbf.g()])
            for k in range(KC):
                tr(BTb[:, k * 128:(k + 1) * 128], hbf.bf()[:, k * 128:(k + 1) * 128], ident_b.bf(), r=[hbf.g(), ident_b.g()], w=[BK[7]])
            cp(hT3[:, :, i * 128:(i + 1) * 128], BTb.rearrange("p (k t) -> p k t", k=KC), r=[BK[7]], w=hT_g(i * 128, (i + 1) * 128), eng='act')
        if dbg and b == 0:
            wa2 = Alloc(arena, wa.p, TOT)
            dtmp = wa2.get(8192)
            cp(dtmp.f32(), hT3[:, 0, :], r=hT_g(0, S), w=[dtmp.g()])
            dump("dbg_hT0", dtmp.f32(), [128, 2048], r=[dtmp.g()])
        if stage <= 1:
            continue

        win_v = win_d.ap().rearrange("(k p) n -> p k n", p=128)
        wa = Alloc(arena, work_base, TOT)
        wp = wa.get(KC * 256 * 2)
        wgp = wa.get(KC * 256 * 2)
        pw = wa.get(2 * 256 * 2)
        pg = wa.get(NT * 256 * 2)
        pT = [wa.get(512 * 2) for _ in range(2)]
        sg = wa.get(512 * 4)
        WP = wp.bf().rearrange("p (k n) -> p k n", k=KC)
        WGP = wgp.bf().rearrange("p (k n) -> p k n", k=KC)
        PW = pw.bf().rearrange("p (c n) -> p c n", c=2)
        PG = pg.bf().rearrange("p (i n) -> p i n", i=NT)
        BAND = band.bf().rearrange("p (j n) -> p j n", j=12)
        for g in range(4):
            dma('pool', WP, win_v[:, :, 3072 + g * 256:3072 + (g + 1) * 256], r=[], w=[wp.g()])
            dma('pool', WGP, win_v[:, :, 5120 + g * 256:5120 + (g + 1) * 256], r=[], w=[wgp.g()])
            dma('pool', PW, poolw_d.ap()[g].rearrange("(c p) n -> p c n", p=128), r=[], w=[pw.g()])
            for i in range(NT):
                for k in range(KC):
                    mm(banks[7][:, (i % 2) * 256:(i % 2 + 1) * 256], hT3[:, k, i * 128:(i + 1) * 128], WP[:, k, :], k == 0, k == KC - 1,
                       r=[hT_g(i * 128, (i + 1) * 128), wp.g()], w=[BK[7]])
                if i % 2 == 1:
                    cp(pg.bf()[:, (i - 1) * 256:(i + 1) * 256], banks[7][:], r=[BK[7]], w=[pg.g((i - 1) * 512, (i + 1) * 512)], eng='act')
            for c in range(4):
                for cc in range(2):
                    for il in range(4):
                        i = c * 4 + il
                        cur = g * 3 + (0 if i == 0 else 1)
                        mm(banks[cc][:, il * 128:(il + 1) * 128], PG[:, i, cc * 128:(cc + 1) * 128], BAND[:, cur, :], True, i == 0,
                           r=[pg.g(i * 512, (i + 1) * 512), band.g()], w=[BK[cc]])
                        if i > 0:
                            mm(banks[cc][:, il * 128:(il + 1) * 128], PG[:, i - 1, cc * 128:(cc + 1) * 128], BAND[:, g * 3 + 2, :], False, True,
                               r=[pg.g((i - 1) * 512, i * 512), band.g()], w=[BK[cc]])
                    cp(pT[cc].bf(), banks[cc][:], r=[BK[cc]], w=[pT[cc].g()], eng='act')
                for ec in range(2):
                    kch = g * 2 + ec
                    mm(banks[2][:], PW[:, 0, ec * 128:(ec + 1) * 128], pT[0].bf(), True, False, r=[pw.g(), pT[0].g()], w=[BK[2]])
                    mm(banks[2][:], PW[:, 1, ec * 128:(ec + 1) * 128], pT[1].bf(), False, True, r=[pw.g(), pT[1].g()], w=[BK[2]])
                    for k in range(KC):
                        mm(banks[3][:], WGP[:, k, ec * 128:(ec + 1) * 128], hT3[:, k, c * 512:(c + 1) * 512], k == 0, k == KC - 1,
                           r=[wgp.g(), hT_g(c * 512, (c + 1) * 512)], w=[BK[3]])
                    act(sg.f32(), banks[3][:], AF.Sigmoid, r=[BK[3]], w=[sg.g()])
                    stt(mT3[:, kch, c * 512:(c + 1) * 512], banks[2][:], pscT.f32()[:, kch:kch + 1], sg.f32(), ALU.mult, ALU.mult,
                        r=[BK[2], pscT.g(), sg.g()], w=[mT_g(kch, c * 512, (c + 1) * 512)])
        if dbg and b == 0 and stage == 2:
            wa2 = Alloc(arena, wa.p, TOT)
            dtmp = wa2.get(8192)
            for kk_ in (0, 5):
                cp(dtmp.f32(), mT3[:, kk_, :], r=[mT.g()], w=[dtmp.g()])
                dump(f"dbg_mT{kk_}", dtmp.f32(), [128, 2048], r=[dtmp.g()])
        if stage <= 2:
            continue

        wa = Alloc(arena, work_base, TOT)
        wq = wa.get(KC * 128 * 2)
        wk_ = wa.get(KC * 128 * 2)
        wv = wa.get(KC * 128 * 2)
        wg = wa.get(KC * 128 * 2)
        QTz = [wa.get(S * 2) for _ in range(2)]
        KT = wa.get(S * 2)
        Vb = wa.get(NT * 128 * 2)
        sgT = wa.get(S * 2)
        PT = [[wa.get(512 * 2) for _ in range(2)] for _ in range(2)]
        ntmp = [wa.get(256 * 4) for _ in range(2)]
        O1s = wa.get(2048)
        O2s = wa.get(2048)
        lnz = wa.get(2048)
        rz = wa.get(2048)
        rz2 = wa.get(2048)
        sq = wa.get(2048)
        t1 = wa.get(2048)
        rs = wa.get(2048)
        biasT = wa.get(8 * 1024)
        BT3 = biasT.f32().rearrange("p (h q) -> p h q", h=8)
        dma('sp', biasT.f32(), bias_scr.ap(), r=["bias_scr"], w=[biasT.g()])
        WQ = wq.bf().rearrange("p (k n) -> p k n", k=KC)
        WK = wk_.bf().rearrange("p (k n) -> p k n", k=KC)
        WV = wv.bf().rearrange("p (k n) -> p k n", k=KC)
        WG = wg.bf().rearrange("p (k n) -> p k n", k=KC)
        V3 = Vb.bf().rearrange("p (i n) -> p i n", i=NT)
        nheads = H if stage > 3 or not dbg else 1
        memset(QTz[0].bf()[64:128, :], 0.0, r=[], w=[QTz[0].g()])
        memset(QTz[1].bf()[0:64, :], 0.0, r=[], w=[QTz[1].g()])
        for h in range(nheads):
            dma('pool', WQ, win_v[:, :, h * 128:(h + 1) * 128], r=[], w=[wq.g()])
            dma('pool', WK, win_v[:, :, 1024 + h * 128:1024 + (h + 1) * 128], r=[], w=[wk_.g()])
            dma('pool', WV, win_v[:, :, 2048 + h * 128:2048 + (h + 1) * 128], r=[], w=[wv.g()])
            dma('pool', WG, win_v[:, :, 4096 + h * 128:4096 + (h + 1) * 128], r=[], w=[wg.g()])
            precast(4)
            pj = 0
            for (W_, wb_, dst, mode) in ((WQ, wq, None, 'q'), (WK, wk_, KT, 'k'), (WG, wg, sgT, 'g')):
                for c in range(4):
                    bk = pj % 4
                    pj += 1
                    for k in range(KC):
                        mm(banks[bk][:], W_[:, k, :], hT3[:, k, c * 512:(c + 1) * 512], k == 0, k == KC - 1,
                           r=[wb_.g(), hT_g(c * 512, (c + 1) * 512)], w=[BK[bk]])
                    if mode == 'q':
                        for m_ in range(2):
                            o_ap = QTz[m_].bf()[m_ * 64:(m_ + 1) * 64, c * 512:(c + 1) * 512]
                            P.add('act', (lambda o_ap=o_ap, bk=bk, m_=m_: (lambda e: e.mul(out=o_ap, in_=banks[bk][m_ * 64:(m_ + 1) * 64, :], mul=0.125)))(),
                                  r=[BK[bk]], w=[QTz[m_].g(c * 1024, (c + 1) * 1024)])
                        continue
                    o_ap = dst.bf()[:, c * 512:(c + 1) * 512]
                    o_g = dst.g(c * 1024, (c + 1) * 1024)
                    if mode == 'k':
                        cp(o_ap, banks[bk][:], r=[BK[bk]], w=[o_g], eng='act')
                    else:
                        act(o_ap, banks[bk][:], AF.Sigmoid, r=[BK[bk]], w=[o_g])
            for i in range(NT):
                bk = (i // 4) % 4
                for k in range(KC):
                    mm(banks[bk][:, (i % 4) * 128:(i % 4 + 1) * 128], hT3[:, k, i * 128:(i + 1) * 128], WV[:, k, :], k == 0, k == KC - 1,
                       r=[hT_g(i * 128, (i + 1) * 128), wv.g()], w=[BK[bk]])
                if i % 4 == 3:
                    cp(Vb.bf()[:, (i - 3) * 128:(i + 1) * 128], banks[bk][:], r=[BK[bk]], w=[Vb.g((i - 3) * 256, (i + 1) * 256)], eng='act')

            def qk(c, j):
                col0 = max(0, j - 4 * c) * 128
                par = j % 2
                for m in range(2):
                    bk = m * 2 + par
                    mm(banks[bk][:, col0:512], KT.bf()[:, j * 128:(j + 1) * 128],
                       QTz[m].bf()[:, c * 512 + col0:(c + 1) * 512], True, True,
                       r=[KT.g(j * 256, (j + 1) * 256), QTz[m].g(c * 1024 + col0 * 2, (c + 1) * 1024)], w=[BK[bk]])

            for c in range(4):
                jmax = 4 * c + 3
                qk(c, 0)
                for j in range(jmax + 1):
                    if j + 1 <= jmax:
                        qk(c, j + 1)
                    col0 = max(0, j - 4 * c) * 128
                    par = j % 2
                    il_lo = max(0, j - 4 * c)
                    il_hi = min(3, j + 1 - 4 * c)
                    far0 = max(0, j + 2 - 4 * c) * 128
                    for m in range(2):
                        bk = m * 2 + par
                        pt = PT[m][par]
                        if il_hi >= il_lo and il_hi >= 0:
                            n0, n1 = il_lo * 128, (il_hi + 1) * 128
                            b0 = (4 * c + il_lo - j) * 128
                            nn = n1 - n0
                            tt(ntmp[m].f32()[:, 0:nn], banks[bk][:, n0:n1], BT3[:, h, b0:b0 + nn], ALU.add,
                               r=[BK[bk], biasT.g(h * 1024, (h + 1) * 1024)], w=[ntmp[m].g()])
                            act(pt.bf()[:, n0:n1], ntmp[m].f32()[:, 0:nn], AF.Exp, r=[ntmp[m].g()], w=[pt.g(n0 * 2, n1 * 2)])
                        if far0 < 512:
                            act(pt.bf()[:, far0:512], banks[bk][:, far0:512], AF.Exp, r=[BK[bk], rb31m.g()], w=[pt.g(far0 * 2, 1024)],
                                bias=rb31m.f32()[:, h:h + 1])
                        mm(banks[4 + m][:, col0:512], V3[:, j, :], pt.bf()[:, col0:512], j == 0, j == jmax,
                           r=[Vb.g(j * 256, (j + 1) * 256), pt.g(col0 * 2, 1024)], w=[BK[4 + m]], skip_group_check=True)
                        mm(banks[6 + m][:, col0:512], ones_b.bf(), pt.bf()[:, col0:512], j == 0, j == jmax,
                           r=[ones_b.g(), pt.g(col0 * 2, 1024)], w=[BK[6 + m]], skip_group_check=True)
                cs = slice(c * 512, (c + 1) * 512)
                cp(O1s.f32(), banks[4][:], r=[BK[4]], w=[O1s.g()], eng='act')
                cp(O2s.f32(), banks[5][:], r=[BK[5]], w=[O2s.g()], eng='act')
                act(lnz.f32(), banks[6][:], AF.Ln, r=[BK[6]], w=[lnz.g()])
                act(rz.f32(), lnz.f32(), AF.Exp, r=[lnz.g()], w=[rz.g()], scale=-1.0)
                act(lnz.f32(), banks[7][:], AF.Ln, r=[BK[7]], w=[lnz.g()])
                act(rz2.f32(), lnz.f32(), AF.Exp, r=[lnz.g()], w=[rz2.g()], scale=-1.0)
                tt(t1.f32(), O1s.f32(), rz.f32(), ALU.mult, r=[O1s.g(), rz.g()], w=[t1.g()])
                tt(O2s.f32(), O2s.f32(), rz2.f32(), ALU.mult, r=[O2s.g(), rz2.g()], w=[O2s.g()])
                stt(t1.f32(), O2s.f32(), sm(SM_NLAM), t1.f32(), ALU.mult, ALU.add, r=[t1.g(), O2s.g(), small.g()], w=[t1.g()])
                act(sq.bf()[:, 0:512], t1.f32(), AF.Square, r=[t1.g()], w=[sq.g()])
                mm(banks[6][:], ones_b.bf(), sq.bf()[:, 0:512], True, True, r=[ones_b.g(), sq.g()], w=[BK[6]])
                ts(rs.f32(), banks[6][:], 1.0 / 128, 1e-5, ALU.mult, ALU.add, r=[BK[6]], w=[rs.g()])
                act(rs.f32(), rs.f32(), AF.Ln, r=[rs.g()], w=[rs.g()])
                act(rs.f32(), rs.f32(), AF.Exp, r=[rs.g()], w=[rs.g()], scale=-0.5)
                tt(t1.f32(), t1.f32(), rs.f32(), ALU.mult, r=[t1.g(), rs.g()], w=[t1.g()])
                stt(t1.f32(), t1.f32(), sm(SM_WSUB), sgT.bf()[:, cs], ALU.mult, ALU.mult, r=[t1.g(), small.g(), sgT.g(c * 1024, (c + 1) * 1024)], w=[t1.g()])
                tt(mT3[:, h, cs], t1.f32(), mT3[:, h, cs], ALU.add, r=[t1.g(), mT_g(h, c * 512, (c + 1) * 512)], w=[mT_g(h, c * 512, (c + 1) * 512)])
        if dbg and b == 0 and stage == 3:
            wa2 = Alloc(arena, wa.p, TOT)
            dtmp = wa2.get(8192)
            cp(dtmp.f32(), mT3[:, 0, :], r=[mT.g()], w=[dtmp.g()])
            dump("dbg_mT0", dtmp.f32(), [128, 2048], r=[dtmp.g()])
        if stage <= 3:
            continue

        precast(64)
        WOUT3 = wout.bf().rearrange("p (k n) -> p k n", k=KC)
        dma('pool', WOUT3, wout_d.ap().rearrange("(k p) n -> p k n", p=128), r=[], w=[wout.g()])
        for k in range(KC):
            tt(WOUT3[:, k, :], WOUT3[:, k, :], g1.f32(), ALU.mult, r=[wout.g(k * 2048, (k + 1) * 2048), g1.g()], w=[wout.g(k * 2048, (k + 1) * 2048)])
        WQ3 = hT.bf().rearrange("p (k n) -> p k n", k=KC)
        dma('pool', WQ3, wqry_d.ap().rearrange("(k p) n -> p k n", p=128), r=[], w=[hT.g()])
        wa = Alloc(arena, work_base, TOT)
        xt5 = g1
        xm = [wa.get(4096) for _ in range(2)]
        h2 = [wa.get(2048) for _ in range(2)]
        eidu = [wa.get(512) for _ in range(2)]
        gate = [wa.get(512) for _ in range(2)]
        tmpf = a1
        ot = sh1
        ssb = wa.get(8192)
        wkb = ssb
        tmpfX = ssb.sub(0, 4096)
        junkbX = ssb.sub(4096, 2048)
        h2T = ssb.sub(0, 2048)
        qTs = ssb.sub(4096, 4096)
        v16 = wa.get(1024)
        ixu = wa.get(1024)
        ixf = wa.get(1024)
        c16, posu, pau, pbu, paf, pbf, i1s, i2s, eidf, scm, ee, actv, gl, wgt = [wa.get(512) for _ in range(14)]
        zz = wa.get(32)
        rzz = wa.get(32)
        dgb = [wa.get(256) for _ in range(4)]
        NBUF = (TOT - wa.p) // 4096
        assert NBUF >= 5, NBUF
        cbuf = [wa.get(4096) for _ in range(NBUF)]
        SINK = mk(sink.bf(), [[0, D]])
        H2T3 = h2T.bf().rearrange("p (k t) -> p k t", k=KC)
        QTS3 = qTs.bf().rearrange("p (q t) -> p q t", q=16)
        SUBK3 = subkT.bf().rearrange("p (q n) -> p q n", q=16)
        SS3 = ssb.f32().rearrange("p (q n) -> p q n", q=16)
        WK3 = wkb.f32().rearrange("p (q n) -> p q n", q=16)
        V16 = v16.f32().rearrange("p (q j) -> p q j", q=16)
        IXU = ixu.u32().rearrange("p (q j) -> p q j", q=16)
        ntiles = NT if not dbg else 2

        def gen(name, r, w, eng='dve', **kw):
            P.add(eng, lambda e: getattr(e, name)(**kw), r=r, w=w)

        def stageX(i):
            par = i % 2
            xm_, h2_, eidu_, gate_ = xm[par], h2[par], eidu[par], gate[par]
            t0 = i * 128
            dma('sp', xt5.f32(), x_d.ap()[b, t0:t0 + 128, :], r=[], w=[xt5.g()])
            for half in range(2):
                for k in range(KC):
                    mm(banks[2 + half][:], mT3[:, k, t0:t0 + 128], WOUT3[:, k, half * 512:(half + 1) * 512], k == 0, k == KC - 1,
                       r=[mT_g(k, t0, t0 + 128), wout.g()], w=[BK[2 + half]])
            for half in range(2):
                hs = slice(half * 512, (half + 1) * 512)
                tt(xm_.f32()[:, hs], banks[2 + half][:], xt5.f32()[:, hs], ALU.add, r=[BK[2 + half], xt5.g()], w=[xm_.g(half * 2048, (half + 1) * 2048)])
            act(junkbX.bf(), xm_.f32(), AF.Square, r=[xm_.g()], w=[junkbX.g(), small.g()], accum_out=sm(SM_SS2))
            rstd_chain(SM_SS2, SM_LN2, SM_RSTD2, D, 1e-6)
            for hf in range(2):
                hs = slice(hf * 512, (hf + 1) * 512)
                stt(tmpfX.f32()[:, hs], xm_.f32()[:, hs], sm(SM_RSTD2), a2.f32()[:, hs], ALU.mult, ALU.mult, r=[xm_.g(), small.g(), a2.g()], w=[tmpfX.g(hf * 2048, (hf + 1) * 2048)])
                tt(h2_.bf()[:, hs], tmpfX.f32()[:, hs], sh2.f32()[:, hs], ALU.add, r=[tmpfX.g(hf * 2048, (hf + 1) * 2048), sh2.g()], w=[h2_.g(hf * 1024, (hf + 1) * 1024)])
            for k in range(KC):
                tr(BTb[:, k * 128:(k + 1) * 128], h2_.bf()[:, k * 128:(k + 1) * 128], ident_b.bf(), r=[h2_.g(), ident_b.g()], w=[BK[7]])
            cp(h2T.bf(), BTb, r=[BK[7]], w=[h2T.g()], eng='act')
            for qc in range(16):
                bk = 2 + (qc // 4) % 2
                for k in range(KC):
                    mm(banks[bk][:, (qc % 4) * 128:(qc % 4 + 1) * 128], WQ3[:, k, qc * 128:(qc + 1) * 128], H2T3[:, k, :], k == 0, k == KC - 1,
                       r=[hT.g(), h2T.g()], w=[BK[bk]])
                if qc % 4 == 3:
                    cp(qTs.bf()[:, (qc - 3) * 128:(qc + 1) * 128], banks[bk][:], r=[BK[bk]], w=[qTs.g((qc - 3) * 256, (qc + 1) * 256)], eng='act')
            for qc in range(16):
                bk = 4 + qc // 4
                mm(banks[bk][:, (qc % 4) * 128:(qc % 4 + 1) * 128], QTS3[:, qc, :], SUBK3[:, qc, :], True, True,
                   r=[qTs.g(qc * 256, (qc + 1) * 256), subkT.g()], w=[BK[bk]])
            for q4 in range(4):
                cp(ssb.f32()[:, q4 * 512:(q4 + 1) * 512], banks[4 + q4][:], r=[BK[4 + q4]], w=[ssb.g(q4 * 2048, (q4 + 1) * 2048)], eng='act')
            if dbg and i == 0:
                dump("dbg_xm", xm_.f32(), [128, 1024], r=[xm_.g()])
                dump("dbg_s", ssb.f32(), [128, 2048], r=[ssb.g()])
            sg_ = lambda qc: ssb.g(qc * 512, (qc + 1) * 512)
            wg_ = lambda qc: wkb.g(qc * 512, (qc + 1) * 512)
            vk = lambda qc, hf: f"v16:{qc}:{hf}"
            ik = lambda qc, hf: f"ixu:{qc}:{hf}"
            ALLV = [vk(q_, h_) for q_ in range(16) for h_ in range(2)]
            ALLI = [ik(q_, h_) for q_ in range(16) for h_ in range(2)]
            for qc in range(16):
                gen('max', [sg_(qc), v16.g()], [vk(qc, 0)], out=V16[:, qc, 0:8], in_=SS3[:, qc, :])
            for qc in range(16):
                gen('max_index', [sg_(qc), vk(qc, 0), ixu.g()], [ik(qc, 0)], out=IXU[:, qc, 0:8], in_max=V16[:, qc, 0:8], in_values=SS3[:, qc, :])
            for qc in range(16):
                gen('match_replace', [sg_(qc), vk(qc, 0)], [wg_(qc)], out=WK3[:, qc, :], in_to_replace=V16[:, qc, 0:8], in_values=SS3[:, qc, :], imm_value=NEG)
            for qc in range(16):
                gen('max', [wg_(qc), v16.g()], [vk(qc, 1)], out=V16[:, qc, 8:16], in_=WK3[:, qc, :])
            for qc in range(16):
                gen('max_index', [wg_(qc), vk(qc, 1), ixu.g()], [ik(qc, 1)], out=IXU[:, qc, 8:16], in_max=V16[:, qc, 8:16], in_values=WK3[:, qc, :])
            cp(ixf.f32(), ixu.u32(), r=ALLI, w=[ixf.g()])
            cand, wk2 = ssb, wkb
            C4 = cand.f32().rearrange("p (h n) -> p h n", h=8)
            W4 = wk2.f32().rearrange("p (h n) -> p h n", h=8)
            vf = v16.f32()
            for hp in range(4):
                tt(mk(cand.f32(), [[256, 2], [16, 16], [1, 16]], hp * 512), mk(vf, [[32, 2], [1, 16], [0, 16]], hp * 64),
                   mk(vf, [[32, 2], [0, 16], [1, 16]], hp * 64 + 16), ALU.add, r=ALLV, w=[cand.g(hp * 2048, (hp + 1) * 2048)])
            C16 = c16.f32().rearrange("p (h j) -> p h j", h=8)
            POS = posu.u32().rearrange("p (h j) -> p h j", h=8)
            cg_ = lambda h_: cand.g(h_ * 1024, (h_ + 1) * 1024)
            w2g_ = lambda h_: wk2.g(h_ * 1024, (h_ + 1) * 1024)
            ck = lambda h_, hf: f"c16:{h_}:{hf}"
            pk = lambda h_, hf: f"pos:{h_}:{hf}"
            ALLC = [ck(h_, f_) for h_ in range(8) for f_ in range(2)]
            ALLP = [pk(h_, f_) for h_ in range(8) for f_ in range(2)]
            for h_ in range(8):
                gen('max', [cg_(h_), c16.g()], [ck(h_, 0)], out=C16[:, h_, 0:8], in_=C4[:, h_, :])
            for h_ in range(8):
                gen('max_index', [cg_(h_), ck(h_, 0), posu.g()], [pk(h_, 0)], out=POS[:, h_, 0:8], in_max=C16[:, h_, 0:8], in_values=C4[:, h_, :])
            for h_ in range(8):
                gen('match_replace', [cg_(h_), ck(h_, 0)], [w2g_(h_)], out=W4[:, h_, :], in_to_replace=C16[:, h_, 0:8], in_values=C4[:, h_, :], imm_value=NEG)
            for h_ in range(8):
                gen('max', [w2g_(h_), c16.g()], [ck(h_, 1)], out=C16[:, h_, 8:16], in_=W4[:, h_, :])
            for h_ in range(8):
                gen('max_index', [w2g_(h_), ck(h_, 1), posu.g()], [pk(h_, 1)], out=POS[:, h_, 8:16], in_max=C16[:, h_, 8:16], in_values=W4[:, h_, :])
            gen('tensor_single_scalar', ALLP, [pau.g()], out=pau.u32(), in_=posu.u32(), scalar=4, op=ALU.logical_shift_right)
            gen('tensor_single_scalar', ALLP, [pbu.g()], out=pbu.u32(), in_=posu.u32(), scalar=15, op=ALU.bitwise_and)
            cp(paf.f32(), pau.u32(), r=[pau.g()], w=[paf.g()])
            cp(pbf.f32(), pbu.u32(), r=[pbu.g()], w=[pbf.g()])
            oh, prod = ssb, wkb
            for (pf_, off_, dst_) in ((paf, 0, i1s), (pbf, 16, i2s)):
                for hp in range(4):
                    og = oh.g(hp * 2048, (hp + 1) * 2048)
                    tt(mk(oh.f32(), [[16, 32], [1, 16]], hp * 512), mk(pf_.f32(), [[1, 32], [0, 16]], hp * 32), mk(iota16.f32(), [[0, 32], [1, 16]]), ALU.is_equal,
                       r=[pf_.g(), iota16.g()], w=[og])
                    tt(mk(prod.f32(), [[256, 2], [16, 16], [1, 16]], hp * 512), mk(oh.f32(), [[256, 2], [16, 16], [1, 16]], hp * 512),
                       mk(ixf.f32(), [[32, 2], [0, 16], [1, 16]], hp * 64 + off_), ALU.mult, r=[og, ixf.g()], w=[og])
                    tred(dst_.f32()[:, hp * 32:(hp + 1) * 32], mk(prod.f32(), [[16, 32], [1, 16]], hp * 512), ALU.add, r=[og], w=[dst_.g()])
            stt(eidf.f32(), i1s.f32(), 128.0, i2s.f32(), ALU.mult, ALU.add, r=[i1s.g(), i2s.g()], w=[eidf.g()])
            cp(eidu_.u32(), eidf.f32(), r=[eidf.g()], w=[eidu_.g()])
            tt(mk(scm.f32(), [[16, 8], [1, 16]]), mk(c16.f32(), [[16, 8], [1, 16]]), mk(c16.f32(), [[16, 8], [0, 16]]), ALU.subtract, r=ALLC, w=[scm.g()])
            act(ee.f32(), scm.f32(), AF.Exp, r=[scm.g()], w=[ee.g()])
            tred(zz.f32(), mk(ee.f32(), [[16, 8], [1, 16]]), ALU.add, r=[ee.g()], w=[zz.g()])
            gen('reciprocal', [zz.g()], [rzz.g()], out=rzz.f32(), in_=zz.f32())
            tt(mk(gate_.f32(), [[16, 8], [1, 16]]), mk(ee.f32(), [[16, 8], [1, 16]]), mk(rzz.f32(), [[1, 8], [0, 16]]), ALU.mult, r=[ee.g(), rzz.g()], w=[gate_.g()])
            if dbg and i == 0:
                dump("dbg_eid", eidf.f32(), [128, 128], r=[eidf.g()])
                dump("dbg_gate", gate_.f32(), [128, 128], r=[gate_.g()])

        def stageY(i, pump):
            par = i % 2
            xm_, h2_, eidu_, gate_ = xm[par], h2[par], eidu[par], gate[par]
            t0 = i * 128
            EID = eidu_.u32()

            def gather(out_ap, slot, r, w):
                P.add('pool', lambda e: e.indirect_dma_start(out=out_ap, out_offset=None, in_=etab_d.ap(),
                                                             in_offset=bass.IndirectOffsetOnAxis(ap=EID[:, slot:slot + 1], axis=0)),
                      r=r, w=w, dma=True)

            def slot_tail(sl):
                cb = cbuf[sl % NBUF]
                dg_ = dgb[sl % 4]
                act(wgt.f32()[:, sl:sl + 1], gl.f32()[:, sl:sl + 1], AF.Copy, r=[f"gl{sl}", gate_.g(), wgt.g()], w=[f"wgt{sl}"],
                    scale=gate_.f32()[:, sl:sl + 1])
                P.add('act', (lambda dg_=dg_, sl=sl: (lambda e: e.activation(out=dg_.bf(), in_=ident_f.f32(), func=AF.Copy, scale=wgt.f32()[:, sl:sl + 1])))(),
                      r=[ident_f.g(), f"wgt{sl}"], w=[dg_.g()])
                for half in range(2):
                    mm(banks[half][:], dg_.bf(), cb.bf()[:, D + half * 512:D + (half + 1) * 512], sl == 0, sl == 127,
                       r=[dg_.g(), cb.g(2048, 4096)], w=[BK[half]])

            for sl in range(128):
                cb = cbuf[sl % NBUF]
                gather(cb.bf(), sl, r=[eidu_.g()] + ETAB_KEYS, w=[cb.g()])
                P.add('dve', (lambda cb=cb, sl=sl: (lambda e: e.scalar_tensor_tensor(out=SINK, in0=cb.bf()[:, 0:D], scalar=1.0, in1=h2_.bf(), op0=ALU.mult, op1=ALU.mult,
                                                                                 accum_out=actv.f32()[:, sl:sl + 1])))(),
                      r=[cb.g(0, 2048), h2_.g(), actv.g()], w=[f"actv{sl}"])
                act(gl.f32()[:, sl:sl + 1], actv.f32()[:, sl:sl + 1], AF.Gelu, r=[f"actv{sl}", gl.g()], w=[f"gl{sl}"])
                if sl >= 1:
                    slot_tail(sl - 1)
                pump(5 if sl < 40 else 2)
            slot_tail(127)
            for half in range(2):
                hs = slice(half * 512, (half + 1) * 512)
                tt(tmpf.f32()[:, hs], banks[half][:], g2.f32()[:, hs], ALU.mult, r=[BK[half], g2.g()], w=[tmpf.g(half * 2048, (half + 1) * 2048)])
                tt(ot.f32()[:, hs], tmpf.f32()[:, hs], xm_.f32()[:, hs], ALU.add, r=[tmpf.g(half * 2048, (half + 1) * 2048), xm_.g()], w=[ot.g(half * 2048, (half + 1) * 2048)])
            if dbg and i == 0:
                dump("dbg_xo", ot.f32(), [128, 1024], r=[ot.g()])
            act(SINK, ot.f32(), AF.Square, r=[ot.g()], w=[small.g()], accum_out=sm(SM_SS3))
            rstd_chain(SM_SS3, SM_LN3, SM_RSTD3, D, 1e-6)
            stt(ot.f32(), ot.f32(), sm(SM_RSTD3), nf_bc.f32(), ALU.mult, ALU.mult, r=[ot.g(), small.g(), nf_bc.g()], w=[ot.g()])
            dma('sp', out_d.ap()[b, t0:t0 + 128, :], ot.f32(), r=[ot.g()], w=[f"out{b}_{i}"])

        def drain(q, n=None):
            k = 0
            while q and (n is None or k < n):
                P.add(*q.pop(0))
                k += 1

        P.capture = []
        stageX(0)
        q = P.capture
        P.capture = None
        drain(q)
        for i in range(ntiles):
            if i + 1 < ntiles:
                P.capture = []
                stageX(i + 1)
                q = P.capture
                P.capture = None
            else:
                q = []
            stageY(i, lambda n: drain(q, n))
            drain(q)

    P.emit(nc, stack)
    stack.close()
    return nc, list(dbg_d.keys())


def _prep_inputs(inputs):
    f = np.float32
    x = np.asarray(inputs['x'], f)
    c = np.asarray(inputs['c'], f)
    cst = _host_consts()
    rep = lambda v, n=128: np.ascontiguousarray(np.broadcast_to(np.asarray(v, f).reshape(1, -1), (n, np.asarray(v).size)))
    nw = np.concatenate([np.asarray(inputs['norm_mix_w'], f).reshape(-1), np.asarray(inputs['norm_ffn_w'], f).reshape(-1),
                         np.asarray(inputs['norm_final_w'], f).reshape(-1)])
    lam = np.concatenate([np.asarray(inputs[k], f).reshape(-1) for k in ('lambda_q1', 'lambda_k1', 'lambda_q2', 'lambda_k2')])
    sk1 = np.asarray(inputs['sub_keys_1'], f)[0]
    sk2 = np.asarray(inputs['sub_keys_2'], f)[0]
    subkT = np.zeros((128, 16, 128), f)
    for h in range(8):
        subkT[:, h * 2 + 0, :] = sk1[h].T
        subkT[:, h * 2 + 1, :] = sk2[h].T
    shared = {
        'w_ada': np.ascontiguousarray(np.asarray(inputs['w_ada'], f)[0]),
        'b_ada_bc': rep(inputs['b_ada']),
        'nw_bc': rep(nw),
        'w_in': np.ascontiguousarray(np.asarray(inputs['w_in'], f)[0]),
        'w_out': np.ascontiguousarray(np.asarray(inputs['w_out'], f)[0]),
        'w_query': np.ascontiguousarray(np.asarray(inputs['w_query'], f)[0]),
        'pool_w': np.ascontiguousarray(np.asarray(inputs['pool_w'], f)[0]),
        'pool_scaleT': np.ascontiguousarray(np.asarray(inputs['pool_scale'], f).reshape(8, 128).T),
        'subkT': np.ascontiguousarray(subkT.reshape(128, 2048)),
        'e_down': np.ascontiguousarray(np.asarray(inputs['expert_down'], f)[0]),
        'e_up': np.ascontiguousarray(np.asarray(inputs['expert_up'], f)[0]),
        'lam_bc': rep(lam),
        'sublnT': np.ascontiguousarray(np.asarray(inputs['subln_w'], f).reshape(128, 1)),
        'relb_bc': rep(np.asarray(inputs['rel_bias'], f).reshape(-1)),
        'masks': np.ascontiguousarray(cst['masks'].reshape(128, -1)),
        'negmask': cst['negmask'],
        'band': np.ascontiguousarray(cst['band'].reshape(128, -1)),
        'ident': cst['ident'],
        'iota16': cst['iota16'],
        'sel0': cst['sel0'],
    }
    in_maps = []
    for core in range(NCORES):
        m = dict(shared)
        m['x'] = np.ascontiguousarray(x[core * NB:(core + 1) * NB])
        cc = c[core * NB:(core + 1) * NB]
        m['cT'] = np.ascontiguousarray(cc.reshape(NB, KC, 128).transpose(2, 0, 1).reshape(128, NB * KC))
        in_maps.append(m)
    return in_maps


_CACHE = {}


def kernel(**inputs):
    in_maps = _prep_inputs(inputs)
    if 'nc' not in _CACHE:
        _CACHE['nc'] = build()[0]
    nc = _CACHE['nc']
    res = run_bass_kernel_spmd(nc, in_maps, core_ids=list(range(NCORES)))
    out = np.concatenate([np.asarray(r['out']) for r in res.results], axis=0)
    return out.astype(np.float32)
```

```python
import os
import math
import contextlib
import numpy as np
import concourse.bass as bass
import concourse.mybir as mybir
from concourse.bass_utils import run_bass_kernel_spmd

dt = mybir.dt
AF = mybir.ActivationFunctionType
ALU = mybir.AluOpType
AX = mybir.AxisListType
F32, BF16, U32 = dt.float32, dt.bfloat16, dt.uint32

NCORES = 8
NB = 2
S = 2048
NT = 16
D = 1024
KC = 8
H = 8
DIN = 6144
NEXP = 16384
OFF = 8.0
NEG = -1.0e30
G = 512


class Prog:
    def __init__(self):
        self.ops = []
        self.last_w = {}
        self.readers = {}
        self.capture = None

    def add(self, eng, fn, r=(), w=(), dma=False):
        if self.capture is not None:
            self.capture.append((eng, fn, r, w, dma))
            return None
        i = len(self.ops)
        deps = set()
        rk = _flat(r)
        wk = _flat(w)
        for k in rk:
            lw = self.last_w.get(k)
            if lw is not None:
                deps.add(lw)
        for k in wk:
            lw = self.last_w.get(k)
            if lw is not None:
                deps.add(lw)
            deps.update(self.readers.get(k, ()))
        deps.discard(i)
        for k in rk:
            self.readers.setdefault(k, []).append(i)
        for k in wk:
            self.last_w[k] = i
            self.readers[k] = []
        self.ops.append(dict(eng=eng, fn=fn, deps=deps, dma=dma, sig=None, need=False, pre=None))
        return i

    def emit(self, nc, stack):
        ops = self.ops
        EPOCH = 30000
        NSLOT = {'sp': 24, 'pool': 24, 'act': 8}
        for o in ops:
            for d in o['deps']:
                p = ops[d]
                if p['eng'] == 'pe' and o['eng'] == 'pe' and not p['dma'] and not o['dma']:
                    continue
                p['need'] = True
        cnt = {e: 0 for e in ('pe', 'act', 'dve', 'pool', 'sp')}
        esems = {e: [] for e in cnt}
        dsems = {q: [stack.enter_context(nc.semaphore(f"d_{q}_{i}")) for i in range(n)] for q, n in NSLOT.items()}
        duse = {q: [0] * n for q, n in NSLOT.items()}
        dnext = {q: 0 for q in NSLOT}
        for o in ops:
            e = o['eng']
            if o['dma']:
                s = dnext[e]
                dnext[e] = (s + 1) % NSLOT[e]
                if duse[e][s] > 0:
                    o['pre'] = (dsems[e][s], 16 * duse[e][s])
                duse[e][s] += 1
                o['sig'] = (dsems[e][s], 16 * duse[e][s])
            elif o['need']:
                ep = cnt[e] // EPOCH
                while len(esems[e]) <= ep:
                    esems[e].append(stack.enter_context(nc.semaphore(f"e_{e}_{len(esems[e])}")))
                cnt[e] += 1
                o['sig'] = (esems[e][ep], cnt[e] - ep * EPOCH)
        by_eng = {e: [o for o in ops if o['eng'] == e] for e in cnt}
        final_waits = []
        for q in NSLOT:
            for s in range(NSLOT[q]):
                if duse[q][s] > 0:
                    final_waits.append((dsems[q][s], 16 * duse[q][s]))

        def run(ename, eng):
            waited = {}
            for o in by_eng[ename]:
                needs = {}
                for d in o['deps']:
                    p = ops[d]
                    if p['eng'] == 'pe' and ename == 'pe' and not p['dma'] and not o['dma']:
                        continue
                    sem, val = p['sig']
                    key = id(sem)
                    if needs.get(key, (None, 0))[1] < val:
                        needs[key] = (sem, val)
                if o['pre'] is not None:
                    sem, val = o['pre']
                    key = id(sem)
                    if needs.get(key, (None, 0))[1] < val:
                        needs[key] = (sem, val)
                for key, (sem, val) in needs.items():
                    if waited.get(key, 0) < val:
                        eng.wait_ge(sem, val)
                        waited[key] = val
                inst = o['fn'](eng)
                if o['sig'] is not None:
                    inst.then_inc(o['sig'][0], 16 if o['dma'] else 1)
            if ename == 'sp':
                for sem, val in final_waits:
                    eng.wait_ge(sem, val)

        with nc.Block() as block:
            @block.tensor
            def _(e):
                run('pe', e)

            @block.scalar
            def _(e):
                run('act', e)

            @block.vector
            def _(e):
                run('dve', e)

            @block.gpsimd
            def _(e):
                run('pool', e)

            @block.sync
            def _(e):
                run('sp', e)


def _flat(x):
    out = []
    for k in x:
        if isinstance(k, (list, tuple, set, range)):
            out.extend(_flat(k))
        else:
            out.append(k)
    return out


class Buf:
    def __init__(self, arena, off, nbytes):
        assert off % 4 == 0 and nbytes % 4 == 0
        self.A, self.off, self.nbytes = arena, off, nbytes

    def g(self, lo=0, hi=None):
        hi = self.nbytes if hi is None else hi
        return range((self.off + lo) // G, (self.off + hi + G - 1) // G)

    def f32(self):
        return self.A[:, self.off // 4:(self.off + self.nbytes) // 4]

    def bf(self):
        return self.A[:, self.off // 4:(self.off + self.nbytes) // 4].bitcast(BF16)

    def u32(self):
        return self.A[:, self.off // 4:(self.off + self.nbytes) // 4].bitcast(U32)

    def sub(self, lo, n):
        return Buf(self.A, self.off + lo, n)


class Alloc:
    def __init__(self, arena, base, limit):
        self.A, self.p, self.limit = arena, base, limit

    def get(self, nbytes):
        nb = (nbytes + G - 1) // G * G
        b = Buf(self.A, self.p, (nbytes + 3) // 4 * 4)
        self.p += nb
        assert self.p <= self.limit, (self.p, self.limit)
        return b


def mk(ap, pattern, extra_off=0):
    return bass.AP(tensor=ap.tensor, offset=ap.offset + extra_off, ap=[list(ap.ap[0])] + [list(p) for p in pattern])


def _t5_bucket(d):
    d = np.maximum(d, 0)
    x = np.maximum(d, 1).astype(np.float32) / np.float32(16)
    large = 16 + (np.log(x).astype(np.float32) / np.float32(math.log(128 / 16)) * np.float32(16)).astype(np.int32)
    large = np.minimum(large, 31)
    return np.where(d < 16, d, large)


def _host_consts():
    kk = np.arange(128)[:, None]
    qq = np.arange(256)[None, :]
    dist = qq - kk
    valid = dist >= 0
    bk = _t5_bucket(dist)
    masks = np.zeros((128, 32, 256), np.float32)
    for b in range(32):
        masks[:, b, :] = (valid & (bk == b)).astype(np.float32)
    negmask = np.where(valid, 0.0, NEG).astype(np.float32)
    band = np.zeros((128, 12, 128), np.float32)
    s = np.arange(128)[:, None]
    t = np.arange(128)[None, :]
    for gi, w in enumerate((2, 4, 8, 16)):
        cnt0 = np.minimum(t + 1, w).astype(np.float32)
        band[:, gi * 3 + 0, :] = ((s <= t) & (s > t - w)) / cnt0 - (s == t)
        band[:, gi * 3 + 1, :] = ((s <= t) & (s > t - w)) / np.float32(w) - (s == t)
        band[:, gi * 3 + 2, :] = (s > 128 + t - w) / np.float32(w)
    ident = np.eye(128, dtype=np.float32)
    iota16 = np.broadcast_to(np.arange(16, dtype=np.float32)[None, :], (128, 16)).copy()
    sel0 = np.zeros((64, 128), np.float32)
    sel0[0, :] = 1.0
    sel0[32, :] = 1.0
    return dict(masks=masks, negmask=negmask, band=band, ident=ident, iota16=iota16, sel0=sel0)


def build(stage=99, dbg=False):
    nc = bass.Bass("TRN2", target_bir_lowering=False)
    P = Prog()

    def din(name, shape, dtype=F32):
        return nc.dram_tensor(name, list(shape), dtype, kind="ExternalInput")

    x_d = din("x", [NB, S, D])
    cT_d = din("cT", [128, NB * KC])
    wada_d = din("w_ada", [D, DIN])
    bada_d = din("b_ada_bc", [128, DIN])
    nw_d = din("nw_bc", [128, 3 * D])
    win_d = din("w_in", [D, DIN])
    wout_d = din("w_out", [D, D])
    wqry_d = din("w_query", [D, 2048])
    poolw_d = din("pool_w", [4, 256, 256])
    psc_d = din("pool_scaleT", [128, 8])
    subk_d = din("subkT", [128, 16 * 128])
    edown_d = din("e_down", [NEXP, D])
    eup_d = din("e_up", [NEXP, D])
    lam_d = din("lam_bc", [128, 256])
    subln_d = din("sublnT", [128, 1])
    relb_d = din("relb_bc", [128, 256])
    masks_d = din("masks", [128, 32 * 256])
    negm_d = din("negmask", [128, 256])
    band_d = din("band", [128, 12 * 128])
    ident_d = din("ident", [128, 128])
    iota_d = din("iota16", [128, 16])
    sel0_d = din("sel0", [64, 128])
    out_d = nc.dram_tensor("out", [NB, S, D], F32, kind="ExternalOutput")
    etab_d = nc.dram_tensor("etab", [NEXP, 2 * D], BF16, kind="Internal")
    dbg_d = {}

    def dbg_out(name, shape):
        dbg_d[name] = nc.dram_tensor(name, list(shape), F32, kind="ExternalOutput")
        return dbg_d[name]

    stack = contextlib.ExitStack()
    TOT = 212480
    arena = stack.enter_context(nc.sbuf_tensor("arena", [128, TOT // 4], F32))
    banks = [stack.enter_context(nc.psum_tensor(f"B{i}", [128, 512], F32)) for i in range(8)]
    BK = [f"B{i}" for i in range(8)]

    al = Alloc(arena, 0, TOT)
    ident_f = al.get(512)
    ident_b = al.get(256)
    ones_b = al.get(256)
    ones_f = al.get(512)
    sel0 = al.get(512)
    relb = al.get(1024)
    rb31m = al.get(32)
    lamv = al.get(1024)
    small = al.get(512)
    pscT = al.get(32)
    band = al.get(12 * 128 * 2)
    subkT = al.get(16 * 128 * 2)
    iota16 = al.get(64)
    sink = al.get(64)
    nf_bc = al.get(4096)
    sh1 = al.get(4096)
    a1 = al.get(4096)
    g1 = al.get(4096)
    sh2 = al.get(4096)
    a2 = al.get(4096)
    g2 = al.get(4096)
    hT = al.get(KC * S * 2)
    mT = al.get(KC * S * 2)
    wout = al.get(KC * D * 2)
    work_base = al.p
    SM = small.f32()
    (SM_S1, SM_S2, SM_E1, SM_E2, SM_NLAM, SM_WSUB, SM_SS, SM_RSTD, SM_LN, SM_SS2, SM_RSTD2, SM_SS3, SM_RSTD3,
     SM_LN2, SM_LN3, SM_SUBLN) = range(16)

    def sm(i):
        return SM[:, i:i + 1]

    def smg(i):
        return small.g()

    def dma(q, out, in_, r, w):
        P.add(q, lambda e: e.dma_start(out=out, in_=in_), r=r, w=w, dma=True)

    def mm(out, lhsT, rhs, start, stop, r, w, **kw):
        P.add('pe', lambda e: e.matmul(out, lhsT, rhs, start=start, stop=stop, **kw), r=r, w=w)

    def tr(out, in_, ident, r, w):
        P.add('pe', lambda e: e.transpose(out, in_, ident), r=r, w=w)

    def act(out, in_, func, r, w, bias=None, scale=None, accum_out=None, eng='act'):
        def f(e):
            kw = {}
            if bias is not None:
                kw['bias'] = bias
            if scale is not None:
                kw['scale'] = scale
            if accum_out is not None:
                kw['accum_out'] = accum_out
            return e.activation(out=out, in_=in_, func=func, **kw)
        P.add('act', f, r=r, w=w)

    def tt(out, in0, in1, op, r, w, eng='dve'):
        P.add(eng, lambda e: e.tensor_tensor(out=out, in0=in0, in1=in1, op=op), r=r, w=w)

    def ts(out, in0, s1, s2, op0, op1, r, w, eng='dve'):
        if op1 is None:
            P.add(eng, lambda e: e.tensor_scalar(out=out, in0=in0, scalar1=s1, scalar2=None, op0=op0), r=r, w=w)
        else:
            P.add(eng, lambda e: e.tensor_scalar(out=out, in0=in0, scalar1=s1, scalar2=s2, op0=op0, op1=op1), r=r, w=w)

    def stt(out, in0, scalar, in1, op0, op1, r, w):
        P.add('dve', lambda e: e.scalar_tensor_tensor(out=out, in0=in0, scalar=scalar, in1=in1, op0=op0, op1=op1), r=r, w=w)

    def cp(out, in_, r, w, eng='dve'):
        if eng == 'act':
            P.add('act', lambda e: e.copy(out=out, in_=in_), r=r, w=w)
        else:
            P.add(eng, lambda e: e.tensor_copy(out=out, in_=in_), r=r, w=w)

    def ttr(out, in0, in1, accum_out, r, w):
        P.add('dve', lambda e: e.scalar_tensor_tensor(out=out, in0=in0, scalar=1.0, in1=in1, op0=ALU.mult, op1=ALU.mult,
                                                     accum_out=accum_out), r=r, w=w)

    def tred(out, in_, op, r, w):
        P.add('dve', lambda e: e.tensor_reduce(out=out, in_=in_, axis=AX.X, op=op), r=r, w=w)

    def memset(ap, val, r, w, eng='dve'):
        P.add(eng, lambda e: e.memset(ap, val), r=r, w=w)

    def rstd_chain(ss_i, ln_i, rstd_i, n, eps):
        ts(sm(ln_i), sm(ss_i), 1.0 / n, eps, ALU.mult, ALU.add, r=[small.g()], w=[small.g()])
        act(sm(ln_i), sm(ln_i), AF.Ln, r=[small.g()], w=[small.g()])
        act(sm(rstd_i), sm(ln_i), AF.Exp, r=[small.g()], w=[small.g()], scale=-0.5)

    def dump(name, ap_sb, shape, r):
        if name in dbg_d:
            return
        d = dbg_out(name, shape)
        dma('sp', d.ap(), ap_sb, r=r, w=[name])

    dma('sp', ident_f.f32(), ident_d.ap(), r=[], w=[ident_f.g()])
    dma('sp', sel0.f32()[0:64, :], sel0_d.ap(), r=[], w=[sel0.g()])
    dma('sp', relb.f32(), relb_d.ap(), r=[], w=[relb.g()])
    dma('sp', lamv.f32(), lam_d.ap(), r=[], w=[lamv.g()])
    dma('sp', pscT.f32(), psc_d.ap(), r=[], w=[pscT.g()])
    dma('sp', iota16.f32(), iota_d.ap(), r=[], w=[iota16.g()])
    dma('sp', nf_bc.f32(), nw_d.ap()[:, 2 * D:3 * D], r=[], w=[nf_bc.g()])
    dma('sp', sm(SM_SUBLN), subln_d.ap(), r=[], w=[small.g()])
    dma('pool', band.bf(), band_d.ap(), r=[], w=[band.g()])
    dma('pool', subkT.bf(), subk_d.ap(), r=[], w=[subkT.g()])
    ETAB_JOBS = [(ti, r0) for ti in range(2) for r0 in range(0, NEXP, 1024)]
    ETAB_KEYS = [f"etab{ti}_{r0}" for (ti, r0) in ETAB_JOBS]

    def precast(n):
        for _ in range(n):
            if not ETAB_JOBS:
                return
            ti, r0 = ETAB_JOBS.pop(0)
            tbl = (edown_d, eup_d)[ti]
            dma('pool', etab_d.ap()[r0:r0 + 1024, ti * D:(ti + 1) * D], tbl.ap()[r0:r0 + 1024, :], r=[], w=[f"etab{ti}_{r0}"])

    cp(ident_b.bf(), ident_f.f32(), r=[ident_f.g()], w=[ident_b.g()])
    memset(ones_b.bf(), 1.0, r=[], w=[ones_b.g()])
    memset(ones_f.f32(), 1.0, r=[], w=[ones_f.g()])
    ts(rb31m.f32(), relb.f32()[:, 31 * 8:32 * 8], -OFF, None, ALU.add, None, r=[relb.g()], w=[rb31m.g()])
    ts(sm(SM_WSUB), sm(SM_SUBLN), 0.8, None, ALU.mult, None, r=[small.g()], w=[small.g()])
    wa = Alloc(arena, work_base, TOT)
    junk64 = wa.get(256)
    LV = lamv.f32()
    ttr(junk64.f32(), LV[:, 0:64], LV[:, 64:128], sm(SM_S1), r=[lamv.g()], w=[junk64.g(), small.g()])
    ttr(junk64.f32(), LV[:, 128:192], LV[:, 192:256], sm(SM_S2), r=[lamv.g()], w=[junk64.g(), small.g()])
    act(sm(SM_E1), sm(SM_S1), AF.Exp, r=[small.g()], w=[small.g()])
    act(sm(SM_E2), sm(SM_S2), AF.Exp, r=[small.g()], w=[small.g()])
    tt(sm(SM_NLAM), sm(SM_E2), sm(SM_E1), ALU.subtract, r=[small.g()], w=[small.g()])
    ts(sm(SM_NLAM), sm(SM_NLAM), -0.2, None, ALU.add, None, r=[small.g()], w=[small.g()])
    bias_scr = nc.dram_tensor("bias_scr", [128, 2048], F32, kind="Internal")
    biasT = wa.get(8 * 1024)
    mk_chunk = wa.get(8 * 256 * 4)
    BT3 = biasT.f32().rearrange("p (h q) -> p h q", h=8)
    for h in range(8):
        dma('sp', BT3[:, h, :], negm_d.ap(), r=[], w=[biasT.g(h * 1024, (h + 1) * 1024)])
    for c in range(4):
        dma('sp', mk_chunk.f32(), masks_d.ap()[:, c * 2048:(c + 1) * 2048], r=[], w=[mk_chunk.g()])
        MC = mk_chunk.f32().rearrange("p (b q) -> p b q", b=8)
        for bb in range(8):
            b = c * 8 + bb
            for h in range(8):
                stt(BT3[:, h, :], MC[:, bb, :], relb.f32()[:, b * 8 + h:b * 8 + h + 1], BT3[:, h, :], ALU.mult, ALU.add,
                    r=[mk_chunk.g(), relb.g(), biasT.g(h * 1024, (h + 1) * 1024)], w=[biasT.g(h * 1024, (h + 1) * 1024)])
    ts(biasT.f32(), biasT.f32(), -OFF, None, ALU.add, None, r=[biasT.g()], w=[biasT.g()])
    dma('sp', bias_scr.ap(), biasT.f32(), r=[biasT.g()], w=["bias_scr"])
    if dbg:
        dump("dbg_bias", biasT.f32(), [128, 2048], r=[biasT.g()])
        dump("dbg_small", small.f32()[:, 0:16], [128, 16], r=[small.g()])

    hT3 = hT.bf().rearrange("p (k t) -> p k t", k=KC)
    mT3 = mT.bf().rearrange("p (k t) -> p k t", k=KC)

    def hT_g(c0, c1):
        return [hT.g(k * S * 2 + c0 * 2, k * S * 2 + c1 * 2) for k in range(KC)]

    def mT_g(k, c0, c1):
        return mT.g(k * S * 2 + c0 * 2, k * S * 2 + c1 * 2)

    BTb = banks[7][:].bitcast(BF16)

    for b in range(NB):
        wa = Alloc(arena, work_base, TOT)
        cact = wa.get(64)
        crep = wa.get(KC * 128 * 4)
        wblk = [wa.get(KC * 512 * 4) for _ in range(2)]
        bblk = [wa.get(2048) for _ in range(2)]
        mtmp = wa.get(2048)
        nwt = wa.get(4096)
        cin = wa.get(64)
        dma('sp', cin.f32()[:, 0:KC], cT_d.ap()[:, b * KC:(b + 1) * KC], r=[], w=[cin.g()])
        act(cact.f32()[:, 0:KC], cin.f32()[:, 0:KC], AF.Silu, r=[cin.g()], w=[cact.g()])
        cp(crep.f32().rearrange("p (k m) -> p k m", k=KC), mk(cact.f32(), [[1, KC], [0, 128]]), r=[cact.g()], w=[crep.g()])
        CR = crep.f32().rearrange("p (k m) -> p k m", k=KC)
        wada_v = wada_d.ap().rearrange("(k p) n -> p k n", p=128)
        dsts = [sh1, sh1, a1, a1, g1, g1, sh2, sh2, a2, a2, g2, g2]
        for nb_ in range(12):
            wb = wblk[nb_ % 2]
            bb_ = bblk[nb_ % 2]
            dma('sp', wb.f32().rearrange("p (k n) -> p k n", k=KC), wada_v[:, :, nb_ * 512:(nb_ + 1) * 512], r=[], w=[wb.g()])
            dma('sp', bb_.f32(), bada_d.ap()[:, nb_ * 512:(nb_ + 1) * 512], r=[], w=[bb_.g()])
            WB = wb.f32().rearrange("p (k n) -> p k n", k=KC)
            for k in range(KC):
                mm(banks[6][:], CR[:, k, :], WB[:, k, :], k == 0, k == KC - 1, r=[crep.g(), wb.g()], w=[BK[6]])
            dst = dsts[nb_]
            half = nb_ % 2
            dst_ap = dst.f32()[:, half * 512:(half + 1) * 512]
            dg_ = dst.g(half * 2048, (half + 1) * 2048)
            if nb_ in (2, 3, 8, 9):
                which = 0 if nb_ in (2, 3) else 1
                dma('sp', nwt.f32()[:, 0:512], nw_d.ap()[:, which * D + half * 512: which * D + (half + 1) * 512], r=[], w=[nwt.g()])
                tt(mtmp.f32(), banks[6][:], bb_.f32(), ALU.add, r=[BK[6], bb_.g()], w=[mtmp.g()])
                stt(dst_ap, mtmp.f32(), 1.0, nwt.f32()[:, 0:512], ALU.add, ALU.mult, r=[mtmp.g(), nwt.g()], w=[dg_])
            else:
                tt(dst_ap, banks[6][:], bb_.f32(), ALU.add, r=[BK[6], bb_.g()], w=[dg_])
        if dbg and b == 0:
            dump("dbg_a1", a1.f32(), [128, 1024], r=[a1.g()])
            dump("dbg_g2", g2.f32(), [128, 1024], r=[g2.g()])

        wa = Alloc(arena, work_base, TOT)
        xt = [wa.get(4096) for _ in range(2)]
        tmpf = wa.get(4096)
        junkb = wa.get(2048)
        hbf = wa.get(2048)
        for i in range(NT):
            xb = xt[i % 2]
            dma('sp', xb.f32(), x_d.ap()[b, i * 128:(i + 1) * 128, :], r=[], w=[xb.g()])
            act(junkb.bf(), xb.f32(), AF.Square, r=[xb.g()], w=[junkb.g(), small.g()], accum_out=sm(SM_SS))
            rstd_chain(SM_SS, SM_LN, SM_RSTD, D, 1e-6)
            stt(tmpf.f32(), xb.f32(), sm(SM_RSTD), a1.f32(), ALU.mult, ALU.mult, r=[xb.g(), small.g(), a1.g()], w=[tmpf.g()])
            tt(hbf.bf(), tmpf.f32(), sh1.f32(), ALU.add, r=[tmpf.g(), sh1.g()], w=[hbf.g()])
            for k in range(KC):
                tr(BTb[:, k * 128:(k + 1) * 128], hbf.bf()[:, k * 128:(k + 1) * 128], ident_b.bf(), r=[hbf.g(), ident_b.g()], w=[BK[7]])
            cp(hT3[:, :, i * 128:(i + 1) * 128], BTb.rearrange("p (k t) -> p k t", k=KC), r=[BK[7]], w=hT_g(i * 128, (i + 1) * 128), eng='act')
        if dbg and b == 0:
            wa2 = Alloc(arena, wa.p, TOT)
            dtmp = wa2.get(8192)
            cp(dtmp.f32(), hT3[:, 0, :], r=hT_g(0, S), w=[dtmp.g()])
            dump("dbg_hT0", dtmp.f32(), [128, 2048], r=[dtmp.g()])
        if stage <= 1:
            continue

        win_v = win_d.ap().rearrange("(k p) n -> p k n", p=128)
        wa = Alloc(arena, work_base, TOT)
        wp = wa.get(KC * 256 * 2)
        wgp = wa.get(KC * 256 * 2)
        pw = wa.get(2 * 256 * 2)
        pg = wa.get(NT * 256 * 2)
        pT = [wa.get(512 * 2) for _ in range(2)]
        sg = wa.get(512 * 4)
        WP = wp.bf().rearrange("p (k n) -> p k n", k=KC)
        WGP = wgp.bf().rearrange("p (k n) -> p k n", k=KC)
        PW = pw.bf().rearrange("p (c n) -> p c n", c=2)
        PG = pg.bf().rearrange("p (i n) -> p i n", i=NT)
        BAND = band.bf().rearrange("p (j n) -> p j n", j=12)
        for g in range(4):
            dma('pool', WP, win_v[:, :, 3072 + g * 256:3072 + (g + 1) * 256], r=[], w=[wp.g()])
            dma('pool', WGP, win_v[:, :, 5120 + g * 256:5120 + (g + 1) * 256], r=[], w=[wgp.g()])
            dma('pool', PW, poolw_d.ap()[g].rearrange("(c p) n -> p c n", p=128), r=[], w=[pw.g()])
            for i in range(NT):
                for k in range(KC):
                    mm(banks[7][:, (i % 2) * 256:(i % 2 + 1) * 256], hT3[:, k, i * 128:(i + 1) * 128], WP[:, k, :], k == 0, k == KC - 1,
                       r=[hT_g(i * 128, (i + 1) * 128), wp.g()], w=[BK[7]])
                if i % 2 == 1:
                    cp(pg.bf()[:, (i - 1) * 256:(i + 1) * 256], banks[7][:], r=[BK[7]], w=[pg.g((i - 1) * 512, (i + 1) * 512)], eng='act')
            for c in range(4):
                for cc in range(2):
                    for il in range(4):
                        i = c * 4 + il
                        cur = g * 3 + (0 if i == 0 else 1)
                        mm(banks[cc][:, il * 128:(il + 1) * 128], PG[:, i, cc * 128:(cc + 1) * 128], BAND[:, cur, :], True, i == 0,
                           r=[pg.g(i * 512, (i + 1) * 512), band.g()], w=[BK[cc]])
                        if i > 0:
                            mm(banks[cc][:, il * 128:(il + 1) * 128], PG[:, i - 1, cc * 128:(cc + 1) * 128], BAND[:, g * 3 + 2, :], False, True,
                               r=[pg.g((i - 1) * 512, i * 512), band.g()], w=[BK[cc]])
                    cp(pT[cc].bf(), banks[cc][:], r=[BK[cc]], w=[pT[cc].g()], eng='act')
                for ec in range(2):
                    kch = g * 2 + ec
                    mm(banks[2][:], PW[:, 0, ec * 128:(ec + 1) * 128], pT[0].bf(), True, False, r=[pw.g(), pT[0].g()], w=[BK[2]])
                    mm(banks[2][:], PW[:, 1, ec * 128:(ec + 1) * 128], pT[1].bf(), False, True, r=[pw.g(), pT[1].g()], w=[BK[2]])
                    for k in range(KC):
                        mm(banks[3][:], WGP[:, k, ec * 128:(ec + 1) * 128], hT3[:, k, c * 512:(c + 1) * 512], k == 0, k == KC - 1,
                           r=[wgp.g(), hT_g(c * 512, (c + 1) * 512)], w=[BK[3]])
                    act(sg.f32(), banks[3][:], AF.Sigmoid, r=[BK[3]], w=[sg.g()])
                    stt(mT3[:, kch, c * 512:(c + 1) * 512], banks[2][:], pscT.f32()[:, kch:kch + 1], sg.f32(), ALU.mult, ALU.mult,
                        r=[BK[2], pscT.g(), sg.g()], w=[mT_g(kch, c * 512, (c + 1) * 512)])
        if dbg and b == 0 and stage == 2:
            wa2 = Alloc(arena, wa.p, TOT)
            dtmp = wa2.get(8192)
            for kk_ in (0, 5):
                cp(dtmp.f32(), mT3[:, kk_, :], r=[mT.g()], w=[dtmp.g()])
                dump(f"dbg_mT{kk_}", dtmp.f32(), [128, 2048], r=[dtmp.g()])
        if stage <= 2:
            continue

        wa = Alloc(arena, work_base, TOT)
        wq = wa.get(KC * 128 * 2)
        wk_ = wa.get(KC * 128 * 2)
        wv = wa.get(KC * 128 * 2)
        wg = wa.get(KC * 128 * 2)
        QTz = [wa.get(S * 2) for _ in range(2)]
        KT = wa.get(S * 2)
        Vb = wa.get(NT * 128 * 2)
        sgT = wa.get(S * 2)
        PT = [[wa.get(512 * 2) for _ in range(2)] for _ in range(2)]
        ntmp = [wa.get(256 * 4) for _ in range(2)]
        O1s = wa.get(2048)
        O2s = wa.get(2048)
        lnz = wa.get(2048)
        rz = wa.get(2048)
        rz2 = wa.get(2048)
        sq = wa.get(2048)
        t1 = wa.get(2048)
        rs = wa.get(2048)
        biasT = wa.get(8 * 1024)
        BT3 = biasT.f32().rearrange("p (h q) -> p h q", h=8)
        dma('sp', biasT.f32(), bias_scr.ap(), r=["bias_scr"], w=[biasT.g()])
        WQ = wq.bf().rearrange("p (k n) -> p k n", k=KC)
        WK = wk_.bf().rearrange("p (k n) -> p k n", k=KC)
        WV = wv.bf().rearrange("p (k n) -> p k n", k=KC)
        WG = wg.bf().rearrange("p (k n) -> p k n", k=KC)
        V3 = Vb.bf().rearrange("p (i n) -> p i n", i=NT)
        nheads = H if stage > 3 or not dbg else 1
        memset(QTz[0].bf()[64:128, :], 0.0, r=[], w=[QTz[0].g()])
        memset(QTz[1].bf()[0:64, :], 0.0, r=[], w=[QTz[1].g()])
        for h in range(nheads):
            dma('pool', WQ, win_v[:, :, h * 128:(h + 1) * 128], r=[], w=[wq.g()])
            dma('pool', WK, win_v[:, :, 1024 + h * 128:1024 + (h + 1) * 128], r=[], w=[wk_.g()])
            dma('pool', WV, win_v[:, :, 2048 + h * 128:2048 + (h + 1) * 128], r=[], w=[wv.g()])
            dma('pool', WG, win_v[:, :, 4096 + h * 128:4096 + (h + 1) * 128], r=[], w=[wg.g()])
            precast(4)
            pj = 0
            for (W_, wb_, dst, mode) in ((WQ, wq, None, 'q'), (WK, wk_, KT, 'k'), (WG, wg, sgT, 'g')):
                for c in range(4):
                    bk = pj % 4
                    pj += 1
                    for k in range(KC):
                        mm(banks[bk][:], W_[:, k, :], hT3[:, k, c * 512:(c + 1) * 512], k == 0, k == KC - 1,
                           r=[wb_.g(), hT_g(c * 512, (c + 1) * 512)], w=[BK[bk]])
                    if mode == 'q':
                        for m_ in range(2):
                            o_ap = QTz[m_].bf()[m_ * 64:(m_ + 1) * 64, c * 512:(c + 1) * 512]
                            P.add('act', (lambda o_ap=o_ap, bk=bk, m_=m_: (lambda e: e.mul(out=o_ap, in_=banks[bk][m_ * 64:(m_ + 1) * 64, :], mul=0.125)))(),
                                  r=[BK[bk]], w=[QTz[m_].g(c * 1024, (c + 1) * 1024)])
                        continue
                    o_ap = dst.bf()[:, c * 512:(c + 1) * 512]
                    o_g = dst.g(c * 1024, (c + 1) * 1024)
                    if mode == 'k':
                        cp(o_ap, banks[bk][:], r=[BK[bk]], w=[o_g], eng='act')
                    else:
                        act(o_ap, banks[bk][:], AF.Sigmoid, r=[BK[bk]], w=[o_g])
            for i in range(NT):
                bk = (i // 4) % 4
                for k in range(KC):
                    mm(banks[bk][:, (i % 4) * 128:(i % 4 + 1) * 128], hT3[:, k, i * 128:(i + 1) * 128], WV[:, k, :], k == 0, k == KC - 1,
                       r=[hT_g(i * 128, (i + 1) * 128), wv.g()], w=[BK[bk]])
                if i % 4 == 3:
                    cp(Vb.bf()[:, (i - 3) * 128:(i + 1) * 128], banks[bk][:], r=[BK[bk]], w=[Vb.g((i - 3) * 256, (i + 1) * 256)], eng='act')

            def qk(c, j):
                col0 = max(0, j - 4 * c) * 128
                par = j % 2
                for m in range(2):
                    bk = m * 2 + par
                    mm(banks[bk][:, col0:512], KT.bf()[:, j * 128:(j + 1) * 128],
                       QTz[m].bf()[:, c * 512 + col0:(c + 1) * 512], True, True,
                       r=[KT.g(j * 256, (j + 1) * 256), QTz[m].g(c * 1024 + col0 * 2, (c + 1) * 1024)], w=[BK[bk]])

            for c in range(4):
                jmax = 4 * c + 3
                qk(c, 0)
                for j in range(jmax + 1):
                    if j + 1 <= jmax:
                        qk(c, j + 1)
                    col0 = max(0, j - 4 * c) * 128
                    par = j % 2
                    il_lo = max(0, j - 4 * c)
                    il_hi = min(3, j + 1 - 4 * c)
                    far0 = max(0, j + 2 - 4 * c) * 128
                    for m in range(2):
                        bk = m * 2 + par
                        pt = PT[m][par]
                        if il_hi >= il_lo and il_hi >= 0:
                            n0, n1 = il_lo * 128, (il_hi + 1) * 128
                            b0 = (4 * c + il_lo - j) * 128
                            nn = n1 - n0
                            tt(ntmp[m].f32()[:, 0:nn], banks[bk][:, n0:n1], BT3[:, h, b0:b0 + nn], ALU.add,
                               r=[BK[bk], biasT.g(h * 1024, (h + 1) * 1024)], w=[ntmp[m].g()])
                            act(pt.bf()[:, n0:n1], ntmp[m].f32()[:, 0:nn], AF.Exp, r=[ntmp[m].g()], w=[pt.g(n0 * 2, n1 * 2)])
                        if far0 < 512:
                            act(pt.bf()[:, far0:512], banks[bk][:, far0:512], AF.Exp, r=[BK[bk], rb31m.g()], w=[pt.g(far0 * 2, 1024)],
                                bias=rb31m.f32()[:, h:h + 1])
                        mm(banks[4 + m][:, col0:512], V3[:, j, :], pt.bf()[:, col0:512], j == 0, j == jmax,
                           r=[Vb.g(j * 256, (j + 1) * 256), pt.g(col0 * 2, 1024)], w=[BK[4 + m]], skip_group_check=True)
                        mm(banks[6 + m][:, col0:512], ones_b.bf(), pt.bf()[:, col0:512], j == 0, j == jmax,
                           r=[ones_b.g(), pt.g(col0 * 2, 1024)], w=[BK[6 + m]], skip_group_check=True)
                cs = slice(c * 512, (c + 1) * 512)
                cp(O1s.f32(), banks[4][:], r=[BK[4]], w=[O1s.g()], eng='act')
                cp(O2s.f32(), banks[5][:], r=[BK[5]], w=[O2s.g()], eng='act')
                act(lnz.f32(), banks[6][:], AF.Ln, r=[BK[6]], w=[lnz.g()])
                act(rz.f32(), lnz.f32(), AF.Exp, r=[lnz.g()], w=[rz.g()], scale=-1.0)
                act(lnz.f32(), banks[7][:], AF.Ln, r=[BK[7]], w=[lnz.g()])
                act(rz2.f32(), lnz.f32(), AF.Exp, r=[lnz.g()], w=[rz2.g()], scale=-1.0)
                tt(t1.f32(), O1s.f32(), rz.f32(), ALU.mult, r=[O1s.g(), rz.g()], w=[t1.g()])
                tt(O2s.f32(), O2s.f32(), rz2.f32(), ALU.mult, r=[O2s.g(), rz2.g()], w=[O2s.g()])
                stt(t1.f32(), O2s.f32(), sm(SM_NLAM), t1.f32(), ALU.mult, ALU.add, r=[t1.g(), O2s.g(), small.g()], w=[t1.g()])
                act(sq.bf()[:, 0:512], t1.f32(), AF.Square, r=[t1.g()], w=[sq.g()])
                mm(banks[6][:], ones_b.bf(), sq.bf()[:, 0:512], True, True, r=[ones_b.g(), sq.g()], w=[BK[6]])
                ts(rs.f32(), banks[6][:], 1.0 / 128, 1e-5, ALU.mult, ALU.add, r=[BK[6]], w=[rs.g()])
                act(rs.f32(), rs.f32(), AF.Ln, r=[rs.g()], w=[rs.g()])
                act(rs.f32(), rs.f32(), AF.Exp, r=[rs.g()], w=[rs.g()], scale=-0.5)
                tt(t1.f32(), t1.f32(), rs.f32(), ALU.mult, r=[t1.g(), rs.g()], w=[t1.g()])
                stt(t1.f32(), t1.f32(), sm(SM_WSUB), sgT.bf()[:, cs], ALU.mult, ALU.mult, r=[t1.g(), small.g(), sgT.g(c * 1024, (c + 1) * 1024)], w=[t1.g()])
                tt(mT3[:, h, cs], t1.f32(), mT3[:, h, cs], ALU.add, r=[t1.g(), mT_g(h, c * 512, (c + 1) * 512)], w=[mT_g(h, c * 512, (c + 1) * 512)])
        if dbg and b == 0 and stage == 3:
            wa2 = Alloc(arena, wa.p, TOT)
            dtmp = wa2.get(8192)
            cp(dtmp.f32(), mT3[:, 0, :], r=[mT.g()], w=[dtmp.g()])
            dump("dbg_mT0", dtmp.f32(), [128, 2048], r=[dtmp.g()])
        if stage <= 3:
            continue

        precast(64)
        WOUT3 = wout.bf().rearrange("p (k n) -> p k n", k=KC)
        dma('pool', WOUT3, wout_d.ap().rearrange("(k p) n -> p k n", p=128), r=[], w=[wout.g()])
        for k in range(KC):
            tt(WOUT3[:, k, :], WOUT3[:, k, :], g1.f32(), ALU.mult, r=[wout.g(k * 2048, (k + 1) * 2048), g1.g()], w=[wout.g(k * 2048, (k + 1) * 2048)])
        WQ3 = hT.bf().rearrange("p (k n) -> p k n", k=KC)
        dma('pool', WQ3, wqry_d.ap().rearrange("(k p) n -> p k n", p=128), r=[], w=[hT.g()])
        wa = Alloc(arena, work_base, TOT)
        xt5 = g1
        xm = [wa.get(4096) for _ in range(2)]
        h2 = [wa.get(2048) for _ in range(2)]
        eidu = [wa.get(512) for _ in range(2)]
        gate = [wa.get(512) for _ in range(2)]
        tmpf = a1
        ot = sh1
        ssb = wa.get(8192)
        wkb = ssb
        tmpfX = ssb.sub(0, 4096)
        junkbX = ssb.sub(4096, 2048)
        h2T = ssb.sub(0, 2048)
        qTs = ssb.sub(4096, 4096)
        v16 = wa.get(1024)
        ixu = wa.get(1024)
        ixf = wa.get(1024)
        c16, posu, pau, pbu, paf, pbf, i1s, i2s, eidf, scm, ee, actv, gl, wgt = [wa.get(512) for _ in range(14)]
        zz = wa.get(32)
        rzz = wa.get(32)
        dgb = [wa.get(256) for _ in range(4)]
        NBUF = (TOT - wa.p) // 4096
        assert NBUF >= 5, NBUF
        cbuf = [wa.get(4096) for _ in range(NBUF)]
        SINK = mk(sink.bf(), [[0, D]])
        H2T3 = h2T.bf().rearrange("p (k t) -> p k t", k=KC)
        QTS3 = qTs.bf().rearrange("p (q t) -> p q t", q=16)
        SUBK3 = subkT.bf().rearrange("p (q n) -> p q n", q=16)
        SS3 = ssb.f32().rearrange("p (q n) -> p q n", q=16)
        WK3 = wkb.f32().rearrange("p (q n) -> p q n", q=16)
        V16 = v16.f32().rearrange("p (q j) -> p q j", q=16)
        IXU = ixu.u32().rearrange("p (q j) -> p q j", q=16)
        ntiles = NT if not dbg else 2

        def gen(name, r, w, eng='dve', **kw):
            P.add(eng, lambda e: getattr(e, name)(**kw), r=r, w=w)

        xmark = [0]

        def stageX(i):
            par = i % 2
            xm_, h2_, eidu_, gate_ = xm[par], h2[par], eidu[par], gate[par]
            t0 = i * 128
            dma('sp', xt5.f32(), x_d.ap()[b, t0:t0 + 128, :], r=[], w=[xt5.g()])
            for half in range(2):
                for k in range(KC):
                    mm(banks[2 + half][:], mT3[:, k, t0:t0 + 128], WOUT3[:, k, half * 512:(half + 1) * 512], k == 0, k == KC - 1,
                       r=[mT_g(k, t0, t0 + 128), wout.g()], w=[BK[2 + half]])
            for half in range(2):
                hs = slice(half * 512, (half + 1) * 512)
                tt(xm_.f32()[:, hs], banks[2 + half][:], xt5.f32()[:, hs], ALU.add, r=[BK[2 + half], xt5.g()], w=[xm_.g(half * 2048, (half + 1) * 2048)])
            act(junkbX.bf(), xm_.f32(), AF.Square, r=[xm_.g()], w=[junkbX.g(), small.g()], accum_out=sm(SM_SS2))
            rstd_chain(SM_SS2, SM_LN2, SM_RSTD2, D, 1e-6)
            for hf in range(2):
                hs = slice(hf * 512, (hf + 1) * 512)
                stt(tmpfX.f32()[:, hs], xm_.f32()[:, hs], sm(SM_RSTD2), a2.f32()[:, hs], ALU.mult, ALU.mult, r=[xm_.g(), small.g(), a2.g()], w=[tmpfX.g(hf * 2048, (hf + 1) * 2048)])
                tt(h2_.bf()[:, hs], tmpfX.f32()[:, hs], sh2.f32()[:, hs], ALU.add, r=[tmpfX.g(hf * 2048, (hf + 1) * 2048), sh2.g()], w=[h2_.g(hf * 1024, (hf + 1) * 1024)])
            for k in range(KC):
                tr(BTb[:, k * 128:(k + 1) * 128], h2_.bf()[:, k * 128:(k + 1) * 128], ident_b.bf(), r=[h2_.g(), ident_b.g()], w=[BK[7]])
            cp(h2T.bf(), BTb, r=[BK[7]], w=[h2T.g()], eng='act')
            for qc in range(16):
                bk = 2 + (qc // 4) % 2
                for k in range(KC):
                    mm(banks[bk][:, (qc % 4) * 128:(qc % 4 + 1) * 128], WQ3[:, k, qc * 128:(qc + 1) * 128], H2T3[:, k, :], k == 0, k == KC - 1,
                       r=[hT.g(), h2T.g()], w=[BK[bk]])
                if qc % 4 == 3:
                    cp(qTs.bf()[:, (qc - 3) * 128:(qc + 1) * 128], banks[bk][:], r=[BK[bk]], w=[qTs.g((qc - 3) * 256, (qc + 1) * 256)], eng='act')
            for qc in range(16):
                bk = 4 + qc // 4
                mm(banks[bk][:, (qc % 4) * 128:(qc % 4 + 1) * 128], QTS3[:, qc, :], SUBK3[:, qc, :], True, True,
                   r=[qTs.g(qc * 256, (qc + 1) * 256), subkT.g()], w=[BK[bk]])
            for q4 in range(4):
                cp(ssb.f32()[:, q4 * 512:(q4 + 1) * 512], banks[4 + q4][:], r=[BK[4 + q4]], w=[ssb.g(q4 * 2048, (q4 + 1) * 2048)], eng='act')
            if dbg and i == 0:
                dump("dbg_xm", xm_.f32(), [128, 1024], r=[xm_.g()])
                dump("dbg_s", ssb.f32(), [128, 2048], r=[ssb.g()])
            xmark[0] = len(P.capture) if P.capture is not None else 0
            sg_ = lambda qc: ssb.g(qc * 512, (qc + 1) * 512)
            wg_ = lambda qc: wkb.g(qc * 512, (qc + 1) * 512)
            vk = lambda qc, hf: f"v16:{qc}:{hf}"
            ik = lambda qc, hf: f"ixu:{qc}:{hf}"
            ALLV = [vk(q_, h_) for q_ in range(16) for h_ in range(2)]
            ALLI = [ik(q_, h_) for q_ in range(16) for h_ in range(2)]
            for qc in range(16):
                gen('max', [sg_(qc), v16.g()], [vk(qc, 0)], out=V16[:, qc, 0:8], in_=SS3[:, qc, :])
            for qc in range(16):
                gen('max_index', [sg_(qc), vk(qc, 0), ixu.g()], [ik(qc, 0)], out=IXU[:, qc, 0:8], in_max=V16[:, qc, 0:8], in_values=SS3[:, qc, :])
            for qc in range(16):
                gen('match_replace', [sg_(qc), vk(qc, 0)], [wg_(qc)], out=WK3[:, qc, :], in_to_replace=V16[:, qc, 0:8], in_values=SS3[:, qc, :], imm_value=NEG)
            for qc in range(16):
                gen('max', [wg_(qc), v16.g()], [vk(qc, 1)], out=V16[:, qc, 8:16], in_=WK3[:, qc, :])
            for qc in range(16):
                gen('max_index', [wg_(qc), vk(qc, 1), ixu.g()], [ik(qc, 1)], out=IXU[:, qc, 8:16], in_max=V16[:, qc, 8:16], in_values=WK3[:, qc, :])
            cp(ixf.f32(), ixu.u32(), r=ALLI, w=[ixf.g()])
            cand, wk2 = ssb, wkb
            C4 = cand.f32().rearrange("p (h n) -> p h n", h=8)
            W4 = wk2.f32().rearrange("p (h n) -> p h n", h=8)
            vf = v16.f32()
            for hp in range(4):
                tt(mk(cand.f32(), [[256, 2], [16, 16], [1, 16]], hp * 512), mk(vf, [[32, 2], [1, 16], [0, 16]], hp * 64),
                   mk(vf, [[32, 2], [0, 16], [1, 16]], hp * 64 + 16), ALU.add, r=ALLV, w=[cand.g(hp * 2048, (hp + 1) * 2048)])
            C16 = c16.f32().rearrange("p (h j) -> p h j", h=8)
            POS = posu.u32().rearrange("p (h j) -> p h j", h=8)
            cg_ = lambda h_: cand.g(h_ * 1024, (h_ + 1) * 1024)
            w2g_ = lambda h_: wk2.g(h_ * 1024, (h_ + 1) * 1024)
            ck = lambda h_, hf: f"c16:{h_}:{hf}"
            pk = lambda h_, hf: f"pos:{h_}:{hf}"
            ALLC = [ck(h_, f_) for h_ in range(8) for f_ in range(2)]
            ALLP = [pk(h_, f_) for h_ in range(8) for f_ in range(2)]
            for h_ in range(8):
                gen('max', [cg_(h_), c16.g()], [ck(h_, 0)], out=C16[:, h_, 0:8], in_=C4[:, h_, :])
            for h_ in range(8):
                gen('max_index', [cg_(h_), ck(h_, 0), posu.g()], [pk(h_, 0)], out=POS[:, h_, 0:8], in_max=C16[:, h_, 0:8], in_values=C4[:, h_, :])
            for h_ in range(8):
                gen('match_replace', [cg_(h_), ck(h_, 0)], [w2g_(h_)], out=W4[:, h_, :], in_to_replace=C16[:, h_, 0:8], in_values=C4[:, h_, :], imm_value=NEG)
            for h_ in range(8):
                gen('max', [w2g_(h_), c16.g()], [ck(h_, 1)], out=C16[:, h_, 8:16], in_=W4[:, h_, :])
            for h_ in range(8):
                gen('max_index', [w2g_(h_), ck(h_, 1), posu.g()], [pk(h_, 1)], out=POS[:, h_, 8:16], in_max=C16[:, h_, 8:16], in_values=W4[:, h_, :])
            gen('tensor_single_scalar', ALLP, [pau.g()], out=pau.u32(), in_=posu.u32(), scalar=4, op=ALU.logical_shift_right)
            gen('tensor_single_scalar', ALLP, [pbu.g()], out=pbu.u32(), in_=posu.u32(), scalar=15, op=ALU.bitwise_and)
            cp(paf.f32(), pau.u32(), r=[pau.g()], w=[paf.g()])
            cp(pbf.f32(), pbu.u32(), r=[pbu.g()], w=[pbf.g()])
            oh, prod = ssb, wkb
            for (pf_, off_, dst_) in ((paf, 0, i1s), (pbf, 16, i2s)):
                for hp in range(4):
                    og = oh.g(hp * 2048, (hp + 1) * 2048)
                    tt(mk(oh.f32(), [[16, 32], [1, 16]], hp * 512), mk(pf_.f32(), [[1, 32], [0, 16]], hp * 32), mk(iota16.f32(), [[0, 32], [1, 16]]), ALU.is_equal,
                       r=[pf_.g(), iota16.g()], w=[og])
                    tt(mk(prod.f32(), [[256, 2], [16, 16], [1, 16]], hp * 512), mk(oh.f32(), [[256, 2], [16, 16], [1, 16]], hp * 512),
                       mk(ixf.f32(), [[32, 2], [0, 16], [1, 16]], hp * 64 + off_), ALU.mult, r=[og, ixf.g()], w=[og])
                    tred(dst_.f32()[:, hp * 32:(hp + 1) * 32], mk(prod.f32(), [[16, 32], [1, 16]], hp * 512), ALU.add, r=[og], w=[dst_.g()])
            stt(eidf.f32(), i1s.f32(), 128.0, i2s.f32(), ALU.mult, ALU.add, r=[i1s.g(), i2s.g()], w=[eidf.g()])
            cp(eidu_.u32(), eidf.f32(), r=[eidf.g()], w=[eidu_.g()])
            tt(mk(scm.f32(), [[16, 8], [1, 16]]), mk(c16.f32(), [[16, 8], [1, 16]]), mk(c16.f32(), [[16, 8], [0, 16]]), ALU.subtract, r=ALLC, w=[scm.g()])
            act(ee.f32(), scm.f32(), AF.Exp, r=[scm.g()], w=[ee.g()])
            tred(zz.f32(), mk(ee.f32(), [[16, 8], [1, 16]]), ALU.add, r=[ee.g()], w=[zz.g()])
            gen('reciprocal', [zz.g()], [rzz.g()], out=rzz.f32(), in_=zz.f32())
            tt(mk(gate_.f32(), [[16, 8], [1, 16]]), mk(ee.f32(), [[16, 8], [1, 16]]), mk(rzz.f32(), [[1, 8], [0, 16]]), ALU.mult, r=[ee.g(), rzz.g()], w=[gate_.g()])
            if dbg and i == 0:
                dump("dbg_eid", eidf.f32(), [128, 128], r=[eidf.g()])
                dump("dbg_gate", gate_.f32(), [128, 128], r=[gate_.g()])

        def stageY(i, pump):
            par = i % 2
            xm_, h2_, eidu_, gate_ = xm[par], h2[par], eidu[par], gate[par]
            t0 = i * 128
            EID = eidu_.u32()

            def gather(out_ap, slot, r, w):
                P.add('pool', lambda e: e.indirect_dma_start(out=out_ap, out_offset=None, in_=etab_d.ap(),
                                                             in_offset=bass.IndirectOffsetOnAxis(ap=EID[:, slot:slot + 1], axis=0)),
                      r=r, w=w, dma=True)

            def slot_tail(sl):
                cb = cbuf[sl % NBUF]
                dg_ = dgb[sl % 4]
                act(wgt.f32()[:, sl:sl + 1], gl.f32()[:, sl:sl + 1], AF.Copy, r=[f"gl{sl}", gate_.g(), wgt.g()], w=[f"wgt{sl}"],
                    scale=gate_.f32()[:, sl:sl + 1])
                P.add('act', (lambda dg_=dg_, sl=sl: (lambda e: e.activation(out=dg_.bf(), in_=ident_f.f32(), func=AF.Copy, scale=wgt.f32()[:, sl:sl + 1])))(),
                      r=[ident_f.g(), f"wgt{sl}"], w=[dg_.g()])
                for half in range(2):
                    mm(banks[half][:], dg_.bf(), cb.bf()[:, D + half * 512:D + (half + 1) * 512], sl == 0, sl == 127,
                       r=[dg_.g(), cb.g(2048, 4096)], w=[BK[half]])

            for sl in range(128):
                cb = cbuf[sl % NBUF]
                gather(cb.bf(), sl, r=[eidu_.g()] + ETAB_KEYS, w=[cb.g()])
                P.add('dve', (lambda cb=cb, sl=sl: (lambda e: e.scalar_tensor_tensor(out=SINK, in0=cb.bf()[:, 0:D], scalar=1.0, in1=h2_.bf(), op0=ALU.mult, op1=ALU.mult,
                                                                                 accum_out=actv.f32()[:, sl:sl + 1])))(),
                      r=[cb.g(0, 2048), h2_.g(), actv.g()], w=[f"actv{sl}"])
                act(gl.f32()[:, sl:sl + 1], actv.f32()[:, sl:sl + 1], AF.Gelu, r=[f"actv{sl}", gl.g()], w=[f"gl{sl}"])
                if sl >= 1:
                    slot_tail(sl - 1)
                pump(sl)
            slot_tail(127)
            for half in range(2):
                hs = slice(half * 512, (half + 1) * 512)
                tt(tmpf.f32()[:, hs], banks[half][:], g2.f32()[:, hs], ALU.mult, r=[BK[half], g2.g()], w=[tmpf.g(half * 2048, (half + 1) * 2048)])
                tt(ot.f32()[:, hs], tmpf.f32()[:, hs], xm_.f32()[:, hs], ALU.add, r=[tmpf.g(half * 2048, (half + 1) * 2048), xm_.g()], w=[ot.g(half * 2048, (half + 1) * 2048)])
            if dbg and i == 0:
                dump("dbg_xo", ot.f32(), [128, 1024], r=[ot.g()])
            act(SINK, ot.f32(), AF.Square, r=[ot.g()], w=[small.g()], accum_out=sm(SM_SS3))
            rstd_chain(SM_SS3, SM_LN3, SM_RSTD3, D, 1e-6)
            stt(ot.f32(), ot.f32(), sm(SM_RSTD3), nf_bc.f32(), ALU.mult, ALU.mult, r=[ot.g(), small.g(), nf_bc.g()], w=[ot.g()])
            dma('sp', out_d.ap()[b, t0:t0 + 128, :], ot.f32(), r=[ot.g()], w=[f"out{b}_{i}"])

        def drain(q, n=None):
            k = 0
            while q and (n is None or k < n):
                P.add(*q.pop(0))
                k += 1

        P.capture = []
        stageX(0)
        q = P.capture
        P.capture = None
        drain(q)
        for i in range(ntiles):
            if i + 1 < ntiles:
                P.capture = []
                stageX(i + 1)
                q = P.capture
                P.capture = None
            else:
                q = []
            n_tot, n_pre = len(q), xmark[0]
            S1, S2 = 24, 122

            def pump_to(sl, q=q, n_tot=n_tot, n_pre=n_pre):
                if sl < S1:
                    target = n_pre * (sl + 1) // S1
                else:
                    target = n_pre + (n_tot - n_pre) * min(sl + 1 - S1, S2 - S1) // (S2 - S1)
                drain(q, max(0, target - (n_tot - len(q))))
            stageY(i, pump_to)
            drain(q)

    P.emit(nc, stack)
    stack.close()
    return nc, list(dbg_d.keys())


def _prep_inputs(inputs):
    f = np.float32
    x = np.asarray(inputs['x'], f)
    c = np.asarray(inputs['c'], f)
    cst = _host_consts()
    rep = lambda v, n=128: np.ascontiguousarray(np.broadcast_to(np.asarray(v, f).reshape(1, -1), (n, np.asarray(v).size)))
    nw = np.concatenate([np.asarray(inputs['norm_mix_w'], f).reshape(-1), np.asarray(inputs['norm_ffn_w'], f).reshape(-1),
                         np.asarray(inputs['norm_final_w'], f).reshape(-1)])
    lam = np.concatenate([np.asarray(inputs[k], f).reshape(-1) for k in ('lambda_q1', 'lambda_k1', 'lambda_q2', 'lambda_k2')])
    sk1 = np.asarray(inputs['sub_keys_1'], f)[0]
    sk2 = np.asarray(inputs['sub_keys_2'], f)[0]
    subkT = np.zeros((128, 16, 128), f)
    for h in range(8):
        subkT[:, h * 2 + 0, :] = sk1[h].T
        subkT[:, h * 2 + 1, :] = sk2[h].T
    shared = {
        'w_ada': np.ascontiguousarray(np.asarray(inputs['w_ada'], f)[0]),
        'b_ada_bc': rep(inputs['b_ada']),
        'nw_bc': rep(nw),
        'w_in': np.ascontiguousarray(np.asarray(inputs['w_in'], f)[0]),
        'w_out': np.ascontiguousarray(np.asarray(inputs['w_out'], f)[0]),
        'w_query': np.ascontiguousarray(np.asarray(inputs['w_query'], f)[0]),
        'pool_w': np.ascontiguousarray(np.asarray(inputs['pool_w'], f)[0]),
        'pool_scaleT': np.ascontiguousarray(np.asarray(inputs['pool_scale'], f).reshape(8, 128).T),
        'subkT': np.ascontiguousarray(subkT.reshape(128, 2048)),
        'e_down': np.ascontiguousarray(np.asarray(inputs['expert_down'], f)[0]),
        'e_up': np.ascontiguousarray(np.asarray(inputs['expert_up'], f)[0]),
        'lam_bc': rep(lam),
        'sublnT': np.ascontiguousarray(np.asarray(inputs['subln_w'], f).reshape(128, 1)),
        'relb_bc': rep(np.asarray(inputs['rel_bias'], f).reshape(-1)),
        'masks': np.ascontiguousarray(cst['masks'].reshape(128, -1)),
        'negmask': cst['negmask'],
        'band': np.ascontiguousarray(cst['band'].reshape(128, -1)),
        'ident': cst['ident'],
        'iota16': cst['iota16'],
        'sel0': cst['sel0'],
    }
    in_maps = []
    for core in range(NCORES):
        m = dict(shared)
        m['x'] = np.ascontiguousarray(x[core * NB:(core + 1) * NB])
        cc = c[core * NB:(core + 1) * NB]
        m['cT'] = np.ascontiguousarray(cc.reshape(NB, KC, 128).transpose(2, 0, 1).reshape(128, NB * KC))
        in_maps.append(m)
    return in_maps


_CACHE = {}


def kernel(**inputs):
    in_maps = _prep_inputs(inputs)
    if 'nc' not in _CACHE:
        _CACHE['nc'] = build()[0]
    nc = _CACHE['nc']
    res = run_bass_kernel_spmd(nc, in_maps, core_ids=list(range(NCORES)))
    out = np.concatenate([np.asarray(r['out']) for r in res.results], axis=0)
    return out.astype(np.float32)
```

```python
import os
import math
import contextlib
import numpy as np
import concourse.bass as bass
import concourse.mybir as mybir
from concourse.bass_utils import run_bass_kernel_spmd

dt = mybir.dt
AF = mybir.ActivationFunctionType
ALU = mybir.AluOpType
AX = mybir.AxisListType
F32, BF16, U32 = dt.float32, dt.bfloat16, dt.uint32

NCORES = 8
NB = 2
S = 2048
NT = 16
D = 1024
KC = 8
H = 8
DIN = 6144
NEXP = 16384
OFF = 8.0
NEG = -1.0e30
G = 512


class Prog:
    def __init__(self):
        self.ops = []
        self.last_w = {}
        self.readers = {}
        self.capture = None

    def add(self, eng, fn, r=(), w=(), dma=False):
        if self.capture is not None:
            self.capture.append((eng, fn, r, w, dma))
            return None
        i = len(self.ops)
        deps = set()
        rk = _flat(r)
        wk = _flat(w)
        for k in rk:
            lw = self.last_w.get(k)
            if lw is not None:
                deps.add(lw)
        for k in wk:
            lw = self.last_w.get(k)
            if lw is not None:
                deps.add(lw)
            deps.update(self.readers.get(k, ()))
        deps.discard(i)
        for k in rk:
            self.readers.setdefault(k, []).append(i)
        for k in wk:
            self.last_w[k] = i
            self.readers[k] = []
        self.ops.append(dict(eng=eng, fn=fn, deps=deps, dma=dma, sig=None, need=False, pre=None))
        return i

    def emit(self, nc, stack):
        ops = self.ops
        EPOCH = 30000
        NSLOT = {'sp': 24, 'pool': 24, 'act': 8}
        for o in ops:
            for d in o['deps']:
                p = ops[d]
                if p['eng'] == 'pe' and o['eng'] == 'pe' and not p['dma'] and not o['dma']:
                    continue
                p['need'] = True
        cnt = {e: 0 for e in ('pe', 'act', 'dve', 'pool', 'sp')}
        esems = {e: [] for e in cnt}
        dsems = {q: [stack.enter_context(nc.semaphore(f"d_{q}_{i}")) for i in range(n)] for q, n in NSLOT.items()}
        duse = {q: [0] * n for q, n in NSLOT.items()}
        dnext = {q: 0 for q in NSLOT}
        for o in ops:
            e = o['eng']
            if o['dma']:
                s = dnext[e]
                dnext[e] = (s + 1) % NSLOT[e]
                if duse[e][s] > 0:
                    o['pre'] = (dsems[e][s], 16 * duse[e][s])
                duse[e][s] += 1
                o['sig'] = (dsems[e][s], 16 * duse[e][s])
            elif o['need']:
                ep = cnt[e] // EPOCH
                while len(esems[e]) <= ep:
                    esems[e].append(stack.enter_context(nc.semaphore(f"e_{e}_{len(esems[e])}")))
                cnt[e] += 1
                o['sig'] = (esems[e][ep], cnt[e] - ep * EPOCH)
        by_eng = {e: [o for o in ops if o['eng'] == e] for e in cnt}
        final_waits = []
        for q in NSLOT:
            for s in range(NSLOT[q]):
                if duse[q][s] > 0:
                    final_waits.append((dsems[q][s], 16 * duse[q][s]))

        def run(ename, eng):
            waited = {}
            for o in by_eng[ename]:
                needs = {}
                for d in o['deps']:
                    p = ops[d]
                    if p['eng'] == 'pe' and ename == 'pe' and not p['dma'] and not o['dma']:
                        continue
                    sem, val = p['sig']
                    key = id(sem)
                    if needs.get(key, (None, 0))[1] < val:
                        needs[key] = (sem, val)
                if o['pre'] is not None:
                    sem, val = o['pre']
                    key = id(sem)
                    if needs.get(key, (None, 0))[1] < val:
                        needs[key] = (sem, val)
                for key, (sem, val) in needs.items():
                    if waited.get(key, 0) < val:
                        eng.wait_ge(sem, val)
                        waited[key] = val
                inst = o['fn'](eng)
                if o['sig'] is not None:
                    inst.then_inc(o['sig'][0], 16 if o['dma'] else 1)
            if ename == 'sp':
                for sem, val in final_waits:
                    eng.wait_ge(sem, val)

        with nc.Block() as block:
            @block.tensor
            def _(e):
                run('pe', e)

            @block.scalar
            def _(e):
                run('act', e)

            @block.vector
            def _(e):
                run('dve', e)

            @block.gpsimd
            def _(e):
                run('pool', e)

            @block.sync
            def _(e):
                run('sp', e)


def _flat(x):
    out = []
    for k in x:
        if isinstance(k, (list, tuple, set, range)):
            out.extend(_flat(k))
        else:
            out.append(k)
    return out


class Buf:
    def __init__(self, arena, off, nbytes):
        assert off % 4 == 0 and nbytes % 4 == 0
        self.A, self.off, self.nbytes = arena, off, nbytes

    def g(self, lo=0, hi=None):
        hi = self.nbytes if hi is None else hi
        return range((self.off + lo) // G, (self.off + hi + G - 1) // G)

    def f32(self):
        return self.A[:, self.off // 4:(self.off + self.nbytes) // 4]

    def bf(self):
        return self.A[:, self.off // 4:(self.off + self.nbytes) // 4].bitcast(BF16)

    def u32(self):
        return self.A[:, self.off // 4:(self.off + self.nbytes) // 4].bitcast(U32)

    def sub(self, lo, n):
        return Buf(self.A, self.off + lo, n)


class Alloc:
    def __init__(self, arena, base, limit):
        self.A, self.p, self.limit = arena, base, limit

    def get(self, nbytes):
        nb = (nbytes + G - 1) // G * G
        b = Buf(self.A, self.p, (nbytes + 3) // 4 * 4)
        self.p += nb
        assert self.p <= self.limit, (self.p, self.limit)
        return b


def mk(ap, pattern, extra_off=0):
    return bass.AP(tensor=ap.tensor, offset=ap.offset + extra_off, ap=[list(ap.ap[0])] + [list(p) for p in pattern])


def _t5_bucket(d):
    d = np.maximum(d, 0)
    x = np.maximum(d, 1).astype(np.float32) / np.float32(16)
    large = 16 + (np.log(x).astype(np.float32) / np.float32(math.log(128 / 16)) * np.float32(16)).astype(np.int32)
    large = np.minimum(large, 31)
    return np.where(d < 16, d, large)


def _host_consts():
    kk = np.arange(128)[:, None]
    qq = np.arange(256)[None, :]
    dist = qq - kk
    valid = dist >= 0
    bk = _t5_bucket(dist)
    masks = np.zeros((128, 32, 256), np.float32)
    for b in range(32):
        masks[:, b, :] = (valid & (bk == b)).astype(np.float32)
    negmask = np.where(valid, 0.0, NEG).astype(np.float32)
    band = np.zeros((128, 12, 128), np.float32)
    s = np.arange(128)[:, None]
    t = np.arange(128)[None, :]
    for gi, w in enumerate((2, 4, 8, 16)):
        cnt0 = np.minimum(t + 1, w).astype(np.float32)
        band[:, gi * 3 + 0, :] = ((s <= t) & (s > t - w)) / cnt0 - (s == t)
        band[:, gi * 3 + 1, :] = ((s <= t) & (s > t - w)) / np.float32(w) - (s == t)
        band[:, gi * 3 + 2, :] = (s > 128 + t - w) / np.float32(w)
    ident = np.eye(128, dtype=np.float32)
    iota16 = np.broadcast_to(np.arange(16, dtype=np.float32)[None, :], (128, 16)).copy()
    sel0 = np.zeros((64, 128), np.float32)
    sel0[0, :] = 1.0
    sel0[32, :] = 1.0
    return dict(masks=masks, negmask=negmask, band=band, ident=ident, iota16=iota16, sel0=sel0)


def build(stage=99, dbg=False):
    nc = bass.Bass("TRN2", target_bir_lowering=False)
    P = Prog()

    def din(name, shape, dtype=F32):
        return nc.dram_tensor(name, list(shape), dtype, kind="ExternalInput")

    x_d = din("x", [NB, S, D])
    cT_d = din("cT", [128, NB * KC])
    wada_d = din("w_ada", [D, DIN])
    bada_d = din("b_ada_bc", [128, DIN])
    nw_d = din("nw_bc", [128, 3 * D])
    win_d = din("w_in", [D, DIN])
    wout_d = din("w_out", [D, D])
    wqry_d = din("w_query", [D, 2048])
    poolw_d = din("pool_w", [4, 256, 256])
    psc_d = din("pool_scaleT", [128, 8])
    subk_d = din("subkT", [128, 16 * 128])
    edown_d = din("e_down", [NEXP, D])
    eup_d = din("e_up", [NEXP, D])
    lam_d = din("lam_bc", [128, 256])
    subln_d = din("sublnT", [128, 1])
    relb_d = din("relb_bc", [128, 256])
    masks_d = din("masks", [128, 32 * 256])
    negm_d = din("negmask", [128, 256])
    band_d = din("band", [128, 12 * 128])
    ident_d = din("ident", [128, 128])
    iota_d = din("iota16", [128, 16])
    sel0_d = din("sel0", [64, 128])
    out_d = nc.dram_tensor("out", [NB, S, D], F32, kind="ExternalOutput")
    etab_d = nc.dram_tensor("etab", [NEXP, 2 * D], BF16, kind="Internal")
    dbg_d = {}

    def dbg_out(name, shape):
        dbg_d[name] = nc.dram_tensor(name, list(shape), F32, kind="ExternalOutput")
        return dbg_d[name]

    stack = contextlib.ExitStack()
    TOT = 212480
    arena = stack.enter_context(nc.sbuf_tensor("arena", [128, TOT // 4], F32))
    banks = [stack.enter_context(nc.psum_tensor(f"B{i}", [128, 512], F32)) for i in range(8)]
    BK = [f"B{i}" for i in range(8)]

    al = Alloc(arena, 0, TOT)
    ident_f = al.get(512)
    ident_b = al.get(256)
    ones_b = al.get(256)
    ones_f = al.get(512)
    sel0 = al.get(512)
    relb = al.get(1024)
    rb31m = al.get(32)
    lamv = al.get(1024)
    small = al.get(512)
    pscT = al.get(32)
    band = al.get(12 * 128 * 2)
    subkT = al.get(16 * 128 * 2)
    iota16 = al.get(64)
    sink = al.get(64)
    nf_bc = al.get(4096)
    sh1 = al.get(4096)
    a1 = al.get(4096)
    g1 = al.get(4096)
    sh2 = al.get(4096)
    a2 = al.get(4096)
    g2 = al.get(4096)
    hT = al.get(KC * S * 2)
    mT = al.get(KC * S * 2)
    wout = al.get(KC * D * 2)
    work_base = al.p
    SM = small.f32()
    (SM_S1, SM_S2, SM_E1, SM_E2, SM_NLAM, SM_WSUB, SM_SS, SM_RSTD, SM_LN, SM_SS2, SM_RSTD2, SM_SS3, SM_RSTD3,
     SM_LN2, SM_LN3, SM_SUBLN) = range(16)

    def sm(i):
        return SM[:, i:i + 1]

    def smg(i):
        return small.g()

    def dma(q, out, in_, r, w):
        P.add(q, lambda e: e.dma_start(out=out, in_=in_), r=r, w=w, dma=True)

    def mm(out, lhsT, rhs, start, stop, r, w, **kw):
        P.add('pe', lambda e: e.matmul(out, lhsT, rhs, start=start, stop=stop, **kw), r=r, w=w)

    def tr(out, in_, ident, r, w):
        P.add('pe', lambda e: e.transpose(out, in_, ident), r=r, w=w)

    def act(out, in_, func, r, w, bias=None, scale=None, accum_out=None, eng='act'):
        def f(e):
            kw = {}
            if bias is not None:
                kw['bias'] = bias
            if scale is not None:
                kw['scale'] = scale
            if accum_out is not None:
                kw['accum_out'] = accum_out
            return e.activation(out=out, in_=in_, func=func, **kw)
        P.add('act', f, r=r, w=w)

    def tt(out, in0, in1, op, r, w, eng='dve'):
        P.add(eng, lambda e: e.tensor_tensor(out=out, in0=in0, in1=in1, op=op), r=r, w=w)

    def ts(out, in0, s1, s2, op0, op1, r, w, eng='dve'):
        if op1 is None:
            P.add(eng, lambda e: e.tensor_scalar(out=out, in0=in0, scalar1=s1, scalar2=None, op0=op0), r=r, w=w)
        else:
            P.add(eng, lambda e: e.tensor_scalar(out=out, in0=in0, scalar1=s1, scalar2=s2, op0=op0, op1=op1), r=r, w=w)

    def stt(out, in0, scalar, in1, op0, op1, r, w):
        P.add('dve', lambda e: e.scalar_tensor_tensor(out=out, in0=in0, scalar=scalar, in1=in1, op0=op0, op1=op1), r=r, w=w)

    def cp(out, in_, r, w, eng='dve'):
        if eng == 'act':
            P.add('act', lambda e: e.copy(out=out, in_=in_), r=r, w=w)
        else:
            P.add(eng, lambda e: e.tensor_copy(out=out, in_=in_), r=r, w=w)

    def ttr(out, in0, in1, accum_out, r, w):
        P.add('dve', lambda e: e.scalar_tensor_tensor(out=out, in0=in0, scalar=1.0, in1=in1, op0=ALU.mult, op1=ALU.mult,
                                                     accum_out=accum_out), r=r, w=w)

    def tred(out, in_, op, r, w):
        P.add('dve', lambda e: e.tensor_reduce(out=out, in_=in_, axis=AX.X, op=op), r=r, w=w)

    def memset(ap, val, r, w, eng='dve'):
        P.add(eng, lambda e: e.memset(ap, val), r=r, w=w)

    def rstd_chain(ss_i, ln_i, rstd_i, n, eps):
        ts(sm(ln_i), sm(ss_i), 1.0 / n, eps, ALU.mult, ALU.add, r=[small.g()], w=[small.g()])
        act(sm(ln_i), sm(ln_i), AF.Ln, r=[small.g()], w=[small.g()])
        act(sm(rstd_i), sm(ln_i), AF.Exp, r=[small.g()], w=[small.g()], scale=-0.5)

    def dump(name, ap_sb, shape, r):
        if name in dbg_d:
            return
        d = dbg_out(name, shape)
        dma('sp', d.ap(), ap_sb, r=r, w=[name])

    dma('sp', ident_f.f32(), ident_d.ap(), r=[], w=[ident_f.g()])
    dma('sp', sel0.f32()[0:64, :], sel0_d.ap(), r=[], w=[sel0.g()])
    dma('sp', relb.f32(), relb_d.ap(), r=[], w=[relb.g()])
    dma('sp', lamv.f32(), lam_d.ap(), r=[], w=[lamv.g()])
    dma('sp', pscT.f32(), psc_d.ap(), r=[], w=[pscT.g()])
    dma('sp', iota16.f32(), iota_d.ap(), r=[], w=[iota16.g()])
    dma('sp', nf_bc.f32(), nw_d.ap()[:, 2 * D:3 * D], r=[], w=[nf_bc.g()])
    dma('sp', sm(SM_SUBLN), subln_d.ap(), r=[], w=[small.g()])
    dma('pool', band.bf(), band_d.ap(), r=[], w=[band.g()])
    dma('pool', subkT.bf(), subk_d.ap(), r=[], w=[subkT.g()])
    ETAB_JOBS = [(ti, r0) for ti in range(2) for r0 in range(0, NEXP, 1024)]
    ETAB_KEYS = [f"etab{ti}_{r0}" for (ti, r0) in ETAB_JOBS]

    def precast(n):
        for _ in range(n):
            if not ETAB_JOBS:
                return
            ti, r0 = ETAB_JOBS.pop(0)
            tbl = (edown_d, eup_d)[ti]
            dma('pool', etab_d.ap()[r0:r0 + 1024, ti * D:(ti + 1) * D], tbl.ap()[r0:r0 + 1024, :], r=[], w=[f"etab{ti}_{r0}"])

    cp(ident_b.bf(), ident_f.f32(), r=[ident_f.g()], w=[ident_b.g()])
    memset(ones_b.bf(), 1.0, r=[], w=[ones_b.g()])
    memset(ones_f.f32(), 1.0, r=[], w=[ones_f.g()])
    ts(rb31m.f32(), relb.f32()[:, 31 * 8:32 * 8], -OFF, None, ALU.add, None, r=[relb.g()], w=[rb31m.g()])
    ts(sm(SM_WSUB), sm(SM_SUBLN), 0.8, None, ALU.mult, None, r=[small.g()], w=[small.g()])
    wa = Alloc(arena, work_base, TOT)
    junk64 = wa.get(256)
    LV = lamv.f32()
    ttr(junk64.f32(), LV[:, 0:64], LV[:, 64:128], sm(SM_S1), r=[lamv.g()], w=[junk64.g(), small.g()])
    ttr(junk64.f32(), LV[:, 128:192], LV[:, 192:256], sm(SM_S2), r=[lamv.g()], w=[junk64.g(), small.g()])
    act(sm(SM_E1), sm(SM_S1), AF.Exp, r=[small.g()], w=[small.g()])
    act(sm(SM_E2), sm(SM_S2), AF.Exp, r=[small.g()], w=[small.g()])
    tt(sm(SM_NLAM), sm(SM_E2), sm(SM_E1), ALU.subtract, r=[small.g()], w=[small.g()])
    ts(sm(SM_NLAM), sm(SM_NLAM), -0.2, None, ALU.add, None, r=[small.g()], w=[small.g()])
    bias_scr = nc.dram_tensor("bias_scr", [128, 2048], F32, kind="Internal")
    biasT = wa.get(8 * 1024)
    mk_chunk = wa.get(8 * 256 * 4)
    BT3 = biasT.f32().rearrange("p (h q) -> p h q", h=8)
    for h in range(8):
        dma('sp', BT3[:, h, :], negm_d.ap(), r=[], w=[biasT.g(h * 1024, (h + 1) * 1024)])
    for c in range(4):
        dma('sp', mk_chunk.f32(), masks_d.ap()[:, c * 2048:(c + 1) * 2048], r=[], w=[mk_chunk.g()])
        MC = mk_chunk.f32().rearrange("p (b q) -> p b q", b=8)
        for bb in range(8):
            b = c * 8 + bb
            for h in range(8):
                stt(BT3[:, h, :], MC[:, bb, :], relb.f32()[:, b * 8 + h:b * 8 + h + 1], BT3[:, h, :], ALU.mult, ALU.add,
                    r=[mk_chunk.g(), relb.g(), biasT.g(h * 1024, (h + 1) * 1024)], w=[biasT.g(h * 1024, (h + 1) * 1024)])
    ts(biasT.f32(), biasT.f32(), -OFF, None, ALU.add, None, r=[biasT.g()], w=[biasT.g()])
    dma('sp', bias_scr.ap(), biasT.f32(), r=[biasT.g()], w=["bias_scr"])
    if dbg:
        dump("dbg_bias", biasT.f32(), [128, 2048], r=[biasT.g()])
        dump("dbg_small", small.f32()[:, 0:16], [128, 16], r=[small.g()])

    hT3 = hT.bf().rearrange("p (k t) -> p k t", k=KC)
    mT3 = mT.bf().rearrange("p (k t) -> p k t", k=KC)

    def hT_g(c0, c1):
        return [hT.g(k * S * 2 + c0 * 2, k * S * 2 + c1 * 2) for k in range(KC)]

    def mT_g(k, c0, c1):
        return mT.g(k * S * 2 + c0 * 2, k * S * 2 + c1 * 2)

    BTb = banks[7][:].bitcast(BF16)

    for b in range(NB):
        wa = Alloc(arena, work_base, TOT)
        cact = wa.get(64)
        crep = wa.get(KC * 128 * 4)
        wblk = [wa.get(KC * 512 * 4) for _ in range(2)]
        bblk = [wa.get(2048) for _ in range(2)]
        mtmp = wa.get(2048)
        nwt = wa.get(4096)
        cin = wa.get(64)
        dma('sp', cin.f32()[:, 0:KC], cT_d.ap()[:, b * KC:(b + 1) * KC], r=[], w=[cin.g()])
        act(cact.f32()[:, 0:KC], cin.f32()[:, 0:KC], AF.Silu, r=[cin.g()], w=[cact.g()])
        cp(crep.f32().rearrange("p (k m) -> p k m", k=KC), mk(cact.f32(), [[1, KC], [0, 128]]), r=[cact.g()], w=[crep.g()])
        CR = crep.f32().rearrange("p (k m) -> p k m", k=KC)
        wada_v = wada_d.ap().rearrange("(k p) n -> p k n", p=128)
        dsts = [sh1, sh1, a1, a1, g1, g1, sh2, sh2, a2, a2, g2, g2]
        for nb_ in range(12):
            wb = wblk[nb_ % 2]
            bb_ = bblk[nb_ % 2]
            dma('sp', wb.f32().rearrange("p (k n) -> p k n", k=KC), wada_v[:, :, nb_ * 512:(nb_ + 1) * 512], r=[], w=[wb.g()])
            dma('sp', bb_.f32(), bada_d.ap()[:, nb_ * 512:(nb_ + 1) * 512], r=[], w=[bb_.g()])
            WB = wb.f32().rearrange("p (k n) -> p k n", k=KC)
            for k in range(KC):
                mm(banks[6][:], CR[:, k, :], WB[:, k, :], k == 0, k == KC - 1, r=[crep.g(), wb.g()], w=[BK[6]])
            dst = dsts[nb_]
            half = nb_ % 2
            dst_ap = dst.f32()[:, half * 512:(half + 1) * 512]
            dg_ = dst.g(half * 2048, (half + 1) * 2048)
            if nb_ in (2, 3, 8, 9):
                which = 0 if nb_ in (2, 3) else 1
                dma('sp', nwt.f32()[:, 0:512], nw_d.ap()[:, which * D + half * 512: which * D + (half + 1) * 512], r=[], w=[nwt.g()])
                tt(mtmp.f32(), banks[6][:], bb_.f32(), ALU.add, r=[BK[6], bb_.g()], w=[mtmp.g()])
                stt(dst_ap, mtmp.f32(), 1.0, nwt.f32()[:, 0:512], ALU.add, ALU.mult, r=[mtmp.g(), nwt.g()], w=[dg_])
            else:
                tt(dst_ap, banks[6][:], bb_.f32(), ALU.add, r=[BK[6], bb_.g()], w=[dg_])
        if dbg and b == 0:
            dump("dbg_a1", a1.f32(), [128, 1024], r=[a1.g()])
            dump("dbg_g2", g2.f32(), [128, 1024], r=[g2.g()])

        wa = Alloc(arena, work_base, TOT)
        xt = [wa.get(4096) for _ in range(2)]
        tmpf2 = [wa.get(4096) for _ in range(2)]
        junkb2 = [wa.get(2048) for _ in range(2)]
        hbf2 = [wa.get(2048) for _ in range(2)]
        for i in range(NT):
            par = i % 2
            xb, tmpf, junkb, hbf = xt[par], tmpf2[par], junkb2[par], hbf2[par]
            ss_, ln_, rs_ = sm(20 + par * 3), sm(21 + par * 3), sm(22 + par * 3)
            kss, kln, krs = f"p1ss{par}", f"p1ln{par}", f"p1rs{par}"
            dma('sp', xb.f32(), x_d.ap()[b, i * 128:(i + 1) * 128, :], r=[], w=[xb.g()])
            act(junkb.bf(), xb.f32(), AF.Square, r=[xb.g(), small.g()], w=[junkb.g(), kss], accum_out=ss_)
            ts(ln_, ss_, 1.0 / D, 1e-6, ALU.mult, ALU.add, r=[kss, small.g()], w=[kln])
            act(ln_, ln_, AF.Ln, r=[kln], w=[kln])
            act(rs_, ln_, AF.Exp, r=[kln, small.g()], w=[krs], scale=-0.5)
            stt(tmpf.f32(), xb.f32(), rs_, a1.f32(), ALU.mult, ALU.mult, r=[xb.g(), krs, a1.g()], w=[tmpf.g()])
            tt(hbf.bf(), tmpf.f32(), sh1.f32(), ALU.add, r=[tmpf.g(), sh1.g()], w=[hbf.g()])
            for k in range(KC):
                tr(BTb[:, k * 128:(k + 1) * 128], hbf.bf()[:, k * 128:(k + 1) * 128], ident_b.bf(), r=[hbf.g(), ident_b.g()], w=[BK[7]])
            cp(hT3[:, :, i * 128:(i + 1) * 128], BTb.rearrange("p (k t) -> p k t", k=KC), r=[BK[7]], w=hT_g(i * 128, (i + 1) * 128), eng='act')
        if dbg and b == 0:
            wa2 = Alloc(arena, wa.p, TOT)
            dtmp = wa2.get(8192)
            cp(dtmp.f32(), hT3[:, 0, :], r=hT_g(0, S), w=[dtmp.g()])
            dump("dbg_hT0", dtmp.f32(), [128, 2048], r=[dtmp.g()])
        if stage <= 1:
            continue

        win_v = win_d.ap().rearrange("(k p) n -> p k n", p=128)
        wa = Alloc(arena, work_base, TOT)
        wp = wa.get(KC * 256 * 2)
        wgp = wa.get(KC * 256 * 2)
        pw = wa.get(2 * 256 * 2)
        pg = wa.get(NT * 256 * 2)
        pT = [wa.get(512 * 2) for _ in range(2)]
        sg = wa.get(512 * 4)
        WP = wp.bf().rearrange("p (k n) -> p k n", k=KC)
        WGP = wgp.bf().rearrange("p (k n) -> p k n", k=KC)
        PW = pw.bf().rearrange("p (c n) -> p c n", c=2)
        PG = pg.bf().rearrange("p (i n) -> p i n", i=NT)
        BAND = band.bf().rearrange("p (j n) -> p j n", j=12)
        for g in range(4):
            dma('pool', WP, win_v[:, :, 3072 + g * 256:3072 + (g + 1) * 256], r=[], w=[wp.g()])
            dma('pool', WGP, win_v[:, :, 5120 + g * 256:5120 + (g + 1) * 256], r=[], w=[wgp.g()])
            dma('pool', PW, poolw_d.ap()[g].rearrange("(c p) n -> p c n", p=128), r=[], w=[pw.g()])
            for i in range(NT):
                for k in range(KC):
                    mm(banks[7][:, (i % 2) * 256:(i % 2 + 1) * 256], hT3[:, k, i * 128:(i + 1) * 128], WP[:, k, :], k == 0, k == KC - 1,
                       r=[hT_g(i * 128, (i + 1) * 128), wp.g()], w=[BK[7]])
                if i % 2 == 1:
                    cp(pg.bf()[:, (i - 1) * 256:(i + 1) * 256], banks[7][:], r=[BK[7]], w=[pg.g((i - 1) * 512, (i + 1) * 512)], eng='act')
            for c in range(4):
                for cc in range(2):
                    for il in range(4):
                        i = c * 4 + il
                        cur = g * 3 + (0 if i == 0 else 1)
                        mm(banks[cc][:, il * 128:(il + 1) * 128], PG[:, i, cc * 128:(cc + 1) * 128], BAND[:, cur, :], True, i == 0,
                           r=[pg.g(i * 512, (i + 1) * 512), band.g()], w=[BK[cc]])
                        if i > 0:
                            mm(banks[cc][:, il * 128:(il + 1) * 128], PG[:, i - 1, cc * 128:(cc + 1) * 128], BAND[:, g * 3 + 2, :], False, True,
                               r=[pg.g((i - 1) * 512, i * 512), band.g()], w=[BK[cc]])
                    cp(pT[cc].bf(), banks[cc][:], r=[BK[cc]], w=[pT[cc].g()], eng='act')
                for ec in range(2):
                    kch = g * 2 + ec
                    mm(banks[2][:], PW[:, 0, ec * 128:(ec + 1) * 128], pT[0].bf(), True, False, r=[pw.g(), pT[0].g()], w=[BK[2]])
                    mm(banks[2][:], PW[:, 1, ec * 128:(ec + 1) * 128], pT[1].bf(), False, True, r=[pw.g(), pT[1].g()], w=[BK[2]])
                    for k in range(KC):
                        mm(banks[3][:], WGP[:, k, ec * 128:(ec + 1) * 128], hT3[:, k, c * 512:(c + 1) * 512], k == 0, k == KC - 1,
                           r=[wgp.g(), hT_g(c * 512, (c + 1) * 512)], w=[BK[3]])
                    act(sg.f32(), banks[3][:], AF.Sigmoid, r=[BK[3]], w=[sg.g()])
                    stt(mT3[:, kch, c * 512:(c + 1) * 512], banks[2][:], pscT.f32()[:, kch:kch + 1], sg.f32(), ALU.mult, ALU.mult,
                        r=[BK[2], pscT.g(), sg.g()], w=[mT_g(kch, c * 512, (c + 1) * 512)])
        if dbg and b == 0 and stage == 2:
            wa2 = Alloc(arena, wa.p, TOT)
            dtmp = wa2.get(8192)
            for kk_ in (0, 5):
                cp(dtmp.f32(), mT3[:, kk_, :], r=[mT.g()], w=[dtmp.g()])
                dump(f"dbg_mT{kk_}", dtmp.f32(), [128, 2048], r=[dtmp.g()])
        if stage <= 2:
            continue

        wa = Alloc(arena, work_base, TOT)
        wq = wa.get(KC * 128 * 2)
        wk_ = wa.get(KC * 128 * 2)
        wv = wa.get(KC * 128 * 2)
        wg = wa.get(KC * 128 * 2)
        QTz = [wa.get(S * 2) for _ in range(2)]
        KT = wa.get(S * 2)
        Vb = wa.get(NT * 128 * 2)
        sgT = wa.get(S * 2)
        PT = [[wa.get(512 * 2) for _ in range(2)] for _ in range(2)]
        ntmp = [wa.get(256 * 4) for _ in range(2)]
        O1s = wa.get(2048)
        O2s = wa.get(2048)
        lnz = wa.get(2048)
        rz = wa.get(2048)
        rz2 = wa.get(2048)
        sq = wa.get(2048)
        t1 = wa.get(2048)
        rs = wa.get(2048)
        biasT = wa.get(8 * 1024)
        BT3 = biasT.f32().rearrange("p (h q) -> p h q", h=8)
        dma('sp', biasT.f32(), bias_scr.ap(), r=["bias_scr"], w=[biasT.g()])
        WQ = wq.bf().rearrange("p (k n) -> p k n", k=KC)
        WK = wk_.bf().rearrange("p (k n) -> p k n", k=KC)
        WV = wv.bf().rearrange("p (k n) -> p k n", k=KC)
        WG = wg.bf().rearrange("p (k n) -> p k n", k=KC)
        V3 = Vb.bf().rearrange("p (i n) -> p i n", i=NT)
        nheads = H if stage > 3 or not dbg else 1
        memset(QTz[0].bf()[64:128, :], 0.0, r=[], w=[QTz[0].g()])
        memset(QTz[1].bf()[0:64, :], 0.0, r=[], w=[QTz[1].g()])
        for h in range(nheads):
            dma('pool', WQ, win_v[:, :, h * 128:(h + 1) * 128], r=[], w=[wq.g()])
            dma('pool', WK, win_v[:, :, 1024 + h * 128:1024 + (h + 1) * 128], r=[], w=[wk_.g()])
            dma('pool', WV, win_v[:, :, 2048 + h * 128:2048 + (h + 1) * 128], r=[], w=[wv.g()])
            dma('pool', WG, win_v[:, :, 4096 + h * 128:4096 + (h + 1) * 128], r=[], w=[wg.g()])
            precast(4)
            pj = 0
            for (W_, wb_, dst, mode) in ((WQ, wq, None, 'q'), (WK, wk_, KT, 'k'), (WG, wg, sgT, 'g')):
                for c in range(4):
                    bk = pj % 4
                    pj += 1
                    for k in range(KC):
                        mm(banks[bk][:], W_[:, k, :], hT3[:, k, c * 512:(c + 1) * 512], k == 0, k == KC - 1,
                           r=[wb_.g(), hT_g(c * 512, (c + 1) * 512)], w=[BK[bk]])
                    if mode == 'q':
                        for m_ in range(2):
                            o_ap = QTz[m_].bf()[m_ * 64:(m_ + 1) * 64, c * 512:(c + 1) * 512]
                            P.add('act', (lambda o_ap=o_ap, bk=bk, m_=m_: (lambda e: e.mul(out=o_ap, in_=banks[bk][m_ * 64:(m_ + 1) * 64, :], mul=0.125)))(),
                                  r=[BK[bk]], w=[QTz[m_].g(c * 1024, (c + 1) * 1024)])
                        continue
                    o_ap = dst.bf()[:, c * 512:(c + 1) * 512]
                    o_g = dst.g(c * 1024, (c + 1) * 1024)
                    if mode == 'k':
                        cp(o_ap, banks[bk][:], r=[BK[bk]], w=[o_g], eng='act')
                    else:
                        act(o_ap, banks[bk][:], AF.Sigmoid, r=[BK[bk]], w=[o_g])
            for i in range(NT):
                bk = (i // 4) % 4
                for k in range(KC):
                    mm(banks[bk][:, (i % 4) * 128:(i % 4 + 1) * 128], hT3[:, k, i * 128:(i + 1) * 128], WV[:, k, :], k == 0, k == KC - 1,
                       r=[hT_g(i * 128, (i + 1) * 128), wv.g()], w=[BK[bk]])
                if i % 4 == 3:
                    cp(Vb.bf()[:, (i - 3) * 128:(i + 1) * 128], banks[bk][:], r=[BK[bk]], w=[Vb.g((i - 3) * 256, (i + 1) * 256)], eng='act')

            def qk(c, j):
                col0 = max(0, j - 4 * c) * 128
                par = j % 2
                for m in range(2):
                    bk = m * 2 + par
                    mm(banks[bk][:, col0:512], KT.bf()[:, j * 128:(j + 1) * 128],
                       QTz[m].bf()[:, c * 512 + col0:(c + 1) * 512], True, True,
                       r=[KT.g(j * 256, (j + 1) * 256), QTz[m].g(c * 1024 + col0 * 2, (c + 1) * 1024)], w=[BK[bk]])

            for c in range(4):
                jmax = 4 * c + 3
                qk(c, 0)
                for j in range(jmax + 1):
                    if j + 1 <= jmax:
                        qk(c, j + 1)
                    col0 = max(0, j - 4 * c) * 128
                    par = j % 2
                    il_lo = max(0, j - 4 * c)
                    il_hi = min(3, j + 1 - 4 * c)
                    far0 = max(0, j + 2 - 4 * c) * 128
                    for m in range(2):
                        bk = m * 2 + par
                        pt = PT[m][par]
                        if il_hi >= il_lo and il_hi >= 0:
                            n0, n1 = il_lo * 128, (il_hi + 1) * 128
                            b0 = (4 * c + il_lo - j) * 128
                            nn = n1 - n0
                            tt(ntmp[m].f32()[:, 0:nn], banks[bk][:, n0:n1], BT3[:, h, b0:b0 + nn], ALU.add,
                               r=[BK[bk], biasT.g(h * 1024, (h + 1) * 1024)], w=[ntmp[m].g()])
                            act(pt.bf()[:, n0:n1], ntmp[m].f32()[:, 0:nn], AF.Exp, r=[ntmp[m].g()], w=[pt.g(n0 * 2, n1 * 2)])
                        if far0 < 512:
                            act(pt.bf()[:, far0:512], banks[bk][:, far0:512], AF.Exp, r=[BK[bk], rb31m.g()], w=[pt.g(far0 * 2, 1024)],
                                bias=rb31m.f32()[:, h:h + 1])
                        mm(banks[4 + m][:, col0:512], V3[:, j, :], pt.bf()[:, col0:512], j == 0, j == jmax,
                           r=[Vb.g(j * 256, (j + 1) * 256), pt.g(col0 * 2, 1024)], w=[BK[4 + m]], skip_group_check=True)
                        mm(banks[6 + m][:, col0:512], ones_b.bf(), pt.bf()[:, col0:512], j == 0, j == jmax,
                           r=[ones_b.g(), pt.g(col0 * 2, 1024)], w=[BK[6 + m]], skip_group_check=True)
                cs = slice(c * 512, (c + 1) * 512)
                cp(O1s.f32(), banks[4][:], r=[BK[4]], w=[O1s.g()], eng='act')
                cp(O2s.f32(), banks[5][:], r=[BK[5]], w=[O2s.g()], eng='act')
                act(lnz.f32(), banks[6][:], AF.Ln, r=[BK[6]], w=[lnz.g()])
                act(rz.f32(), lnz.f32(), AF.Exp, r=[lnz.g()], w=[rz.g()], scale=-1.0)
                act(lnz.f32(), banks[7][:], AF.Ln, r=[BK[7]], w=[lnz.g()])
                act(rz2.f32(), lnz.f32(), AF.Exp, r=[lnz.g()], w=[rz2.g()], scale=-1.0)
                tt(t1.f32(), O1s.f32(), rz.f32(), ALU.mult, r=[O1s.g(), rz.g()], w=[t1.g()])
                tt(O2s.f32(), O2s.f32(), rz2.f32(), ALU.mult, r=[O2s.g(), rz2.g()], w=[O2s.g()])
                stt(t1.f32(), O2s.f32(), sm(SM_NLAM), t1.f32(), ALU.mult, ALU.add, r=[t1.g(), O2s.g(), small.g()], w=[t1.g()])
                act(sq.bf()[:, 0:512], t1.f32(), AF.Square, r=[t1.g()], w=[sq.g()])
                mm(banks[6][:], ones_b.bf(), sq.bf()[:, 0:512], True, True, r=[ones_b.g(), sq.g()], w=[BK[6]])
                ts(rs.f32(), banks[6][:], 1.0 / 128, 1e-5, ALU.mult, ALU.add, r=[BK[6]], w=[rs.g()])
                act(rs.f32(), rs.f32(), AF.Ln, r=[rs.g()], w=[rs.g()])
                act(rs.f32(), rs.f32(), AF.Exp, r=[rs.g()], w=[rs.g()], scale=-0.5)
                tt(t1.f32(), t1.f32(), rs.f32(), ALU.mult, r=[t1.g(), rs.g()], w=[t1.g()])
                stt(t1.f32(), t1.f32(), sm(SM_WSUB), sgT.bf()[:, cs], ALU.mult, ALU.mult, r=[t1.g(), small.g(), sgT.g(c * 1024, (c + 1) * 1024)], w=[t1.g()])
                tt(mT3[:, h, cs], t1.f32(), mT3[:, h, cs], ALU.add, r=[t1.g(), mT_g(h, c * 512, (c + 1) * 512)], w=[mT_g(h, c * 512, (c + 1) * 512)])
        if dbg and b == 0 and stage == 3:
            wa2 = Alloc(arena, wa.p, TOT)
            dtmp = wa2.get(8192)
            cp(dtmp.f32(), mT3[:, 0, :], r=[mT.g()], w=[dtmp.g()])
            dump("dbg_mT0", dtmp.f32(), [128, 2048], r=[dtmp.g()])
        if stage <= 3:
            continue

        precast(64)
        WOUT3 = wout.bf().rearrange("p (k n) -> p k n", k=KC)
        dma('pool', WOUT3, wout_d.ap().rearrange("(k p) n -> p k n", p=128), r=[], w=[wout.g()])
        for k in range(KC):
            tt(WOUT3[:, k, :], WOUT3[:, k, :], g1.f32(), ALU.mult, r=[wout.g(k * 2048, (k + 1) * 2048), g1.g()], w=[wout.g(k * 2048, (k + 1) * 2048)])
        WQ3 = hT.bf().rearrange("p (k n) -> p k n", k=KC)
        dma('pool', WQ3, wqry_d.ap().rearrange("(k p) n -> p k n", p=128), r=[], w=[hT.g()])
        wa = Alloc(arena, work_base, TOT)
        xt5 = g1
        xm = [wa.get(4096) for _ in range(2)]
        h2 = [wa.get(2048) for _ in range(2)]
        eidu = [wa.get(512) for _ in range(2)]
        gate = [wa.get(512) for _ in range(2)]
        tmpf = a1
        ot = sh1
        ssb = wa.get(8192)
        wkb = ssb
        tmpfX = ssb.sub(0, 4096)
        junkbX = ssb.sub(4096, 2048)
        h2T = ssb.sub(0, 2048)
        qTs = ssb.sub(4096, 4096)
        v16 = wa.get(1024)
        ixu = wa.get(1024)
        ixf = wa.get(1024)
        c16, posu, pau, pbu, paf, pbf, i1s, i2s, eidf, scm, ee, actv, gl, wgt = [wa.get(512) for _ in range(14)]
        zz = wa.get(32)
        rzz = wa.get(32)
        dgb = [wa.get(256) for _ in range(4)]
        NBUF = (TOT - wa.p) // 4096
        assert NBUF >= 5, NBUF
        cbuf = [wa.get(4096) for _ in range(NBUF)]
        SINK = mk(sink.bf(), [[0, D]])
        H2T3 = h2T.bf().rearrange("p (k t) -> p k t", k=KC)
        QTS3 = qTs.bf().rearrange("p (q t) -> p q t", q=16)
        SUBK3 = subkT.bf().rearrange("p (q n) -> p q n", q=16)
        SS3 = ssb.f32().rearrange("p (q n) -> p q n", q=16)
        WK3 = wkb.f32().rearrange("p (q n) -> p q n", q=16)
        V16 = v16.f32().rearrange("p (q j) -> p q j", q=16)
        IXU = ixu.u32().rearrange("p (q j) -> p q j", q=16)
        ntiles = NT if not dbg else 2

        def gen(name, r, w, eng='dve', **kw):
            P.add(eng, lambda e: getattr(e, name)(**kw), r=r, w=w)

        xmark = [0]

        def stageX(i):
            par = i % 2
            xm_, h2_, eidu_, gate_ = xm[par], h2[par], eidu[par], gate[par]
            t0 = i * 128
            dma('sp', xt5.f32(), x_d.ap()[b, t0:t0 + 128, :], r=[], w=[xt5.g()])
            for half in range(2):
                for k in range(KC):
                    mm(banks[2 + half][:], mT3[:, k, t0:t0 + 128], WOUT3[:, k, half * 512:(half + 1) * 512], k == 0, k == KC - 1,
                       r=[mT_g(k, t0, t0 + 128), wout.g()], w=[BK[2 + half]])
            for half in range(2):
                hs = slice(half * 512, (half + 1) * 512)
                tt(xm_.f32()[:, hs], banks[2 + half][:], xt5.f32()[:, hs], ALU.add, r=[BK[2 + half], xt5.g()], w=[xm_.g(half * 2048, (half + 1) * 2048)])
            act(junkbX.bf(), xm_.f32(), AF.Square, r=[xm_.g()], w=[junkbX.g(), small.g()], accum_out=sm(SM_SS2))
            rstd_chain(SM_SS2, SM_LN2, SM_RSTD2, D, 1e-6)
            for hf in range(2):
                hs = slice(hf * 512, (hf + 1) * 512)
                stt(tmpfX.f32()[:, hs], xm_.f32()[:, hs], sm(SM_RSTD2), a2.f32()[:, hs], ALU.mult, ALU.mult, r=[xm_.g(), small.g(), a2.g()], w=[tmpfX.g(hf * 2048, (hf + 1) * 2048)])
                tt(h2_.bf()[:, hs], tmpfX.f32()[:, hs], sh2.f32()[:, hs], ALU.add, r=[tmpfX.g(hf * 2048, (hf + 1) * 2048), sh2.g()], w=[h2_.g(hf * 1024, (hf + 1) * 1024)])
            for k in range(KC):
                tr(BTb[:, k * 128:(k + 1) * 128], h2_.bf()[:, k * 128:(k + 1) * 128], ident_b.bf(), r=[h2_.g(), ident_b.g()], w=[BK[7]])
            cp(h2T.bf(), BTb, r=[BK[7]], w=[h2T.g()], eng='act')
            for qc in range(16):
                bk = 2 + (qc // 4) % 2
                for k in range(KC):
                    mm(banks[bk][:, (qc % 4) * 128:(qc % 4 + 1) * 128], WQ3[:, k, qc * 128:(qc + 1) * 128], H2T3[:, k, :], k == 0, k == KC - 1,
                       r=[hT.g(), h2T.g()], w=[BK[bk]])
                if qc % 4 == 3:
                    cp(qTs.bf()[:, (qc - 3) * 128:(qc + 1) * 128], banks[bk][:], r=[BK[bk]], w=[qTs.g((qc - 3) * 256, (qc + 1) * 256)], eng='act')
            for qc in range(16):
                bk = 4 + qc // 4
                mm(banks[bk][:, (qc % 4) * 128:(qc % 4 + 1) * 128], QTS3[:, qc, :], SUBK3[:, qc, :], True, True,
                   r=[qTs.g(qc * 256, (qc + 1) * 256), subkT.g()], w=[BK[bk]])
            for q4 in range(4):
                cp(ssb.f32()[:, q4 * 512:(q4 + 1) * 512], banks[4 + q4][:], r=[BK[4 + q4]], w=[ssb.g(q4 * 2048, (q4 + 1) * 2048)], eng='act')
            if dbg and i == 0:
                dump("dbg_xm", xm_.f32(), [128, 1024], r=[xm_.g()])
                dump("dbg_s", ssb.f32(), [128, 2048], r=[ssb.g()])
            xmark[0] = len(P.capture) if P.capture is not None else 0
            sg_ = lambda qc: ssb.g(qc * 512, (qc + 1) * 512)
            wg_ = lambda qc: wkb.g(qc * 512, (qc + 1) * 512)
            vk = lambda qc, hf: f"v16:{qc}:{hf}"
            ik = lambda qc, hf: f"ixu:{qc}:{hf}"
            ALLV = [vk(q_, h_) for q_ in range(16) for h_ in range(2)]
            ALLI = [ik(q_, h_) for q_ in range(16) for h_ in range(2)]
            for qc in range(16):
                gen('max', [sg_(qc), v16.g()], [vk(qc, 0)], out=V16[:, qc, 0:8], in_=SS3[:, qc, :])
            for qc in range(16):
                gen('max_index', [sg_(qc), vk(qc, 0), ixu.g()], [ik(qc, 0)], out=IXU[:, qc, 0:8], in_max=V16[:, qc, 0:8], in_values=SS3[:, qc, :])
            for qc in range(16):
                gen('match_replace', [sg_(qc), vk(qc, 0)], [wg_(qc)], out=WK3[:, qc, :], in_to_replace=V16[:, qc, 0:8], in_values=SS3[:, qc, :], imm_value=NEG)
            for qc in range(16):
                gen('max', [wg_(qc), v16.g()], [vk(qc, 1)], out=V16[:, qc, 8:16], in_=WK3[:, qc, :])
            for qc in range(16):
                gen('max_index', [wg_(qc), vk(qc, 1), ixu.g()], [ik(qc, 1)], out=IXU[:, qc, 8:16], in_max=V16[:, qc, 8:16], in_values=WK3[:, qc, :])
            cp(ixf.f32(), ixu.u32(), r=ALLI, w=[ixf.g()])
            cand, wk2 = ssb, wkb
            C4 = cand.f32().rearrange("p (h n) -> p h n", h=8)
            W4 = wk2.f32().rearrange("p (h n) -> p h n", h=8)
            vf = v16.f32()
            for hp in range(4):
                tt(mk(cand.f32(), [[256, 2], [16, 16], [1, 16]], hp * 512), mk(vf, [[32, 2], [1, 16], [0, 16]], hp * 64),
                   mk(vf, [[32, 2], [0, 16], [1, 16]], hp * 64 + 16), ALU.add, r=ALLV, w=[cand.g(hp * 2048, (hp + 1) * 2048)])
            C16 = c16.f32().rearrange("p (h j) -> p h j", h=8)
            POS = posu.u32().rearrange("p (h j) -> p h j", h=8)
            cg_ = lambda h_: cand.g(h_ * 1024, (h_ + 1) * 1024)
            w2g_ = lambda h_: wk2.g(h_ * 1024, (h_ + 1) * 1024)
            ck = lambda h_, hf: f"c16:{h_}:{hf}"
            pk = lambda h_, hf: f"pos:{h_}:{hf}"
            ALLC = [ck(h_, f_) for h_ in range(8) for f_ in range(2)]
            ALLP = [pk(h_, f_) for h_ in range(8) for f_ in range(2)]
            for h_ in range(8):
                gen('max', [cg_(h_), c16.g()], [ck(h_, 0)], out=C16[:, h_, 0:8], in_=C4[:, h_, :])
            for h_ in range(8):
                gen('max_index', [cg_(h_), ck(h_, 0), posu.g()], [pk(h_, 0)], out=POS[:, h_, 0:8], in_max=C16[:, h_, 0:8], in_values=C4[:, h_, :])
            for h_ in range(8):
                gen('match_replace', [cg_(h_), ck(h_, 0)], [w2g_(h_)], out=W4[:, h_, :], in_to_replace=C16[:, h_, 0:8], in_values=C4[:, h_, :], imm_value=NEG)
            for h_ in range(8):
                gen('max', [w2g_(h_), c16.g()], [ck(h_, 1)], out=C16[:, h_, 8:16], in_=W4[:, h_, :])
            for h_ in range(8):
                gen('max_index', [w2g_(h_), ck(h_, 1), posu.g()], [pk(h_, 1)], out=POS[:, h_, 8:16], in_max=C16[:, h_, 8:16], in_values=W4[:, h_, :])
            gen('tensor_single_scalar', ALLP, [pau.g()], out=pau.u32(), in_=posu.u32(), scalar=4, op=ALU.logical_shift_right)
            gen('tensor_single_scalar', ALLP, [pbu.g()], out=pbu.u32(), in_=posu.u32(), scalar=15, op=ALU.bitwise_and)
            cp(paf.f32(), pau.u32(), r=[pau.g()], w=[paf.g()])
            cp(pbf.f32(), pbu.u32(), r=[pbu.g()], w=[pbf.g()])
            oh, prod = ssb, wkb
            for (pf_, off_, dst_) in ((paf, 0, i1s), (pbf, 16, i2s)):
                for hp in range(4):
                    og = oh.g(hp * 2048, (hp + 1) * 2048)
                    tt(mk(oh.f32(), [[16, 32], [1, 16]], hp * 512), mk(pf_.f32(), [[1, 32], [0, 16]], hp * 32), mk(iota16.f32(), [[0, 32], [1, 16]]), ALU.is_equal,
                       r=[pf_.g(), iota16.g()], w=[og])
                    tt(mk(prod.f32(), [[256, 2], [16, 16], [1, 16]], hp * 512), mk(oh.f32(), [[256, 2], [16, 16], [1, 16]], hp * 512),
                       mk(ixf.f32(), [[32, 2], [0, 16], [1, 16]], hp * 64 + off_), ALU.mult, r=[og, ixf.g()], w=[og])
                    tred(dst_.f32()[:, hp * 32:(hp + 1) * 32], mk(prod.f32(), [[16, 32], [1, 16]], hp * 512), ALU.add, r=[og], w=[dst_.g()])
            stt(eidf.f32(), i1s.f32(), 128.0, i2s.f32(), ALU.mult, ALU.add, r=[i1s.g(), i2s.g()], w=[eidf.g()])
            cp(eidu_.u32(), eidf.f32(), r=[eidf.g()], w=[eidu_.g()])
            tt(mk(scm.f32(), [[16, 8], [1, 16]]), mk(c16.f32(), [[16, 8], [1, 16]]), mk(c16.f32(), [[16, 8], [0, 16]]), ALU.subtract, r=ALLC, w=[scm.g()])
            act(ee.f32(), scm.f32(), AF.Exp, r=[scm.g()], w=[ee.g()])
            tred(zz.f32(), mk(ee.f32(), [[16, 8], [1, 16]]), ALU.add, r=[ee.g()], w=[zz.g()])
            gen('reciprocal', [zz.g()], [rzz.g()], out=rzz.f32(), in_=zz.f32())
            tt(mk(gate_.f32(), [[16, 8], [1, 16]]), mk(ee.f32(), [[16, 8], [1, 16]]), mk(rzz.f32(), [[1, 8], [0, 16]]), ALU.mult, r=[ee.g(), rzz.g()], w=[gate_.g()])
            if dbg and i == 0:
                dump("dbg_eid", eidf.f32(), [128, 128], r=[eidf.g()])
                dump("dbg_gate", gate_.f32(), [128, 128], r=[gate_.g()])

        def stageY(i, pump):
            par = i % 2
            xm_, h2_, eidu_, gate_ = xm[par], h2[par], eidu[par], gate[par]
            t0 = i * 128
            EID = eidu_.u32()

            def gather(out_ap, slot, r, w):
                P.add('pool', lambda e: e.indirect_dma_start(out=out_ap, out_offset=None, in_=etab_d.ap(),
                                                             in_offset=bass.IndirectOffsetOnAxis(ap=EID[:, slot:slot + 1], axis=0)),
                      r=r, w=w, dma=True)

            def slot_tail(sl):
                cb = cbuf[sl % NBUF]
                dg_ = dgb[sl % 4]
                act(wgt.f32()[:, sl:sl + 1], gl.f32()[:, sl:sl + 1], AF.Copy, r=[f"gl{sl}", gate_.g(), wgt.g()], w=[f"wgt{sl}"],
                    scale=gate_.f32()[:, sl:sl + 1])
                P.add('act', (lambda dg_=dg_, sl=sl: (lambda e: e.activation(out=dg_.bf(), in_=ident_f.f32(), func=AF.Copy, scale=wgt.f32()[:, sl:sl + 1])))(),
                      r=[ident_f.g(), f"wgt{sl}"], w=[dg_.g()])
                for half in range(2):
                    mm(banks[half][:], dg_.bf(), cb.bf()[:, D + half * 512:D + (half + 1) * 512], sl == 0, sl == 127,
                       r=[dg_.g(), cb.g(2048, 4096)], w=[BK[half]])

            for sl in range(128):
                cb = cbuf[sl % NBUF]
                gather(cb.bf(), sl, r=[eidu_.g()] + ETAB_KEYS, w=[cb.g()])
                P.add('dve', (lambda cb=cb, sl=sl: (lambda e: e.scalar_tensor_tensor(out=SINK, in0=cb.bf()[:, 0:D], scalar=1.0, in1=h2_.bf(), op0=ALU.mult, op1=ALU.mult,
                                                                                 accum_out=actv.f32()[:, sl:sl + 1])))(),
                      r=[cb.g(0, 2048), h2_.g(), actv.g()], w=[f"actv{sl}"])
                act(gl.f32()[:, sl:sl + 1], actv.f32()[:, sl:sl + 1], AF.Gelu, r=[f"actv{sl}", gl.g()], w=[f"gl{sl}"])
                if sl >= 1:
                    slot_tail(sl - 1)
                pump(sl)
            slot_tail(127)
            for half in range(2):
                hs = slice(half * 512, (half + 1) * 512)
                tt(tmpf.f32()[:, hs], banks[half][:], g2.f32()[:, hs], ALU.mult, r=[BK[half], g2.g()], w=[tmpf.g(half * 2048, (half + 1) * 2048)])
                tt(ot.f32()[:, hs], tmpf.f32()[:, hs], xm_.f32()[:, hs], ALU.add, r=[tmpf.g(half * 2048, (half + 1) * 2048), xm_.g()], w=[ot.g(half * 2048, (half + 1) * 2048)])
            if dbg and i == 0:
                dump("dbg_xo", ot.f32(), [128, 1024], r=[ot.g()])
            act(SINK, ot.f32(), AF.Square, r=[ot.g()], w=[small.g()], accum_out=sm(SM_SS3))
            rstd_chain(SM_SS3, SM_LN3, SM_RSTD3, D, 1e-6)
            stt(ot.f32(), ot.f32(), sm(SM_RSTD3), nf_bc.f32(), ALU.mult, ALU.mult, r=[ot.g(), small.g(), nf_bc.g()], w=[ot.g()])
            dma('sp', out_d.ap()[b, t0:t0 + 128, :], ot.f32(), r=[ot.g()], w=[f"out{b}_{i}"])

        def drain(q, n=None):
            k = 0
            while q and (n is None or k < n):
                P.add(*q.pop(0))
                k += 1

        P.capture = []
        stageX(0)
        q = P.capture
        P.capture = None
        drain(q)
        for i in range(ntiles):
            if i + 1 < ntiles:
                P.capture = []
                stageX(i + 1)
                q = P.capture
                P.capture = None
            else:
                q = []
            n_tot, n_pre = len(q), xmark[0]
            S1, S2 = 24, 122

            def pump_to(sl, q=q, n_tot=n_tot, n_pre=n_pre):
                if sl < S1:
                    target = n_pre * (sl + 1) // S1
                else:
                    target = n_pre + (n_tot - n_pre) * min(sl + 1 - S1, S2 - S1) // (S2 - S1)
                drain(q, max(0, target - (n_tot - len(q))))
            stageY(i, pump_to)
            drain(q)

    P.emit(nc, stack)
    stack.close()
    return nc, list(dbg_d.keys())


def _prep_inputs(inputs):
    f = np.float32
    x = np.asarray(inputs['x'], f)
    c = np.asarray(inputs['c'], f)
    cst = _host_consts()
    rep = lambda v, n=128: np.ascontiguousarray(np.broadcast_to(np.asarray(v, f).reshape(1, -1), (n, np.asarray(v).size)))
    nw = np.concatenate([np.asarray(inputs['norm_mix_w'], f).reshape(-1), np.asarray(inputs['norm_ffn_w'], f).reshape(-1),
                         np.asarray(inputs['norm_final_w'], f).reshape(-1)])
    lam = np.concatenate([np.asarray(inputs[k], f).reshape(-1) for k in ('lambda_q1', 'lambda_k1', 'lambda_q2', 'lambda_k2')])
    sk1 = np.asarray(inputs['sub_keys_1'], f)[0]
    sk2 = np.asarray(inputs['sub_keys_2'], f)[0]
    subkT = np.zeros((128, 16, 128), f)
    for h in range(8):
        subkT[:, h * 2 + 0, :] = sk1[h].T
        subkT[:, h * 2 + 1, :] = sk2[h].T
    shared = {
        'w_ada': np.ascontiguousarray(np.asarray(inputs['w_ada'], f)[0]),
        'b_ada_bc': rep(inputs['b_ada']),
        'nw_bc': rep(nw),
        'w_in': np.ascontiguousarray(np.asarray(inputs['w_in'], f)[0]),
        'w_out': np.ascontiguousarray(np.asarray(inputs['w_out'], f)[0]),
        'w_query': np.ascontiguousarray(np.asarray(inputs['w_query'], f)[0]),
        'pool_w': np.ascontiguousarray(np.asarray(inputs['pool_w'], f)[0]),
        'pool_scaleT': np.ascontiguousarray(np.asarray(inputs['pool_scale'], f).reshape(8, 128).T),
        'subkT': np.ascontiguousarray(subkT.reshape(128, 2048)),
        'e_down': np.ascontiguousarray(np.asarray(inputs['expert_down'], f)[0]),
        'e_up': np.ascontiguousarray(np.asarray(inputs['expert_up'], f)[0]),
        'lam_bc': rep(lam),
        'sublnT': np.ascontiguousarray(np.asarray(inputs['subln_w'], f).reshape(128, 1)),
        'relb_bc': rep(np.asarray(inputs['rel_bias'], f).reshape(-1)),
        'masks': np.ascontiguousarray(cst['masks'].reshape(128, -1)),
        'negmask': cst['negmask'],
        'band': np.ascontiguousarray(cst['band'].reshape(128, -1)),
        'ident': cst['ident'],
        'iota16': cst['iota16'],
        'sel0': cst['sel0'],
    }
    in_maps = []
    for core in range(NCORES):
        m = dict(shared)
        m['x'] = np.ascontiguousarray(x[core * NB:(core + 1) * NB])
        cc = c[core * NB:(core + 1) * NB]
        m['cT'] = np.ascontiguousarray(cc.reshape(NB, KC, 128).transpose(2, 0, 1).reshape(128, NB * KC))
        in_maps.append(m)
    return in_maps


_CACHE = {}


def kernel(**inputs):
    in_maps = _prep_inputs(inputs)
    if 'nc' not in _CACHE:
        _CACHE['nc'] = build()[0]
    nc = _CACHE['nc']
    res = run_bass_kernel_spmd(nc, in_maps, core_ids=list(range(NCORES)))
    out = np.concatenate([np.asarray(r['out']) for r in res.results], axis=0)
    return out.astype(np.float32)
```

```python
import os
import math
import contextlib
import numpy as np
import concourse.bass as bass
import concourse.mybir as mybir
from concourse.bass_utils import run_bass_kernel_spmd

dt = mybir.dt
AF = mybir.ActivationFunctionType
ALU = mybir.AluOpType
AX = mybir.AxisListType
F32, BF16, U32 = dt.float32, dt.bfloat16, dt.uint32

NCORES = 8
NB = 2
S = 2048
NT = 16
D = 1024
KC = 8
H = 8
DIN = 6144
NEXP = 16384
OFF = 8.0
NEG = -1.0e30
G = 512


class Prog:
    def __init__(self):
        self.ops = []
        self.last_w = {}
        self.readers = {}
        self.capture = None

    def add(self, eng, fn, r=(), w=(), dma=False):
        if self.capture is not None:
            self.capture.append((eng, fn, r, w, dma))
            return None
        i = len(self.ops)
        deps = set()
        rk = _flat(r)
        wk = _flat(w)
        for k in rk:
            lw = self.last_w.get(k)
            if lw is not None:
                deps.add(lw)
        for k in wk:
            lw = self.last_w.get(k)
            if lw is not None:
                deps.add(lw)
            deps.update(self.readers.get(k, ()))
        deps.discard(i)
        for k in rk:
            self.readers.setdefault(k, []).append(i)
        for k in wk:
            self.last_w[k] = i
            self.readers[k] = []
        self.ops.append(dict(eng=eng, fn=fn, deps=deps, dma=dma, sig=None, need=False, pre=None))
        return i

    def emit(self, nc, stack):
        ops = self.ops
        EPOCH = 30000
        NSLOT = {'sp': 24, 'pool': 24, 'act': 8}
        for o in ops:
            for d in o['deps']:
                p = ops[d]
                if p['eng'] == 'pe' and o['eng'] == 'pe' and not p['dma'] and not o['dma']:
                    continue
                p['need'] = True
        cnt = {e: 0 for e in ('pe', 'act', 'dve', 'pool', 'sp')}
        esems = {e: [] for e in cnt}
        dsems = {q: [stack.enter_context(nc.semaphore(f"d_{q}_{i}")) for i in range(n)] for q, n in NSLOT.items()}
        duse = {q: [0] * n for q, n in NSLOT.items()}
        dnext = {q: 0 for q in NSLOT}
        for o in ops:
            e = o['eng']
            if o['dma']:
                s = dnext[e]
                dnext[e] = (s + 1) % NSLOT[e]
                if duse[e][s] > 0:
                    o['pre'] = (dsems[e][s], 16 * duse[e][s])
                duse[e][s] += 1
                o['sig'] = (dsems[e][s], 16 * duse[e][s])
            elif o['need']:
                ep = cnt[e] // EPOCH
                while len(esems[e]) <= ep:
                    esems[e].append(stack.enter_context(nc.semaphore(f"e_{e}_{len(esems[e])}")))
                cnt[e] += 1
                o['sig'] = (esems[e][ep], cnt[e] - ep * EPOCH)
        by_eng = {e: [o for o in ops if o['eng'] == e] for e in cnt}
        final_waits = []
        for q in NSLOT:
            for s in range(NSLOT[q]):
                if duse[q][s] > 0:
                    final_waits.append((dsems[q][s], 16 * duse[q][s]))

        def run(ename, eng):
            waited = {}
            for o in by_eng[ename]:
                needs = {}
                for d in o['deps']:
                    p = ops[d]
                    if p['eng'] == 'pe' and ename == 'pe' and not p['dma'] and not o['dma']:
                        continue
                    sem, val = p['sig']
                    key = id(sem)
                    if needs.get(key, (None, 0))[1] < val:
                        needs[key] = (sem, val)
                if o['pre'] is not None:
                    sem, val = o['pre']
                    key = id(sem)
                    if needs.get(key, (None, 0))[1] < val:
                        needs[key] = (sem, val)
                for key, (sem, val) in needs.items():
                    if waited.get(key, 0) < val:
                        eng.wait_ge(sem, val)
                        waited[key] = val
                inst = o['fn'](eng)
                if o['sig'] is not None:
                    inst.then_inc(o['sig'][0], 16 if o['dma'] else 1)
            if ename == 'sp':
                for sem, val in final_waits:
                    eng.wait_ge(sem, val)

        with nc.Block() as block:
            @block.tensor
            def _(e):
                run('pe', e)

            @block.scalar
            def _(e):
                run('act', e)

            @block.vector
            def _(e):
                run('dve', e)

            @block.gpsimd
            def _(e):
                run('pool', e)

            @block.sync
            def _(e):
                run('sp', e)


def _flat(x):
    out = []
    for k in x:
        if isinstance(k, (list, tuple, set, range)):
            out.extend(_flat(k))
        else:
            out.append(k)
    return out


class Buf:
    def __init__(self, arena, off, nbytes):
        assert off % 4 == 0 and nbytes % 4 == 0
        self.A, self.off, self.nbytes = arena, off, nbytes

    def g(self, lo=0, hi=None):
        hi = self.nbytes if hi is None else hi
        return range((self.off + lo) // G, (self.off + hi + G - 1) // G)

    def f32(self):
        return self.A[:, self.off // 4:(self.off + self.nbytes) // 4]

    def bf(self):
        return self.A[:, self.off // 4:(self.off + self.nbytes) // 4].bitcast(BF16)

    def u32(self):
        return self.A[:, self.off // 4:(self.off + self.nbytes) // 4].bitcast(U32)

    def sub(self, lo, n):
        return Buf(self.A, self.off + lo, n)


class Alloc:
    def __init__(self, arena, base, limit):
        self.A, self.p, self.limit = arena, base, limit

    def get(self, nbytes):
        nb = (nbytes + G - 1) // G * G
        b = Buf(self.A, self.p, (nbytes + 3) // 4 * 4)
        self.p += nb
        assert self.p <= self.limit, (self.p, self.limit)
        return b


def mk(ap, pattern, extra_off=0):
    return bass.AP(tensor=ap.tensor, offset=ap.offset + extra_off, ap=[list(ap.ap[0])] + [list(p) for p in pattern])


def _t5_bucket(d):
    d = np.maximum(d, 0)
    x = np.maximum(d, 1).astype(np.float32) / np.float32(16)
    large = 16 + (np.log(x).astype(np.float32) / np.float32(math.log(128 / 16)) * np.float32(16)).astype(np.int32)
    large = np.minimum(large, 31)
    return np.where(d < 16, d, large)


def _host_consts():
    kk = np.arange(128)[:, None]
    qq = np.arange(256)[None, :]
    dist = qq - kk
    valid = dist >= 0
    bk = _t5_bucket(dist)
    masks = np.zeros((128, 32, 256), np.float32)
    for b in range(32):
        masks[:, b, :] = (valid & (bk == b)).astype(np.float32)
    negmask = np.where(valid, 0.0, NEG).astype(np.float32)
    band = np.zeros((128, 12, 128), np.float32)
    s = np.arange(128)[:, None]
    t = np.arange(128)[None, :]
    for gi, w in enumerate((2, 4, 8, 16)):
        cnt0 = np.minimum(t + 1, w).astype(np.float32)
        band[:, gi * 3 + 0, :] = ((s <= t) & (s > t - w)) / cnt0 - (s == t)
        band[:, gi * 3 + 1, :] = ((s <= t) & (s > t - w)) / np.float32(w) - (s == t)
        band[:, gi * 3 + 2, :] = (s > 128 + t - w) / np.float32(w)
    ident = np.eye(128, dtype=np.float32)
    iota16 = np.broadcast_to(np.arange(16, dtype=np.float32)[None, :], (128, 16)).copy()
    sel0 = np.zeros((64, 128), np.float32)
    sel0[0, :] = 1.0
    sel0[32, :] = 1.0
    return dict(masks=masks, negmask=negmask, band=band, ident=ident, iota16=iota16, sel0=sel0)


def build(stage=99, dbg=False):
    nc = bass.Bass("TRN2", target_bir_lowering=False)
    P = Prog()

    def din(name, shape, dtype=F32):
        return nc.dram_tensor(name, list(shape), dtype, kind="ExternalInput")

    x_d = din("x", [NB, S, D])
    cT_d = din("cT", [128, NB * KC])
    wada_d = din("w_ada", [D, DIN])
    bada_d = din("b_ada_bc", [128, DIN])
    nw_d = din("nw_bc", [128, 3 * D])
    win_d = din("w_in", [D, DIN])
    wout_d = din("w_out", [D, D])
    wqry_d = din("w_query", [D, 2048])
    poolw_d = din("pool_w", [4, 256, 256])
    psc_d = din("pool_scaleT", [128, 8])
    subk_d = din("subkT", [128, 16 * 128])
    edown_d = din("e_down", [NEXP, D])
    eup_d = din("e_up", [NEXP, D])
    lam_d = din("lam_bc", [128, 256])
    subln_d = din("sublnT", [128, 1])
    relb_d = din("relb_bc", [128, 256])
    masks_d = din("masks", [128, 32 * 256])
    negm_d = din("negmask", [128, 256])
    band_d = din("band", [128, 12 * 128])
    ident_d = din("ident", [128, 128])
    iota_d = din("iota16", [128, 16])
    sel0_d = din("sel0", [64, 128])
    out_d = nc.dram_tensor("out", [NB, S, D], F32, kind="ExternalOutput")
    etab_d = nc.dram_tensor("etab", [NEXP, 2 * D], BF16, kind="Internal")
    dbg_d = {}

    def dbg_out(name, shape):
        dbg_d[name] = nc.dram_tensor(name, list(shape), F32, kind="ExternalOutput")
        return dbg_d[name]

    stack = contextlib.ExitStack()
    TOT = 212480
    arena = stack.enter_context(nc.sbuf_tensor("arena", [128, TOT // 4], F32))
    banks = [stack.enter_context(nc.psum_tensor(f"B{i}", [128, 512], F32)) for i in range(8)]
    BK = [f"B{i}" for i in range(8)]

    al = Alloc(arena, 0, TOT)
    ident_f = al.get(512)
    ident_b = al.get(256)
    ones_b = al.get(256)
    ones_f = al.get(512)
    sel0 = al.get(512)
    relb = al.get(1024)
    rb31m = al.get(32)
    lamv = al.get(1024)
    small = al.get(512)
    pscT = al.get(32)
    band = al.get(12 * 128 * 2)
    subkT = al.get(16 * 128 * 2)
    iota16 = al.get(64)
    sink = al.get(64)
    nf_bc = al.get(4096)
    sh1 = al.get(4096)
    a1 = al.get(4096)
    g1 = al.get(4096)
    sh2 = al.get(4096)
    a2 = al.get(4096)
    g2 = al.get(4096)
    hT = al.get(KC * S * 2)
    mT = al.get(KC * S * 2)
    wout = al.get(KC * D * 2)
    work_base = al.p
    SM = small.f32()
    (SM_S1, SM_S2, SM_E1, SM_E2, SM_NLAM, SM_WSUB, SM_SS, SM_RSTD, SM_LN, SM_SS2, SM_RSTD2, SM_SS3, SM_RSTD3,
     SM_LN2, SM_LN3, SM_SUBLN) = range(16)

    def sm(i):
        return SM[:, i:i + 1]

    def smg(i):
        return small.g()

    def dma(q, out, in_, r, w):
        P.add(q, lambda e: e.dma_start(out=out, in_=in_), r=r, w=w, dma=True)

    def mm(out, lhsT, rhs, start, stop, r, w, **kw):
        P.add('pe', lambda e: e.matmul(out, lhsT, rhs, start=start, stop=stop, **kw), r=r, w=w)

    def tr(out, in_, ident, r, w):
        P.add('pe', lambda e: e.transpose(out, in_, ident), r=r, w=w)

    def act(out, in_, func, r, w, bias=None, scale=None, accum_out=None, eng='act'):
        def f(e):
            kw = {}
            if bias is not None:
                kw['bias'] = bias
            if scale is not None:
                kw['scale'] = scale
            if accum_out is not None:
                kw['accum_out'] = accum_out
            return e.activation(out=out, in_=in_, func=func, **kw)
        P.add('act', f, r=r, w=w)

    def tt(out, in0, in1, op, r, w, eng='dve'):
        P.add(eng, lambda e: e.tensor_tensor(out=out, in0=in0, in1=in1, op=op), r=r, w=w)

    def ts(out, in0, s1, s2, op0, op1, r, w, eng='dve'):
        if op1 is None:
            P.add(eng, lambda e: e.tensor_scalar(out=out, in0=in0, scalar1=s1, scalar2=None, op0=op0), r=r, w=w)
        else:
            P.add(eng, lambda e: e.tensor_scalar(out=out, in0=in0, scalar1=s1, scalar2=s2, op0=op0, op1=op1), r=r, w=w)

    def stt(out, in0, scalar, in1, op0, op1, r, w):
        P.add('dve', lambda e: e.scalar_tensor_tensor(out=out, in0=in0, scalar=scalar, in1=in1, op0=op0, op1=op1), r=r, w=w)

    def cp(out, in_, r, w, eng='dve'):
        if eng == 'act':
            P.add('act', lambda e: e.copy(out=out, in_=in_), r=r, w=w)
        else:
            P.add(eng, lambda e: e.tensor_copy(out=out, in_=in_), r=r, w=w)

    def ttr(out, in0, in1, accum_out, r, w):
        P.add('dve', lambda e: e.scalar_tensor_tensor(out=out, in0=in0, scalar=1.0, in1=in1, op0=ALU.mult, op1=ALU.mult,
                                                     accum_out=accum_out), r=r, w=w)

    def tred(out, in_, op, r, w):
        P.add('dve', lambda e: e.tensor_reduce(out=out, in_=in_, axis=AX.X, op=op), r=r, w=w)

    def memset(ap, val, r, w, eng='dve'):
        P.add(eng, lambda e: e.memset(ap, val), r=r, w=w)

    def rstd_chain(ss_i, ln_i, rstd_i, n, eps):
        ts(sm(ln_i), sm(ss_i), 1.0 / n, eps, ALU.mult, ALU.add, r=[small.g()], w=[small.g()])
        act(sm(ln_i), sm(ln_i), AF.Ln, r=[small.g()], w=[small.g()])
        act(sm(rstd_i), sm(ln_i), AF.Exp, r=[small.g()], w=[small.g()], scale=-0.5)

    def dump(name, ap_sb, shape, r):
        if name in dbg_d:
            return
        d = dbg_out(name, shape)
        dma('sp', d.ap(), ap_sb, r=r, w=[name])

    dma('sp', ident_f.f32(), ident_d.ap(), r=[], w=[ident_f.g()])
    dma('sp', sel0.f32()[0:64, :], sel0_d.ap(), r=[], w=[sel0.g()])
    dma('sp', relb.f32(), relb_d.ap(), r=[], w=[relb.g()])
    dma('sp', lamv.f32(), lam_d.ap(), r=[], w=[lamv.g()])
    dma('sp', pscT.f32(), psc_d.ap(), r=[], w=[pscT.g()])
    dma('sp', iota16.f32(), iota_d.ap(), r=[], w=[iota16.g()])
    dma('sp', nf_bc.f32(), nw_d.ap()[:, 2 * D:3 * D], r=[], w=[nf_bc.g()])
    dma('sp', sm(SM_SUBLN), subln_d.ap(), r=[], w=[small.g()])
    dma('pool', band.bf(), band_d.ap(), r=[], w=[band.g()])
    dma('pool', subkT.bf(), subk_d.ap(), r=[], w=[subkT.g()])
    ETAB_JOBS = [(ti, r0) for ti in range(2) for r0 in range(0, NEXP, 1024)]
    ETAB_KEYS = [f"etab{ti}_{r0}" for (ti, r0) in ETAB_JOBS]

    def precast(n):
        for _ in range(n):
            if not ETAB_JOBS:
                return
            ti, r0 = ETAB_JOBS.pop(0)
            tbl = (edown_d, eup_d)[ti]
            dma('pool', etab_d.ap()[r0:r0 + 1024, ti * D:(ti + 1) * D], tbl.ap()[r0:r0 + 1024, :], r=[], w=[f"etab{ti}_{r0}"])

    cp(ident_b.bf(), ident_f.f32(), r=[ident_f.g()], w=[ident_b.g()])
    memset(ones_b.bf(), 1.0, r=[], w=[ones_b.g()])
    memset(ones_f.f32(), 1.0, r=[], w=[ones_f.g()])
    ts(rb31m.f32(), relb.f32()[:, 31 * 8:32 * 8], -OFF, None, ALU.add, None, r=[relb.g()], w=[rb31m.g()])
    ts(sm(SM_WSUB), sm(SM_SUBLN), 0.8, None, ALU.mult, None, r=[small.g()], w=[small.g()])
    wa = Alloc(arena, work_base, TOT)
    junk64 = wa.get(256)
    LV = lamv.f32()
    ttr(junk64.f32(), LV[:, 0:64], LV[:, 64:128], sm(SM_S1), r=[lamv.g()], w=[junk64.g(), small.g()])
    ttr(junk64.f32(), LV[:, 128:192], LV[:, 192:256], sm(SM_S2), r=[lamv.g()], w=[junk64.g(), small.g()])
    act(sm(SM_E1), sm(SM_S1), AF.Exp, r=[small.g()], w=[small.g()])
    act(sm(SM_E2), sm(SM_S2), AF.Exp, r=[small.g()], w=[small.g()])
    tt(sm(SM_NLAM), sm(SM_E2), sm(SM_E1), ALU.subtract, r=[small.g()], w=[small.g()])
    ts(sm(SM_NLAM), sm(SM_NLAM), -0.2, None, ALU.add, None, r=[small.g()], w=[small.g()])
    bias_scr = nc.dram_tensor("bias_scr", [128, 2048], F32, kind="Internal")
    biasT = wa.get(8 * 1024)
    mk_chunk = wa.get(8 * 256 * 4)
    BT3 = biasT.f32().rearrange("p (h q) -> p h q", h=8)
    for h in range(8):
        dma('sp', BT3[:, h, :], negm_d.ap(), r=[], w=[biasT.g(h * 1024, (h + 1) * 1024)])
    for c in range(4):
        dma('sp', mk_chunk.f32(), masks_d.ap()[:, c * 2048:(c + 1) * 2048], r=[], w=[mk_chunk.g()])
        MC = mk_chunk.f32().rearrange("p (b q) -> p b q", b=8)
        for bb in range(8):
            b = c * 8 + bb
            for h in range(8):
                stt(BT3[:, h, :], MC[:, bb, :], relb.f32()[:, b * 8 + h:b * 8 + h + 1], BT3[:, h, :], ALU.mult, ALU.add,
                    r=[mk_chunk.g(), relb.g(), biasT.g(h * 1024, (h + 1) * 1024)], w=[biasT.g(h * 1024, (h + 1) * 1024)])
    ts(biasT.f32(), biasT.f32(), -OFF, None, ALU.add, None, r=[biasT.g()], w=[biasT.g()])
    dma('sp', bias_scr.ap(), biasT.f32(), r=[biasT.g()], w=["bias_scr"])
    if dbg:
        dump("dbg_bias", biasT.f32(), [128, 2048], r=[biasT.g()])
        dump("dbg_small", small.f32()[:, 0:16], [128, 16], r=[small.g()])

    hT3 = hT.bf().rearrange("p (k t) -> p k t", k=KC)
    mT3 = mT.bf().rearrange("p (k t) -> p k t", k=KC)

    def hT_g(c0, c1):
        return [hT.g(k * S * 2 + c0 * 2, k * S * 2 + c1 * 2) for k in range(KC)]

    def mT_g(k, c0, c1):
        return mT.g(k * S * 2 + c0 * 2, k * S * 2 + c1 * 2)

    BTb = banks[7][:].bitcast(BF16)

    for b in range(NB):
        wa = Alloc(arena, work_base, TOT)
        cact = wa.get(64)
        crep = wa.get(KC * 128 * 4)
        wblk = [wa.get(KC * 512 * 4) for _ in range(2)]
        bblk = [wa.get(2048) for _ in range(2)]
        mtmp = wa.get(2048)
        nwt = wa.get(4096)
        cin = wa.get(64)
        dma('sp', cin.f32()[:, 0:KC], cT_d.ap()[:, b * KC:(b + 1) * KC], r=[], w=[cin.g()])
        act(cact.f32()[:, 0:KC], cin.f32()[:, 0:KC], AF.Silu, r=[cin.g()], w=[cact.g()])
        cp(crep.f32().rearrange("p (k m) -> p k m", k=KC), mk(cact.f32(), [[1, KC], [0, 128]]), r=[cact.g()], w=[crep.g()])
        CR = crep.f32().rearrange("p (k m) -> p k m", k=KC)
        wada_v = wada_d.ap().rearrange("(k p) n -> p k n", p=128)
        dsts = [sh1, sh1, a1, a1, g1, g1, sh2, sh2, a2, a2, g2, g2]
        for nb_ in range(12):
            wb = wblk[nb_ % 2]
            bb_ = bblk[nb_ % 2]
            dma('sp', wb.f32().rearrange("p (k n) -> p k n", k=KC), wada_v[:, :, nb_ * 512:(nb_ + 1) * 512], r=[], w=[wb.g()])
            dma('sp', bb_.f32(), bada_d.ap()[:, nb_ * 512:(nb_ + 1) * 512], r=[], w=[bb_.g()])
            WB = wb.f32().rearrange("p (k n) -> p k n", k=KC)
            for k in range(KC):
                mm(banks[6][:], CR[:, k, :], WB[:, k, :], k == 0, k == KC - 1, r=[crep.g(), wb.g()], w=[BK[6]])
            dst = dsts[nb_]
            half = nb_ % 2
            dst_ap = dst.f32()[:, half * 512:(half + 1) * 512]
            dg_ = dst.g(half * 2048, (half + 1) * 2048)
            if nb_ in (2, 3, 8, 9):
                which = 0 if nb_ in (2, 3) else 1
                dma('sp', nwt.f32()[:, 0:512], nw_d.ap()[:, which * D + half * 512: which * D + (half + 1) * 512], r=[], w=[nwt.g()])
                tt(mtmp.f32(), banks[6][:], bb_.f32(), ALU.add, r=[BK[6], bb_.g()], w=[mtmp.g()])
                stt(dst_ap, mtmp.f32(), 1.0, nwt.f32()[:, 0:512], ALU.add, ALU.mult, r=[mtmp.g(), nwt.g()], w=[dg_])
            else:
                tt(dst_ap, banks[6][:], bb_.f32(), ALU.add, r=[BK[6], bb_.g()], w=[dg_])
        if dbg and b == 0:
            dump("dbg_a1", a1.f32(), [128, 1024], r=[a1.g()])
            dump("dbg_g2", g2.f32(), [128, 1024], r=[g2.g()])

        wa = Alloc(arena, work_base, TOT)
        xt = [wa.get(4096) for _ in range(2)]
        tmpf2 = [wa.get(4096) for _ in range(2)]
        junkb2 = [wa.get(2048) for _ in range(2)]
        hbf2 = [wa.get(2048) for _ in range(2)]
        for i in range(NT):
            par = i % 2
            xb, tmpf, junkb, hbf = xt[par], tmpf2[par], junkb2[par], hbf2[par]
            ss_, ln_, rs_ = sm(20 + par * 3), sm(21 + par * 3), sm(22 + par * 3)
            kss, kln, krs = f"p1ss{par}", f"p1ln{par}", f"p1rs{par}"
            dma('sp', xb.f32(), x_d.ap()[b, i * 128:(i + 1) * 128, :], r=[], w=[xb.g()])
            act(junkb.bf(), xb.f32(), AF.Square, r=[xb.g(), small.g()], w=[junkb.g(), kss], accum_out=ss_)
            ts(ln_, ss_, 1.0 / D, 1e-6, ALU.mult, ALU.add, r=[kss, small.g()], w=[kln])
            act(ln_, ln_, AF.Ln, r=[kln], w=[kln])
            act(rs_, ln_, AF.Exp, r=[kln, small.g()], w=[krs], scale=-0.5)
            stt(tmpf.f32(), xb.f32(), rs_, a1.f32(), ALU.mult, ALU.mult, r=[xb.g(), krs, a1.g()], w=[tmpf.g()])
            tt(hbf.bf(), tmpf.f32(), sh1.f32(), ALU.add, r=[tmpf.g(), sh1.g()], w=[hbf.g()])
            for k in range(KC):
                tr(BTb[:, k * 128:(k + 1) * 128], hbf.bf()[:, k * 128:(k + 1) * 128], ident_b.bf(), r=[hbf.g(), ident_b.g()], w=[BK[7]])
            cp(hT3[:, :, i * 128:(i + 1) * 128], BTb.rearrange("p (k t) -> p k t", k=KC), r=[BK[7]], w=hT_g(i * 128, (i + 1) * 128), eng='act')
        if dbg and b == 0:
            wa2 = Alloc(arena, wa.p, TOT)
            dtmp = wa2.get(8192)
            cp(dtmp.f32(), hT3[:, 0, :], r=hT_g(0, S), w=[dtmp.g()])
            dump("dbg_hT0", dtmp.f32(), [128, 2048], r=[dtmp.g()])
        if stage <= 1:
            continue

        win_v = win_d.ap().rearrange("(k p) n -> p k n", p=128)
        wa = Alloc(arena, work_base, TOT)
        wp = wa.get(KC * 256 * 2)
        wgp = wa.get(KC * 256 * 2)
        pw = wa.get(2 * 256 * 2)
        pg = wa.get(NT * 256 * 2)
        pT = [wa.get(512 * 2) for _ in range(2)]
        sg = wa.get(512 * 4)
        WP = wp.bf().rearrange("p (k n) -> p k n", k=KC)
        WGP = wgp.bf().rearrange("p (k n) -> p k n", k=KC)
        PW = pw.bf().rearrange("p (c n) -> p c n", c=2)
        PG = pg.bf().rearrange("p (i n) -> p i n", i=NT)
        BAND = band.bf().rearrange("p (j n) -> p j n", j=12)
        for g in range(4):
            dma('pool', WP, win_v[:, :, 3072 + g * 256:3072 + (g + 1) * 256], r=[], w=[wp.g()])
            dma('pool', WGP, win_v[:, :, 5120 + g * 256:5120 + (g + 1) * 256], r=[], w=[wgp.g()])
            dma('pool', PW, poolw_d.ap()[g].rearrange("(c p) n -> p c n", p=128), r=[], w=[pw.g()])
            for i in range(NT):
                for k in range(KC):
                    mm(banks[7][:, (i % 2) * 256:(i % 2 + 1) * 256], hT3[:, k, i * 128:(i + 1) * 128], WP[:, k, :], k == 0, k == KC - 1,
                       r=[hT_g(i * 128, (i + 1) * 128), wp.g()], w=[BK[7]])
                if i % 2 == 1:
                    cp(pg.bf()[:, (i - 1) * 256:(i + 1) * 256], banks[7][:], r=[BK[7]], w=[pg.g((i - 1) * 512, (i + 1) * 512)], eng='act')
            for c in range(4):
                for cc in range(2):
                    for il in range(4):
                        i = c * 4 + il
                        cur = g * 3 + (0 if i == 0 else 1)
                        mm(banks[cc][:, il * 128:(il + 1) * 128], PG[:, i, cc * 128:(cc + 1) * 128], BAND[:, cur, :], True, i == 0,
                           r=[pg.g(i * 512, (i + 1) * 512), band.g()], w=[BK[cc]])
                        if i > 0:
                            mm(banks[cc][:, il * 128:(il + 1) * 128], PG[:, i - 1, cc * 128:(cc + 1) * 128], BAND[:, g * 3 + 2, :], False, True,
                               r=[pg.g((i - 1) * 512, i * 512), band.g()], w=[BK[cc]])
                    cp(pT[cc].bf(), banks[cc][:], r=[BK[cc]], w=[pT[cc].g()], eng='act')
                for ec in range(2):
                    kch = g * 2 + ec
                    mm(banks[2][:], PW[:, 0, ec * 128:(ec + 1) * 128], pT[0].bf(), True, False, r=[pw.g(), pT[0].g()], w=[BK[2]])
                    mm(banks[2][:], PW[:, 1, ec * 128:(ec + 1) * 128], pT[1].bf(), False, True, r=[pw.g(), pT[1].g()], w=[BK[2]])
                    for k in range(KC):
                        mm(banks[3][:], WGP[:, k, ec * 128:(ec + 1) * 128], hT3[:, k, c * 512:(c + 1) * 512], k == 0, k == KC - 1,
                           r=[wgp.g(), hT_g(c * 512, (c + 1) * 512)], w=[BK[3]])
                    act(sg.f32(), banks[3][:], AF.Sigmoid, r=[BK[3]], w=[sg.g()])
                    stt(mT3[:, kch, c * 512:(c + 1) * 512], banks[2][:], pscT.f32()[:, kch:kch + 1], sg.f32(), ALU.mult, ALU.mult,
                        r=[BK[2], pscT.g(), sg.g()], w=[mT_g(kch, c * 512, (c + 1) * 512)])
        if dbg and b == 0 and stage == 2:
            wa2 = Alloc(arena, wa.p, TOT)
            dtmp = wa2.get(8192)
            for kk_ in (0, 5):
                cp(dtmp.f32(), mT3[:, kk_, :], r=[mT.g()], w=[dtmp.g()])
                dump(f"dbg_mT{kk_}", dtmp.f32(), [128, 2048], r=[dtmp.g()])
        if stage <= 2:
            continue

        wa = Alloc(arena, work_base, TOT)
        wq = wa.get(KC * 128 * 2)
        wk_ = wa.get(KC * 128 * 2)
        wv = wa.get(KC * 128 * 2)
        wg = wa.get(KC * 128 * 2)
        QTz = [wa.get(S * 2) for _ in range(2)]
        KT = wa.get(S * 2)
        Vb = wa.get(NT * 128 * 2)
        sgT = wa.get(S * 2)
        PT = [[wa.get(512 * 2) for _ in range(2)] for _ in range(2)]
        ntmp = [wa.get(256 * 4) for _ in range(2)]
        O1s = wa.get(2048)
        O2s = wa.get(2048)
        lnz = wa.get(2048)
        rz = wa.get(2048)
        rz2 = wa.get(2048)
        lnz2 = wa.get(2048)
        sq = wa.get(2048)
        t1 = wa.get(2048)
        rs = wa.get(2048)
        biasT = wa.get(8 * 1024)
        BT3 = biasT.f32().rearrange("p (h q) -> p h q", h=8)
        dma('sp', biasT.f32(), bias_scr.ap(), r=["bias_scr"], w=[biasT.g()])
        WQ = wq.bf().rearrange("p (k n) -> p k n", k=KC)
        WK = wk_.bf().rearrange("p (k n) -> p k n", k=KC)
        WV = wv.bf().rearrange("p (k n) -> p k n", k=KC)
        WG = wg.bf().rearrange("p (k n) -> p k n", k=KC)
        V3 = Vb.bf().rearrange("p (i n) -> p i n", i=NT)
        nheads = H if stage > 3 or not dbg else 1
        memset(QTz[0].bf()[64:128, :], 0.0, r=[], w=[QTz[0].g()])
        memset(QTz[1].bf()[0:64, :], 0.0, r=[], w=[QTz[1].g()])
        for h in range(nheads):
            dma('pool', WQ, win_v[:, :, h * 128:(h + 1) * 128], r=[], w=[wq.g()])
            dma('pool', WK, win_v[:, :, 1024 + h * 128:1024 + (h + 1) * 128], r=[], w=[wk_.g()])
            dma('pool', WV, win_v[:, :, 2048 + h * 128:2048 + (h + 1) * 128], r=[], w=[wv.g()])
            dma('pool', WG, win_v[:, :, 4096 + h * 128:4096 + (h + 1) * 128], r=[], w=[wg.g()])
            precast(4)
            pj = 0
            for (W_, wb_, dst, mode) in ((WQ, wq, None, 'q'), (WK, wk_, KT, 'k'), (WG, wg, sgT, 'g')):
                for c in range(4):
                    bk = pj % 4
                    pj += 1
                    for k in range(KC):
                        mm(banks[bk][:], W_[:, k, :], hT3[:, k, c * 512:(c + 1) * 512], k == 0, k == KC - 1,
                           r=[wb_.g(), hT_g(c * 512, (c + 1) * 512)], w=[BK[bk]])
                    if mode == 'q':
                        for m_ in range(2):
                            o_ap = QTz[m_].bf()[m_ * 64:(m_ + 1) * 64, c * 512:(c + 1) * 512]
                            P.add('act', (lambda o_ap=o_ap, bk=bk, m_=m_: (lambda e: e.mul(out=o_ap, in_=banks[bk][m_ * 64:(m_ + 1) * 64, :], mul=0.125)))(),
                                  r=[BK[bk]], w=[QTz[m_].g(c * 1024, (c + 1) * 1024)])
                        continue
                    o_ap = dst.bf()[:, c * 512:(c + 1) * 512]
                    o_g = dst.g(c * 1024, (c + 1) * 1024)
                    if mode == 'k':
                        cp(o_ap, banks[bk][:], r=[BK[bk]], w=[o_g], eng='act')
                    else:
                        act(o_ap, banks[bk][:], AF.Sigmoid, r=[BK[bk]], w=[o_g])
            for i in range(NT):
                bk = (i // 4) % 4
                for k in range(KC):
                    mm(banks[bk][:, (i % 4) * 128:(i % 4 + 1) * 128], hT3[:, k, i * 128:(i + 1) * 128], WV[:, k, :], k == 0, k == KC - 1,
                       r=[hT_g(i * 128, (i + 1) * 128), wv.g()], w=[BK[bk]])
                if i % 4 == 3:
                    cp(Vb.bf()[:, (i - 3) * 128:(i + 1) * 128], banks[bk][:], r=[BK[bk]], w=[Vb.g((i - 3) * 256, (i + 1) * 256)], eng='act')

            def qk(c, j):
                col0 = max(0, j - 4 * c) * 128
                par = j % 2
                for m in range(2):
                    bk = m * 2 + par
                    mm(banks[bk][:, col0:512], KT.bf()[:, j * 128:(j + 1) * 128],
                       QTz[m].bf()[:, c * 512 + col0:(c + 1) * 512], True, True,
                       r=[KT.g(j * 256, (j + 1) * 256), QTz[m].g(c * 1024 + col0 * 2, (c + 1) * 1024)], w=[BK[bk]])

            for c in range(4):
                jmax = 4 * c + 3
                qk(c, 0)
                for j in range(jmax + 1):
                    if j + 1 <= jmax:
                        qk(c, j + 1)
                    col0 = max(0, j - 4 * c) * 128
                    par = j % 2
                    il_lo = max(0, j - 4 * c)
                    il_hi = min(3, j + 1 - 4 * c)
                    far0 = max(0, j + 2 - 4 * c) * 128
                    for m in range(2):
                        bk = m * 2 + par
                        pt = PT[m][par]
                        if il_hi >= il_lo and il_hi >= 0:
                            n0, n1 = il_lo * 128, (il_hi + 1) * 128
                            b0 = (4 * c + il_lo - j) * 128
                            nn = n1 - n0
                            tt(ntmp[m].f32()[:, 0:nn], banks[bk][:, n0:n1], BT3[:, h, b0:b0 + nn], ALU.add,
                               r=[BK[bk], biasT.g(h * 1024, (h + 1) * 1024)], w=[ntmp[m].g()])
                            act(pt.bf()[:, n0:n1], ntmp[m].f32()[:, 0:nn], AF.Exp, r=[ntmp[m].g()], w=[pt.g(n0 * 2, n1 * 2)])
                        if far0 < 512:
                            act(pt.bf()[:, far0:512], banks[bk][:, far0:512], AF.Exp, r=[BK[bk], rb31m.g()], w=[pt.g(far0 * 2, 1024)],
                                bias=rb31m.f32()[:, h:h + 1])
                        mm(banks[4 + m][:, col0:512], V3[:, j, :], pt.bf()[:, col0:512], j == 0, j == jmax,
                           r=[Vb.g(j * 256, (j + 1) * 256), pt.g(col0 * 2, 1024)], w=[BK[4 + m]], skip_group_check=True)
                        mm(banks[6 + m][:, col0:512], ones_b.bf(), pt.bf()[:, col0:512], j == 0, j == jmax,
                           r=[ones_b.g(), pt.g(col0 * 2, 1024)], w=[BK[6 + m]], skip_group_check=True)
                cs = slice(c * 512, (c + 1) * 512)
                act(lnz.f32(), banks[6][:], AF.Ln, r=[BK[6]], w=[lnz.g()])
                act(rz.f32(), lnz.f32(), AF.Exp, r=[lnz.g()], w=[rz.g()], scale=-1.0)
                act(lnz2.f32(), banks[7][:], AF.Ln, r=[BK[7]], w=[lnz2.g()])
                act(rz2.f32(), lnz2.f32(), AF.Exp, r=[lnz2.g()], w=[rz2.g()], scale=-1.0)
                tt(t1.f32(), banks[4][:], rz.f32(), ALU.mult, r=[BK[4], rz.g()], w=[t1.g()])
                tt(O2s.f32(), banks[5][:], rz2.f32(), ALU.mult, r=[BK[5], rz2.g()], w=[O2s.g()])
                stt(t1.f32(), O2s.f32(), sm(SM_NLAM), t1.f32(), ALU.mult, ALU.add, r=[t1.g(), O2s.g(), small.g()], w=[t1.g()])
                act(sq.bf()[:, 0:512], t1.f32(), AF.Square, r=[t1.g()], w=[sq.g()])
                mm(banks[6][:], ones_b.bf(), sq.bf()[:, 0:512], True, True, r=[ones_b.g(), sq.g()], w=[BK[6]])
                ts(rs.f32(), banks[6][:], 1.0 / 128, 1e-5, ALU.mult, ALU.add, r=[BK[6]], w=[rs.g()])
                act(rs.f32(), rs.f32(), AF.Ln, r=[rs.g()], w=[rs.g()])
                act(rs.f32(), rs.f32(), AF.Exp, r=[rs.g()], w=[rs.g()], scale=-0.5)
                tt(t1.f32(), t1.f32(), rs.f32(), ALU.mult, r=[t1.g(), rs.g()], w=[t1.g()])
                stt(t1.f32(), t1.f32(), sm(SM_WSUB), sgT.bf()[:, cs], ALU.mult, ALU.mult, r=[t1.g(), small.g(), sgT.g(c * 1024, (c + 1) * 1024)], w=[t1.g()])
                tt(mT3[:, h, cs], t1.f32(), mT3[:, h, cs], ALU.add, r=[t1.g(), mT_g(h, c * 512, (c + 1) * 512)], w=[mT_g(h, c * 512, (c + 1) * 512)])
        if dbg and b == 0 and stage == 3:
            wa2 = Alloc(arena, wa.p, TOT)
            dtmp = wa2.get(8192)
            cp(dtmp.f32(), mT3[:, 0, :], r=[mT.g()], w=[dtmp.g()])
            dump("dbg_mT0", dtmp.f32(), [128, 2048], r=[dtmp.g()])
        if stage <= 3:
            continue

        precast(64)
        WOUT3 = wout.bf().rearrange("p (k n) -> p k n", k=KC)
        dma('pool', WOUT3, wout_d.ap().rearrange("(k p) n -> p k n", p=128), r=[], w=[wout.g()])
        for k in range(KC):
            tt(WOUT3[:, k, :], WOUT3[:, k, :], g1.f32(), ALU.mult, r=[wout.g(k * 2048, (k + 1) * 2048), g1.g()], w=[wout.g(k * 2048, (k + 1) * 2048)])
        WQ3 = hT.bf().rearrange("p (k n) -> p k n", k=KC)
        dma('pool', WQ3, wqry_d.ap().rearrange("(k p) n -> p k n", p=128), r=[], w=[hT.g()])
        wa = Alloc(arena, work_base, TOT)
        xt5 = g1
        xm = [wa.get(4096) for _ in range(2)]
        h2 = [wa.get(2048) for _ in range(2)]
        eidu = [wa.get(512) for _ in range(2)]
        gate = [wa.get(512) for _ in range(2)]
        tmpf = a1
        ot = sh1
        ssb = wa.get(8192)
        wkb = ssb
        tmpfX = ssb.sub(0, 4096)
        junkbX = ssb.sub(4096, 2048)
        h2T = ssb.sub(0, 2048)
        qTs = ssb.sub(4096, 4096)
        v16 = wa.get(1024)
        ixu = wa.get(1024)
        ixf = wa.get(1024)
        c16, posu, pau, pbu, paf, pbf, i1s, i2s, eidf, scm, ee, actv, gl, wgt = [wa.get(512) for _ in range(14)]
        zz = wa.get(32)
        rzz = wa.get(32)
        dgb = [wa.get(256) for _ in range(4)]
        NBUF = (TOT - wa.p) // 4096
        assert NBUF >= 5, NBUF
        cbuf = [wa.get(4096) for _ in range(NBUF)]
        SINK = mk(sink.bf(), [[0, D]])
        H2T3 = h2T.bf().rearrange("p (k t) -> p k t", k=KC)
        QTS3 = qTs.bf().rearrange("p (q t) -> p q t", q=16)
        SUBK3 = subkT.bf().rearrange("p (q n) -> p q n", q=16)
        SS3 = ssb.f32().rearrange("p (q n) -> p q n", q=16)
        WK3 = wkb.f32().rearrange("p (q n) -> p q n", q=16)
        V16 = v16.f32().rearrange("p (q j) -> p q j", q=16)
        IXU = ixu.u32().rearrange("p (q j) -> p q j", q=16)
        ntiles = NT if not dbg else 2

        def gen(name, r, w, eng='dve', **kw):
            P.add(eng, lambda e: getattr(e, name)(**kw), r=r, w=w)

        xmark = [0]

        def stageX(i):
            par = i % 2
            xm_, h2_, eidu_, gate_ = xm[par], h2[par], eidu[par], gate[par]
            t0 = i * 128
            dma('sp', xt5.f32(), x_d.ap()[b, t0:t0 + 128, :], r=[], w=[xt5.g()])
            for half in range(2):
                for k in range(KC):
                    mm(banks[2 + half][:], mT3[:, k, t0:t0 + 128], WOUT3[:, k, half * 512:(half + 1) * 512], k == 0, k == KC - 1,
                       r=[mT_g(k, t0, t0 + 128), wout.g()], w=[BK[2 + half]])
            for half in range(2):
                hs = slice(half * 512, (half + 1) * 512)
                tt(xm_.f32()[:, hs], banks[2 + half][:], xt5.f32()[:, hs], ALU.add, r=[BK[2 + half], xt5.g()], w=[xm_.g(half * 2048, (half + 1) * 2048)])
            act(junkbX.bf(), xm_.f32(), AF.Square, r=[xm_.g()], w=[junkbX.g(), small.g()], accum_out=sm(SM_SS2))
            rstd_chain(SM_SS2, SM_LN2, SM_RSTD2, D, 1e-6)
            for hf in range(2):
                hs = slice(hf * 512, (hf + 1) * 512)
                stt(tmpfX.f32()[:, hs], xm_.f32()[:, hs], sm(SM_RSTD2), a2.f32()[:, hs], ALU.mult, ALU.mult, r=[xm_.g(), small.g(), a2.g()], w=[tmpfX.g(hf * 2048, (hf + 1) * 2048)])
                tt(h2_.bf()[:, hs], tmpfX.f32()[:, hs], sh2.f32()[:, hs], ALU.add, r=[tmpfX.g(hf * 2048, (hf + 1) * 2048), sh2.g()], w=[h2_.g(hf * 1024, (hf + 1) * 1024)])
            for k in range(KC):
                tr(BTb[:, k * 128:(k + 1) * 128], h2_.bf()[:, k * 128:(k + 1) * 128], ident_b.bf(), r=[h2_.g(), ident_b.g()], w=[BK[7]])
            cp(h2T.bf(), BTb, r=[BK[7]], w=[h2T.g()], eng='act')
            for qc in range(16):
                bk = 2 + (qc // 4) % 2
                for k in range(KC):
                    mm(banks[bk][:, (qc % 4) * 128:(qc % 4 + 1) * 128], WQ3[:, k, qc * 128:(qc + 1) * 128], H2T3[:, k, :], k == 0, k == KC - 1,
                       r=[hT.g(), h2T.g()], w=[BK[bk]])
                if qc % 4 == 3:
                    cp(qTs.bf()[:, (qc - 3) * 128:(qc + 1) * 128], banks[bk][:], r=[BK[bk]], w=[qTs.g((qc - 3) * 256, (qc + 1) * 256)], eng='act')
            for qc in range(16):
                bk = 4 + qc // 4
                mm(banks[bk][:, (qc % 4) * 128:(qc % 4 + 1) * 128], QTS3[:, qc, :], SUBK3[:, qc, :], True, True,
                   r=[qTs.g(qc * 256, (qc + 1) * 256), subkT.g()], w=[BK[bk]])
            for q4 in range(4):
                cp(ssb.f32()[:, q4 * 512:(q4 + 1) * 512], banks[4 + q4][:], r=[BK[4 + q4]], w=[ssb.g(q4 * 2048, (q4 + 1) * 2048)], eng='act')
            if dbg and i == 0:
                dump("dbg_xm", xm_.f32(), [128, 1024], r=[xm_.g()])
                dump("dbg_s", ssb.f32(), [128, 2048], r=[ssb.g()])
            xmark[0] = len(P.capture) if P.capture is not None else 0
            sg_ = lambda qc: ssb.g(qc * 512, (qc + 1) * 512)
            wg_ = lambda qc: wkb.g(qc * 512, (qc + 1) * 512)
            vk = lambda qc, hf: f"v16:{qc}:{hf}"
            ik = lambda qc, hf: f"ixu:{qc}:{hf}"
            ALLV = [vk(q_, h_) for q_ in range(16) for h_ in range(2)]
            ALLI = [ik(q_, h_) for q_ in range(16) for h_ in range(2)]
            for qc in range(16):
                gen('max', [sg_(qc), v16.g()], [vk(qc, 0)], out=V16[:, qc, 0:8], in_=SS3[:, qc, :])
            for qc in range(16):
                gen('max_index', [sg_(qc), vk(qc, 0), ixu.g()], [ik(qc, 0)], out=IXU[:, qc, 0:8], in_max=V16[:, qc, 0:8], in_values=SS3[:, qc, :])
            for qc in range(16):
                gen('match_replace', [sg_(qc), vk(qc, 0)], [wg_(qc)], out=WK3[:, qc, :], in_to_replace=V16[:, qc, 0:8], in_values=SS3[:, qc, :], imm_value=NEG)
            for qc in range(16):
                gen('max', [wg_(qc), v16.g()], [vk(qc, 1)], out=V16[:, qc, 8:16], in_=WK3[:, qc, :])
            for qc in range(16):
                gen('max_index', [wg_(qc), vk(qc, 1), ixu.g()], [ik(qc, 1)], out=IXU[:, qc, 8:16], in_max=V16[:, qc, 8:16], in_values=WK3[:, qc, :])
            cp(ixf.f32(), ixu.u32(), r=ALLI, w=[ixf.g()])
            cand, wk2 = ssb, wkb
            C4 = cand.f32().rearrange("p (h n) -> p h n", h=8)
            W4 = wk2.f32().rearrange("p (h n) -> p h n", h=8)
            vf = v16.f32()
            for hp in range(4):
                tt(mk(cand.f32(), [[256, 2], [16, 16], [1, 16]], hp * 512), mk(vf, [[32, 2], [1, 16], [0, 16]], hp * 64),
                   mk(vf, [[32, 2], [0, 16], [1, 16]], hp * 64 + 16), ALU.add, r=ALLV, w=[cand.g(hp * 2048, (hp + 1) * 2048)])
            C16 = c16.f32().rearrange("p (h j) -> p h j", h=8)
            POS = posu.u32().rearrange("p (h j) -> p h j", h=8)
            cg_ = lambda h_: cand.g(h_ * 1024, (h_ + 1) * 1024)
            w2g_ = lambda h_: wk2.g(h_ * 1024, (h_ + 1) * 1024)
            ck = lambda h_, hf: f"c16:{h_}:{hf}"
            pk = lambda h_, hf: f"pos:{h_}:{hf}"
            ALLC = [ck(h_, f_) for h_ in range(8) for f_ in range(2)]
            ALLP = [pk(h_, f_) for h_ in range(8) for f_ in range(2)]
            for h_ in range(8):
                gen('max', [cg_(h_), c16.g()], [ck(h_, 0)], out=C16[:, h_, 0:8], in_=C4[:, h_, :])
            for h_ in range(8):
                gen('max_index', [cg_(h_), ck(h_, 0), posu.g()], [pk(h_, 0)], out=POS[:, h_, 0:8], in_max=C16[:, h_, 0:8], in_values=C4[:, h_, :])
            for h_ in range(8):
                gen('match_replace', [cg_(h_), ck(h_, 0)], [w2g_(h_)], out=W4[:, h_, :], in_to_replace=C16[:, h_, 0:8], in_values=C4[:, h_, :], imm_value=NEG)
            for h_ in range(8):
                gen('max', [w2g_(h_), c16.g()], [ck(h_, 1)], out=C16[:, h_, 8:16], in_=W4[:, h_, :])
            for h_ in range(8):
                gen('max_index', [w2g_(h_), ck(h_, 1), posu.g()], [pk(h_, 1)], out=POS[:, h_, 8:16], in_max=C16[:, h_, 8:16], in_values=W4[:, h_, :])
            gen('tensor_single_scalar', ALLP, [pau.g()], out=pau.u32(), in_=posu.u32(), scalar=4, op=ALU.logical_shift_right)
            gen('tensor_single_scalar', ALLP, [pbu.g()], out=pbu.u32(), in_=posu.u32(), scalar=15, op=ALU.bitwise_and)
            cp(paf.f32(), pau.u32(), r=[pau.g()], w=[paf.g()])
            cp(pbf.f32(), pbu.u32(), r=[pbu.g()], w=[pbf.g()])
            oh, prod = ssb, wkb
            for (pf_, off_, dst_) in ((paf, 0, i1s), (pbf, 16, i2s)):
                for hp in range(4):
                    og = oh.g(hp * 2048, (hp + 1) * 2048)
                    tt(mk(oh.f32(), [[16, 32], [1, 16]], hp * 512), mk(pf_.f32(), [[1, 32], [0, 16]], hp * 32), mk(iota16.f32(), [[0, 32], [1, 16]]), ALU.is_equal,
                       r=[pf_.g(), iota16.g()], w=[og])
                    tt(mk(prod.f32(), [[256, 2], [16, 16], [1, 16]], hp * 512), mk(oh.f32(), [[256, 2], [16, 16], [1, 16]], hp * 512),
                       mk(ixf.f32(), [[32, 2], [0, 16], [1, 16]], hp * 64 + off_), ALU.mult, r=[og, ixf.g()], w=[og])
                    tred(dst_.f32()[:, hp * 32:(hp + 1) * 32], mk(prod.f32(), [[16, 32], [1, 16]], hp * 512), ALU.add, r=[og], w=[dst_.g()])
            stt(eidf.f32(), i1s.f32(), 128.0, i2s.f32(), ALU.mult, ALU.add, r=[i1s.g(), i2s.g()], w=[eidf.g()])
            cp(eidu_.u32(), eidf.f32(), r=[eidf.g()], w=[eidu_.g()])
            tt(mk(scm.f32(), [[16, 8], [1, 16]]), mk(c16.f32(), [[16, 8], [1, 16]]), mk(c16.f32(), [[16, 8], [0, 16]]), ALU.subtract, r=ALLC, w=[scm.g()])
            act(ee.f32(), scm.f32(), AF.Exp, r=[scm.g()], w=[ee.g()])
            tred(zz.f32(), mk(ee.f32(), [[16, 8], [1, 16]]), ALU.add, r=[ee.g()], w=[zz.g()])
            gen('reciprocal', [zz.g()], [rzz.g()], out=rzz.f32(), in_=zz.f32())
            tt(mk(gate_.f32(), [[16, 8], [1, 16]]), mk(ee.f32(), [[16, 8], [1, 16]]), mk(rzz.f32(), [[1, 8], [0, 16]]), ALU.mult, r=[ee.g(), rzz.g()], w=[gate_.g()])
            if dbg and i == 0:
                dump("dbg_eid", eidf.f32(), [128, 128], r=[eidf.g()])
                dump("dbg_gate", gate_.f32(), [128, 128], r=[gate_.g()])

        def stageY(i, pump):
            par = i % 2
            xm_, h2_, eidu_, gate_ = xm[par], h2[par], eidu[par], gate[par]
            t0 = i * 128
            EID = eidu_.u32()

            def gather(out_ap, slot, r, w):
                P.add('pool', lambda e: e.indirect_dma_start(out=out_ap, out_offset=None, in_=etab_d.ap(),
                                                             in_offset=bass.IndirectOffsetOnAxis(ap=EID[:, slot:slot + 1], axis=0)),
                      r=r, w=w, dma=True)

            def slot_tail(sl):
                cb = cbuf[sl % NBUF]
                dg_ = dgb[sl % 4]
                act(wgt.f32()[:, sl:sl + 1], gl.f32()[:, sl:sl + 1], AF.Copy, r=[f"gl{sl}", gate_.g(), wgt.g()], w=[f"wgt{sl}"],
                    scale=gate_.f32()[:, sl:sl + 1])
                P.add('act', (lambda dg_=dg_, sl=sl: (lambda e: e.activation(out=dg_.bf(), in_=ident_f.f32(), func=AF.Copy, scale=wgt.f32()[:, sl:sl + 1])))(),
                      r=[ident_f.g(), f"wgt{sl}"], w=[dg_.g()])
                for half in range(2):
                    mm(banks[half][:], dg_.bf(), cb.bf()[:, D + half * 512:D + (half + 1) * 512], sl == 0, sl == 127,
                       r=[dg_.g(), cb.g(2048, 4096)], w=[BK[half]])

            for sl in range(128):
                cb = cbuf[sl % NBUF]
                gather(cb.bf(), sl, r=[eidu_.g()] + ETAB_KEYS, w=[cb.g()])
                P.add('dve', (lambda cb=cb, sl=sl: (lambda e: e.scalar_tensor_tensor(out=SINK, in0=cb.bf()[:, 0:D], scalar=1.0, in1=h2_.bf(), op0=ALU.mult, op1=ALU.mult,
                                                                                 accum_out=actv.f32()[:, sl:sl + 1])))(),
                      r=[cb.g(0, 2048), h2_.g(), actv.g()], w=[f"actv{sl}"])
                act(gl.f32()[:, sl:sl + 1], actv.f32()[:, sl:sl + 1], AF.Gelu, r=[f"actv{sl}", gl.g()], w=[f"gl{sl}"])
                if sl >= 1:
                    slot_tail(sl - 1)
                pump(sl)
            slot_tail(127)
            for half in range(2):
                hs = slice(half * 512, (half + 1) * 512)
                tt(tmpf.f32()[:, hs], banks[half][:], g2.f32()[:, hs], ALU.mult, r=[BK[half], g2.g()], w=[tmpf.g(half * 2048, (half + 1) * 2048)])
                tt(ot.f32()[:, hs], tmpf.f32()[:, hs], xm_.f32()[:, hs], ALU.add, r=[tmpf.g(half * 2048, (half + 1) * 2048), xm_.g()], w=[ot.g(half * 2048, (half + 1) * 2048)])
            if dbg and i == 0:
                dump("dbg_xo", ot.f32(), [128, 1024], r=[ot.g()])
            act(SINK, ot.f32(), AF.Square, r=[ot.g()], w=[small.g()], accum_out=sm(SM_SS3))
            rstd_chain(SM_SS3, SM_LN3, SM_RSTD3, D, 1e-6)
            stt(ot.f32(), ot.f32(), sm(SM_RSTD3), nf_bc.f32(), ALU.mult, ALU.mult, r=[ot.g(), small.g(), nf_bc.g()], w=[ot.g()])
            dma('sp', out_d.ap()[b, t0:t0 + 128, :], ot.f32(), r=[ot.g()], w=[f"out{b}_{i}"])

        def drain(q, n=None):
            k = 0
            while q and (n is None or k < n):
                P.add(*q.pop(0))
                k += 1

        P.capture = []
        stageX(0)
        q = P.capture
        P.capture = None
        drain(q)
        for i in range(ntiles):
            if i + 1 < ntiles:
                P.capture = []
                stageX(i + 1)
                q = P.capture
                P.capture = None
            else:
                q = []
            n_tot, n_pre = len(q), xmark[0]
            S1, S2 = 24, 122

            def pump_to(sl, q=q, n_tot=n_tot, n_pre=n_pre):
                if sl < S1:
                    target = n_pre * (sl + 1) // S1
                else:
                    target = n_pre + (n_tot - n_pre) * min(sl + 1 - S1, S2 - S1) // (S2 - S1)
                drain(q, max(0, target - (n_tot - len(q))))
            stageY(i, pump_to)
            drain(q)

    P.emit(nc, stack)
    stack.close()
    return nc, list(dbg_d.keys())


def _prep_inputs(inputs):
    f = np.float32
    x = np.asarray(inputs['x'], f)
    c = np.asarray(inputs['c'], f)
    cst = _host_consts()
    rep = lambda v, n=128: np.ascontiguousarray(np.broadcast_to(np.asarray(v, f).reshape(1, -1), (n, np.asarray(v).size)))
    nw = np.concatenate([np.asarray(inputs['norm_mix_w'], f).reshape(-1), np.asarray(inputs['norm_ffn_w'], f).reshape(-1),
                         np.asarray(inputs['norm_final_w'], f).reshape(-1)])
    lam = np.concatenate([np.asarray(inputs[k], f).reshape(-1) for k in ('lambda_q1', 'lambda_k1', 'lambda_q2', 'lambda_k2')])
    sk1 = np.asarray(inputs['sub_keys_1'], f)[0]
    sk2 = np.asarray(inputs['sub_keys_2'], f)[0]
    subkT = np.zeros((128, 16, 128), f)
    for h in range(8):
        subkT[:, h * 2 + 0, :] = sk1[h].T
        subkT[:, h * 2 + 1, :] = sk2[h].T
    shared = {
        'w_ada': np.ascontiguousarray(np.asarray(inputs['w_ada'], f)[0]),
        'b_ada_bc': rep(inputs['b_ada']),
        'nw_bc': rep(nw),
        'w_in': np.ascontiguousarray(np.asarray(inputs['w_in'], f)[0]),
        'w_out': np.ascontiguousarray(np.asarray(inputs['w_out'], f)[0]),
        'w_query': np.ascontiguousarray(np.asarray(inputs['w_query'], f)[0]),
        'pool_w': np.ascontiguousarray(np.asarray(inputs['pool_w'], f)[0]),
        'pool_scaleT': np.ascontiguousarray(np.asarray(inputs['pool_scale'], f).reshape(8, 128).T),
        'subkT': np.ascontiguousarray(subkT.reshape(128, 2048)),
        'e_down': np.ascontiguousarray(np.asarray(inputs['expert_down'], f)[0]),
        'e_up': np.ascontiguousarray(np.asarray(inputs['expert_up'], f)[0]),
        'lam_bc': rep(lam),
        'sublnT': np.ascontiguousarray(np.asarray(inputs['subln_w'], f).reshape(128, 1)),
        'relb_bc': rep(np.asarray(inputs['rel_bias'], f).reshape(-1)),
        'masks': np.ascontiguousarray(cst['masks'].reshape(128, -1)),
        'negmask': cst['negmask'],
        'band': np.ascontiguousarray(cst['band'].reshape(128, -1)),
        'ident': cst['ident'],
        'iota16': cst['iota16'],
        'sel0': cst['sel0'],
    }
    in_maps = []
    for core in range(NCORES):
        m = dict(shared)
        m['x'] = np.ascontiguousarray(x[core * NB:(core + 1) * NB])
        cc = c[core * NB:(core + 1) * NB]
        m['cT'] = np.ascontiguousarray(cc.reshape(NB, KC, 128).transpose(2, 0, 1).reshape(128, NB * KC))
        in_maps.append(m)
    return in_maps


_CACHE = {}


def kernel(**inputs):
    in_maps = _prep_inputs(inputs)
    if 'nc' not in _CACHE:
        _CACHE['nc'] = build()[0]
    nc = _CACHE['nc']
    res = run_bass_kernel_spmd(nc, in_maps, core_ids=list(range(NCORES)))
    out = np.concatenate([np.asarray(r['out']) for r in res.results], axis=0)
    return out.astype(np.float32)
```
